# Optimizing a Trainium2 kernel written in Bass

```python
import math
import jax
import jax.numpy as jnp
from jax import lax
import numpy as np

D_MODEL = 2048
BATCH = 16
SEQ = 256
DEPTH = 2
DEC_BATCH = 4
DEC_SEQ = 2048
PAST_LEN = 512

GRID_W = 64
EPS = 1e-6
Q_BLOCK = 128
N_BRANCH = 4
BRANCH_W = D_MODEL // 2

MLA_HEADS = 8
MLA_NOPE = 128
MLA_ROPE = 64
MLA_V = BRANCH_W // MLA_HEADS
MLA_Q_RANK = D_MODEL // 4
MLA_KV_RANK = D_MODEL // 8
MLA_SCALE = (MLA_NOPE + MLA_ROPE) ** -0.5
ROPE_THETA = 10000.0

DN_HEADS = 8
DN_DK = 128
DN_DV = BRANCH_W // DN_HEADS
DN_CONV = 5
DN_CHUNK = 64
DN_CONV_CH = 2 * DN_HEADS * DN_DK + DN_HEADS * DN_DV

HY_WIDTH = BRANCH_W
HY_ORDER = 2
HY_SHORT = 3
HY_EMB = 33
HY_HID = 64
HY_FAST_DECAY = 0.3
HY_SLOW_DECAY = 1.5
HY_DECAY_TARGET = 1e-2

NA_HEADS = 8
NA_HD = BRANCH_W // NA_HEADS
NA_KH = 8
NA_KW = 16
NA_SCALE = NA_HD ** -0.5

PEER_HEADS = 8
PEER_NKEYS = 128
PEER_N = PEER_NKEYS * PEER_NKEYS
PEER_DKEY = 256
PEER_TOPK = 16
PEER_BLOCK = 128

IN_SIZES = (MLA_Q_RANK, MLA_KV_RANK, MLA_ROPE, DN_CONV_CH, DN_HEADS * DN_DV, 2 * DN_HEADS, 2 * DN_HEADS,
            (HY_ORDER + 1) * HY_WIDTH, 3 * NA_HEADS * NA_HD, N_BRANCH * D_MODEL)
IN_COLS = sum(IN_SIZES)

kernel_name = "hybrid_flow_trunk_step"

F32 = jnp.float32


def rms_norm(x, g):
    xf = x.astype(F32)
    y = xf * lax.rsqrt(jnp.mean(xf * xf, axis=-1, keepdims=True) + EPS)
    return (y * g.astype(F32)).astype(x.dtype)


def l2_norm(x):
    xf = x.astype(F32)
    return xf * lax.rsqrt(jnp.sum(xf * xf, axis=-1, keepdims=True) + EPS)


def split_in(p):
    cuts, acc = [], 0
    for s in IN_SIZES[:-1]:
        acc += s
        cuts.append(acc)
    return jnp.split(p, cuts, axis=-1)


def ada_modulation(cond, lp):
    m = jax.nn.silu(cond) @ lp["w_ada"] + lp["b_ada"]
    return jnp.split(m, 6, axis=-1)


def modulate(x, g, shift, scale):
    return rms_norm(x, g) * (1 + scale) + shift


def depthwise_conv(x, w):
    k, ch = w.shape
    pad = k // 2
    return lax.conv_general_dilated(x, w[:, None, :].astype(x.dtype), (1,), [(pad, pad)],
                                    dimension_numbers=("NWC", "WIO", "NWC"), feature_group_count=ch)


def block_attention(q, k, v, scale):
    B, Lq, H, dq = q.shape
    dv = v.shape[-1]
    nb = Lq // Q_BLOCK
    qb = q.reshape(B, nb, Q_BLOCK, H, dq).transpose(1, 0, 2, 3, 4)

    def one(qi):
        s = jnp.einsum("bqhd,bkhd->bhqk", qi, k, preferred_element_type=F32) * scale
        p = jax.nn.softmax(s, axis=-1).astype(v.dtype)
        return jnp.einsum("bhqk,bkhd->bqhd", p, v)

    o = lax.map(one, qb)
    return o.transpose(1, 0, 2, 3, 4).reshape(B, Lq, H, dv)


def apply_axial_rope(x):
    L = x.shape[1]
    half = x.shape[-1] // 2
    t = jnp.arange(L)
    inv = ROPE_THETA ** (-jnp.arange(0, half, 2, dtype=F32) / half)
    xf = x.astype(F32)
    out = []
    for pos, xa in ((t // GRID_W, xf[..., :half]), (t % GRID_W, xf[..., half:])):
        ang = pos.astype(F32)[:, None] * inv[None, :]
        cos = jnp.cos(ang)[:, None, :]
        sin = jnp.sin(ang)[:, None, :]
        x1, x2 = xa[..., : half // 2], xa[..., half // 2:]
        out += [x1 * cos - x2 * sin, x2 * cos + x1 * sin]
    return jnp.concatenate(out, axis=-1).astype(x.dtype)


def mla_queries(cq, lp, rotate):
    B, L, _ = cq.shape
    q = (rms_norm(cq, lp["mla_q_norm"]) @ lp["mla_w_qb"]).reshape(B, L, MLA_HEADS, MLA_NOPE + MLA_ROPE)
    if rotate:
        q = jnp.concatenate([q[..., :MLA_NOPE], apply_axial_rope(q[..., MLA_NOPE:])], axis=-1)
    return q


def mla_keys_values(ckv_n, krope, lp):
    B, L, _ = ckv_n.shape
    kv = (ckv_n @ lp["mla_w_kvb"]).reshape(B, L, MLA_HEADS, MLA_NOPE + MLA_V)
    k = jnp.concatenate([kv[..., :MLA_NOPE],
                         jnp.broadcast_to(krope[:, :, None, :], (B, L, MLA_HEADS, MLA_ROPE))], axis=-1)
    return k, kv[..., MLA_NOPE:]


def chunk_gated_delta(q, k, v, g, beta, s0):
    B, H, L, DK = q.shape
    DV = v.shape[-1]
    C = DN_CHUNK
    N = L // C
    q = q.reshape(B, H, N, C, DK)
    k = k.reshape(B, H, N, C, DK)
    v = v.reshape(B, H, N, C, DV)
    beta = beta.reshape(B, H, N, C)
    gc = jnp.cumsum(g.reshape(B, H, N, C), axis=-1)
    tri_incl = jnp.tril(jnp.ones((C, C), bool))
    tri_strict = jnp.tril(jnp.ones((C, C), bool), -1)
    decay = jnp.exp(jnp.where(tri_incl, gc[..., :, None] - gc[..., None, :], -jnp.inf))
    kk = jnp.einsum("bhncd,bhnsd->bhncs", k, k)
    a_low = jnp.where(tri_strict, beta[..., None] * kk * decay, 0.0)
    rhs = jnp.concatenate([v * beta[..., None], k * (beta * jnp.exp(gc))[..., None]], axis=-1)
    sol = lax.linalg.triangular_solve(jnp.eye(C, dtype=F32) + a_low, rhs, left_side=True, lower=True,
                                      unit_diagonal=True)
    u, w = sol[..., :DV], sol[..., DV:]
    qk = jnp.where(tri_incl, jnp.einsum("bhncd,bhnsd->bhncs", q, k) * decay, 0.0)

    def step(S, inp):
        qi, ki, ui, wi, gi, qki = inp
        v_new = ui - jnp.einsum("bhck,bhkv->bhcv", wi, S)
        o = (jnp.einsum("bhck,bhkv->bhcv", qi * jnp.exp(gi)[..., None], S)
             + jnp.einsum("bhcs,bhsv->bhcv", qki, v_new))
        g_last = gi[..., -1]
        S = (S * jnp.exp(g_last)[..., None, None]
             + jnp.einsum("bhck,bhcv->bhkv", ki * jnp.exp(g_last[..., None] - gi)[..., None], v_new))
        return S, o

    xs = tuple(jnp.moveaxis(t, 2, 0) for t in (q, k, u, w, gc, qk))
    S, o = lax.scan(step, s0, xs)
    return jnp.moveaxis(o, 0, 2).reshape(B, H, L, DV), S


def gated_deltanet(qkv_raw, z, a, b, lp, s_f0, s_b0):
    B, L, _ = qkv_raw.shape
    qkv = jax.nn.silu(depthwise_conv(qkv_raw, lp["dn_conv"]))
    q, k, v = jnp.split(qkv, [DN_HEADS * DN_DK, 2 * DN_HEADS * DN_DK], axis=-1)
    q = (l2_norm(q.reshape(B, L, DN_HEADS, DN_DK)) * DN_DK ** -0.5).transpose(0, 2, 1, 3)
    k = l2_norm(k.reshape(B, L, DN_HEADS, DN_DK)).transpose(0, 2, 1, 3)
    v = v.reshape(B, L, DN_HEADS, DN_DV).astype(F32).transpose(0, 2, 1, 3)
    a = a.reshape(B, L, 2, DN_HEADS).astype(F32)
    g = -jnp.exp(lp["dn_a_log"].astype(F32)) * jax.nn.softplus(a + lp["dn_dt_bias"].astype(F32))
    beta = jax.nn.sigmoid(b.reshape(B, L, 2, DN_HEADS).astype(F32))
    g = g.transpose(2, 0, 3, 1)
    beta = beta.transpose(2, 0, 3, 1)
    o_f, s_f = chunk_gated_delta(q, k, v, g[0], beta[0], s_f0.astype(F32))
    fl = lambda t: jnp.flip(t, axis=2)
    o_b, s_b = chunk_gated_delta(fl(q), fl(k), fl(v), fl(g[1]), fl(beta[1]), s_b0.astype(F32))
    o = (o_f + fl(o_b)).transpose(0, 2, 1, 3)
    o = rms_norm(o, lp["dn_out_norm"]) * jax.nn.silu(z.reshape(B, L, DN_HEADS, DN_DV).astype(F32))
    return o.reshape(B, L, BRANCH_W).astype(qkv_raw.dtype), s_f, s_b


def hyena_filters(L, lp):
    t01 = jnp.linspace(0.0, 1.0, L, dtype=F32)[:, None]
    bands = (HY_EMB - 1) // 2
    w = 2.0 * math.pi * jnp.arange(L, dtype=F32)[:, None] / L
    f = jnp.linspace(1e-4, bands - 1, bands, dtype=F32)[None, :]
    z = jnp.concatenate([t01, jnp.cos(f * w), -jnp.sin(f * w)], axis=-1)
    h = jnp.sin(z @ lp["hy_w1"].astype(F32) + lp["hy_b1"].astype(F32))
    h = jnp.sin(h @ lp["hy_w2"].astype(F32) + lp["hy_b2"].astype(F32))
    h = (h @ lp["hy_w3"].astype(F32)).reshape(L, HY_ORDER, 2, HY_WIDTH)
    max_decay = math.log(HY_DECAY_TARGET) / HY_FAST_DECAY
    min_decay = math.log(HY_DECAY_TARGET) / HY_SLOW_DECAY
    deltas = jnp.linspace(min_decay, max_decay, HY_WIDTH, dtype=F32)
    window = jnp.exp(-t01 * jnp.abs(deltas)[None, :])
    return h * window[:, None, None, :]


def long_conv(u, hf, hb, bias):
    L = u.shape[1]
    kern = jnp.concatenate([hf, jnp.zeros_like(hf[:1]), jnp.flip(hb[1:], axis=0)], axis=0)
    uf = jnp.fft.rfft(u, n=2 * L, axis=1)
    kf = jnp.fft.rfft(kern, axis=0)
    y = jnp.fft.irfft(uf * kf[None], n=2 * L, axis=1)[:, :L]
    return y + u * bias


def hyena(hy_raw, lp):
    L = hy_raw.shape[1]
    zc = depthwise_conv(hy_raw, lp["hy_conv"]).astype(F32)
    v, x1, x2 = jnp.split(zc, 3, axis=-1)
    filt = hyena_filters(L, lp)
    bias = lp["hy_bias"].astype(F32)
    y = v
    for o, gate in enumerate((x1, x2)):
        y = gate * long_conv(y, filt[:, o, 0], filt[:, o, 1], bias[o])
    return y.astype(hy_raw.dtype)


def na_split(na_in):
    B, L, _ = na_in.shape
    q, k, v = jnp.split(na_in, 3, axis=-1)
    return tuple(t.reshape(B, L, NA_HEADS, NA_HD) for t in (q, k, v))


def na_latent(q, k, v, k_ctx, v_ctx, rpb):
    B, L, H, d = q.shape
    rows = L // GRID_W
    kh = min(NA_KH, rows)
    kw = NA_KW
    nk = kh * kw
    cols = jnp.arange(GRID_W)
    col_idx = jnp.clip(cols - kw // 2, 0, GRID_W - kw)[:, None] + jnp.arange(kw)[None, :]
    dcol = col_idx - cols[:, None] + (NA_KW - 1)
    q_rows = q.reshape(B, rows, GRID_W, H, d).transpose(1, 0, 2, 3, 4)

    def one_row(args):
        r, q_row = args
        key_rows = jnp.clip(r - kh // 2, 0, rows - kh) + jnp.arange(kh)
        idx = (key_rows[None, :, None] * GRID_W + col_idx[:, None, :]).reshape(GRID_W, nk)
        k_win = k[:, idx]
        v_win = v[:, idx]
        drow = key_rows - r + (NA_KH - 1)
        bias = rpb[:, drow[None, :, None], dcol[:, None, :]].reshape(H, GRID_W, nk).astype(F32)
        s_win = jnp.einsum("bqhd,bqkhd->bhqk", q_row, k_win, preferred_element_type=F32) * NA_SCALE + bias[None]
        s_ctx = jnp.einsum("bqhd,bkhd->bhqk", q_row, k_ctx, preferred_element_type=F32) * NA_SCALE
        p = jax.nn.softmax(jnp.concatenate([s_win, s_ctx], axis=-1), axis=-1).astype(v.dtype)
        return (jnp.einsum("bhqk,bqkhd->bqhd", p[..., :nk], v_win)
                + jnp.einsum("bhqk,bkhd->bqhd", p[..., nk:], v_ctx))

    o = lax.map(one_row, (jnp.arange(rows), q_rows))
    return o.transpose(1, 0, 2, 3, 4).reshape(B, L, H * d)


def merge_branches(branches, gate_logits, lp):
    B, L, _ = gate_logits.shape
    br = jnp.stack(branches, axis=2)
    proj = jnp.einsum("blnw,nwd->blnd", br, lp["w_branch"])
    g = jax.nn.sigmoid(gate_logits.reshape(B, L, N_BRANCH, D_MODEL))
    return jnp.sum(g * proj, axis=2) @ lp["w_out"]


def peer(h, lp):
    B, L, D = h.shape
    T = B * L
    xt = h.reshape(T, D)
    q = (xt @ lp["peer_wq"]).reshape(T, PEER_HEADS, 2, PEER_DKEY // 2)
    s = jnp.einsum("thsc,snc->thsn", q, lp["peer_keys"], preferred_element_type=F32)
    top_s, top_i = lax.top_k(s, PEER_TOPK)
    cand_s = (top_s[:, :, 0, :, None] + top_s[:, :, 1, None, :]).reshape(T, PEER_HEADS, PEER_TOPK * PEER_TOPK)
    cand_i = (top_i[:, :, 0, :, None] * PEER_NKEYS + top_i[:, :, 1, None, :]).reshape(T, PEER_HEADS, PEER_TOPK * PEER_TOPK)
    best_s, best_j = lax.top_k(cand_s, PEER_TOPK)
    experts = jnp.take_along_axis(cand_i, best_j, axis=-1)
    gates = jax.nn.softmax(best_s, axis=-1)
    nb = T // PEER_BLOCK
    ne = PEER_HEADS * PEER_TOPK

    def block(args):
        xb, eb, gb = args
        act = jax.nn.gelu(jnp.einsum("td,ted->te", xb, lp["peer_u"][eb], preferred_element_type=F32),
                          approximate=False)
        return jnp.einsum("te,ted->td", (gb * act).astype(xb.dtype), lp["peer_v"][eb])

    y = lax.map(block, (xt.reshape(nb, PEER_BLOCK, D), experts.reshape(nb, PEER_BLOCK, ne),
                        gates.reshape(nb, PEER_BLOCK, ne)))
    return y.reshape(B, L, D)


def context_layer(x, lp, c_ctx):
    B, L, _ = x.shape
    sh1, sc1, g1, sh2, sc2, g2 = ada_modulation(c_ctx[None, None, :], lp)
    h = modulate(x, lp["norm1_g"], sh1, sc1)
    cq, ckv, krope, dn_qkv, dn_z, dn_a, dn_b, hy_in, na_in, gate_logits = split_in(h @ lp["w_in"])
    ckv_n = rms_norm(ckv, lp["mla_kv_norm"])
    k, v = mla_keys_values(ckv_n, krope, lp)
    o_a = block_attention(mla_queries(cq, lp, rotate=False), k, v, MLA_SCALE).reshape(B, L, BRANCH_W)
    s0 = jnp.zeros((B, DN_HEADS, DN_DK, DN_DV), F32)
    o_b, s_f, s_b = gated_deltanet(dn_qkv, dn_z, dn_a, dn_b, lp, s0, s0)
    o_c = hyena(hy_in, lp)
    qn, kn, vn = na_split(na_in)
    o_d = block_attention(qn, kn, vn, NA_SCALE).reshape(B, L, BRANCH_W)
    x = x + g1 * merge_branches((o_a, o_b, o_c, o_d), gate_logits, lp)
    x = x + g2 * peer(modulate(x, lp["norm2_g"], sh2, sc2), lp)
    return x, ckv_n, krope, kn, vn, s_f.astype(x.dtype), s_b.astype(x.dtype)


def latent_layer(x, lp, c, ckv_ctx, krope_ctx, na_k_ctx, na_v_ctx, s_f0, s_b0):
    B, L, _ = x.shape
    sh1, sc1, g1, sh2, sc2, g2 = ada_modulation(c[:, None, :], lp)
    h = modulate(x, lp["norm1_g"], sh1, sc1)
    cq, ckv, krope, dn_qkv, dn_z, dn_a, dn_b, hy_in, na_in, gate_logits = split_in(h @ lp["w_in"])
    q = mla_queries(cq, lp, rotate=True)
    k_lat, v_lat = mla_keys_values(rms_norm(ckv, lp["mla_kv_norm"]),
                                   apply_axial_rope(krope[:, :, None, :])[:, :, 0, :], lp)
    k_ctx, v_ctx = mla_keys_values(ckv_ctx, krope_ctx, lp)
    o_a = block_attention(q, jnp.concatenate([k_lat, k_ctx], axis=1), jnp.concatenate([v_lat, v_ctx], axis=1),
                          MLA_SCALE).reshape(B, L, BRANCH_W)
    o_b, _, _ = gated_deltanet(dn_qkv, dn_z, dn_a, dn_b, lp, s_f0, s_b0)
    o_c = hyena(hy_in, lp)
    qn, kn, vn = na_split(na_in)
    o_d = na_latent(qn, kn, vn, na_k_ctx, na_v_ctx, lp["na_rpb"])
    x = x + g1 * merge_branches((o_a, o_b, o_c, o_d), gate_logits, lp)
    x = x + g2 * peer(modulate(x, lp["norm2_g"], sh2, sc2), lp)
    return x


def setup_inputs(seed: int = 0) -> dict:
    key = jax.random.key(seed)
    ks = iter(jax.random.split(key, 48))
    nrm = lambda shape, s: jax.random.normal(next(ks), shape, F32) * s
    gain = lambda shape: 1.0 + 0.01 * jax.random.normal(next(ks), shape, F32)
    dt = jnp.exp(jax.random.uniform(next(ks), (DEPTH, 2, DN_HEADS), F32, math.log(1e-3), math.log(1e-1)))
    return {
        "x_prompt": nrm((BATCH, SEQ, D_MODEL), 1.0),
        "x_sample": nrm((DEC_BATCH, DEC_SEQ, D_MODEL), 1.0),
        "cache_mla_ckv": nrm((DEC_BATCH, DEPTH, PAST_LEN, MLA_KV_RANK), 1.0),
        "cache_mla_krope": nrm((DEC_BATCH, DEPTH, PAST_LEN, MLA_ROPE), 1.0),
        "cache_na_k": nrm((DEC_BATCH, DEPTH, PAST_LEN, NA_HEADS, NA_HD), 1.0),
        "cache_na_v": nrm((DEC_BATCH, DEPTH, PAST_LEN, NA_HEADS, NA_HD), 1.0),
        "state_dn_fwd": nrm((DEC_BATCH, DEPTH, DN_HEADS, DN_DK, DN_DV), 0.1),
        "state_dn_bwd": nrm((DEC_BATCH, DEPTH, DN_HEADS, DN_DK, DN_DV), 0.1),
        "c": nrm((DEC_BATCH, D_MODEL), 1.0),
        "c_ctx": nrm((D_MODEL,), 1.0),
        "norm1_g": gain((DEPTH, D_MODEL)),
        "w_ada": nrm((DEPTH, D_MODEL, 6 * D_MODEL), 0.5 * D_MODEL ** -0.5),
        "b_ada": nrm((DEPTH, 6 * D_MODEL), 0.01),
        "w_in": nrm((DEPTH, D_MODEL, IN_COLS), D_MODEL ** -0.5),
        "mla_q_norm": gain((DEPTH, MLA_Q_RANK)),
        "mla_w_qb": nrm((DEPTH, MLA_Q_RANK, MLA_HEADS * (MLA_NOPE + MLA_ROPE)), MLA_Q_RANK ** -0.5),
        "mla_kv_norm": gain((DEPTH, MLA_KV_RANK)),
        "mla_w_kvb": nrm((DEPTH, MLA_KV_RANK, MLA_HEADS * (MLA_NOPE + MLA_V)), MLA_KV_RANK ** -0.5),
        "dn_conv": nrm((DEPTH, DN_CONV, DN_CONV_CH), DN_CONV ** -0.5),
        "dn_a_log": jnp.log(jax.random.uniform(next(ks), (DEPTH, 2, DN_HEADS), F32, 1.0, 16.0)),
        "dn_dt_bias": dt + jnp.log(-jnp.expm1(-dt)),
        "dn_out_norm": gain((DEPTH, DN_DV)),
        "hy_conv": nrm((DEPTH, HY_SHORT, (HY_ORDER + 1) * HY_WIDTH), HY_SHORT ** -0.5),
        "hy_w1": nrm((DEPTH, HY_EMB, HY_HID), HY_EMB ** -0.5),
        "hy_b1": nrm((DEPTH, HY_HID), 0.01),
        "hy_w2": nrm((DEPTH, HY_HID, HY_HID), HY_HID ** -0.5),
        "hy_b2": nrm((DEPTH, HY_HID), 0.01),
        "hy_w3": nrm((DEPTH, HY_HID, HY_ORDER * 2 * HY_WIDTH), 0.02),
        "hy_bias": nrm((DEPTH, HY_ORDER, HY_WIDTH), 0.1),
        "na_rpb": nrm((DEPTH, NA_HEADS, 2 * NA_KH - 1, 2 * NA_KW - 1), 0.02),
        "w_branch": nrm((DEPTH, N_BRANCH, BRANCH_W, D_MODEL), BRANCH_W ** -0.5),
        "w_out": nrm((DEPTH, D_MODEL, D_MODEL), D_MODEL ** -0.5),
        "norm2_g": gain((DEPTH, D_MODEL)),
        "peer_wq": nrm((DEPTH, D_MODEL, PEER_HEADS * PEER_DKEY), D_MODEL ** -0.5),
        "peer_keys": nrm((DEPTH, 2, PEER_NKEYS, PEER_DKEY // 2), (PEER_DKEY // 2) ** -0.5),
        "peer_u": nrm((DEPTH, PEER_N, D_MODEL), D_MODEL ** -0.5),
        "peer_v": nrm((DEPTH, PEER_N, D_MODEL), PEER_HEADS ** -0.5),
        "final_g": gain((D_MODEL,)),
    }


def reference(x_prompt, x_sample, cache_mla_ckv, cache_mla_krope, cache_na_k, cache_na_v, state_dn_fwd,
              state_dn_bwd, c, c_ctx, norm1_g, w_ada, b_ada, w_in, mla_q_norm, mla_w_qb, mla_kv_norm, mla_w_kvb,
              dn_conv, dn_a_log, dn_dt_bias, dn_out_norm, hy_conv, hy_w1, hy_b1, hy_w2, hy_b2, hy_w3, hy_bias,
              na_rpb, w_branch, w_out, norm2_g, peer_wq, peer_keys, peer_u, peer_v, final_g):
    layers = [dict(norm1_g=norm1_g[l], w_ada=w_ada[l], b_ada=b_ada[l], w_in=w_in[l], mla_q_norm=mla_q_norm[l],
                   mla_w_qb=mla_w_qb[l], mla_kv_norm=mla_kv_norm[l], mla_w_kvb=mla_w_kvb[l], dn_conv=dn_conv[l],
                   dn_a_log=dn_a_log[l], dn_dt_bias=dn_dt_bias[l], dn_out_norm=dn_out_norm[l], hy_conv=hy_conv[l],
                   hy_w1=hy_w1[l], hy_b1=hy_b1[l], hy_w2=hy_w2[l], hy_b2=hy_b2[l], hy_w3=hy_w3[l],
                   hy_bias=hy_bias[l], na_rpb=na_rpb[l], w_branch=w_branch[l], w_out=w_out[l],
                   norm2_g=norm2_g[l], peer_wq=peer_wq[l], peer_keys=peer_keys[l], peer_u=peer_u[l],
                   peer_v=peer_v[l])
              for l in range(DEPTH)]

    xp = x_prompt
    ctx = []
    for l in range(DEPTH):
        xp, ckv_n, krope, na_k, na_v, s_f, s_b = context_layer(xp, layers[l], c_ctx)
        ctx.append((ckv_n, krope, na_k, na_v, s_f, s_b))
    y_prompt = rms_norm(xp, final_g)

    xs = x_sample
    for l in range(DEPTH):
        xs = latent_layer(xs, layers[l], c, cache_mla_ckv[:, l], cache_mla_krope[:, l], cache_na_k[:, l],
                          cache_na_v[:, l], state_dn_fwd[:, l], state_dn_bwd[:, l])
    y_sample = rms_norm(xs, final_g)

    new_mla_ckv = jnp.stack([t[0] for t in ctx], axis=1)
    new_mla_krope = jnp.stack([t[1] for t in ctx], axis=1)
    new_na_k = jnp.stack([t[2] for t in ctx], axis=1)
    new_na_v = jnp.stack([t[3] for t in ctx], axis=1)
    new_dn_fwd = jnp.stack([t[4] for t in ctx], axis=1)
    new_dn_bwd = jnp.stack([t[5] for t in ctx], axis=1)
    return (y_prompt, y_sample, new_mla_ckv, new_mla_krope, new_na_k, new_na_v, new_dn_fwd, new_dn_bwd)
```

```python
import numpy as np
from contextlib import ExitStack
import concourse.bass as bass
import concourse.mybir as mybir
from concourse.bass_utils import run_bass_kernel_spmd

F32 = mybir.dt.float32
BF16 = mybir.dt.bfloat16
I32 = mybir.dt.int32
U32 = mybir.dt.uint32
U16 = mybir.dt.uint16
AF = mybir.ActivationFunctionType
ALU = mybir.AluOpType
AX = mybir.AxisListType

D = 2048
DEPTH = 2
NCORE = 8
LC = 256
LL = 2048
PAST = 512
NCTX = 2
TT = NCTX * LC + LL
NTILE = TT // 128
EPS = 1e-6

T_COLS = {}
_o = 0
for _n, _s in (("cq", 512), ("ckv", 256), ("kr", 64), ("z", 1024), ("a", 16), ("b", 16),
               ("nak", 1024), ("nav", 1024), ("gate", 8192)):
    T_COLS[_n] = (_o, _s)
    _o += _s
NT_COLS = _o
F_ROWS = {}
_o = 0
for _n, _s in (("krT", 64), ("krswT", 64), ("dnT", 3072), ("hyT", 3072), ("naqT", 1024), ("nakT", 1024)):
    F_ROWS[_n] = (_o, _s)
    _o += _s
NF_ROWS = _o

IN_SIZES = (512, 256, 64, 3072, 1024, 16, 16, 3072, 3072, 8192)
IN_OFF = np.concatenate([[0], np.cumsum(IN_SIZES)]).astype(int)
(O_CQ, O_CKV, O_KR, O_DN, O_Z, O_A, O_B, O_HY, O_NA, O_GATE) = [int(v) for v in IN_OFF[:-1]]


class Buf:
    __slots__ = ("t", "last_w", "readers", "name", "root")

    def __init__(self, t, name, root=None):
        self.t = t
        self.name = name
        self.last_w = None
        self.readers = {}
        self.root = root if root is not None else self

    def __getitem__(self, idx):
        return self.t[idx]


class Sched:
    ENG = ("pe", "act", "dve", "pool", "sp")
    NDMA = 6

    def __init__(self, nc, es):
        self.nc = nc
        self.es = es
        self.eng = {"pe": nc.tensor, "act": nc.scalar, "dve": nc.vector, "pool": nc.gpsimd, "sp": nc.sync}
        self.sem = {}
        self.cnt = {}
        for e in self.ENG:
            self.sem[e] = es.enter_context(nc.semaphore("sem_" + e))
            self.cnt[e] = 0
        self.dq = {}
        for q in ("sp", "act", "pool"):
            sl = []
            for i in range(self.NDMA):
                key = ("dma", q, i)
                self.sem[key] = es.enter_context(nc.semaphore("dsem_%s_%d" % (q, i)))
                self.cnt[key] = 0
                sl.append(key)
            self.dq[q] = [sl, 0]
        self.seen = {e: {} for e in self.ENG}
        self.ninstr = 0
        self.scope = es

    def _nm(self, name):
        self.nid = getattr(self, "nid", 0) + 1
        return "%s_%d" % (name, self.nid)

    def sbuf(self, name, shape, dtype=F32):
        name = self._nm(name)
        return Buf(self.scope.enter_context(self.nc.sbuf_tensor(name, list(shape), dtype)), name)

    def psum(self, name, shape, dtype=F32):
        name = self._nm(name)
        return Buf(self.scope.enter_context(self.nc.psum_tensor(name, list(shape), dtype)), name)

    def dram(self, name, shape, dtype=F32, kind="Internal"):
        t = self.nc.dram_tensor(name, list(shape), dtype, kind=kind)
        return Buf(t.ap(), name)

    def _wait(self, e, key, val):
        if self.seen[e].get(key, 0) >= val:
            return
        self.eng[e].wait_ge(self.sem[key], val)
        self.seen[e][key] = val
        self.ninstr += 1

    def _deps(self, e, reads, writes):
        reads = [b.root for b in reads]
        writes = [b.root for b in writes]
        need = {}
        for b in list(reads) + list(writes):
            if b.last_w is not None:
                k, v = b.last_w
                if need.get(k, 0) < v:
                    need[k] = v
        for b in writes:
            for k, v in b.readers.items():
                if need.get(k, 0) < v:
                    need[k] = v
        for k, v in need.items():
            if k == "pe" and e == "pe":
                continue
            self._wait(e, k, v)

    def _mark(self, ev, reads, writes):
        reads = [b.root for b in reads]
        writes = [b.root for b in writes]
        k, v = ev
        for b in writes:
            b.last_w = ev
            b.readers = {}
        for b in reads:
            if b.readers.get(k, 0) < v:
                b.readers[k] = v

    def op(self, e, fn, reads=(), writes=()):
        self._deps(e, reads, writes)
        ins = fn(self.eng[e])
        self.cnt[e] += 1
        ins.then_inc(self.sem[e], 1)
        self._mark((e, self.cnt[e]), reads, writes)
        self.ninstr += 1
        return ins

    def dma(self, q, fn, reads=(), writes=()):
        sl, i = self.dq[q]
        key = sl[i % self.NDMA]
        self.dq[q][1] = i + 1
        if self.cnt[key] > 0:
            self._wait(q, key, self.cnt[key])
        self._deps(q, reads, writes)
        ins = fn(self.eng[q])
        self.cnt[key] += 16
        ins.then_inc(self.sem[key], 16)
        self._mark((key, self.cnt[key]), reads, writes)
        self.ninstr += 1
        return ins

    def barrier(self):
        for e in self.ENG:
            for key, v in self.cnt.items():
                if v > 0:
                    self._wait(e, key, v)


import os as _os
DBG_BR = bool(int(_os.environ.get("KDBG_BR", "0")))
DBG_FILL = (2,)
DNSTOP = int(_os.environ.get("KDN_STOP", "9"))
DNVAR = int(_os.environ.get("KDN_VAR", "0"))
DNSEQ = int(_os.environ.get("KDN_SEQ", "3"))


def _hy_inputs(R, ein):
    R["hy_tab"] = []
    R["hy_zemb"] = []
    R["hy_wf"] = []
    R["hy_win"] = []
    for i, L in enumerate((LC, LL)):
        nf, KT, NB = (L + 1), (L + 1 + 127) // 128, (L + 1 + 511) // 512
        R["hy_tab"].append(ein("hy_tab%d" % i, [2, NB, 128, KT, 512], BF16))
        R["hy_zemb"].append(ein("hy_zemb%d" % i, [33, L]))
        R["hy_wf"].append(ein("hy_wf%d" % i, [128, KT]))
        R["hy_win"].append(ein("hy_win%d" % i, [L, 1024]))
    R["hy_w1"] = ein("hy_w1", [DEPTH, 33, 64])
    R["hy_w2"] = ein("hy_w2", [DEPTH, 64, 64])
    R["hy_w3"] = ein("hy_w3", [DEPTH, 64, 4096])
    R["hy_b12"] = ein("hy_b12", [DEPTH, 64, 2])
    R["hy_convT"] = ein("hy_convT", [DEPTH, 128, 24, 3])
    R["hy_biasT"] = ein("hy_biasT", [DEPTH, 2, 128, 8])


def _hy_scratch(S, R):
    R["hyZ"] = S.dram("hyZ", [3072, TT])
    R["hyY1"] = S.dram("hyY1", [1024, TT])
    R["hyU"] = S.dram("hyU", [TT, 1024], BF16)
    R["hyPQ"] = [S.dram("hyPQ%d" % i, [2, 2, ((L + 1 + 127) // 128) * 128, 1024]) for i, L in enumerate((LC, LL))]
    R["hyYS"] = S.dram("hyYS", [2, ((LL + 1 + 127) // 128) * 128, 1024], BF16)


def _hy_host(inp):
    import ml_dtypes
    H = {}
    for i, L in enumerate((LC, LL)):
        nf, KT, NB = (L + 1), (L + 1 + 127) // 128, (L + 1 + 511) // 512
        r = np.arange(KT * 128, dtype=np.int64)
        q = np.arange(NB * 512, dtype=np.int64)
        prod = (r[:, None] * q[None, :]) % (2 * L)
        ang = np.pi * prod.astype(np.float64) / L
        valid = (r[:, None] <= L) & (q[None, :] <= L)
        tabs = []
        for fn in (np.cos, np.sin):
            T = np.where(valid, fn(ang), 0.0).astype(np.float32)
            T = T.reshape(KT, 128, NB, 512).transpose(2, 1, 0, 3)
            tabs.append(T)
        H["hy_tab%d" % i] = np.ascontiguousarray(np.stack(tabs, 0)).astype(ml_dtypes.bfloat16)
        f = np.arange(KT * 128)
        wf = np.where((f == 0) | (f == L), 1.0, np.where(f < L, 2.0, 0.0)) / (2.0 * L)
        H["hy_wf%d" % i] = np.ascontiguousarray(wf.reshape(KT, 128).T).astype(np.float32)
        t01 = np.linspace(0.0, 1.0, L, dtype=np.float32)[:, None]
        w = (np.float32(2.0 * np.pi) * np.arange(L, dtype=np.float32)[:, None] / np.float32(L)).astype(np.float32)
        fr = np.linspace(1e-4, 15.0, 16, dtype=np.float32)[None, :]
        z = np.concatenate([t01, np.cos(fr * w), -np.sin(fr * w)], axis=-1).astype(np.float32)
        H["hy_zemb%d" % i] = np.ascontiguousarray(z.T)
        max_decay = np.log(1e-2) / 0.3
        min_decay = np.log(1e-2) / 1.5
        deltas = np.linspace(min_decay, max_decay, 1024, dtype=np.float32)
        H["hy_win%d" % i] = np.exp(-t01 * np.abs(deltas)[None, :]).astype(np.float32)
    H["hy_w1"] = inp["hy_w1"]
    H["hy_w2"] = inp["hy_w2"]
    H["hy_w3"] = inp["hy_w3"]
    H["hy_b12"] = np.ascontiguousarray(np.stack([inp["hy_b1"], inp["hy_b2"]], axis=-1))
    H["hy_convT"] = np.ascontiguousarray(inp["hy_conv"].reshape(DEPTH, 3, 24, 128).transpose(0, 3, 2, 1))
    H["hy_biasT"] = np.ascontiguousarray(inp["hy_bias"].reshape(DEPTH, 2, 8, 128).transpose(0, 1, 3, 2))
    return H


class K:
    pass


def _evac(S, i, out_ap, in_ap, reads, writes):
    if i % 2 == 0:
        S.op("act", lambda e: e.activation(out=out_ap, in_=in_ap, func=AF.Copy), reads=reads, writes=writes)
    else:
        S.op("dve", lambda e: e.tensor_copy(out=out_ap, in_=in_ap), reads=reads, writes=writes)


def _rmsnorm_rows(S, xt, g, n, jk, st):
    S.op("act", lambda e: e.activation(out=jk[:, 0:n], in_=xt[:, 0:n], func=AF.Square, accum_out=st[:]),
         reads=[xt], writes=[jk, st])
    S.op("act", lambda e: e.activation(out=st[:], in_=st[:], func=AF.Sqrt, scale=1.0 / n, bias=EPS),
         reads=[st], writes=[st])
    S.op("dve", lambda e: e.reciprocal(out=st[:], in_=st[:]), reads=[st], writes=[st])
    S.op("dve", lambda e: e.scalar_tensor_tensor(out=xt[:, 0:n], in0=xt[:, 0:n], scalar=st[:, 0:1], in1=g[:, 0:n],
                                                 op0=ALU.mult, op1=ALU.mult),
         reads=[xt, st, g], writes=[xt])


def _attn(S, A, qparts, kparts, v_ap, ktl, q0, QB, scale, bias_fn, out_dst):
    psO, psD = A["psO"][A["n"] % 2], A["psD"][A["n"] % 2]
    A["n"] += 1
    n = len(ktl)
    for i, kt in enumerate(ktl):
        ps = A["psS"][A["ns"] % 2]
        pT = A["pT"][A["ns"] % 3]
        A["ns"] += 1
        for pi in range(len(qparts)):
            kb, kf = kparts[pi]
            qb_, qf = qparts[pi]
            S.op("pe", lambda e: e.matmul(ps[:, 0:QB], lhsT=kf(kt), rhs=qf(q0, QB), start=(pi == 0),
                                          stop=(pi == len(qparts) - 1)), reads=[kb, qb_], writes=[ps])
        bb = bias_fn(kt) if bias_fn is not None else None
        if bb is not None:
            tf = A["tf"][A["ns"] % 2]
            S.op("dve", lambda e: e.scalar_tensor_tensor(out=tf[:, 0:QB], in0=ps[:, 0:QB], scalar=float(scale),
                                                         in1=bb[:, 0:QB], op0=ALU.mult, op1=ALU.add),
                 reads=[ps, bb], writes=[tf])
            S.op("act", lambda e: e.activation(out=pT[:, 0:QB], in_=tf[:, 0:QB], func=AF.Exp), reads=[tf], writes=[pT])
        else:
            S.op("act", lambda e: e.activation(out=pT[:, 0:QB], in_=ps[:, 0:QB], func=AF.Exp, scale=float(scale)),
                 reads=[ps], writes=[pT])
        vb, va = v_ap(kt)
        S.op("pe", lambda e: e.matmul(psO[:, 0:QB], lhsT=va, rhs=pT[:, 0:QB], start=(i == 0), stop=(i == n - 1)),
             reads=[vb, pT], writes=[psO])
        S.op("pe", lambda e: e.matmul(psD[:, 0:QB], lhsT=A["ones"][:, :], rhs=pT[:, 0:QB], start=(i == 0),
                                      stop=(i == n - 1)), reads=[A["ones"], pT], writes=[psD])
    rd = A["rd"]
    ob = A["ob"][A["n"] % 2]
    S.op("dve", lambda e: e.reciprocal(out=rd[:, 0:QB], in_=psD[:, 0:QB]), reads=[psD], writes=[rd])
    S.op("dve", lambda e: e.tensor_tensor(out=ob[:, 0:QB], in0=psO[:, 0:QB], in1=rd[:, 0:QB], op=ALU.mult),
         reads=[psO, rd], writes=[ob])
    dbuf, dap = out_dst
    S.dma("sp", lambda e: e.dma_start(out=dap, in_=ob[:, 0:QB]), reads=[ob], writes=[dbuf])


def _attn_res(S):
    A = {"n": 0, "ns": 0}
    A["psS"] = [S.psum("psS%d" % i, [128, 512], F32) for i in range(2)]
    A["psO"] = [S.psum("psO%d" % i, [128, 512], F32) for i in range(2)]
    A["psD"] = [S.psum("psD%d" % i, [128, 512], F32) for i in range(2)]
    A["pT"] = [S.sbuf("pT%d" % i, [128, 512], BF16) for i in range(3)]
    A["tf"] = [S.sbuf("tf%d" % i, [128, 512], F32) for i in range(2)]
    A["rd"] = S.sbuf("rd", [128, 512], F32)
    A["ob"] = [S.sbuf("ob%d" % i, [128, 512], BF16) for i in range(2)]
    ones = S.sbuf("onesb", [128, 128], BF16)
    S.op("dve", lambda e: e.memset(ones[:], 1.0), writes=[ones])
    A["ones"] = ones
    return A


def _transpose_rows(S, src, ncol, dst, dst_fn, ident, ptr, ev0=0):
    nch = ncol // 128
    for c0 in range(0, nch, 8):
        pt = ptr[(ev0 + c0 // 8) % 2]
        nn = min(8, nch - c0)
        for j in range(nn):
            S.op("pe", lambda e: e.transpose(pt[:, j, :], src[:, (c0 + j) * 128:(c0 + j + 1) * 128], ident[:]),
                 reads=[src, ident], writes=[pt])
        for j in range(nn):
            _evac(S, j, dst_fn(c0 + j), pt[:, j, :], reads=[pt], writes=[dst])


def stage_mla(S, l, R):
    ident = R["ident"]
    P_T, P_F, brT = R["P_T"], R["P_F"], R["brT"]
    with ExitStack() as sc:
        S.scope = sc
        A = _attn_res(S)
        psP = [S.psum("psP%d" % i, [128, 512], F32) for i in range(2)]
        wqb = S.sbuf("wqb", [128, 4, 2048], BF16)
        wkvb = S.sbuf("wkvb", [128, 2, 2048], BF16)
        S.dma("pool", lambda e: e.dma_start(out=wqb[:], in_=R["w_qb"][l].rearrange("(k p) c -> p k c", p=128)),
              reads=[R["w_qb"]], writes=[wqb])
        S.dma("pool", lambda e: e.dma_start(out=wkvb[:], in_=R["w_kvb"][l].rearrange("(k p) c -> p k c", p=128)),
              reads=[R["w_kvb"]], writes=[wkvb])
        gq = S.sbuf("gq", [128, 512], F32)
        gk = S.sbuf("gk", [128, 256], F32)
        S.dma("sp", lambda e: e.dma_start(out=gq[:], in_=R["mla_q_norm"][l, :].partition_broadcast(128)),
              reads=[R["mla_q_norm"]], writes=[gq])
        S.dma("sp", lambda e: e.dma_start(out=gk[:], in_=R["mla_kv_norm"][l, :].partition_broadcast(128)),
              reads=[R["mla_kv_norm"]], writes=[gk])
        ropc = S.sbuf("ropc", [64, LL], F32)
        rops = S.sbuf("rops", [64, LL], F32)
        S.dma("sp", lambda e: e.dma_start(out=ropc[:], in_=R["ropeT"][0]), reads=[R["ropeT"]], writes=[ropc])
        S.dma("sp", lambda e: e.dma_start(out=rops[:], in_=R["ropeT"][1]), reads=[R["ropeT"]], writes=[rops])
        cqT = S.sbuf("cqT", [128, 4, LL], BF16)
        ckvT = S.sbuf("ckvT", [128, 2, LL + PAST], BF16)
        krT = S.sbuf("krT", [128, LL + PAST], BF16)
        vall = S.sbuf("vall", [128, (LL + PAST) // 128, 1024], BF16)
        knT = S.sbuf("knT", [128, LL + PAST], BF16)
        qnT = S.sbuf("qnT", [128, LL], BF16)
        qrT = S.sbuf("qrT", [128, LL], BF16)
        S.op("dve", lambda e: e.memset(krT[64:128, :], 0.0), writes=[krT])
        S.op("dve", lambda e: e.memset(qrT[64:128, :], 0.0), writes=[qrT])
        xt = [S.sbuf("mx%d" % i, [128, 512], F32) for i in range(2)]
        xb = [S.sbuf("mxb%d" % i, [128, 512], BF16) for i in range(2)]
        jk = S.sbuf("mjk", [128, 512], F32)
        st = [S.sbuf("mst%d" % i, [128, 1], F32) for i in range(2)]
        r1 = S.sbuf("mr1", [64, 512], F32)
        r2 = S.sbuf("mr2", [64, 512], F32)
        ptr = [S.psum("mptr%d" % i, [128, 8, 128], BF16) for i in range(0)]
        aq, _ = T_COLS["cq"]
        ak, _ = T_COLS["ckv"]
        akr, _ = T_COLS["kr"]
        fkr, _ = F_ROWS["krT"]
        fks, _ = F_ROWS["krswT"]

        def tr_bf(src, ncol, dst, dst_fn, ev):
            nch = ncol // 128
            pt = psP[ev % 2]
            ptv = pt[:, :].bitcast(BF16)
            for j in range(nch):
                S.op("pe", lambda e: e.transpose(ptv[:, j * 128:(j + 1) * 128], src[:, j * 128:(j + 1) * 128], ident[:]),
                     reads=[src, ident], writes=[pt])
            for j in range(nch):
                _evac(S, j, dst_fn(j), ptv[:, j * 128:(j + 1) * 128], reads=[pt], writes=[dst])

        seqs = [(s * LC, LC, False) for s in range(NCTX)] + [(NCTX * LC, LL, True)]
        for si, (tok0, L, latent) in enumerate(seqs):
            Lk = L + (PAST if latent else 0)
            nkt = Lk // 128
            QB = 512 if latent else 256
            for t in range(L // 128):
                g0 = tok0 + t * 128
                x1, x1b, s1 = xt[t % 2], xb[t % 2], st[t % 2]
                S.dma("sp", lambda e: e.dma_start(out=x1[:, 0:512], in_=P_T[g0:g0 + 128, aq:aq + 512]),
                      reads=[P_T], writes=[x1])
                _rmsnorm_rows(S, x1, gq, 512, jk, s1)
                S.op("pool", lambda e: e.tensor_copy(out=x1b[:, 0:512], in_=x1[:, 0:512]), reads=[x1], writes=[x1b])
                tr_bf(x1b, 512, cqT, lambda j: cqT[:, j, t * 128:(t + 1) * 128], t)
            for t in range(nkt):
                x1, x1b, s1 = xt[t % 2], xb[t % 2], st[t % 2]
                if t < L // 128:
                    g0 = tok0 + t * 128
                    S.dma("sp", lambda e: e.dma_start(out=x1[:, 0:256], in_=P_T[g0:g0 + 128, ak:ak + 256]),
                          reads=[P_T], writes=[x1])
                    _rmsnorm_rows(S, x1, gk, 256, jk, s1)
                    if not latent:
                        S.dma("sp", lambda e: e.dma_start(out=R["o_ckv"][si, l, t * 128:(t + 1) * 128, :], in_=x1[:, 0:256]),
                              reads=[x1], writes=[R["o_ckv"]])
                else:
                    p0 = (t - L // 128) * 128
                    S.dma("sp", lambda e: e.dma_start(out=x1[:, 0:256], in_=R["cache_ckv"][l, p0:p0 + 128, :]),
                          reads=[R["cache_ckv"]], writes=[x1])
                S.op("pool", lambda e: e.tensor_copy(out=x1b[:, 0:256], in_=x1[:, 0:256]), reads=[x1], writes=[x1b])
                tr_bf(x1b, 256, ckvT, lambda j: ckvT[:, j, t * 128:(t + 1) * 128], t)
                if t >= L // 128:
                    p0 = (t - L // 128) * 128
                    S.dma("sp", lambda e: e.dma_start(out=x1[:, 256:320], in_=R["cache_kr"][l, p0:p0 + 128, :]),
                          reads=[R["cache_kr"]], writes=[x1])
                    S.op("pool", lambda e: e.tensor_copy(out=x1b[:, 256:320], in_=x1[:, 256:320]), reads=[x1], writes=[x1b])
                    pt = psP[(t + 1) % 2]
                    ptv = pt[:, :].bitcast(BF16)
                    S.op("pe", lambda e: e.transpose(ptv[0:64, 0:128], x1b[:, 256:320], ident[:]),
                         reads=[x1b, ident], writes=[pt])
                    _evac(S, t, krT[0:64, t * 128:(t + 1) * 128], ptv[0:64, 0:128], reads=[pt], writes=[krT])
            for g in range(L // QB):
                g0 = tok0 + g * QB
                S.dma("sp", lambda e: e.dma_start(out=r1[:, 0:QB], in_=P_F[fkr:fkr + 64, g0:g0 + QB]), reads=[P_F], writes=[r1])
                if latent:
                    S.dma("sp", lambda e: e.dma_start(out=r2[:, 0:QB], in_=P_F[fks:fks + 64, g0:g0 + QB]),
                          reads=[P_F], writes=[r2])
                    S.op("dve", lambda e: e.tensor_tensor(out=r1[:, 0:QB], in0=r1[:, 0:QB], in1=ropc[:, g * QB:(g + 1) * QB],
                                                          op=ALU.mult), reads=[r1, ropc], writes=[r1])
                    S.op("dve", lambda e: e.tensor_tensor(out=r2[:, 0:QB], in0=r2[:, 0:QB], in1=rops[:, g * QB:(g + 1) * QB],
                                                          op=ALU.mult), reads=[r2, rops], writes=[r2])
                    S.op("dve", lambda e: e.tensor_tensor(out=krT[0:64, g * QB:(g + 1) * QB], in0=r1[:, 0:QB], in1=r2[:, 0:QB],
                                                          op=ALU.add), reads=[r1, r2], writes=[krT])
                else:
                    S.op("dve", lambda e: e.tensor_copy(out=krT[0:64, g * QB:(g + 1) * QB], in_=r1[:, 0:QB]),
                         reads=[r1], writes=[krT])
            ev = 0
            for t in range(nkt):
                for hb in range(2):
                    ps = psP[ev % 2]
                    for kc in range(2):
                        S.op("pe", lambda e: e.matmul(ps[:, :], lhsT=ckvT[:, kc, t * 128:(t + 1) * 128],
                                                      rhs=wkvb[:, kc, 1024 + hb * 512:1024 + (hb + 1) * 512],
                                                      start=(kc == 0), stop=(kc == 1)), reads=[ckvT, wkvb], writes=[ps])
                    _evac(S, ev, vall[:, t, hb * 512:(hb + 1) * 512], ps[:, :], reads=[ps], writes=[vall])
                    ev += 1
            for h in range(8):
                for g in range((Lk + 511) // 512):
                    w = min(512, Lk - g * 512)
                    ps = psP[ev % 2]
                    for kc in range(2):
                        S.op("pe", lambda e: e.matmul(ps[:, 0:w], lhsT=wkvb[:, kc, h * 128:(h + 1) * 128],
                                                      rhs=ckvT[:, kc, g * 512:g * 512 + w], start=(kc == 0), stop=(kc == 1)),
                             reads=[ckvT, wkvb], writes=[ps])
                    _evac(S, ev, knT[:, g * 512:g * 512 + w], ps[:, 0:w], reads=[ps], writes=[knT])
                    ev += 1
                for g in range(L // QB):
                    ps = psP[ev % 2]
                    for kc in range(4):
                        S.op("pe", lambda e: e.matmul(ps[:, 0:QB], lhsT=wqb[:, kc, h * 128:(h + 1) * 128],
                                                      rhs=cqT[:, kc, g * QB:(g + 1) * QB], start=(kc == 0), stop=(kc == 3)),
                             reads=[cqT, wqb], writes=[ps])
                    _evac(S, ev, qnT[:, g * QB:(g + 1) * QB], ps[:, 0:QB], reads=[ps], writes=[qnT])
                    ev += 1
                    ps = psP[ev % 2]
                    for kc in range(4):
                        S.op("pe", lambda e: e.matmul(ps[0:64, 0:QB], lhsT=wqb[:, kc, 1024 + h * 64:1024 + (h + 1) * 64],
                                                      rhs=cqT[:, kc, g * QB:(g + 1) * QB], start=(kc == 0), stop=(kc == 3)),
                             reads=[cqT, wqb], writes=[ps])
                    if latent:
                        ps2 = psP[(ev + 1) % 2]
                        for kc in range(4):
                            S.op("pe", lambda e: e.matmul(ps2[0:64, 0:QB], lhsT=wqb[:, kc, 1536 + h * 64:1536 + (h + 1) * 64],
                                                          rhs=cqT[:, kc, g * QB:(g + 1) * QB], start=(kc == 0), stop=(kc == 3)),
                                 reads=[cqT, wqb], writes=[ps2])
                        S.op("dve", lambda e: e.tensor_tensor(out=r1[:, 0:QB], in0=ps[0:64, 0:QB],
                                                              in1=ropc[:, g * QB:(g + 1) * QB], op=ALU.mult),
                             reads=[ps, ropc], writes=[r1])
                        S.op("dve", lambda e: e.tensor_tensor(out=r2[:, 0:QB], in0=ps2[0:64, 0:QB],
                                                              in1=rops[:, g * QB:(g + 1) * QB], op=ALU.mult),
                             reads=[ps2, rops], writes=[r2])
                        S.op("dve", lambda e: e.tensor_tensor(out=qrT[0:64, g * QB:(g + 1) * QB], in0=r1[:, 0:QB],
                                                              in1=r2[:, 0:QB], op=ALU.add), reads=[r1, r2], writes=[qrT])
                        ev += 2
                    else:
                        _evac(S, ev, qrT[0:64, g * QB:(g + 1) * QB], ps[0:64, 0:QB], reads=[ps], writes=[qrT])
                        ev += 1
                for g in range(L // QB):
                    _attn(S, A,
                          qparts=[(qnT, lambda q0, n: qnT[:, q0:q0 + n]), (qrT, lambda q0, n: qrT[:, q0:q0 + n])],
                          kparts=[(knT, lambda kt: knT[:, kt * 128:(kt + 1) * 128]),
                                  (krT, lambda kt: krT[:, kt * 128:(kt + 1) * 128])],
                          v_ap=lambda kt: (vall, vall[:, kt, h * 128:(h + 1) * 128]),
                          ktl=list(range(nkt)), q0=g * QB, QB=QB, scale=192 ** -0.5, bias_fn=None,
                          out_dst=(brT[0], brT[0][h * 128:(h + 1) * 128, tok0 + g * QB:tok0 + (g + 1) * QB]))
        S.barrier()
    S.scope = S.es


NA_QB_TILES = {0: list(range(0, 6)), 1: list(range(2, 10)), 2: list(range(6, 14)), 3: list(range(10, 16))}
NA_MASK_IDX = {}
_i = 0
for _qb in range(4):
    for _kt in NA_QB_TILES[_qb]:
        NA_MASK_IDX[(_qb, _kt)] = _i
        _i += 1
NA_NMASK = _i


def stage_na(S, l, R):
    ident = R["ident"]
    P_T, P_F, brT = R["P_T"], R["P_F"], R["brT"]
    with ExitStack() as sc:
        S.scope = sc
        A = _attn_res(S)
        psP = [S.psum("psP%d" % i, [128, 512], F32) for i in range(2)]
        qT = S.sbuf("naqT", [128, LL], BF16)
        kT = S.sbuf("nakT", [128, LL + PAST], BF16)
        vall = S.sbuf("navall", [128, (LL + PAST) // 128, 1024], BF16)
        kc_f = [S.sbuf("nakc%d" % i, [128, 1024], F32) for i in range(2)]
        kc_b = S.sbuf("nakcb", [128, 4, 1024], BF16)
        bias = [S.sbuf("nabias%d" % i, [128, 512], F32) for i in range(2)]
        mask = [S.sbuf("namask%d" % i, [128, 512], F32) for i in range(2)]
        fq, _ = F_ROWS["naqT"]
        fk, _ = F_ROWS["nakT"]
        av, _ = T_COLS["nav"]
        seqs = [(s * LC, LC, False) for s in range(NCTX)] + [(NCTX * LC, LL, True)]
        nb = 0
        for si, (tok0, L, latent) in enumerate(seqs):
            Lk = L + (PAST if latent else 0)
            nkt = Lk // 128
            QB = 512 if latent else 256
            for t in range(L // 128):
                g0 = tok0 + t * 128
                S.dma("pool", lambda e: e.dma_start(out=vall[:, t, :], in_=P_T[g0:g0 + 128, av:av + 1024]),
                      reads=[P_T], writes=[vall])
            if latent:
                for t in range(4):
                    S.dma("pool", lambda e: e.dma_start(out=vall[:, L // 128 + t, :],
                                                        in_=R["cache_nav"][l, t * 128:(t + 1) * 128, :]),
                          reads=[R["cache_nav"]], writes=[vall])
                    S.dma("pool", lambda e: e.dma_start(out=kc_b[:, t, :], in_=R["cache_nak"][l, t * 128:(t + 1) * 128, :]),
                          reads=[R["cache_nak"]], writes=[kc_b])
            for h in range(8):
                S.dma("pool", lambda e: e.dma_start(out=qT[:, 0:L], in_=P_F[fq + h * 128:fq + (h + 1) * 128, tok0:tok0 + L]),
                      reads=[P_F], writes=[qT])
                S.dma("pool", lambda e: e.dma_start(out=kT[:, 0:L], in_=P_F[fk + h * 128:fk + (h + 1) * 128, tok0:tok0 + L]),
                      reads=[P_F], writes=[kT])
                if latent:
                    pt = psP[h % 2]
                    ptv = pt[:, :].bitcast(BF16)
                    for t in range(4):
                        S.op("pe", lambda e: e.transpose(ptv[:, t * 128:(t + 1) * 128], kc_b[:, t, h * 128:(h + 1) * 128],
                                                         ident[:]), reads=[kc_b, ident], writes=[pt])
                    _evac(S, h, kT[:, L:L + 512], ptv[:, 0:512], reads=[pt], writes=[kT])
                for g in range(L // QB):
                    if latent:
                        ktl = NA_QB_TILES[g] + [16, 17, 18, 19]

                        def bias_fn(kt, g=g, h=h):
                            if kt >= 16:
                                return None
                            bb, mm = bias[bias_fn.n % 2], mask[bias_fn.n % 2]
                            bias_fn.n += 1
                            for rr in range(2):
                                rp = 2 * kt + rr
                                mm0 = 15 - rp + 8 * g
                                S.dma("sp", lambda e: e.dma_start(
                                    out=bb[rr * 64:(rr + 1) * 64, :],
                                    in_=R["na_ctab"][l, h, :, mm0:mm0 + 8, :].rearrange("c m q -> c (m q)")),
                                    reads=[R["na_ctab"]], writes=[bb])
                            mi = NA_MASK_IDX[(g, kt)]
                            S.dma("sp", lambda e: e.dma_start(out=mm[:], in_=R["na_mask"][mi]), reads=[R["na_mask"]], writes=[mm])
                            S.op("pool", lambda e: e.tensor_tensor(out=bb[:], in0=bb[:], in1=mm[:], op=ALU.add),
                                 reads=[bb, mm], writes=[bb])
                            return bb
                        bias_fn.n = nb
                    else:
                        ktl = list(range(nkt))
                        bias_fn = None
                    _attn(S, A,
                          qparts=[(qT, lambda q0, n: qT[:, q0:q0 + n])],
                          kparts=[(kT, lambda kt: kT[:, kt * 128:(kt + 1) * 128])],
                          v_ap=lambda kt: (vall, vall[:, kt, h * 128:(h + 1) * 128]),
                          ktl=ktl, q0=g * QB, QB=QB, scale=128 ** -0.5, bias_fn=bias_fn,
                          out_dst=(brT[3], brT[3][h * 128:(h + 1) * 128, tok0 + g * QB:tok0 + (g + 1) * QB]))
                    if latent:
                        nb = bias_fn.n
        S.barrier()
    S.scope = S.es


def stage_merge(S, l, R, x_src):
    ident = R["ident"]
    P_T, brT, mrg, xbuf, modd = R["P_T"], R["brT"], R["mrg"], R["xbuf"], R["modd"]
    ag, _ = T_COLS["gate"]
    for b in range(4):
        with ExitStack() as sc:
            S.scope = sc
            wbr = S.sbuf("wbr", [128, 8, 2048], BF16)
            S.dma("pool", lambda e: e.dma_start(out=wbr[:], in_=R["w_branch"][l, b].rearrange("(k p) c -> p k c", p=128)),
                  reads=[R["w_branch"]], writes=[wbr])
            brt = [S.sbuf("brt%d" % i, [128, 8, 128], BF16) for i in range(2)]
            gt = [S.sbuf("gt%d" % i, [128, 2048], F32) for i in range(2)]
            acc = [S.sbuf("acc%d" % i, [128, 2048], F32) for i in range(2)]
            pm = [S.psum("pm%d" % i, [128, 512], F32) for i in range(4)]
            ev = 0
            for t in range(NTILE):
                bt, g, a = brt[t % 2], gt[t % 2], acc[t % 2]
                S.dma("sp", lambda e: e.dma_start(out=bt[:], in_=brT[b][:, t * 128:(t + 1) * 128].rearrange("(k p) t -> p k t", p=128)),
                      reads=[brT[b]], writes=[bt])
                S.dma("sp", lambda e: e.dma_start(out=g[:], in_=P_T[t * 128:(t + 1) * 128, ag + b * 2048:ag + (b + 1) * 2048]),
                      reads=[P_T], writes=[g])
                if b > 0:
                    S.dma("sp", lambda e: e.dma_start(out=a[:], in_=mrg[t * 128:(t + 1) * 128, :]), reads=[mrg], writes=[a])
                S.op("act", lambda e: e.activation(out=g[:], in_=g[:], func=AF.Sigmoid), reads=[g], writes=[g])
                for nb in range(4):
                    ps = pm[ev % 4]
                    ev += 1
                    for kc in range(8):
                        S.op("pe", lambda e: e.matmul(ps[:, :], lhsT=bt[:, kc, :], rhs=wbr[:, kc, nb * 512:(nb + 1) * 512],
                                                      start=(kc == 0), stop=(kc == 7)), reads=[bt, wbr], writes=[ps])
                    S.op("dve", lambda e: e.tensor_tensor(out=g[:, nb * 512:(nb + 1) * 512], in0=ps[:, :],
                                                          in1=g[:, nb * 512:(nb + 1) * 512], op=ALU.mult),
                         reads=[ps, g], writes=[g])
                if b > 0:
                    S.op("pool", lambda e: e.tensor_tensor(out=g[:], in0=g[:], in1=a[:], op=ALU.add), reads=[g, a], writes=[g])
                S.dma("sp", lambda e: e.dma_start(out=mrg[t * 128:(t + 1) * 128, :], in_=g[:]), reads=[g], writes=[mrg])
            S.barrier()
    with ExitStack() as sc:
        S.scope = sc
        wout = S.sbuf("wout", [128, 16, 2048], BF16)
        S.dma("pool", lambda e: e.dma_start(out=wout[:], in_=R["w_out"][l].rearrange("(k p) c -> p k c", p=128)),
              reads=[R["w_out"]], writes=[wout])
        g1b = [S.sbuf("g1b%d" % c, [128, 2048], F32) for c in range(2)]
        for c in range(2):
            S.dma("sp", lambda e: e.dma_start(out=g1b[c][:], in_=modd[c, 2 * D:3 * D].partition_broadcast(128)),
                  reads=[modd], writes=[g1b[c]])
        mt = [S.sbuf("mt%d" % i, [128, 2048], F32) for i in range(2)]
        mb = [S.sbuf("mb%d" % i, [128, 2048], BF16) for i in range(2)]
        mT = [S.sbuf("mT%d" % i, [128, 16, 128], BF16) for i in range(2)]
        xt = [S.sbuf("xo%d" % i, [128, 2048], F32) for i in range(2)]
        ptr = [S.psum("optr%d" % i, [128, 8, 128], BF16) for i in range(2)]
        pm = [S.psum("pmo%d" % i, [128, 512], F32) for i in range(4)]
        ev = 0
        for t in range(NTILE):
            c = 0 if t < (NCTX * LC) // 128 else 1
            m, mbb, mTT, x = mt[t % 2], mb[t % 2], mT[t % 2], xt[t % 2]
            S.dma("sp", lambda e: e.dma_start(out=m[:], in_=mrg[t * 128:(t + 1) * 128, :]), reads=[mrg], writes=[m])
            S.dma("sp", lambda e: e.dma_start(out=x[:], in_=x_src[t * 128:(t + 1) * 128, :]), reads=[x_src], writes=[x])
            S.op("pool", lambda e: e.tensor_copy(out=mbb[:], in_=m[:]), reads=[m], writes=[mbb])
            for half in range(2):
                pt = ptr[half]
                for j in range(8):
                    kc = half * 8 + j
                    S.op("pe", lambda e: e.transpose(pt[:, j, :], mbb[:, kc * 128:(kc + 1) * 128], ident[:]),
                         reads=[mbb, ident], writes=[pt])
                _evac(S, half, mTT[:, half * 8:(half + 1) * 8, :], pt[:, :, :], reads=[pt], writes=[mTT])
            for nb in range(4):
                ps = pm[ev % 4]
                ev += 1
                for kc in range(16):
                    S.op("pe", lambda e: e.matmul(ps[:, :], lhsT=mTT[:, kc, :], rhs=wout[:, kc, nb * 512:(nb + 1) * 512],
                                                  start=(kc == 0), stop=(kc == 15)), reads=[mTT, wout], writes=[ps])
                S.op("dve", lambda e: e.tensor_tensor(out=m[:, nb * 512:(nb + 1) * 512], in0=ps[:, :],
                                                      in1=g1b[c][:, nb * 512:(nb + 1) * 512], op=ALU.mult),
                     reads=[ps, g1b[c]], writes=[m])
            S.op("pool", lambda e: e.tensor_tensor(out=x[:], in0=x[:], in1=m[:], op=ALU.add), reads=[x, m], writes=[x])
            S.dma("sp", lambda e: e.dma_start(out=xbuf[t * 128:(t + 1) * 128, :], in_=x[:]), reads=[x], writes=[xbuf])
        S.barrier()
    S.scope = S.es


def stage_peer(S, l, R, last):
    ident = R["ident"]
    xbuf, modd = R["xbuf"], R["modd"]
    with ExitStack() as sc:
        S.scope = sc
        wq = S.sbuf("pwq", [128, 16, 2048], BF16)
        S.dma("pool", lambda e: e.dma_start(out=wq[:], in_=R["peer_wq"][l].rearrange("(k p) c -> p k c", p=128)),
              reads=[R["peer_wq"]], writes=[wq])
        identf = S.sbuf("identf", [128, 128], F32)
        S.dma("sp", lambda e: e.dma_start(out=identf[:], in_=R["ident_in"][:, :]), reads=[R["ident_in"]], writes=[identf])
        keysT = S.sbuf("keysT", [128, 2, 128], F32)
        S.dma("sp", lambda e: e.dma_start(out=keysT[:], in_=R["peer_keysT"][l].rearrange("s c n -> c s n")),
              reads=[R["peer_keysT"]], writes=[keysT])
        gm2 = S.sbuf("gm2", [128, 2048], F32)
        sh2 = S.sbuf("sh2", [128, 2048], F32)
        x1 = S.sbuf("px1", [128, 2048], F32)
        h2 = S.sbuf("ph2", [128, 2048], F32)
        h2b = S.sbuf("ph2b", [128, 2048], BF16)
        h2T = S.sbuf("ph2T", [128, 16, 128], BF16)
        qTf = S.sbuf("pqTf", [128, 16, 128], F32)
        scr = S.sbuf("pscr", [128, 16, 128], F32)
        scw = S.sbuf("pscw", [128, 128], F32)
        vals = S.sbuf("pvals", [128, 16, 16], F32)
        idx = S.sbuf("pidx", [128, 16, 16], U32)
        idxf = S.sbuf("pidxf", [128, 16, 16], F32)
        cand = S.sbuf("pcand", [128, 256], F32)
        candw = S.sbuf("pcandw", [128, 256], F32)
        cid = S.sbuf("pcid", [128, 256], F32)
        bs = S.sbuf("pbs", [128, 8, 16], F32)
        eq = S.sbuf("peq", [128, 8, 256], F32)
        eidf = S.sbuf("peidf", [128, 8, 16], F32)
        eidi = S.sbuf("peidi", [128, 128], I32)
        negb = S.sbuf("pnegb", [128, 8], F32)
        zs = S.sbuf("pzs", [128, 8], F32)
        gat = S.sbuf("pgat", [128, 8, 16], F32)
        actv = S.sbuf("pact", [128, 128], F32)
        coef = S.sbuf("pcoef", [128, 128], F32)
        yacc = S.sbuf("pyacc", [128, 2048], F32)
        qf = yacc
        jk = S.sbuf("pjk", [128, 2048], F32)
        st = S.sbuf("pst", [128, 1], F32)
        ug = [S.sbuf("pug%d" % i, [128, 2048], F32) for i in range(2)]
        ptr = [S.psum("pptr%d" % i, [128, 8, 128], BF16) for i in range(2)]
        pm = [S.psum("ppm%d" % i, [128, 512], F32) for i in range(4)]
        cur_c = -1
        for t in range(NTILE):
            c = 0 if t < (NCTX * LC) // 128 else 1
            if c != cur_c:
                cur_c = c
                S.dma("sp", lambda e: e.dma_start(out=sh2[:], in_=modd[c, 3 * D:4 * D].partition_broadcast(128)),
                      reads=[modd], writes=[sh2])
                S.dma("sp", lambda e: e.dma_start(out=gm2[:], in_=modd[c, 4 * D:5 * D].partition_broadcast(128)),
                      reads=[modd], writes=[gm2])
                S.dma("sp", lambda e: e.dma_start(out=jk[:], in_=R["norm2_g"][l, :].partition_broadcast(128)),
                      reads=[R["norm2_g"]], writes=[jk])
                S.op("dve", lambda e: e.scalar_tensor_tensor(out=gm2[:], in0=gm2[:], scalar=1.0, in1=jk[:],
                                                             op0=ALU.add, op1=ALU.mult), reads=[gm2, jk], writes=[gm2])
            S.dma("sp", lambda e: e.dma_start(out=x1[:], in_=xbuf[t * 128:(t + 1) * 128, :]), reads=[xbuf], writes=[x1])
            S.op("act", lambda e: e.activation(out=jk[:], in_=x1[:], func=AF.Square, accum_out=st[:]),
                 reads=[x1], writes=[jk, st])
            S.op("act", lambda e: e.activation(out=st[:], in_=st[:], func=AF.Sqrt, scale=1.0 / D, bias=EPS),
                 reads=[st], writes=[st])
            S.op("dve", lambda e: e.reciprocal(out=st[:], in_=st[:]), reads=[st], writes=[st])
            S.op("dve", lambda e: e.scalar_tensor_tensor(out=h2[:], in0=x1[:], scalar=st[:, 0:1], in1=gm2[:],
                                                         op0=ALU.mult, op1=ALU.mult), reads=[x1, st, gm2], writes=[h2])
            S.op("dve", lambda e: e.tensor_tensor(out=h2[:], in0=h2[:], in1=sh2[:], op=ALU.add), reads=[h2, sh2], writes=[h2])
            S.op("act", lambda e: e.activation(out=h2b[:], in_=h2[:], func=AF.Copy), reads=[h2], writes=[h2b])
            for half in range(2):
                pt = ptr[half]
                for j in range(8):
                    kc = half * 8 + j
                    S.op("pe", lambda e: e.transpose(pt[:, j, :], h2b[:, kc * 128:(kc + 1) * 128], ident[:]),
                         reads=[h2b, ident], writes=[pt])
                _evac(S, half, h2T[:, half * 8:(half + 1) * 8, :], pt[:, :, :], reads=[pt], writes=[h2T])
            for nb in range(4):
                ps = pm[nb]
                for kc in range(16):
                    S.op("pe", lambda e: e.matmul(ps[:, :], lhsT=h2T[:, kc, :], rhs=wq[:, kc, nb * 512:(nb + 1) * 512],
                                                  start=(kc == 0), stop=(kc == 15)), reads=[h2T, wq], writes=[ps])
                _evac(S, nb, qf[:, nb * 512:(nb + 1) * 512], ps[:, :], reads=[ps], writes=[qf])
            for b4 in range(4):
                ps = pm[b4]
                for j in range(4):
                    ch = b4 * 4 + j
                    S.op("pe", lambda e: e.transpose(ps[:, j * 128:(j + 1) * 128], qf[:, ch * 128:(ch + 1) * 128], identf[:]),
                         reads=[qf, identf], writes=[ps])
                _evac(S, b4, qTf[:, b4 * 4:(b4 + 1) * 4, :], ps[:, :].rearrange("p (a b) -> p a b", a=4), reads=[ps], writes=[qTf])
            for b4 in range(4):
                ps = pm[b4]
                for j in range(4):
                    ch = b4 * 4 + j
                    S.op("pe", lambda e: e.matmul(ps[:, j * 128:(j + 1) * 128], lhsT=qTf[:, ch, :], rhs=keysT[:, ch % 2, :],
                                                  start=True, stop=True), reads=[qTf, keysT], writes=[ps])
                _evac(S, b4 + 1, scr[:, b4 * 4:(b4 + 1) * 4, :], ps[:, :].rearrange("p (a b) -> p a b", a=4), reads=[ps], writes=[scr])
            for ch in range(16):
                S.op("dve", lambda e: e.max(out=vals[:, ch, 0:8], in_=scr[:, ch, :]), reads=[scr], writes=[vals])
                S.op("dve", lambda e: e.match_replace(out=scw[:, :], in_to_replace=vals[:, ch, 0:8], in_values=scr[:, ch, :],
                                                      imm_value=-1e30), reads=[vals, scr], writes=[scw])
                S.op("dve", lambda e: e.max(out=vals[:, ch, 8:16], in_=scw[:, :]), reads=[scw], writes=[vals])
                S.op("dve", lambda e: e.max_index(out=idx[:, ch, 0:8], in_max=vals[:, ch, 0:8], in_values=scr[:, ch, :]),
                     reads=[vals, scr], writes=[idx])
                S.op("dve", lambda e: e.max_index(out=idx[:, ch, 8:16], in_max=vals[:, ch, 8:16], in_values=scr[:, ch, :]),
                     reads=[vals, scr], writes=[idx])
            S.op("dve", lambda e: e.tensor_copy(out=idxf[:], in_=idx[:]), reads=[idx], writes=[idxf])
            for h in range(8):
                c3 = cand[:, :].rearrange("p (a b) -> p a b", a=16)
                i3 = cid[:, :].rearrange("p (a b) -> p a b", a=16)
                S.op("dve", lambda e: e.tensor_tensor(out=c3, in0=vals[:, 2 * h, :].unsqueeze(2).to_broadcast([128, 16, 16]),
                                                      in1=vals[:, 2 * h + 1, :].unsqueeze(1).to_broadcast([128, 16, 16]),
                                                      op=ALU.add), reads=[vals], writes=[cand])
                S.op("dve", lambda e: e.scalar_tensor_tensor(out=i3, in0=idxf[:, 2 * h, :].unsqueeze(2).to_broadcast([128, 16, 16]),
                                                             scalar=128.0,
                                                             in1=idxf[:, 2 * h + 1, :].unsqueeze(1).to_broadcast([128, 16, 16]),
                                                             op0=ALU.mult, op1=ALU.add), reads=[idxf], writes=[cid])
                S.op("dve", lambda e: e.max(out=bs[:, h, 0:8], in_=cand[:, :]), reads=[cand], writes=[bs])
                S.op("dve", lambda e: e.match_replace(out=candw[:, :], in_to_replace=bs[:, h, 0:8], in_values=cand[:, :],
                                                      imm_value=-1e30), reads=[bs, cand], writes=[candw])
                S.op("dve", lambda e: e.max(out=bs[:, h, 8:16], in_=candw[:, :]), reads=[candw], writes=[bs])
                for hf in range(2):
                    S.op("dve", lambda e: e.tensor_tensor(out=eq[:], in0=cand[:, :].unsqueeze(1).to_broadcast([128, 8, 256]),
                                                          in1=bs[:, h, hf * 8:(hf + 1) * 8].unsqueeze(2).to_broadcast([128, 8, 256]),
                                                          op=ALU.is_equal), reads=[cand, bs], writes=[eq])
                    S.op("dve", lambda e: e.tensor_tensor(out=eq[:], in0=eq[:],
                                                          in1=cid[:, :].unsqueeze(1).to_broadcast([128, 8, 256]),
                                                          op=ALU.mult), reads=[eq, cid], writes=[eq])
                    S.op("dve", lambda e: e.tensor_reduce(out=eidf[:, h, hf * 8:(hf + 1) * 8], in_=eq[:], axis=AX.X, op=ALU.add),
                         reads=[eq], writes=[eidf])
            S.op("dve", lambda e: e.tensor_copy(out=eidi[:, :].rearrange("p (a b) -> p a b", a=8), in_=eidf[:]),
                 reads=[eidf], writes=[eidi])
            S.op("dve", lambda e: e.tensor_scalar(out=negb[:], in0=bs[:, :, 0], scalar1=-1.0, scalar2=None, op0=ALU.mult),
                 reads=[bs], writes=[negb])
            for h in range(8):
                S.op("act", lambda e: e.activation(out=gat[:, h, :], in_=bs[:, h, :], func=AF.Exp, bias=negb[:, h:h + 1],
                                                   accum_out=zs[:, h:h + 1]), reads=[bs, negb], writes=[gat, zs])
            S.op("dve", lambda e: e.reciprocal(out=zs[:], in_=zs[:]), reads=[zs], writes=[zs])
            S.op("dve", lambda e: e.tensor_tensor(out=gat[:], in0=gat[:], in1=zs[:, :].unsqueeze(2).to_broadcast([128, 8, 16]),
                                                  op=ALU.mult), reads=[gat, zs], writes=[gat])
            for s in range(128):
                u = ug[s % 2]
                S.dma("pool", lambda e: e.indirect_dma_start(
                    out=u[:], out_offset=None, in_=R["peer_u"][l][:, :],
                    in_offset=bass.IndirectOffsetOnAxis(ap=eidi[:, s:s + 1], axis=0)),
                    reads=[R["peer_u"][l], eidi], writes=[u])
                S.op("dve", lambda e: e.scalar_tensor_tensor(out=jk[:], in0=u[:], scalar=1.0, in1=h2[:],
                                                             op0=ALU.mult, op1=ALU.mult, accum_out=actv[:, s:s + 1]),
                     reads=[u, h2], writes=[jk, actv])
            S.op("act", lambda e: e.activation(out=actv[:], in_=actv[:], func=AF.Gelu), reads=[actv], writes=[actv])
            S.op("dve", lambda e: e.tensor_tensor(out=coef[:], in0=actv[:], in1=gat[:, :, :].rearrange("p a b -> p (a b)"),
                                                  op=ALU.mult), reads=[actv, gat], writes=[coef])
            for s in range(128):
                u = ug[s % 2]
                S.dma("pool", lambda e: e.indirect_dma_start(
                    out=u[:], out_offset=None, in_=R["peer_v"][l][:, :],
                    in_offset=bass.IndirectOffsetOnAxis(ap=eidi[:, s:s + 1], axis=0)),
                    reads=[R["peer_v"][l], eidi], writes=[u])
                if s == 0:
                    S.op("dve", lambda e: e.tensor_scalar(out=yacc[:], in0=u[:], scalar1=coef[:, 0:1], scalar2=None,
                                                          op0=ALU.mult), reads=[u, coef], writes=[yacc])
                else:
                    S.op("dve", lambda e: e.scalar_tensor_tensor(out=yacc[:], in0=u[:], scalar=coef[:, s:s + 1], in1=yacc[:],
                                                                 op0=ALU.mult, op1=ALU.add), reads=[u, coef, yacc], writes=[yacc])
            S.dma("sp", lambda e: e.dma_start(out=jk[:], in_=modd[c, 5 * D:6 * D].partition_broadcast(128)),
                  reads=[modd], writes=[jk])
            S.op("dve", lambda e: e.tensor_tensor(out=yacc[:], in0=yacc[:], in1=jk[:], op=ALU.mult), reads=[yacc, jk], writes=[yacc])
            S.op("dve", lambda e: e.tensor_tensor(out=x1[:], in0=x1[:], in1=yacc[:], op=ALU.add), reads=[x1, yacc], writes=[x1])
            if not last:
                S.dma("sp", lambda e: e.dma_start(out=xbuf[t * 128:(t + 1) * 128, :], in_=x1[:]), reads=[x1], writes=[xbuf])
            else:
                S.op("act", lambda e: e.activation(out=jk[:], in_=x1[:], func=AF.Square, accum_out=st[:]),
                     reads=[x1], writes=[jk, st])
                S.op("act", lambda e: e.activation(out=st[:], in_=st[:], func=AF.Sqrt, scale=1.0 / D, bias=EPS),
                     reads=[st], writes=[st])
                S.op("dve", lambda e: e.reciprocal(out=st[:], in_=st[:]), reads=[st], writes=[st])
                S.dma("sp", lambda e: e.dma_start(out=jk[:], in_=R["final_g"][0, :].partition_broadcast(128)),
                      reads=[R["final_g"]], writes=[jk])
                S.op("dve", lambda e: e.scalar_tensor_tensor(out=x1[:], in0=x1[:], scalar=st[:, 0:1], in1=jk[:],
                                                             op0=ALU.mult, op1=ALU.mult), reads=[x1, st, jk], writes=[x1])
                S.dma("sp", lambda e: e.dma_start(out=R["y_out"][t * 128:(t + 1) * 128, :], in_=x1[:]), reads=[x1], writes=[R["y_out"]])
        S.barrier()
    S.scope = S.es


def stage_dn(S, l, R):
    P_T, P_F, brT = R["P_T"], R["P_F"], R["brT"]
    fdn, _ = F_ROWS["dnT"]
    az, _ = T_COLS["z"]
    aa, _ = T_COLS["a"]
    with ExitStack() as sc:
        S.scope = sc
        cst = S.sbuf("dncst", [128, 10, 128], F32)
        S.dma("sp", lambda e: e.dma_start(out=cst[:], in_=R["dn_consts"][:, :, :].rearrange("m p q -> p m q")),
              reads=[R["dn_consts"]], writes=[cst])
        onesf = S.sbuf("dnones", [128, 128], F32)
        S.op("dve", lambda e: e.memset(onesf[:], 1.0), writes=[onesf])
        identf = S.sbuf("dnidentf", [128, 128], F32)
        S.dma("sp", lambda e: e.dma_start(out=identf[:], in_=R["dn_consts"][2]), reads=[R["dn_consts"]], writes=[identf])
        identb = R["ident"]
        cw = S.sbuf("dncw", [128, 24, 5], F32)
        S.dma("sp", lambda e: e.dma_start(out=cw[:], in_=R["dn_convT"][l]), reads=[R["dn_convT"]], writes=[cw])
        alog = S.sbuf("dnalog", [128, 16], F32)
        dtb = S.sbuf("dndtb", [128, 16], F32)
        S.dma("sp", lambda e: e.dma_start(out=alog[:], in_=R["dn_a_log"][l, :].partition_broadcast(128)),
              reads=[R["dn_a_log"]], writes=[alog])
        S.dma("sp", lambda e: e.dma_start(out=dtb[:], in_=R["dn_dt_bias"][l, :].partition_broadcast(128)),
              reads=[R["dn_dt_bias"]], writes=[dtb])
        S.op("act", lambda e: e.activation(out=alog[:], in_=alog[:], func=AF.Exp), reads=[alog], writes=[alog])
        gon = S.sbuf("dngon", [128, 128], F32)
        S.dma("sp", lambda e: e.dma_start(out=gon[:], in_=R["dn_out_norm"][l, :].partition_broadcast(128)),
              reads=[R["dn_out_norm"]], writes=[gon])
        NCH = LL // 128
        xin = S.sbuf("dnxin", [128, LL + 4], F32)
        acc = S.sbuf("dnacc", [128, LL], F32)
        qT = S.sbuf("dnqT", [128, LL], F32)
        kT = S.sbuf("dnkT", [128, LL], F32)
        vT = S.sbuf("dnvT", [128, LL], F32)
        ktok = S.sbuf("dnktok", [128, NCH, 128], F32)
        vtok = S.sbuf("dnvtok", [128, NCH, 128], F32)
        oall = S.sbuf("dnoall", [128, NCH, 1024], F32)
        gt = S.sbuf("dng", [128, NCH, 16], F32)
        bt = S.sbuf("dnb", [128, NCH, 16], F32)
        gc = S.sbuf("dngc", [128, NCH, 16], F32)
        egc = S.sbuf("dnegc", [128, NCH, 16], F32)
        bg = S.sbuf("dnbg", [128, NCH, 16], F32)
        egl = S.sbuf("dnegl", [128, NCH, 16], F32)
        edl = S.sbuf("dnedl", [128, NCH, 16], F32)
        ab = S.sbuf("dnab", [128, 32], F32)
        sq = S.sbuf("dnsq", [128, 512], F32)
        rn = S.sbuf("dnrn", [128, 512], F32)
        Sst = S.sbuf("dnS", [128, 128], F32)
        Gm = S.sbuf("dnGm", [128, 128], F32)
        EA = S.sbuf("dnEA", [128, 128], F32)
        ET = S.sbuf("dnET", [128, 128], F32)
        Am = S.sbuf("dnA", [128, 128], F32)
        Bk = [S.sbuf("dnB%d" % i, [128, 128], F32) for i in range(6)]
        Ad = S.sbuf("dnAd", [128, 128], F32)
        Aoff = S.sbuf("dnAoff", [128, 128], F32)
        Boff = S.sbuf("dnBoff", [128, 128], F32)
        Zb = S.sbuf("dnZb", [128, 256], F32)
        Ck = [S.sbuf("dnC%d" % i, [128, 128], F32) for i in range(2)]
        qkT = S.sbuf("dnqkT", [128, 128], F32)
        X = S.sbuf("dnX", [128, 256], F32)
        wT = S.sbuf("dnwT", [128, 128], F32)
        vnew = S.sbuf("dnvnew", [128, 128], F32)
        kd = S.sbuf("dnkd", [128, 128], F32)
        tmp = S.sbuf("dntmp", [128, 128], F32)
        zt = S.sbuf("dnz", [128, 1024], F32)
        ob = S.sbuf("dnob", [128, 1024], BF16)
        obT = S.sbuf("dnobT", [128, 8, 128], BF16)
        ssq = S.sbuf("dnssq", [128, 8], F32)
        pqb = [S.psum("dnpqb%d" % i, [128, 512], F32) for i in range(6)]
        pbig = pqb[0:2]
        pq = [Buf(pqb[i].t[:, 0:128], "dnpq%d" % i, root=pqb[i]) for i in range(6)]
        pxb = S.psum("dnpxb", [128, 512], F32)
        px = [Buf(pxb.t[:, i * 256:(i + 1) * 256], "dnpx%d" % i, root=pxb) for i in range(2)]
        ptr = S.psum("dnptr", [128, 8, 128], BF16)
        npq = [0]

        def PQ():
            npq[0] += 1
            return pq[npq[0] % 6]

        seqs = [(s * LC, LC, False) for s in range(NCTX)] + [(NCTX * LC, LL, True)]
        seqs = seqs[:DNSEQ]
        for si, (tok0, L, latent) in enumerate(seqs):
            nch = L // 128
            for n in range(nch):
                g0 = tok0 + n * 128
                S.dma("sp", lambda e: e.dma_start(out=ab[:], in_=P_T[g0:g0 + 128, aa:aa + 32]), reads=[P_T], writes=[ab])
                S.op("dve", lambda e: e.tensor_tensor(out=gt[:, n, :], in0=ab[:, 0:16], in1=dtb[:], op=ALU.add),
                     reads=[ab, dtb], writes=[gt])
                S.op("act", lambda e: e.activation(out=gt[:, n, :], in_=gt[:, n, :], func=AF.Exp), reads=[gt], writes=[gt])
                S.op("act", lambda e: e.activation(out=gt[:, n, :], in_=gt[:, n, :], func=AF.Ln, bias=1.0), reads=[gt], writes=[gt])
                S.op("dve", lambda e: e.scalar_tensor_tensor(out=gt[:, n, :], in0=gt[:, n, :], scalar=-1.0, in1=alog[:],
                                                             op0=ALU.mult, op1=ALU.mult), reads=[gt, alog], writes=[gt])
                S.op("act", lambda e: e.activation(out=bt[:, n, :], in_=ab[:, 16:32], func=AF.Sigmoid), reads=[ab], writes=[bt])
                p1 = PQ()
                for d in range(2):
                    S.op("pe", lambda e: e.matmul(p1[:, d * 8:(d + 1) * 8], lhsT=cst[:, d, :], rhs=gt[:, n, d * 8:(d + 1) * 8],
                                                  start=True, stop=True), reads=[cst, gt], writes=[p1])
                S.op("pe", lambda e: e.matmul(p1[:, 16:32], lhsT=onesf[:, :], rhs=gt[:, n, :], start=True, stop=True),
                     reads=[onesf, gt], writes=[p1])
                S.op("dve", lambda e: e.tensor_copy(out=gc[:, n, :], in_=p1[:, 0:16]), reads=[p1], writes=[gc])
                S.op("act", lambda e: e.activation(out=egc[:, n, :], in_=p1[:, 0:16], func=AF.Exp), reads=[p1], writes=[egc])
                S.op("act", lambda e: e.activation(out=egl[:, n, :], in_=p1[:, 16:32], func=AF.Exp), reads=[p1], writes=[egl])
                S.op("dve", lambda e: e.tensor_tensor(out=edl[:, n, :], in0=p1[:, 16:32], in1=gc[:, n, :], op=ALU.subtract),
                     reads=[p1, gc], writes=[edl])
                S.op("act", lambda e: e.activation(out=edl[:, n, :], in_=edl[:, n, :], func=AF.Exp), reads=[edl], writes=[edl])
                S.op("dve", lambda e: e.tensor_tensor(out=bg[:, n, :], in0=bt[:, n, :], in1=egc[:, n, :], op=ALU.mult),
                     reads=[bt, egc], writes=[bg])
            for h in range(8):
                if DNSTOP < 2:
                    continue
                for which, dst in ((0, qT), (1, kT), (2, vT)):
                    ch = which * 8 + h
                    r0 = fdn + ch * 128
                    S.op("pool", lambda e: e.memset(xin[:, 0:2], 0.0), writes=[xin])
                    S.op("pool", lambda e: e.memset(xin[:, L + 2:L + 4], 0.0), writes=[xin])
                    S.dma("sp", lambda e: e.dma_start(out=xin[:, 2:L + 2], in_=P_F[r0:r0 + 128, tok0:tok0 + L]),
                          reads=[P_F], writes=[xin])
                    S.op("dve", lambda e: e.tensor_scalar(out=acc[:, 0:L], in0=xin[:, 0:L], scalar1=cw[:, ch, 0:1], scalar2=None,
                                                          op0=ALU.mult), reads=[xin, cw], writes=[acc])
                    for kk in range(1, 5):
                        S.op("dve", lambda e: e.scalar_tensor_tensor(out=acc[:, 0:L], in0=xin[:, kk:kk + L],
                                                                     scalar=cw[:, ch, kk:kk + 1], in1=acc[:, 0:L],
                                                                     op0=ALU.mult, op1=ALU.add), reads=[xin, cw, acc], writes=[acc])
                    S.op("act", lambda e: e.activation(out=dst[:, 0:L], in_=acc[:, 0:L], func=AF.Silu), reads=[acc], writes=[dst])
                    if which < 2:
                        for g in range((L + 511) // 512):
                            w = min(512, L - g * 512)
                            pb = pbig[g % 2]
                            S.op("act", lambda e: e.activation(out=sq[:, 0:w], in_=dst[:, g * 512:g * 512 + w], func=AF.Square),
                                 reads=[dst], writes=[sq])
                            S.op("pe", lambda e: e.matmul(pb[:, 0:w], lhsT=onesf[:, :], rhs=sq[:, 0:w], start=True, stop=True),
                                 reads=[onesf, sq], writes=[pb])
                            sc_ = 128.0 if which == 0 else 1.0
                            S.op("act", lambda e: e.activation(out=rn[:, 0:w], in_=pb[:, 0:w], func=AF.Sqrt, scale=sc_,
                                                               bias=sc_ * EPS), reads=[pb], writes=[rn])
                            S.op("dve", lambda e: e.reciprocal(out=rn[:, 0:w], in_=rn[:, 0:w]), reads=[rn], writes=[rn])
                            S.op("dve", lambda e: e.tensor_tensor(out=dst[:, g * 512:g * 512 + w], in0=dst[:, g * 512:g * 512 + w],
                                                                  in1=rn[:, 0:w], op=ALU.mult), reads=[dst, rn], writes=[dst])
                if DNSTOP < 3:
                    continue
                for n in range(nch):
                    for src, dd in ((kT, ktok), (vT, vtok)):
                        p1 = PQ()
                        S.op("pe", lambda e: e.transpose(p1[:, 0:128], src[:, n * 128:(n + 1) * 128], identf[:]),
                             reads=[src, identf], writes=[p1])
                        _evac(S, n, dd[:, n, :], p1[:, 0:128], reads=[p1], writes=[dd])
                for d in range(2):
                    if DNSTOP < 4:
                        continue
                    dh = d * 8 + h
                    if latent:
                        S.dma("sp", lambda e: e.dma_start(out=Sst[:], in_=R["dn_state"][d][l, h, :, :]),
                              reads=[R["dn_state"][d]], writes=[Sst])
                    else:
                        S.op("dve", lambda e: e.memset(Sst[:], 0.0), writes=[Sst])
                    order = range(nch) if d == 0 else range(nch - 1, -1, -1)
                    for n in order:
                        c0 = n * 128
                        S.op("dve", lambda e: e.tensor_scalar(out=Gm[:], in0=cst[:, 3 + d, :], scalar1=gt[:, n, dh:dh + 1],
                                                              scalar2=None, op0=ALU.mult), reads=[cst, gt], writes=[Gm])
                        pA, pT_, pkk, pkq = PQ(), PQ(), PQ(), PQ()
                        S.op("pe", lambda e: e.matmul(pA[:, :], lhsT=cst[:, d, :], rhs=Gm[:, :], start=True, stop=False),
                             reads=[cst, Gm], writes=[pA])
                        S.op("pe", lambda e: e.matmul(pA[:, :], lhsT=cst[:, 2, :], rhs=cst[:, 5 + d, :], start=False, stop=True),
                             reads=[cst], writes=[pA])
                        S.op("pe", lambda e: e.matmul(pT_[:, :], lhsT=Gm[:, :], rhs=cst[:, d, :], start=True, stop=False),
                             reads=[cst, Gm], writes=[pT_])
                        S.op("pe", lambda e: e.matmul(pT_[:, :], lhsT=cst[:, 2, :], rhs=cst[:, 7 + d, :], start=False, stop=True),
                             reads=[cst], writes=[pT_])
                        S.op("pe", lambda e: e.matmul(pkk[:, :], lhsT=kT[:, c0:c0 + 128], rhs=kT[:, c0:c0 + 128], start=True,
                                                      stop=True), reads=[kT], writes=[pkk])
                        S.op("pe", lambda e: e.matmul(pkq[:, :], lhsT=kT[:, c0:c0 + 128], rhs=qT[:, c0:c0 + 128], start=True,
                                                      stop=True), reads=[kT, qT], writes=[pkq])
                        S.op("act", lambda e: e.activation(out=EA[:], in_=pA[:, :], func=AF.Exp), reads=[pA], writes=[EA])
                        S.op("act", lambda e: e.activation(out=ET[:], in_=pT_[:, :], func=AF.Exp), reads=[pT_], writes=[ET])
                        S.op("dve", lambda e: e.scalar_tensor_tensor(out=Am[:], in0=pkk[:, :], scalar=bt[:, n, dh:dh + 1], in1=EA[:],
                                                                     op0=ALU.mult, op1=ALU.mult), reads=[pkk, bt, EA], writes=[Am])
                        S.op("dve", lambda e: e.tensor_tensor(out=qkT[:], in0=pkq[:, :], in1=ET[:], op=ALU.mult),
                             reads=[pkq, ET], writes=[qkT])
                        S.op("pool", lambda e: e.tensor_scalar(out=X[:, 0:128], in0=vtok[:, n, :], scalar1=bt[:, n, dh:dh + 1],
                                                               scalar2=None, op0=ALU.mult), reads=[vtok, bt], writes=[X])
                        S.op("pool", lambda e: e.tensor_scalar(out=X[:, 128:256], in0=ktok[:, n, :], scalar1=bg[:, n, dh:dh + 1],
                                                               scalar2=None, op0=ALU.mult), reads=[ktok, bg], writes=[X])
                        if DNSTOP < 5:
                            continue
                        S.op("dve", lambda e: e.tensor_tensor(out=Ad[:], in0=Am[:], in1=cst[:, 9, :], op=ALU.mult),
                             reads=[Am, cst], writes=[Ad])
                        S.op("pool", lambda e: e.tensor_tensor(out=Aoff[:], in0=Am[:], in1=Ad[:], op=ALU.subtract),
                             reads=[Am, Ad], writes=[Aoff])
                        pt1, pt2 = PQ(), PQ()
                        S.op("pe", lambda e: e.transpose(pt1[:, :], Ad[:, :], identf[:]), reads=[Ad, identf], writes=[pt1])
                        S.op("pe", lambda e: e.transpose(pt2[:, :], Aoff[:, :], identf[:]), reads=[Aoff, identf], writes=[pt2])
                        S.op("act", lambda e: e.activation(out=Bk[0][:], in_=pt1[:, :], func=AF.Copy), reads=[pt1], writes=[Bk[0]])
                        S.op("act", lambda e: e.activation(out=Boff[:], in_=pt2[:, :], func=AF.Copy), reads=[pt2], writes=[Boff])
                        Cc = Ad
                        for k_ in range(6):
                            Bc = Bk[k_]
                            pxx = px[k_ % 2]
                            S.op("pe", lambda e: e.matmul(pxx[:, :], lhsT=Bc[:, :], rhs=X[:, :], start=True, stop=True),
                                 reads=[Bc, X], writes=[pxx])
                            S.op("dve", lambda e: e.tensor_tensor(out=X[:], in0=X[:], in1=pxx[:, :],
                                                                  op=(ALU.subtract if k_ == 0 else ALU.add)),
                                 reads=[X, pxx], writes=[X])
                            if k_ < 5:
                                Bn, Cn = Bk[k_ + 1], Ck[(k_ + 1) % 2]
                                pb_, pc_ = PQ(), PQ()
                                S.op("pe", lambda e: e.matmul(pb_[:, :], lhsT=Cc[:, :], rhs=Bc[:, :], start=True, stop=True),
                                     reads=[Cc, Bc], writes=[pb_])
                                S.op("pe", lambda e: e.matmul(pc_[:, :], lhsT=Bc[:, :], rhs=Cc[:, :], start=True, stop=True),
                                     reads=[Cc, Bc], writes=[pc_])
                                S.op("act", lambda e: e.activation(out=Bn[:], in_=pb_[:, :], func=AF.Copy), reads=[pb_], writes=[Bn])
                                S.op("dve", lambda e: e.tensor_copy(out=Cn[:], in_=pc_[:, :]), reads=[pc_], writes=[Cn])
                                Cc = Cn
                        pz = px[0]
                        S.op("pe", lambda e: e.matmul(pz[:, :], lhsT=Boff[:, :], rhs=X[:, :], start=True, stop=True),
                             reads=[Boff, X], writes=[pz])
                        S.op("act", lambda e: e.activation(out=Zb[:], in_=pz[:, :], func=AF.Copy), reads=[pz], writes=[Zb])
                        for k_ in range(6):
                            pxx = px[(k_ + 1) % 2]
                            S.op("pe", lambda e: e.matmul(pxx[:, :], lhsT=Bk[k_][:, :], rhs=Zb[:, :], start=True, stop=True),
                                 reads=[Bk[k_], Zb], writes=[pxx])
                            S.op("dve", lambda e: e.tensor_tensor(out=Zb[:], in0=Zb[:], in1=pxx[:, :],
                                                                  op=(ALU.subtract if k_ == 0 else ALU.add)),
                                 reads=[Zb, pxx], writes=[Zb])
                        S.op("dve", lambda e: e.tensor_tensor(out=X[:], in0=X[:], in1=Zb[:], op=ALU.subtract),
                             reads=[X, Zb], writes=[X])
                        if DNSTOP < 6:
                            continue
                        pw = PQ()
                        S.op("pe", lambda e: e.transpose(pw[:, :], X[:, 128:256], identf[:]), reads=[X, identf], writes=[pw])
                        S.op("act", lambda e: e.activation(out=wT[:], in_=pw[:, :], func=AF.Copy), reads=[pw], writes=[wT])
                        p1, p2, p3, p4 = PQ(), PQ(), PQ(), PQ()
                        S.op("pe", lambda e: e.matmul(p1[:, :], lhsT=wT[:, :], rhs=Sst[:, :], start=True, stop=True),
                             reads=[wT, Sst], writes=[p1])
                        S.op("dve", lambda e: e.tensor_tensor(out=vnew[:], in0=X[:, 0:128], in1=p1[:, :], op=ALU.subtract),
                             reads=[X, p1], writes=[vnew])
                        S.op("pe", lambda e: e.matmul(p2[:, :], lhsT=qT[:, c0:c0 + 128], rhs=Sst[:, :], start=True, stop=True),
                             reads=[qT, Sst], writes=[p2])
                        S.op("pe", lambda e: e.matmul(p3[:, :], lhsT=qkT[:, :], rhs=vnew[:, :], start=True, stop=True),
                             reads=[qkT, vnew], writes=[p3])
                        S.op("dve", lambda e: e.tensor_scalar(out=tmp[:], in0=p2[:, :], scalar1=egc[:, n, dh:dh + 1], scalar2=None,
                                                              op0=ALU.mult), reads=[p2, egc], writes=[tmp])
                        osl = oall[:, n, h * 128:(h + 1) * 128]
                        if d == 0:
                            S.op("dve", lambda e: e.tensor_tensor(out=osl, in0=tmp[:], in1=p3[:, :], op=ALU.add),
                                 reads=[tmp, p3], writes=[oall])
                        else:
                            S.op("dve", lambda e: e.tensor_tensor(out=tmp[:], in0=tmp[:], in1=p3[:, :], op=ALU.add),
                                 reads=[tmp, p3], writes=[tmp])
                            S.op("pool", lambda e: e.tensor_tensor(out=osl, in0=osl, in1=tmp[:], op=ALU.add),
                                 reads=[tmp, oall], writes=[oall])
                        S.op("pool", lambda e: e.tensor_scalar(out=kd[:], in0=ktok[:, n, :], scalar1=edl[:, n, dh:dh + 1],
                                                               scalar2=None, op0=ALU.mult), reads=[ktok, edl], writes=[kd])
                        S.op("pe", lambda e: e.matmul(p4[:, :], lhsT=kd[:, :], rhs=vnew[:, :], start=True, stop=True),
                             reads=[kd, vnew], writes=[p4])
                        S.op("dve", lambda e: e.scalar_tensor_tensor(out=Sst[:], in0=Sst[:], scalar=egl[:, n, dh:dh + 1], in1=p4[:, :],
                                                                     op0=ALU.mult, op1=ALU.add), reads=[Sst, egl, p4], writes=[Sst])
                    if not latent:
                        ost = R["o_dn"][d]
                        S.dma("sp", lambda e: e.dma_start(out=ost[si, l, h, :, :], in_=Sst[:]), reads=[Sst], writes=[ost])
            for n in range(nch):
                if DNSTOP < 7:
                    continue
                g0 = tok0 + n * 128
                S.dma("sp", lambda e: e.dma_start(out=zt[:], in_=P_T[g0:g0 + 128, az:az + 1024]), reads=[P_T], writes=[zt])
                S.op("act", lambda e: e.activation(out=zt[:], in_=zt[:], func=AF.Silu), reads=[zt], writes=[zt])
                o3 = oall[:, n, :].rearrange("p (h v) -> p h v", h=8)
                for h in range(8):
                    S.op("act", lambda e: e.activation(out=tmp[:], in_=oall[:, n, h * 128:(h + 1) * 128], func=AF.Square,
                                                       accum_out=ssq[:, h:h + 1]), reads=[oall], writes=[tmp, ssq])
                S.op("act", lambda e: e.activation(out=ssq[:], in_=ssq[:], func=AF.Sqrt, scale=1.0 / 128, bias=EPS),
                     reads=[ssq], writes=[ssq])
                S.op("dve", lambda e: e.reciprocal(out=ssq[:], in_=ssq[:]), reads=[ssq], writes=[ssq])
                S.op("dve", lambda e: e.tensor_tensor(out=o3, in0=o3, in1=ssq[:, :].unsqueeze(2).to_broadcast([128, 8, 128]),
                                                      op=ALU.mult), reads=[oall, ssq], writes=[oall])
                S.op("dve", lambda e: e.tensor_tensor(out=o3, in0=o3, in1=gon[:, :].unsqueeze(1).to_broadcast([128, 8, 128]),
                                                      op=ALU.mult), reads=[oall, gon], writes=[oall])
                S.op("dve", lambda e: e.tensor_tensor(out=ob[:], in0=oall[:, n, :], in1=zt[:], op=ALU.mult),
                     reads=[oall, zt], writes=[ob])
                for h in range(8):
                    S.op("pe", lambda e: e.transpose(ptr[:, h, :], ob[:, h * 128:(h + 1) * 128], identb[:]),
                         reads=[ob, identb], writes=[ptr])
                _evac(S, n, obT[:], ptr[:, :, :], reads=[ptr], writes=[obT])
                S.dma("sp", lambda e: e.dma_start(out=brT[1][:, g0:g0 + 128].rearrange("(h p) t -> p h t", p=128), in_=obT[:]),
                      reads=[obT], writes=[brT[1]])
        S.barrier()
    S.scope = S.es


def _hy_geom(L):
    nf = L + 1
    KT = (nf + 127) // 128
    NB = (nf + 511) // 512
    return nf, KT, NB


def stage_hyena(S, l, R):
    P_F, brT = R["P_F"], R["brT"]
    ident = R["ident"]
    fhy, _ = F_ROWS["hyT"]
    hyZ, hyY1, hyU, hyPQ, hyYS = R["hyZ"], R["hyY1"], R["hyU"], R["hyPQ"], R["hyYS"]
    PI = float(np.pi)
    seqs = [(s * LC, LC, False) for s in range(NCTX)] + [(NCTX * LC, LL, True)]
    for (Lx, tabi, seq_list) in ((LC, 0, seqs[:NCTX]), (LL, 1, seqs[NCTX:])):
        L = Lx
        nf, KT, NB = _hy_geom(L)
        KU = L // 128
        tab = R["hy_tab"][tabi]
        with ExitStack() as sc:
            S.scope = sc
            w1 = S.sbuf("hyw1", [33, 64], F32)
            w2 = S.sbuf("hyw2", [64, 64], F32)
            w3 = S.sbuf("hyw3", [64, 4096], F32)
            b12 = S.sbuf("hyb12", [64, 2], F32)
            S.dma("sp", lambda e: e.dma_start(out=w1[:], in_=R["hy_w1"][l]), reads=[R["hy_w1"]], writes=[w1])
            S.dma("sp", lambda e: e.dma_start(out=w2[:], in_=R["hy_w2"][l]), reads=[R["hy_w2"]], writes=[w2])
            S.dma("sp", lambda e: e.dma_start(out=w3[:], in_=R["hy_w3"][l]), reads=[R["hy_w3"]], writes=[w3])
            S.dma("sp", lambda e: e.dma_start(out=b12[:], in_=R["hy_b12"][l]), reads=[R["hy_b12"]], writes=[b12])
            zT = S.sbuf("hyzT", [33, L], F32)
            S.dma("sp", lambda e: e.dma_start(out=zT[:], in_=R["hy_zemb"][tabi][:, :]), reads=[R["hy_zemb"][tabi]], writes=[zT])
            wfs = S.sbuf("hywf", [128, KT], F32)
            S.dma("sp", lambda e: e.dma_start(out=wfs[:], in_=R["hy_wf"][tabi][:, :]), reads=[R["hy_wf"][tabi]], writes=[wfs])
            h1 = S.sbuf("hyh1", [64, L], F32)
            h2 = S.sbuf("hyh2", [64, L], F32)
            xa = S.sbuf("hyxa", [64, 512], F32)
            xb_ = S.sbuf("hyxb", [64, 512], F32)
            xc = S.sbuf("hyxc", [64, 512], F32)
            win = S.sbuf("hywin", [128, 1024], F32)
            hs = S.sbuf("hyhs", [128, KU, 1024], BF16)
            hd = S.sbuf("hyhd", [128, KU, 1024], BF16)
            tf = [S.sbuf("hytf%d" % i, [128, 512], F32) for i in range(2)]
            slab = [S.sbuf("hyslab%d" % i, [128, KT, 512], BF16) for i in range(2)]
            pst = S.sbuf("hypst", [128, 512], F32)
            pm = [S.psum("hypm%d" % i, [128, 512], F32) for i in range(4)]
            npm = [0]

            def PM():
                npm[0] += 1
                return pm[npm[0] % 4]

            def sin_layer(dst, wmat, kdim, src, bcol):
                for g in range((L + 511) // 512):
                    w = min(512, L - g * 512)
                    ps = PM()
                    S.op("pe", lambda e: e.matmul(ps[0:64, 0:w], lhsT=wmat[0:kdim, :], rhs=src[0:kdim, g * 512:g * 512 + w],
                                                  start=True, stop=True), reads=[wmat, src], writes=[ps])
                    S.op("dve", lambda e: e.tensor_scalar(out=xa[:, 0:w], in0=ps[0:64, 0:w], scalar1=b12[:, bcol:bcol + 1],
                                                          scalar2=None, op0=ALU.add), reads=[ps, b12], writes=[xa])
                    S.op("dve", lambda e: e.tensor_scalar(out=xb_[:, 0:w], in0=xa[:, 0:w], scalar1=PI, scalar2=-2 * PI,
                                                          op0=ALU.is_gt, op1=ALU.mult), reads=[xa], writes=[xb_])
                    S.op("dve", lambda e: e.tensor_scalar(out=xc[:, 0:w], in0=xa[:, 0:w], scalar1=-PI, scalar2=2 * PI,
                                                          op0=ALU.is_lt, op1=ALU.mult), reads=[xa], writes=[xc])
                    S.op("dve", lambda e: e.tensor_tensor(out=xa[:, 0:w], in0=xa[:, 0:w], in1=xb_[:, 0:w], op=ALU.add),
                         reads=[xa, xb_], writes=[xa])
                    S.op("dve", lambda e: e.tensor_tensor(out=xa[:, 0:w], in0=xa[:, 0:w], in1=xc[:, 0:w], op=ALU.add),
                         reads=[xa, xc], writes=[xa])
                    S.op("act", lambda e: e.activation(out=dst[:, g * 512:g * 512 + w], in_=xa[:, 0:w], func=AF.Sin),
                         reads=[xa], writes=[dst])

            sin_layer(h1, w1, 33, zT, 0)
            sin_layer(h2, w2, 64, h1, 1)
            for o in range(2):
                for t in range(KU):
                    S.dma("sp", lambda e: e.dma_start(out=win[:], in_=R["hy_win"][tabi][t * 128:(t + 1) * 128, :]),
                          reads=[R["hy_win"][tabi]], writes=[win])
                    for cb in range(2):
                        pf, pb = PM(), PM()
                        cf = (o * 2 + 0) * 1024 + cb * 512
                        cbk = (o * 2 + 1) * 1024 + cb * 512
                        S.op("pe", lambda e: e.matmul(pf[:, :], lhsT=h2[:, t * 128:(t + 1) * 128], rhs=w3[:, cf:cf + 512],
                                                      start=True, stop=True), reads=[h2, w3], writes=[pf])
                        S.op("pe", lambda e: e.matmul(pb[:, :], lhsT=h2[:, t * 128:(t + 1) * 128], rhs=w3[:, cbk:cbk + 512],
                                                      start=True, stop=True), reads=[h2, w3], writes=[pb])
                        S.op("dve", lambda e: e.tensor_tensor(out=tf[0][:], in0=pf[:, :], in1=win[:, cb * 512:(cb + 1) * 512],
                                                              op=ALU.mult), reads=[pf, win], writes=[tf[0]])
                        S.op("dve", lambda e: e.tensor_tensor(out=tf[1][:], in0=pb[:, :], in1=win[:, cb * 512:(cb + 1) * 512],
                                                              op=ALU.mult), reads=[pb, win], writes=[tf[1]])
                        if t == 0:
                            S.op("dve", lambda e: e.memset(tf[1][0:1, :], 0.0), writes=[tf[1]])
                        S.op("dve", lambda e: e.tensor_tensor(out=hs[:, t, cb * 512:(cb + 1) * 512], in0=tf[0][:], in1=tf[1][:],
                                                              op=ALU.add), reads=[tf[0], tf[1]], writes=[hs])
                        S.op("pool", lambda e: e.tensor_tensor(out=hd[:, t, cb * 512:(cb + 1) * 512], in0=tf[0][:], in1=tf[1][:],
                                                               op=ALU.subtract), reads=[tf[0], tf[1]], writes=[hd])
                for fb in range(NB):
                    for cs in range(2):
                        S.dma("sp", lambda e: e.dma_start(out=slab[cs][:], in_=tab[cs, fb]), reads=[tab], writes=[slab[cs]])
                    for j in range(4):
                        m = fb * 4 + j
                        if m >= KT:
                            break
                        fm = min(128, nf - m * 128)
                        for cs, src in ((0, hs), (1, hd)):
                            for cb in range(2):
                                ps = PM()
                                for kt in range(KU):
                                    S.op("pe", lambda e: e.matmul(ps[0:fm, :], lhsT=slab[cs][:, kt, j * 128:j * 128 + fm],
                                                                  rhs=src[:, kt, cb * 512:(cb + 1) * 512], start=(kt == 0),
                                                                  stop=(kt == KU - 1)), reads=[slab[cs], src], writes=[ps])
                                S.op("dve", lambda e: e.tensor_scalar(out=pst[0:fm, :], in0=ps[0:fm, :], scalar1=wfs[0:fm, m:m + 1],
                                                                      scalar2=None, op0=ALU.mult), reads=[ps, wfs], writes=[pst])
                                S.dma("sp", lambda e: e.dma_start(
                                    out=hyPQ[tabi][o, cs, m * 128:m * 128 + fm, cb * 512:(cb + 1) * 512], in_=pst[0:fm, :]),
                                    reads=[pst], writes=[hyPQ[tabi]])
            S.barrier()
        for (tok0, L_, latent) in seq_list:
            si = tok0 // LC if not latent else NCTX
            with ExitStack() as sc:
                S.scope = sc
                cw = S.sbuf("hycw", [128, 24, 3], F32)
                S.dma("sp", lambda e: e.dma_start(out=cw[:], in_=R["hy_convT"][l]), reads=[R["hy_convT"]], writes=[cw])
                xin = [S.sbuf("hyxin%d" % i, [128, L + 2], F32) for i in range(2)]
                acc = [S.sbuf("hyacc%d" % i, [128, L], F32) for i in range(2)]
                accb = S.sbuf("hyaccb", [128, L], BF16)
                ut = S.sbuf("hyut", [128, KU, 128], BF16)
                ptr = [S.psum("hyptr%d" % i, [128, 8, 128], BF16) for i in range(2)]
                for ch in range(24):
                    xi, ac = xin[ch % 2], acc[ch % 2]
                    r0 = fhy + ch * 128
                    S.op("pool", lambda e: e.memset(xi[:, 0:1], 0.0), writes=[xi])
                    S.op("pool", lambda e: e.memset(xi[:, L + 1:L + 2], 0.0), writes=[xi])
                    S.dma("sp", lambda e: e.dma_start(out=xi[:, 1:L + 1], in_=P_F[r0:r0 + 128, tok0:tok0 + L]), reads=[P_F], writes=[xi])
                    S.op("dve", lambda e: e.tensor_scalar(out=ac[:], in0=xi[:, 0:L], scalar1=cw[:, ch, 0:1], scalar2=None, op0=ALU.mult),
                         reads=[xi, cw], writes=[ac])
                    for kk in (1, 2):
                        S.op("dve", lambda e: e.scalar_tensor_tensor(out=ac[:], in0=xi[:, kk:kk + L], scalar=cw[:, ch, kk:kk + 1], in1=ac[:],
                                                                     op0=ALU.mult, op1=ALU.add), reads=[xi, cw, ac], writes=[ac])
                    S.dma("sp", lambda e: e.dma_start(out=hyZ[ch * 128:(ch + 1) * 128, tok0:tok0 + L], in_=ac[:]), reads=[ac], writes=[hyZ])
                    if ch < 8:
                        S.op("act", lambda e: e.activation(out=accb[:], in_=ac[:], func=AF.Copy), reads=[ac], writes=[accb])
                        for t0 in range(0, KU, 8):
                            pt = ptr[(t0 // 8) % 2]
                            nn = min(8, KU - t0)
                            for j in range(nn):
                                S.op("pe", lambda e: e.transpose(pt[:, j, :], accb[:, (t0 + j) * 128:(t0 + j + 1) * 128], ident[:]),
                                     reads=[accb, ident], writes=[pt])
                            _evac(S, t0 // 8, ut[:, t0:t0 + nn, :], pt[:, 0:nn, :], reads=[pt], writes=[ut])
                        S.dma("sp", lambda e: e.dma_start(
                            out=hyU[tok0:tok0 + L, ch * 128:(ch + 1) * 128].rearrange("(k p) c -> p k c", p=128), in_=ut[:]),
                            reads=[ut], writes=[hyU])
                S.barrier()
            for o in range(2):
                with ExitStack() as sc:
                    S.scope = sc
                    u = S.sbuf("hyu", [128, KU, 1024], BF16)
                    S.dma("sp", lambda e: e.dma_start(out=u[:], in_=hyU[tok0:tok0 + L, :].rearrange("(k p) c -> p k c", p=128)),
                          reads=[hyU], writes=[u])
                    slab = [S.sbuf("hyslabf%d" % i, [128, KT, 512], BF16) for i in range(2)]
                    PQt = [S.sbuf("hyPQt%d" % i, [128, 1024], F32) for i in range(2)]
                    ta = S.sbuf("hyta", [128, 512], F32)
                    tb_ = S.sbuf("hytb", [128, 512], F32)
                    yc = S.sbuf("hyyc", [128, 512], BF16)
                    ys = S.sbuf("hyys", [128, 512], BF16)
                    pm = [S.psum("hypmf%d" % i, [128, 512], F32) for i in range(4)]
                    for fb in range(NB):
                        for cs in range(2):
                            S.dma("sp", lambda e: e.dma_start(out=slab[cs][:], in_=tab[cs, fb]), reads=[tab], writes=[slab[cs]])
                        for j in range(4):
                            m = fb * 4 + j
                            if m >= KT:
                                break
                            fm = min(128, nf - m * 128)
                            for cs in range(2):
                                S.dma("sp", lambda e: e.dma_start(out=PQt[cs][0:fm, :], in_=hyPQ[tabi][o, cs, m * 128:m * 128 + fm, :]),
                                      reads=[hyPQ[tabi]], writes=[PQt[cs]])
                            for cb in range(2):
                                pa, pb = pm[(2 * cb) % 4], pm[(2 * cb + 1) % 4]
                                for kt in range(KU):
                                    S.op("pe", lambda e: e.matmul(pa[0:fm, :], lhsT=slab[0][:, kt, j * 128:j * 128 + fm],
                                                                  rhs=u[:, kt, cb * 512:(cb + 1) * 512], start=(kt == 0), stop=(kt == KU - 1)),
                                         reads=[slab[0], u], writes=[pa])
                                for kt in range(KU):
                                    S.op("pe", lambda e: e.matmul(pb[0:fm, :], lhsT=slab[1][:, kt, j * 128:j * 128 + fm],
                                                                  rhs=u[:, kt, cb * 512:(cb + 1) * 512], start=(kt == 0), stop=(kt == KU - 1)),
                                         reads=[slab[1], u], writes=[pb])
                                Pc = PQt[0][0:fm, cb * 512:(cb + 1) * 512]
                                Qc = PQt[1][0:fm, cb * 512:(cb + 1) * 512]
                                S.op("dve", lambda e: e.tensor_tensor(out=ta[0:fm, :], in0=pa[0:fm, :], in1=Pc, op=ALU.mult),
                                     reads=[pa, PQt[0]], writes=[ta])
                                S.op("dve", lambda e: e.tensor_tensor(out=tb_[0:fm, :], in0=pb[0:fm, :], in1=Qc, op=ALU.mult),
                                     reads=[pb, PQt[1]], writes=[tb_])
                                S.op("pool", lambda e: e.tensor_tensor(out=yc[0:fm, :], in0=ta[0:fm, :], in1=tb_[0:fm, :], op=ALU.subtract),
                                     reads=[ta, tb_], writes=[yc])
                                S.op("dve", lambda e: e.tensor_tensor(out=ta[0:fm, :], in0=pa[0:fm, :], in1=Qc, op=ALU.mult),
                                     reads=[pa, PQt[1]], writes=[ta])
                                S.op("dve", lambda e: e.tensor_tensor(out=tb_[0:fm, :], in0=pb[0:fm, :], in1=Pc, op=ALU.mult),
                                     reads=[pb, PQt[0]], writes=[tb_])
                                S.op("pool", lambda e: e.tensor_tensor(out=ys[0:fm, :], in0=ta[0:fm, :], in1=tb_[0:fm, :], op=ALU.add),
                                     reads=[ta, tb_], writes=[ys])
                                S.dma("sp", lambda e: e.dma_start(out=hyYS[0, m * 128:m * 128 + fm, cb * 512:(cb + 1) * 512], in_=yc[0:fm, :]),
                                      reads=[yc], writes=[hyYS])
                                S.dma("sp", lambda e: e.dma_start(out=hyYS[1, m * 128:m * 128 + fm, cb * 512:(cb + 1) * 512], in_=ys[0:fm, :]),
                                      reads=[ys], writes=[hyYS])
                    S.barrier()
                with ExitStack() as sc:
                    S.scope = sc
                    ycs = [S.sbuf("hyYc%d" % i, [128, KT, 1024], BF16) for i in range(2)]
                    for cs in range(2):
                        S.op("dve", lambda e: e.memset(ycs[cs][:, KT - 1, :], 0.0), writes=[ycs[cs]])
                        S.dma("sp", lambda e: e.dma_start(
                            out=ycs[cs][:, 0:KT - 1, :], in_=hyYS[cs, 0:(KT - 1) * 128, :].rearrange("(k p) c -> p k c", p=128)),
                            reads=[hyYS], writes=[ycs[cs]])
                        S.dma("sp", lambda e: e.dma_start(out=ycs[cs][0:1, KT - 1, :], in_=hyYS[cs, (KT - 1) * 128:(KT - 1) * 128 + 1, :]),
                              reads=[hyYS], writes=[ycs[cs]])
                    slab = [S.sbuf("hyslabi%d" % i, [128, KT, 512], BF16) for i in range(2)]
                    bia = S.sbuf("hybia", [128, 8], F32)
                    S.dma("sp", lambda e: e.dma_start(out=bia[:], in_=R["hy_biasT"][l, o]), reads=[R["hy_biasT"]], writes=[bia])
                    uT = [S.sbuf("hyuT%d" % i, [128, 512], F32) for i in range(2)]
                    gT = [S.sbuf("hygT%d" % i, [128, 512], F32) for i in range(2)]
                    yo = [S.sbuf("hyyo%d" % i, [128, 512], F32) for i in range(2)]
                    yb = [S.sbuf("hyyb%d" % i, [128, 512], BF16) for i in range(2)]
                    ut = [S.sbuf("hyuti%d" % i, [128, 4, 128], BF16) for i in range(2)]
                    pm = [S.psum("hypmi%d" % i, [128, 512], F32) for i in range(4)]
                    ptr = [S.psum("hyptri%d" % i, [128, 8, 128], BF16) for i in range(2)]
                    it = 0
                    for tb in range((L + 511) // 512):
                        tw = min(512, L - tb * 512)
                        for cs in range(2):
                            S.dma("sp", lambda e: e.dma_start(out=slab[cs][:], in_=tab[cs, tb]), reads=[tab], writes=[slab[cs]])
                        for cc in range(8):
                            ps = pm[it % 4]
                            u_, g_, y_, yb_, ut_ = uT[it % 2], gT[it % 2], yo[it % 2], yb[it % 2], ut[it % 2]
                            it += 1
                            usrc = hyZ if o == 0 else hyY1
                            S.dma("sp", lambda e: e.dma_start(out=u_[:, 0:tw], in_=usrc[cc * 128:(cc + 1) * 128, tok0 + tb * 512:tok0 + tb * 512 + tw]),
                                  reads=[usrc], writes=[u_])
                            gr = (1 + o) * 1024 + cc * 128
                            S.dma("sp", lambda e: e.dma_start(out=g_[:, 0:tw], in_=hyZ[gr:gr + 128, tok0 + tb * 512:tok0 + tb * 512 + tw]),
                                  reads=[hyZ], writes=[g_])
                            n_mm = 2 * KT
                            i_mm = 0
                            for cs in range(2):
                                for kt in range(KT):
                                    kp = 128 if kt < KT - 1 else 1
                                    S.op("pe", lambda e: e.matmul(ps[:, 0:tw], lhsT=ycs[cs][0:kp, kt, cc * 128:(cc + 1) * 128],
                                                                  rhs=slab[cs][0:kp, kt, 0:tw], start=(i_mm == 0), stop=(i_mm == n_mm - 1)),
                                         reads=[ycs[cs], slab[cs]], writes=[ps])
                                    i_mm += 1
                            S.op("dve", lambda e: e.scalar_tensor_tensor(out=y_[:, 0:tw], in0=u_[:, 0:tw], scalar=bia[:, cc:cc + 1],
                                                                         in1=ps[:, 0:tw], op0=ALU.mult, op1=ALU.add),
                                 reads=[u_, bia, ps], writes=[y_])
                            if o == 0:
                                S.op("pool", lambda e: e.tensor_tensor(out=y_[:, 0:tw], in0=y_[:, 0:tw], in1=g_[:, 0:tw], op=ALU.mult),
                                     reads=[y_, g_], writes=[y_])
                                S.dma("sp", lambda e: e.dma_start(out=hyY1[cc * 128:(cc + 1) * 128, tok0 + tb * 512:tok0 + tb * 512 + tw],
                                                                  in_=y_[:, 0:tw]), reads=[y_], writes=[hyY1])
                                S.op("act", lambda e: e.activation(out=yb_[:, 0:tw], in_=y_[:, 0:tw], func=AF.Copy), reads=[y_], writes=[yb_])
                                pt = ptr[it % 2]
                                nt = tw // 128
                                for j in range(nt):
                                    S.op("pe", lambda e: e.transpose(pt[:, j, :], yb_[:, j * 128:(j + 1) * 128], ident[:]),
                                         reads=[yb_, ident], writes=[pt])
                                _evac(S, it, ut_[:, 0:nt, :], pt[:, 0:nt, :], reads=[pt], writes=[ut_])
                                S.dma("sp", lambda e: e.dma_start(
                                    out=hyU[tok0 + tb * 512:tok0 + tb * 512 + tw, cc * 128:(cc + 1) * 128].rearrange("(k p) c -> p k c", p=128),
                                    in_=ut_[:, 0:nt, :]), reads=[ut_], writes=[hyU])
                            else:
                                S.op("pool", lambda e: e.tensor_tensor(out=yb_[:, 0:tw], in0=y_[:, 0:tw], in1=g_[:, 0:tw], op=ALU.mult),
                                     reads=[y_, g_], writes=[yb_])
                                S.dma("sp", lambda e: e.dma_start(out=brT[2][cc * 128:(cc + 1) * 128, tok0 + tb * 512:tok0 + tb * 512 + tw],
                                                                  in_=yb_[:, 0:tw]), reads=[yb_], writes=[brT[2]])
                    S.barrier()
    S.scope = S.es


def build_program(dbg=False):
    nc = bass.Bass("TRN2", target_bir_lowering=False)
    k = K()
    k.nc = nc

    def ein(name, shape, dtype=F32):
        return Buf(nc.dram_tensor(name, list(shape), dtype, kind="ExternalInput").ap(), name)

    def eout(name, shape, dtype=F32):
        return Buf(nc.dram_tensor(name, list(shape), dtype, kind="ExternalOutput").ap(), name)

    x_in = ein("x_in", [TT, D])
    c2T = ein("c2T", [128, 16, 2])
    ident_in = ein("ident", [128, 128])
    norm1_g = ein("norm1_g", [DEPTH, D])
    w_ada = ein("w_ada", [DEPTH, D, 6 * D])
    b_ada = ein("b_ada", [DEPTH, 6 * D])
    w_in_T = ein("w_in_T", [DEPTH, D, NT_COLS])
    w_in_F = ein("w_in_F", [DEPTH, D, NF_ROWS])
    mla_kv_norm = ein("mla_kv_norm", [DEPTH, 256])
    R = {}
    R["ident_in"] = ident_in
    R["mla_kv_norm"] = mla_kv_norm
    R["mla_q_norm"] = ein("mla_q_norm", [DEPTH, 512])
    R["w_qb"] = ein("w_qb_p", [DEPTH, 512, 2048])
    R["w_kvb"] = ein("w_kvb_p", [DEPTH, 256, 2048])
    R["cache_ckv"] = ein("cache_ckv", [DEPTH, PAST, 256])
    R["cache_kr"] = ein("cache_kr", [DEPTH, PAST, 64])
    R["cache_nak"] = ein("cache_nak", [DEPTH, PAST, 1024])
    R["cache_nav"] = ein("cache_nav", [DEPTH, PAST, 1024])
    R["ropeT"] = ein("ropeT", [2, 64, LL])
    R["na_ctab"] = ein("na_ctab", [DEPTH, 8, 64, 31, 64])
    R["na_mask"] = ein("na_mask", [NA_NMASK, 128, 512])
    R["w_branch"] = ein("w_branch", [DEPTH, 4, 1024, 2048])
    R["w_out"] = ein("w_out", [DEPTH, D, D])
    R["norm2_g"] = ein("norm2_g", [DEPTH, D])
    R["peer_wq"] = ein("peer_wq", [DEPTH, D, D])
    R["peer_keysT"] = ein("peer_keysT", [DEPTH, 2, 128, 128])
    R["peer_u"] = [ein("peer_u%d" % i, [16384, D]) for i in range(DEPTH)]
    R["peer_v"] = [ein("peer_v%d" % i, [16384, D]) for i in range(DEPTH)]
    R["final_g"] = ein("final_g", [1, D])
    _hy_inputs(R, ein)
    R["dn_consts"] = ein("dn_consts", [10, 128, 128])
    R["dn_convT"] = ein("dn_convT", [DEPTH, 128, 24, 5])
    R["dn_a_log"] = ein("dn_a_log", [DEPTH, 16])
    R["dn_dt_bias"] = ein("dn_dt_bias", [DEPTH, 16])
    R["dn_out_norm"] = ein("dn_out_norm", [DEPTH, 128])
    R["dn_state"] = [ein("dn_state%d" % i, [DEPTH, 8, 128, 128]) for i in range(2)]
    if DBG_BR:
        R["dbg_br"] = ein("dbg_br", [DEPTH, 2, 1024, TT], BF16)

    o_ckv = eout("o_ckv", [NCTX, DEPTH, LC, 256])
    o_kr = eout("o_kr", [NCTX, DEPTH, LC, 64])
    o_nak = eout("o_nak", [NCTX, DEPTH, LC, 1024])
    o_nav = eout("o_nav", [NCTX, DEPTH, LC, 1024])
    y_out = eout("y_out", [TT, D])
    o_dn = [eout("o_dn%d" % i, [NCTX, DEPTH, 8, 128, 128]) for i in range(2)]
    R["o_dn"] = o_dn
    outs = [o_ckv, o_kr, o_nak, o_nav, y_out] + o_dn
    R["o_ckv"] = o_ckv
    R["y_out"] = y_out

    with ExitStack() as es:
        S = Sched(nc, es)
        k.S = S
        modd = S.dram("modd", [2, 6 * D])
        P_T = S.dram("P_T", [TT, NT_COLS])
        P_F = S.dram("P_F", [NF_ROWS, TT])
        brT = [S.dram("brT%d" % b_, [1024, TT], BF16) for b_ in range(4)]
        R.update(P_T=P_T, P_F=P_F, brT=brT, modd=modd)
        R["mrg"] = S.dram("mrg", [TT, D])
        R["xbuf"] = S.dram("xbuf", [TT, D])
        _hy_scratch(S, R)

        ident = S.sbuf("identb", [128, 128], BF16)
        S.dma("pool", lambda e: e.dma_start(out=ident[:], in_=ident_in[:, :]), reads=[ident_in], writes=[ident])
        R["ident"] = ident

        for l in range(1 if DBG_BR else DEPTH):
            with ExitStack() as sc:
                S.scope = sc
                cT = S.sbuf("cT", [128, 16, 2], F32)
                sT = S.sbuf("sT", [128, 16, 2], BF16)
                S.dma("sp", lambda e: e.dma_start(out=cT[:], in_=c2T[:, :, :]), reads=[c2T], writes=[cT])
                S.op("act", lambda e: e.activation(out=sT[:], in_=cT[:], func=AF.Silu), reads=[cT], writes=[sT])
                wts = [S.sbuf("adaw%d" % i, [128, 16, 512], BF16) for i in range(2)]
                bts = [S.sbuf("adab%d" % i, [2, 512], F32) for i in range(2)]
                mts = [S.sbuf("adam%d" % i, [2, 512], F32) for i in range(2)]
                pss = [S.psum("adap%d" % i, [2, 512], F32) for i in range(2)]
                for cb in range(24):
                    wt, bt, mt, ps = wts[cb % 2], bts[cb % 2], mts[cb % 2], pss[cb % 2]
                    c0 = cb * 512
                    S.dma("pool", lambda e: e.dma_start(
                        out=wt[:], in_=w_ada[l, :, c0:c0 + 512].rearrange("(k p) c -> p k c", p=128)),
                        reads=[w_ada], writes=[wt])
                    S.dma("sp", lambda e: e.dma_start(out=bt[:], in_=b_ada[l, c0:c0 + 512].partition_broadcast(2)),
                          reads=[b_ada], writes=[bt])
                    for kc in range(16):
                        S.op("pe", lambda e: e.matmul(ps[:, :], lhsT=sT[:, kc, :], rhs=wt[:, kc, :],
                                                      start=(kc == 0), stop=(kc == 15)),
                             reads=[sT, wt], writes=[ps])
                    S.op("dve", lambda e: e.tensor_tensor(out=mt[:], in0=ps[:], in1=bt[:], op=ALU.add),
                         reads=[ps, bt], writes=[mt])
                    S.dma("sp", lambda e: e.dma_start(out=modd[:, c0:c0 + 512], in_=mt[:]), reads=[mt], writes=[modd])
            S.barrier()

            x_cur = x_in if l == 0 else R["xbuf"]
            with ExitStack() as sc:
                S.scope = sc
                hT = S.sbuf("hT", [128, 16, TT], BF16)
                with ExitStack() as sc2:
                    S.scope = sc2
                    gm = [S.sbuf("gm%d" % c, [128, D], F32) for c in range(2)]
                    sh = [S.sbuf("sh%d" % c, [128, D], F32) for c in range(2)]
                    gt = S.sbuf("gt", [128, D], F32)
                    S.dma("sp", lambda e: e.dma_start(out=gt[:], in_=norm1_g[l, :].partition_broadcast(128)),
                          reads=[norm1_g], writes=[gt])
                    for c in range(2):
                        S.dma("sp", lambda e: e.dma_start(out=sh[c][:], in_=modd[c, 0:D].partition_broadcast(128)),
                              reads=[modd], writes=[sh[c]])
                        S.dma("sp", lambda e: e.dma_start(out=gm[c][:], in_=modd[c, D:2 * D].partition_broadcast(128)),
                              reads=[modd], writes=[gm[c]])
                        S.op("dve", lambda e: e.scalar_tensor_tensor(out=gm[c][:], in0=gm[c][:], scalar=1.0, in1=gt[:],
                                                                     op0=ALU.add, op1=ALU.mult),
                             reads=[gm[c], gt], writes=[gm[c]])
                    xts = [S.sbuf("xt%d" % i, [128, D], F32) for i in range(2)]
                    junk = S.sbuf("junk", [128, D], F32)
                    hb = [S.sbuf("hb%d" % i, [128, D], BF16) for i in range(2)]
                    ss = [S.sbuf("ss%d" % i, [128, 1], F32) for i in range(2)]
                    rs = [S.sbuf("rs%d" % i, [128, 1], F32) for i in range(2)]
                    ptr = [S.psum("ptr%d" % i, [128, 8, 128], BF16) for i in range(2)]
                    for t in range(NTILE):
                        c = 0 if t < (NCTX * LC) // 128 else 1
                        xt, hbt, sst, rst = xts[t % 2], hb[t % 2], ss[t % 2], rs[t % 2]
                        S.dma("sp", lambda e: e.dma_start(out=xt[:], in_=x_cur[t * 128:(t + 1) * 128, :]),
                              reads=[x_cur], writes=[xt])
                        S.op("act", lambda e: e.activation(out=junk[:], in_=xt[:], func=AF.Square, accum_out=sst[:]),
                             reads=[xt], writes=[junk, sst])
                        S.op("act", lambda e: e.activation(out=rst[:], in_=sst[:], func=AF.Sqrt, scale=1.0 / D, bias=EPS),
                             reads=[sst], writes=[rst])
                        S.op("dve", lambda e: e.reciprocal(out=rst[:], in_=rst[:]), reads=[rst], writes=[rst])
                        S.op("dve", lambda e: e.scalar_tensor_tensor(out=xt[:], in0=xt[:], scalar=rst[:, 0:1], in1=gm[c][:],
                                                                     op0=ALU.mult, op1=ALU.mult),
                             reads=[xt, rst, gm[c]], writes=[xt])
                        S.op("pool", lambda e: e.tensor_tensor(out=hbt[:], in0=xt[:], in1=sh[c][:], op=ALU.add),
                             reads=[xt, sh[c]], writes=[hbt])
                        for half in range(2):
                            pt = ptr[half]
                            for j in range(8):
                                kc = half * 8 + j
                                S.op("pe", lambda e: e.transpose(pt[:, j, :], hbt[:, kc * 128:(kc + 1) * 128], ident[:]),
                                     reads=[hbt, ident], writes=[pt])
                            _evac(S, half, hT[:, half * 8:(half + 1) * 8, t * 128:(t + 1) * 128], pt[:, :, :],
                                  reads=[pt], writes=[hT])
                    S.barrier()
                S.scope = sc
                wts = [S.sbuf("wint%d" % i, [128, 16, 512], BF16) for i in range(2)]
                stg = [S.sbuf("stg%d" % i, [128, 512], F32) for i in range(4)]
                pmm = [S.psum("pmm%d" % i, [128, 512], F32) for i in range(4)]
                nblk = (NT_COLS + 511) // 512
                ev = 0
                for cb in range(nblk):
                    c0 = cb * 512
                    cw = min(512, NT_COLS - c0)
                    wt = wts[cb % 2]
                    S.dma("pool", lambda e: e.dma_start(
                        out=wt[:, :, 0:cw], in_=w_in_T[l, :, c0:c0 + cw].rearrange("(k p) c -> p k c", p=128)),
                        reads=[w_in_T], writes=[wt])
                    for t in range(NTILE):
                        ps, st = pmm[ev % 4], stg[ev % 4]
                        for kc in range(16):
                            S.op("pe", lambda e: e.matmul(ps[:, 0:cw], lhsT=hT[:, kc, t * 128:(t + 1) * 128],
                                                          rhs=wt[:, kc, 0:cw], start=(kc == 0), stop=(kc == 15)),
                                 reads=[hT, wt], writes=[ps])
                        _evac(S, ev, st[:, 0:cw], ps[:, 0:cw], reads=[ps], writes=[st])
                        S.dma("sp", lambda e: e.dma_start(out=P_T[t * 128:(t + 1) * 128, c0:c0 + cw], in_=st[:, 0:cw]),
                              reads=[st], writes=[P_T])
                        ev += 1
                nblk = (NF_ROWS + 511) // 512
                for cb in range(nblk):
                    c0 = cb * 512
                    cw = min(512, NF_ROWS - c0)
                    wt = wts[cb % 2]
                    S.dma("pool", lambda e: e.dma_start(
                        out=wt[:, :, 0:cw], in_=w_in_F[l, :, c0:c0 + cw].rearrange("(k p) c -> p k c", p=128)),
                        reads=[w_in_F], writes=[wt])
                    for sb in range((cw + 127) // 128):
                        r0 = c0 + sb * 128
                        rw = min(128, NF_ROWS - r0)
                        for g in range(TT // 512):
                            ps, st = pmm[ev % 4], stg[ev % 4]
                            for kc in range(16):
                                S.op("pe", lambda e: e.matmul(ps[0:rw, :], lhsT=wt[:, kc, sb * 128:sb * 128 + rw],
                                                              rhs=hT[:, kc, g * 512:(g + 1) * 512],
                                                              start=(kc == 0), stop=(kc == 15)),
                                     reads=[hT, wt], writes=[ps])
                            _evac(S, ev, st[0:rw, :], ps[0:rw, :], reads=[ps], writes=[st])
                            S.dma("sp", lambda e: e.dma_start(out=P_F[r0:r0 + rw, g * 512:(g + 1) * 512], in_=st[0:rw, :]),
                                  reads=[st], writes=[P_F])
                            ev += 1
            S.barrier()
            S.scope = es

            for s_ in range(NCTX):
                for (nm, ob) in (("kr", o_kr), ("nak", o_nak), ("nav", o_nav)):
                    a0, aw = T_COLS[nm]
                    S.dma("sp", lambda e: e.dma_start(out=ob[s_, l, :, :], in_=P_T[s_ * LC:(s_ + 1) * LC, a0:a0 + aw]),
                          reads=[P_T], writes=[ob])
            if DBG_BR:
                for b_ in DBG_FILL:
                    S.dma("sp", lambda e: e.dma_start(out=brT[b_][:, :], in_=R["dbg_br"][l, b_ - 1]),
                          reads=[R["dbg_br"]], writes=[brT[b_]])
            stage_dn(S, l, R)
            if not DBG_BR:
                stage_hyena(S, l, R)
                stage_mla(S, l, R)
                stage_na(S, l, R)
            if DBG_BR:
                for nm_, b_ in (("d_brT1", 1),):
                    db = eout(nm_, [1024, TT], BF16)
                    outs.append(db)
                    S.dma("sp", lambda e: e.dma_start(out=db[:, :], in_=brT[b_][:, :]), reads=[brT[b_]], writes=[db])
            if not DBG_BR:
                stage_merge(S, l, R, x_in if l == 0 else R["xbuf"])
                stage_peer(S, l, R, last=(l == DEPTH - 1))

        for b in outs:
            if b.last_w is not None:
                S._wait("sp", b.last_w[0], b.last_w[1])
        S.barrier()
        k.ninstr = S.ninstr
    return nc, k


def _prep_weights(inp):
    w_in = inp["w_in"]
    tcols = np.concatenate([
        np.arange(O_CQ, O_CQ + 512), np.arange(O_CKV, O_CKV + 256), np.arange(O_KR, O_KR + 64),
        np.arange(O_Z, O_Z + 1024), np.arange(O_A, O_A + 16), np.arange(O_B, O_B + 16),
        np.arange(O_NA + 1024, O_NA + 2048), np.arange(O_NA + 2048, O_NA + 3072),
        np.arange(O_GATE, O_GATE + 8192)])
    sw = np.concatenate([np.arange(16, 32), np.arange(0, 16), np.arange(48, 64), np.arange(32, 48)])
    fcols = np.concatenate([
        np.arange(O_KR, O_KR + 64), O_KR + sw, np.arange(O_DN, O_DN + 3072), np.arange(O_HY, O_HY + 3072),
        np.arange(O_NA, O_NA + 1024), np.arange(O_NA + 1024, O_NA + 2048)])
    assert len(tcols) == NT_COLS and len(fcols) == NF_ROWS
    return np.ascontiguousarray(w_in[:, :, tcols]), np.ascontiguousarray(w_in[:, :, fcols])


def _prep_consts(inp):
    C = {}
    sw = np.concatenate([np.arange(16, 32), np.arange(0, 16), np.arange(48, 64), np.arange(32, 48)])
    nope = np.concatenate([np.arange(h * 192, h * 192 + 128) for h in range(8)])
    rope = np.concatenate([np.arange(h * 192 + 128, h * 192 + 192) for h in range(8)])
    ropesw = np.concatenate([h * 192 + 128 + sw for h in range(8)])
    C["w_qb_p"] = np.ascontiguousarray(inp["mla_w_qb"][:, :, np.concatenate([nope, rope, ropesw])])
    kn = np.concatenate([np.arange(h * 256, h * 256 + 128) for h in range(8)])
    vv = np.concatenate([np.arange(h * 256 + 128, h * 256 + 256) for h in range(8)])
    C["w_kvb_p"] = np.ascontiguousarray(inp["mla_w_kvb"][:, :, np.concatenate([kn, vv])])
    t = np.arange(LL)
    pos = [t // 64, t % 64]
    inv = 10000.0 ** (-np.arange(0, 32, 2, dtype=np.float32) / 32.0)
    cosT = np.zeros((64, LL), np.float32)
    sinT = np.zeros((64, LL), np.float32)
    for d in range(64):
        half, j = d // 32, d % 32
        ang = pos[half].astype(np.float32) * inv[j % 16]
        cosT[d] = np.cos(ang)
        sinT[d] = (-np.sin(ang)) if j < 16 else np.sin(ang)
    C["ropeT"] = np.stack([cosT, sinT], 0).astype(np.float32)
    rpb = inp["na_rpb"]
    NEG = np.float32(-30000.0)
    cp = np.arange(64)[:, None]
    c = np.arange(64)[None, :]
    cstart = np.clip(c - 8, 0, 48)
    cvalid = (cp >= cstart) & (cp < cstart + 16)
    dcol = np.clip(cp - c + 15, 0, 30)
    ctab = np.full((DEPTH, 8, 64, 31, 64), NEG, np.float32)
    for mm in range(31):
        dr = 22 - mm
        if 0 <= dr <= 14:
            g = rpb[:, :, dr, :][:, :, dcol]
            ctab[:, :, :, mm, :] = np.where(cvalid[None, None], g, NEG)
    C["na_ctab"] = ctab
    mask = np.full((NA_NMASK, 128, 512), NEG, np.float32)
    for (qb, kt), mi in NA_MASK_IDX.items():
        for rr in range(2):
            rp = 2 * kt + rr
            for j in range(8):
                r = 8 * qb + j
                st_ = min(max(r - 4, 0), 24)
                if st_ <= rp < st_ + 8:
                    mask[mi, rr * 64:(rr + 1) * 64, j * 64:(j + 1) * 64] = 0.0
    C["na_mask"] = mask
    C["peer_keysT"] = np.ascontiguousarray(inp["peer_keys"].transpose(0, 1, 3, 2))
    ii = np.arange(128)[:, None]
    jj = np.arange(128)[None, :]
    NEGM = np.float32(-30000.0)
    dc = np.zeros((10, 128, 128), np.float32)
    dc[9] = ((ii // 64) == (jj // 64))
    dc[0] = (ii <= jj)
    dc[1] = (ii >= jj)
    dc[2] = np.eye(128)
    dc[3] = (ii > jj)
    dc[4] = (ii < jj)
    dc[5] = np.where(ii > jj, 0.0, NEGM)
    dc[6] = np.where(ii < jj, 0.0, NEGM)
    dc[7] = np.where(jj >= ii, 0.0, NEGM)
    dc[8] = np.where(jj <= ii, 0.0, NEGM)
    C["dn_consts"] = dc
    C["dn_convT"] = np.ascontiguousarray(inp["dn_conv"].reshape(DEPTH, 5, 24, 128).transpose(0, 3, 2, 1))
    return C


_CACHE = {}
_DBG = {}


def kernel(**inp):
    inp = {k_: np.asarray(v) for k_, v in inp.items()}
    if "prog" not in _CACHE:
        _CACHE["prog"] = build_program()
    nc, kk = _CACHE["prog"]
    w_in_T, w_in_F = _prep_weights(inp)
    C = _prep_consts(inp)
    H = _hy_host(inp)
    ident = np.eye(128, dtype=np.float32)
    in_maps = []
    for i in range(NCORE):
        b = i // 2
        x_in = np.concatenate([inp["x_prompt"][2 * i].reshape(LC, D), inp["x_prompt"][2 * i + 1].reshape(LC, D),
                               inp["x_sample"][b].reshape(LL, D)], axis=0)
        c2 = np.stack([inp["c_ctx"], inp["c"][b]], axis=0)
        c2T = np.ascontiguousarray(c2.reshape(2, 16, 128).transpose(2, 1, 0))
        in_maps.append({
            "x_in": np.ascontiguousarray(x_in), "c2T": c2T, "ident": ident,
            "norm1_g": inp["norm1_g"], "w_ada": inp["w_ada"], "b_ada": inp["b_ada"],
            "w_in_T": w_in_T, "w_in_F": w_in_F, "mla_kv_norm": inp["mla_kv_norm"],
            "mla_q_norm": inp["mla_q_norm"], "w_qb_p": C["w_qb_p"], "w_kvb_p": C["w_kvb_p"],
            "cache_ckv": inp["cache_mla_ckv"][b], "cache_kr": inp["cache_mla_krope"][b],
            "cache_nak": np.ascontiguousarray(inp["cache_na_k"][b].reshape(DEPTH, PAST, 1024)),
            "cache_nav": np.ascontiguousarray(inp["cache_na_v"][b].reshape(DEPTH, PAST, 1024)),
            "ropeT": C["ropeT"], "na_ctab": C["na_ctab"], "na_mask": C["na_mask"],
            "w_branch": inp["w_branch"], "w_out": inp["w_out"], "norm2_g": inp["norm2_g"],
            "peer_wq": inp["peer_wq"], "peer_keysT": C["peer_keysT"],
            "peer_u0": inp["peer_u"][0], "peer_u1": inp["peer_u"][1],
            "peer_v0": inp["peer_v"][0], "peer_v1": inp["peer_v"][1],
            "final_g": inp["final_g"].reshape(1, D),
            "dn_consts": C["dn_consts"], "dn_convT": C["dn_convT"],
            "dn_a_log": inp["dn_a_log"].reshape(DEPTH, 16), "dn_dt_bias": inp["dn_dt_bias"].reshape(DEPTH, 16),
            "dn_out_norm": inp["dn_out_norm"],
            "dn_state0": inp["state_dn_fwd"][b], "dn_state1": inp["state_dn_bwd"][b],
        })
        in_maps[-1].update(H)
        if DBG_BR:
            in_maps[-1]["dbg_br"] = _DBG["br"]
    import os
    ndev = int(os.environ.get("KDEV_CORES", NCORE))
    res = run_bass_kernel_spmd(nc, in_maps[:ndev], core_ids=list(range(ndev)))
    r = list(res.results) + [res.results[0]] * (NCORE - ndev)
    _DBG["res"] = res.results[0]
    B = 16
    y_prompt = np.concatenate([r[i]["y_out"][:NCTX * LC].reshape(NCTX, LC, D) for i in range(NCORE)], axis=0)
    y_sample = np.stack([r[2 * j]["y_out"][NCTX * LC:] for j in range(4)], axis=0)
    new_ckv = np.concatenate([r[i]["o_ckv"] for i in range(NCORE)], axis=0)
    new_kr = np.concatenate([r[i]["o_kr"] for i in range(NCORE)], axis=0)
    new_nak = np.concatenate([r[i]["o_nak"] for i in range(NCORE)], axis=0).reshape(B, DEPTH, LC, 8, 128)
    new_nav = np.concatenate([r[i]["o_nav"] for i in range(NCORE)], axis=0).reshape(B, DEPTH, LC, 8, 128)
    new_dnf = np.concatenate([r[i]["o_dn0"] for i in range(NCORE)], axis=0)
    new_dnb = np.concatenate([r[i]["o_dn1"] for i in range(NCORE)], axis=0)
    return (y_prompt, y_sample, new_ckv, new_kr, new_nak, new_nav, new_dnf, new_dnb)
```

```python
import numpy as np
from contextlib import ExitStack
import concourse.bass as bass
import concourse.mybir as mybir
from concourse.bass_utils import run_bass_kernel_spmd

F32 = mybir.dt.float32
BF16 = mybir.dt.bfloat16
I32 = mybir.dt.int32
U32 = mybir.dt.uint32
U16 = mybir.dt.uint16
AF = mybir.ActivationFunctionType
ALU = mybir.AluOpType
AX = mybir.AxisListType

D = 2048
DEPTH = 2
NCORE = 8
LC = 256
LL = 2048
PAST = 512
NCTX = 2
TT = NCTX * LC + LL
NTILE = TT // 128
EPS = 1e-6

T_COLS = {}
_o = 0
for _n, _s in (("cq", 512), ("ckv", 256), ("kr", 64), ("z", 1024), ("a", 16), ("b", 16),
               ("nak", 1024), ("nav", 1024), ("gate", 8192)):
    T_COLS[_n] = (_o, _s)
    _o += _s
NT_COLS = _o
F_ROWS = {}
_o = 0
for _n, _s in (("krT", 64), ("krswT", 64), ("dnT", 3072), ("hyT", 3072), ("naqT", 1024), ("nakT", 1024)):
    F_ROWS[_n] = (_o, _s)
    _o += _s
NF_ROWS = _o

IN_SIZES = (512, 256, 64, 3072, 1024, 16, 16, 3072, 3072, 8192)
IN_OFF = np.concatenate([[0], np.cumsum(IN_SIZES)]).astype(int)
(O_CQ, O_CKV, O_KR, O_DN, O_Z, O_A, O_B, O_HY, O_NA, O_GATE) = [int(v) for v in IN_OFF[:-1]]


class Buf:
    __slots__ = ("t", "last_w", "readers", "name", "root")

    def __init__(self, t, name, root=None):
        self.t = t
        self.name = name
        self.last_w = None
        self.readers = {}
        self.root = root if root is not None else self

    def __getitem__(self, idx):
        return self.t[idx]


class Sched:
    ENG = ("pe", "act", "dve", "pool", "sp")
    NDMA = 6

    def __init__(self, nc, es):
        self.nc = nc
        self.es = es
        self.eng = {"pe": nc.tensor, "act": nc.scalar, "dve": nc.vector, "pool": nc.gpsimd, "sp": nc.sync}
        self.sem = {}
        self.cnt = {}
        for e in self.ENG:
            self.sem[e] = es.enter_context(nc.semaphore("sem_" + e))
            self.cnt[e] = 0
        self.dq = {}
        for q in ("sp", "act", "pool"):
            sl = []
            for i in range(self.NDMA):
                key = ("dma", q, i)
                self.sem[key] = es.enter_context(nc.semaphore("dsem_%s_%d" % (q, i)))
                self.cnt[key] = 0
                sl.append(key)
            self.dq[q] = [sl, 0]
        self.seen = {e: {} for e in self.ENG}
        self.ninstr = 0
        self.scope = es

    def _nm(self, name):
        self.nid = getattr(self, "nid", 0) + 1
        return "%s_%d" % (name, self.nid)

    def sbuf(self, name, shape, dtype=F32):
        name = self._nm(name)
        return Buf(self.scope.enter_context(self.nc.sbuf_tensor(name, list(shape), dtype)), name)

    def psum(self, name, shape, dtype=F32):
        name = self._nm(name)
        return Buf(self.scope.enter_context(self.nc.psum_tensor(name, list(shape), dtype)), name)

    def dram(self, name, shape, dtype=F32, kind="Internal"):
        t = self.nc.dram_tensor(name, list(shape), dtype, kind=kind)
        return Buf(t.ap(), name)

    def _wait(self, e, key, val):
        if self.seen[e].get(key, 0) >= val:
            return
        self.eng[e].wait_ge(self.sem[key], val)
        self.seen[e][key] = val
        self.ninstr += 1

    def _deps(self, e, reads, writes):
        reads = [b.root for b in reads]
        writes = [b.root for b in writes]
        need = {}
        for b in list(reads) + list(writes):
            if b.last_w is not None:
                k, v = b.last_w
                if need.get(k, 0) < v:
                    need[k] = v
        for b in writes:
            for k, v in b.readers.items():
                if need.get(k, 0) < v:
                    need[k] = v
        for k, v in need.items():
            if k == "pe" and e == "pe":
                continue
            self._wait(e, k, v)

    def _mark(self, ev, reads, writes):
        reads = [b.root for b in reads]
        writes = [b.root for b in writes]
        k, v = ev
        for b in writes:
            b.last_w = ev
            b.readers = {}
        for b in reads:
            if b.readers.get(k, 0) < v:
                b.readers[k] = v

    def op(self, e, fn, reads=(), writes=()):
        self._deps(e, reads, writes)
        ins = fn(self.eng[e])
        self.cnt[e] += 1
        ins.then_inc(self.sem[e], 1)
        self._mark((e, self.cnt[e]), reads, writes)
        self.ninstr += 1
        return ins

    def dma(self, q, fn, reads=(), writes=()):
        sl, i = self.dq[q]
        key = sl[i % self.NDMA]
        self.dq[q][1] = i + 1
        if self.cnt[key] > 0:
            self._wait(q, key, self.cnt[key])
        self._deps(q, reads, writes)
        ins = fn(self.eng[q])
        self.cnt[key] += 16
        ins.then_inc(self.sem[key], 16)
        self._mark((key, self.cnt[key]), reads, writes)
        self.ninstr += 1
        return ins

    def barrier(self):
        for e in self.ENG:
            for key, v in self.cnt.items():
                if v > 0:
                    self._wait(e, key, v)


import os as _os
DBG_BR = bool(int(_os.environ.get("KDBG_BR", "0")))
DBG_FILL = (2,)
DNSTOP = int(_os.environ.get("KDN_STOP", "9"))
DNVAR = int(_os.environ.get("KDN_VAR", "0"))
DNSEQ = int(_os.environ.get("KDN_SEQ", "3"))


def _hy_inputs(R, ein):
    R["hy_tab"] = []
    R["hy_zemb"] = []
    R["hy_wf"] = []
    R["hy_win"] = []
    for i, L in enumerate((LC, LL)):
        nf, KT, NB = (L + 1), (L + 1 + 127) // 128, (L + 1 + 511) // 512
        R["hy_tab"].append(ein("hy_tab%d" % i, [2, NB, 128, KT, 512], BF16))
        R["hy_zemb"].append(ein("hy_zemb%d" % i, [33, L]))
        R["hy_wf"].append(ein("hy_wf%d" % i, [128, KT]))
        R["hy_win"].append(ein("hy_win%d" % i, [L, 1024]))
    R["hy_w1"] = ein("hy_w1", [DEPTH, 33, 64])
    R["hy_w2"] = ein("hy_w2", [DEPTH, 64, 64])
    R["hy_w3"] = ein("hy_w3", [DEPTH, 64, 4096])
    R["hy_b12"] = ein("hy_b12", [DEPTH, 64, 2])
    R["hy_convT"] = ein("hy_convT", [DEPTH, 128, 24, 3])
    R["hy_biasT"] = ein("hy_biasT", [DEPTH, 2, 128, 8])


def _hy_scratch(S, R):
    R["hyZ"] = S.dram("hyZ", [3072, TT])
    R["hyY1"] = S.dram("hyY1", [1024, TT])
    R["hyU"] = S.dram("hyU", [TT, 1024], BF16)
    R["hyPQ"] = [S.dram("hyPQ%d" % i, [2, 2, ((L + 1 + 127) // 128) * 128, 1024]) for i, L in enumerate((LC, LL))]
    R["hyYS"] = S.dram("hyYS", [2, ((LL + 1 + 127) // 128) * 128, 1024], BF16)


def _hy_host(inp):
    import ml_dtypes
    H = {}
    for i, L in enumerate((LC, LL)):
        nf, KT, NB = (L + 1), (L + 1 + 127) // 128, (L + 1 + 511) // 512
        r = np.arange(KT * 128, dtype=np.int64)
        q = np.arange(NB * 512, dtype=np.int64)
        prod = (r[:, None] * q[None, :]) % (2 * L)
        ang = np.pi * prod.astype(np.float64) / L
        valid = (r[:, None] <= L) & (q[None, :] <= L)
        tabs = []
        for fn in (np.cos, np.sin):
            T = np.where(valid, fn(ang), 0.0).astype(np.float32)
            T = T.reshape(KT, 128, NB, 512).transpose(2, 1, 0, 3)
            tabs.append(T)
        H["hy_tab%d" % i] = np.ascontiguousarray(np.stack(tabs, 0)).astype(ml_dtypes.bfloat16)
        f = np.arange(KT * 128)
        wf = np.where((f == 0) | (f == L), 1.0, np.where(f < L, 2.0, 0.0)) / (2.0 * L)
        H["hy_wf%d" % i] = np.ascontiguousarray(wf.reshape(KT, 128).T).astype(np.float32)
        t01 = np.linspace(0.0, 1.0, L, dtype=np.float32)[:, None]
        w = (np.float32(2.0 * np.pi) * np.arange(L, dtype=np.float32)[:, None] / np.float32(L)).astype(np.float32)
        fr = np.linspace(1e-4, 15.0, 16, dtype=np.float32)[None, :]
        z = np.concatenate([t01, np.cos(fr * w), -np.sin(fr * w)], axis=-1).astype(np.float32)
        H["hy_zemb%d" % i] = np.ascontiguousarray(z.T)
        max_decay = np.log(1e-2) / 0.3
        min_decay = np.log(1e-2) / 1.5
        deltas = np.linspace(min_decay, max_decay, 1024, dtype=np.float32)
        H["hy_win%d" % i] = np.exp(-t01 * np.abs(deltas)[None, :]).astype(np.float32)
    H["hy_w1"] = inp["hy_w1"]
    H["hy_w2"] = inp["hy_w2"]
    H["hy_w3"] = inp["hy_w3"]
    H["hy_b12"] = np.ascontiguousarray(np.stack([inp["hy_b1"], inp["hy_b2"]], axis=-1))
    H["hy_convT"] = np.ascontiguousarray(inp["hy_conv"].reshape(DEPTH, 3, 24, 128).transpose(0, 3, 2, 1))
    H["hy_biasT"] = np.ascontiguousarray(inp["hy_bias"].reshape(DEPTH, 2, 8, 128).transpose(0, 1, 3, 2))
    return H


class K:
    pass


def _evac(S, i, out_ap, in_ap, reads, writes):
    if i % 2 == 0:
        S.op("act", lambda e: e.activation(out=out_ap, in_=in_ap, func=AF.Copy), reads=reads, writes=writes)
    else:
        S.op("dve", lambda e: e.tensor_copy(out=out_ap, in_=in_ap), reads=reads, writes=writes)


def _rmsnorm_rows(S, xt, g, n, jk, st):
    S.op("act", lambda e: e.activation(out=jk[:, 0:n], in_=xt[:, 0:n], func=AF.Square, accum_out=st[:]),
         reads=[xt], writes=[jk, st])
    S.op("act", lambda e: e.activation(out=st[:], in_=st[:], func=AF.Sqrt, scale=1.0 / n, bias=EPS),
         reads=[st], writes=[st])
    S.op("dve", lambda e: e.reciprocal(out=st[:], in_=st[:]), reads=[st], writes=[st])
    S.op("dve", lambda e: e.scalar_tensor_tensor(out=xt[:, 0:n], in0=xt[:, 0:n], scalar=st[:, 0:1], in1=g[:, 0:n],
                                                 op0=ALU.mult, op1=ALU.mult),
         reads=[xt, st, g], writes=[xt])


def _attn(S, A, qparts, kparts, v_ap, ktl, q0, QB, scale, bias_fn, out_dst):
    psO, psD = A["psO"][A["n"] % 2], A["psD"][A["n"] % 2]
    A["n"] += 1
    n = len(ktl)
    for i, kt in enumerate(ktl):
        ps = A["psS"][A["ns"] % 2]
        pT = A["pT"][A["ns"] % 3]
        A["ns"] += 1
        for pi in range(len(qparts)):
            kb, kf = kparts[pi]
            qb_, qf = qparts[pi]
            S.op("pe", lambda e: e.matmul(ps[:, 0:QB], lhsT=kf(kt), rhs=qf(q0, QB), start=(pi == 0),
                                          stop=(pi == len(qparts) - 1)), reads=[kb, qb_], writes=[ps])
        bb = bias_fn(kt) if bias_fn is not None else None
        if bb is not None:
            tf = A["tf"][A["ns"] % 2]
            S.op("dve", lambda e: e.scalar_tensor_tensor(out=tf[:, 0:QB], in0=ps[:, 0:QB], scalar=float(scale),
                                                         in1=bb[:, 0:QB], op0=ALU.mult, op1=ALU.add),
                 reads=[ps, bb], writes=[tf])
            S.op("act", lambda e: e.activation(out=pT[:, 0:QB], in_=tf[:, 0:QB], func=AF.Exp), reads=[tf], writes=[pT])
        else:
            S.op("act", lambda e: e.activation(out=pT[:, 0:QB], in_=ps[:, 0:QB], func=AF.Exp, scale=float(scale)),
                 reads=[ps], writes=[pT])
        vb, va = v_ap(kt)
        S.op("pe", lambda e: e.matmul(psO[:, 0:QB], lhsT=va, rhs=pT[:, 0:QB], start=(i == 0), stop=(i == n - 1)),
             reads=[vb, pT], writes=[psO])
        S.op("pe", lambda e: e.matmul(psD[:, 0:QB], lhsT=A["ones"][:, :], rhs=pT[:, 0:QB], start=(i == 0),
                                      stop=(i == n - 1)), reads=[A["ones"], pT], writes=[psD])
    rd = A["rd"]
    ob = A["ob"][A["n"] % 2]
    S.op("dve", lambda e: e.reciprocal(out=rd[:, 0:QB], in_=psD[:, 0:QB]), reads=[psD], writes=[rd])
    S.op("dve", lambda e: e.tensor_tensor(out=ob[:, 0:QB], in0=psO[:, 0:QB], in1=rd[:, 0:QB], op=ALU.mult),
         reads=[psO, rd], writes=[ob])
    dbuf, dap = out_dst
    S.dma("sp", lambda e: e.dma_start(out=dap, in_=ob[:, 0:QB]), reads=[ob], writes=[dbuf])


def _attn_res(S):
    A = {"n": 0, "ns": 0}
    A["psS"] = [S.psum("psS%d" % i, [128, 512], F32) for i in range(2)]
    A["psO"] = [S.psum("psO%d" % i, [128, 512], F32) for i in range(2)]
    A["psD"] = [S.psum("psD%d" % i, [128, 512], F32) for i in range(2)]
    A["pT"] = [S.sbuf("pT%d" % i, [128, 512], BF16) for i in range(3)]
    A["tf"] = [S.sbuf("tf%d" % i, [128, 512], F32) for i in range(2)]
    A["rd"] = S.sbuf("rd", [128, 512], F32)
    A["ob"] = [S.sbuf("ob%d" % i, [128, 512], BF16) for i in range(2)]
    ones = S.sbuf("onesb", [128, 128], BF16)
    S.op("dve", lambda e: e.memset(ones[:], 1.0), writes=[ones])
    A["ones"] = ones
    return A


def _transpose_rows(S, src, ncol, dst, dst_fn, ident, ptr, ev0=0):
    nch = ncol // 128
    for c0 in range(0, nch, 8):
        pt = ptr[(ev0 + c0 // 8) % 2]
        nn = min(8, nch - c0)
        for j in range(nn):
            S.op("pe", lambda e: e.transpose(pt[:, j, :], src[:, (c0 + j) * 128:(c0 + j + 1) * 128], ident[:]),
                 reads=[src, ident], writes=[pt])
        for j in range(nn):
            _evac(S, j, dst_fn(c0 + j), pt[:, j, :], reads=[pt], writes=[dst])


def stage_mla(S, l, R):
    ident = R["ident"]
    P_T, P_F, brT = R["P_T"], R["P_F"], R["brT"]
    with ExitStack() as sc:
        S.scope = sc
        A = _attn_res(S)
        psP = [S.psum("psP%d" % i, [128, 512], F32) for i in range(2)]
        wqb = S.sbuf("wqb", [128, 4, 2048], BF16)
        wkvb = S.sbuf("wkvb", [128, 2, 2048], BF16)
        S.dma("pool", lambda e: e.dma_start(out=wqb[:], in_=R["w_qb"][l].rearrange("(k p) c -> p k c", p=128)),
              reads=[R["w_qb"]], writes=[wqb])
        S.dma("pool", lambda e: e.dma_start(out=wkvb[:], in_=R["w_kvb"][l].rearrange("(k p) c -> p k c", p=128)),
              reads=[R["w_kvb"]], writes=[wkvb])
        gq = S.sbuf("gq", [128, 512], F32)
        gk = S.sbuf("gk", [128, 256], F32)
        S.dma("sp", lambda e: e.dma_start(out=gq[:], in_=R["mla_q_norm"][l, :].partition_broadcast(128)),
              reads=[R["mla_q_norm"]], writes=[gq])
        S.dma("sp", lambda e: e.dma_start(out=gk[:], in_=R["mla_kv_norm"][l, :].partition_broadcast(128)),
              reads=[R["mla_kv_norm"]], writes=[gk])
        ropc = S.sbuf("ropc", [64, LL], F32)
        rops = S.sbuf("rops", [64, LL], F32)
        S.dma("sp", lambda e: e.dma_start(out=ropc[:], in_=R["ropeT"][0]), reads=[R["ropeT"]], writes=[ropc])
        S.dma("sp", lambda e: e.dma_start(out=rops[:], in_=R["ropeT"][1]), reads=[R["ropeT"]], writes=[rops])
        cqT = S.sbuf("cqT", [128, 4, LL], BF16)
        ckvT = S.sbuf("ckvT", [128, 2, LL + PAST], BF16)
        krT = S.sbuf("krT", [128, LL + PAST], BF16)
        vall = S.sbuf("vall", [128, (LL + PAST) // 128, 1024], BF16)
        knT = S.sbuf("knT", [128, LL + PAST], BF16)
        qnT = S.sbuf("qnT", [128, LL], BF16)
        qrT = S.sbuf("qrT", [128, LL], BF16)
        S.op("dve", lambda e: e.memset(krT[64:128, :], 0.0), writes=[krT])
        S.op("dve", lambda e: e.memset(qrT[64:128, :], 0.0), writes=[qrT])
        xt = [S.sbuf("mx%d" % i, [128, 512], F32) for i in range(2)]
        xb = [S.sbuf("mxb%d" % i, [128, 512], BF16) for i in range(2)]
        jk = S.sbuf("mjk", [128, 512], F32)
        st = [S.sbuf("mst%d" % i, [128, 1], F32) for i in range(2)]
        r1 = S.sbuf("mr1", [64, 512], F32)
        r2 = S.sbuf("mr2", [64, 512], F32)
        ptr = [S.psum("mptr%d" % i, [128, 8, 128], BF16) for i in range(0)]
        aq, _ = T_COLS["cq"]
        ak, _ = T_COLS["ckv"]
        akr, _ = T_COLS["kr"]
        fkr, _ = F_ROWS["krT"]
        fks, _ = F_ROWS["krswT"]

        def tr_bf(src, ncol, dst, dst_fn, ev):
            nch = ncol // 128
            pt = psP[ev % 2]
            ptv = pt[:, :].bitcast(BF16)
            for j in range(nch):
                S.op("pe", lambda e: e.transpose(ptv[:, j * 128:(j + 1) * 128], src[:, j * 128:(j + 1) * 128], ident[:]),
                     reads=[src, ident], writes=[pt])
            for j in range(nch):
                _evac(S, j, dst_fn(j), ptv[:, j * 128:(j + 1) * 128], reads=[pt], writes=[dst])

        seqs = [(s * LC, LC, False) for s in range(NCTX)] + [(NCTX * LC, LL, True)]
        for si, (tok0, L, latent) in enumerate(seqs):
            Lk = L + (PAST if latent else 0)
            nkt = Lk // 128
            QB = 512 if latent else 256
            for t in range(L // 128):
                g0 = tok0 + t * 128
                x1, x1b, s1 = xt[t % 2], xb[t % 2], st[t % 2]
                S.dma("sp", lambda e: e.dma_start(out=x1[:, 0:512], in_=P_T[g0:g0 + 128, aq:aq + 512]),
                      reads=[P_T], writes=[x1])
                _rmsnorm_rows(S, x1, gq, 512, jk, s1)
                S.op("pool", lambda e: e.tensor_copy(out=x1b[:, 0:512], in_=x1[:, 0:512]), reads=[x1], writes=[x1b])
                tr_bf(x1b, 512, cqT, lambda j: cqT[:, j, t * 128:(t + 1) * 128], t)
            for t in range(nkt):
                x1, x1b, s1 = xt[t % 2], xb[t % 2], st[t % 2]
                if t < L // 128:
                    g0 = tok0 + t * 128
                    S.dma("sp", lambda e: e.dma_start(out=x1[:, 0:256], in_=P_T[g0:g0 + 128, ak:ak + 256]),
                          reads=[P_T], writes=[x1])
                    _rmsnorm_rows(S, x1, gk, 256, jk, s1)
                    if not latent:
                        S.dma("sp", lambda e: e.dma_start(out=R["o_ckv"][si, l, t * 128:(t + 1) * 128, :], in_=x1[:, 0:256]),
                              reads=[x1], writes=[R["o_ckv"]])
                else:
                    p0 = (t - L // 128) * 128
                    S.dma("sp", lambda e: e.dma_start(out=x1[:, 0:256], in_=R["cache_ckv"][l, p0:p0 + 128, :]),
                          reads=[R["cache_ckv"]], writes=[x1])
                S.op("pool", lambda e: e.tensor_copy(out=x1b[:, 0:256], in_=x1[:, 0:256]), reads=[x1], writes=[x1b])
                tr_bf(x1b, 256, ckvT, lambda j: ckvT[:, j, t * 128:(t + 1) * 128], t)
                if t >= L // 128:
                    p0 = (t - L // 128) * 128
                    S.dma("sp", lambda e: e.dma_start(out=x1[:, 256:320], in_=R["cache_kr"][l, p0:p0 + 128, :]),
                          reads=[R["cache_kr"]], writes=[x1])
                    S.op("pool", lambda e: e.tensor_copy(out=x1b[:, 256:320], in_=x1[:, 256:320]), reads=[x1], writes=[x1b])
                    pt = psP[(t + 1) % 2]
                    ptv = pt[:, :].bitcast(BF16)
                    S.op("pe", lambda e: e.transpose(ptv[0:64, 0:128], x1b[:, 256:320], ident[:]),
                         reads=[x1b, ident], writes=[pt])
                    _evac(S, t, krT[0:64, t * 128:(t + 1) * 128], ptv[0:64, 0:128], reads=[pt], writes=[krT])
            for g in range(L // QB):
                g0 = tok0 + g * QB
                S.dma("sp", lambda e: e.dma_start(out=r1[:, 0:QB], in_=P_F[fkr:fkr + 64, g0:g0 + QB]), reads=[P_F], writes=[r1])
                if latent:
                    S.dma("sp", lambda e: e.dma_start(out=r2[:, 0:QB], in_=P_F[fks:fks + 64, g0:g0 + QB]),
                          reads=[P_F], writes=[r2])
                    S.op("dve", lambda e: e.tensor_tensor(out=r1[:, 0:QB], in0=r1[:, 0:QB], in1=ropc[:, g * QB:(g + 1) * QB],
                                                          op=ALU.mult), reads=[r1, ropc], writes=[r1])
                    S.op("dve", lambda e: e.tensor_tensor(out=r2[:, 0:QB], in0=r2[:, 0:QB], in1=rops[:, g * QB:(g + 1) * QB],
                                                          op=ALU.mult), reads=[r2, rops], writes=[r2])
                    S.op("dve", lambda e: e.tensor_tensor(out=krT[0:64, g * QB:(g + 1) * QB], in0=r1[:, 0:QB], in1=r2[:, 0:QB],
                                                          op=ALU.add), reads=[r1, r2], writes=[krT])
                else:
                    S.op("dve", lambda e: e.tensor_copy(out=krT[0:64, g * QB:(g + 1) * QB], in_=r1[:, 0:QB]),
                         reads=[r1], writes=[krT])
            ev = 0
            for t in range(nkt):
                for hb in range(2):
                    ps = psP[ev % 2]
                    for kc in range(2):
                        S.op("pe", lambda e: e.matmul(ps[:, :], lhsT=ckvT[:, kc, t * 128:(t + 1) * 128],
                                                      rhs=wkvb[:, kc, 1024 + hb * 512:1024 + (hb + 1) * 512],
                                                      start=(kc == 0), stop=(kc == 1)), reads=[ckvT, wkvb], writes=[ps])
                    _evac(S, ev, vall[:, t, hb * 512:(hb + 1) * 512], ps[:, :], reads=[ps], writes=[vall])
                    ev += 1
            for h in range(8):
                for g in range((Lk + 511) // 512):
                    w = min(512, Lk - g * 512)
                    ps = psP[ev % 2]
                    for kc in range(2):
                        S.op("pe", lambda e: e.matmul(ps[:, 0:w], lhsT=wkvb[:, kc, h * 128:(h + 1) * 128],
                                                      rhs=ckvT[:, kc, g * 512:g * 512 + w], start=(kc == 0), stop=(kc == 1)),
                             reads=[ckvT, wkvb], writes=[ps])
                    _evac(S, ev, knT[:, g * 512:g * 512 + w], ps[:, 0:w], reads=[ps], writes=[knT])
                    ev += 1
                for g in range(L // QB):
                    ps = psP[ev % 2]
                    for kc in range(4):
                        S.op("pe", lambda e: e.matmul(ps[:, 0:QB], lhsT=wqb[:, kc, h * 128:(h + 1) * 128],
                                                      rhs=cqT[:, kc, g * QB:(g + 1) * QB], start=(kc == 0), stop=(kc == 3)),
                             reads=[cqT, wqb], writes=[ps])
                    _evac(S, ev, qnT[:, g * QB:(g + 1) * QB], ps[:, 0:QB], reads=[ps], writes=[qnT])
                    ev += 1
                    ps = psP[ev % 2]
                    for kc in range(4):
                        S.op("pe", lambda e: e.matmul(ps[0:64, 0:QB], lhsT=wqb[:, kc, 1024 + h * 64:1024 + (h + 1) * 64],
                                                      rhs=cqT[:, kc, g * QB:(g + 1) * QB], start=(kc == 0), stop=(kc == 3)),
                             reads=[cqT, wqb], writes=[ps])
                    if latent:
                        ps2 = psP[(ev + 1) % 2]
                        for kc in range(4):
                            S.op("pe", lambda e: e.matmul(ps2[0:64, 0:QB], lhsT=wqb[:, kc, 1536 + h * 64:1536 + (h + 1) * 64],
                                                          rhs=cqT[:, kc, g * QB:(g + 1) * QB], start=(kc == 0), stop=(kc == 3)),
                                 reads=[cqT, wqb], writes=[ps2])
                        S.op("dve", lambda e: e.tensor_tensor(out=r1[:, 0:QB], in0=ps[0:64, 0:QB],
                                                              in1=ropc[:, g * QB:(g + 1) * QB], op=ALU.mult),
                             reads=[ps, ropc], writes=[r1])
                        S.op("dve", lambda e: e.tensor_tensor(out=r2[:, 0:QB], in0=ps2[0:64, 0:QB],
                                                              in1=rops[:, g * QB:(g + 1) * QB], op=ALU.mult),
                             reads=[ps2, rops], writes=[r2])
                        S.op("dve", lambda e: e.tensor_tensor(out=qrT[0:64, g * QB:(g + 1) * QB], in0=r1[:, 0:QB],
                                                              in1=r2[:, 0:QB], op=ALU.add), reads=[r1, r2], writes=[qrT])
                        ev += 2
                    else:
                        _evac(S, ev, qrT[0:64, g * QB:(g + 1) * QB], ps[0:64, 0:QB], reads=[ps], writes=[qrT])
                        ev += 1
                for g in range(L // QB):
                    _attn(S, A,
                          qparts=[(qnT, lambda q0, n: qnT[:, q0:q0 + n]), (qrT, lambda q0, n: qrT[:, q0:q0 + n])],
                          kparts=[(knT, lambda kt: knT[:, kt * 128:(kt + 1) * 128]),
                                  (krT, lambda kt: krT[:, kt * 128:(kt + 1) * 128])],
                          v_ap=lambda kt: (vall, vall[:, kt, h * 128:(h + 1) * 128]),
                          ktl=list(range(nkt)), q0=g * QB, QB=QB, scale=192 ** -0.5, bias_fn=None,
                          out_dst=(brT[0], brT[0][h * 128:(h + 1) * 128, tok0 + g * QB:tok0 + (g + 1) * QB]))
        S.barrier()
    S.scope = S.es


NA_QB_TILES = {0: list(range(0, 6)), 1: list(range(2, 10)), 2: list(range(6, 14)), 3: list(range(10, 16))}
NA_MASK_IDX = {}
_i = 0
for _qb in range(4):
    for _kt in NA_QB_TILES[_qb]:
        NA_MASK_IDX[(_qb, _kt)] = _i
        _i += 1
NA_NMASK = _i


def stage_na(S, l, R):
    ident = R["ident"]
    P_T, P_F, brT = R["P_T"], R["P_F"], R["brT"]
    with ExitStack() as sc:
        S.scope = sc
        A = _attn_res(S)
        psP = [S.psum("psP%d" % i, [128, 512], F32) for i in range(2)]
        qT = S.sbuf("naqT", [128, LL], BF16)
        kT = S.sbuf("nakT", [128, LL + PAST], BF16)
        vall = S.sbuf("navall", [128, (LL + PAST) // 128, 1024], BF16)
        kc_f = [S.sbuf("nakc%d" % i, [128, 1024], F32) for i in range(2)]
        kc_b = S.sbuf("nakcb", [128, 4, 1024], BF16)
        bias = [S.sbuf("nabias%d" % i, [128, 512], F32) for i in range(2)]
        mask = [S.sbuf("namask%d" % i, [128, 512], F32) for i in range(2)]
        fq, _ = F_ROWS["naqT"]
        fk, _ = F_ROWS["nakT"]
        av, _ = T_COLS["nav"]
        seqs = [(s * LC, LC, False) for s in range(NCTX)] + [(NCTX * LC, LL, True)]
        nb = 0
        for si, (tok0, L, latent) in enumerate(seqs):
            Lk = L + (PAST if latent else 0)
            nkt = Lk // 128
            QB = 512 if latent else 256
            for t in range(L // 128):
                g0 = tok0 + t * 128
                S.dma("pool", lambda e: e.dma_start(out=vall[:, t, :], in_=P_T[g0:g0 + 128, av:av + 1024]),
                      reads=[P_T], writes=[vall])
            if latent:
                for t in range(4):
                    S.dma("pool", lambda e: e.dma_start(out=vall[:, L // 128 + t, :],
                                                        in_=R["cache_nav"][l, t * 128:(t + 1) * 128, :]),
                          reads=[R["cache_nav"]], writes=[vall])
                    S.dma("pool", lambda e: e.dma_start(out=kc_b[:, t, :], in_=R["cache_nak"][l, t * 128:(t + 1) * 128, :]),
                          reads=[R["cache_nak"]], writes=[kc_b])
            for h in range(8):
                S.dma("pool", lambda e: e.dma_start(out=qT[:, 0:L], in_=P_F[fq + h * 128:fq + (h + 1) * 128, tok0:tok0 + L]),
                      reads=[P_F], writes=[qT])
                S.dma("pool", lambda e: e.dma_start(out=kT[:, 0:L], in_=P_F[fk + h * 128:fk + (h + 1) * 128, tok0:tok0 + L]),
                      reads=[P_F], writes=[kT])
                if latent:
                    pt = psP[h % 2]
                    ptv = pt[:, :].bitcast(BF16)
                    for t in range(4):
                        S.op("pe", lambda e: e.transpose(ptv[:, t * 128:(t + 1) * 128], kc_b[:, t, h * 128:(h + 1) * 128],
                                                         ident[:]), reads=[kc_b, ident], writes=[pt])
                    _evac(S, h, kT[:, L:L + 512], ptv[:, 0:512], reads=[pt], writes=[kT])
                for g in range(L // QB):
                    if latent:
                        ktl = NA_QB_TILES[g] + [16, 17, 18, 19]

                        def bias_fn(kt, g=g, h=h):
                            if kt >= 16:
                                return None
                            bb, mm = bias[bias_fn.n % 2], mask[bias_fn.n % 2]
                            bias_fn.n += 1
                            for rr in range(2):
                                rp = 2 * kt + rr
                                mm0 = 15 - rp + 8 * g
                                S.dma("sp", lambda e: e.dma_start(
                                    out=bb[rr * 64:(rr + 1) * 64, :],
                                    in_=R["na_ctab"][l, h, :, mm0:mm0 + 8, :].rearrange("c m q -> c (m q)")),
                                    reads=[R["na_ctab"]], writes=[bb])
                            mi = NA_MASK_IDX[(g, kt)]
                            S.dma("sp", lambda e: e.dma_start(out=mm[:], in_=R["na_mask"][mi]), reads=[R["na_mask"]], writes=[mm])
                            S.op("pool", lambda e: e.tensor_tensor(out=bb[:], in0=bb[:], in1=mm[:], op=ALU.add),
                                 reads=[bb, mm], writes=[bb])
                            return bb
                        bias_fn.n = nb
                    else:
                        ktl = list(range(nkt))
                        bias_fn = None
                    _attn(S, A,
                          qparts=[(qT, lambda q0, n: qT[:, q0:q0 + n])],
                          kparts=[(kT, lambda kt: kT[:, kt * 128:(kt + 1) * 128])],
                          v_ap=lambda kt: (vall, vall[:, kt, h * 128:(h + 1) * 128]),
                          ktl=ktl, q0=g * QB, QB=QB, scale=128 ** -0.5, bias_fn=bias_fn,
                          out_dst=(brT[3], brT[3][h * 128:(h + 1) * 128, tok0 + g * QB:tok0 + (g + 1) * QB]))
                    if latent:
                        nb = bias_fn.n
        S.barrier()
    S.scope = S.es


def stage_merge(S, l, R, x_src):
    ident = R["ident"]
    P_T, brT, mrg, xbuf, modd = R["P_T"], R["brT"], R["mrg"], R["xbuf"], R["modd"]
    ag, _ = T_COLS["gate"]
    for b in range(4):
        with ExitStack() as sc:
            S.scope = sc
            wbr = S.sbuf("wbr", [128, 8, 2048], BF16)
            S.dma("pool", lambda e: e.dma_start(out=wbr[:], in_=R["w_branch"][l, b].rearrange("(k p) c -> p k c", p=128)),
                  reads=[R["w_branch"]], writes=[wbr])
            brt = [S.sbuf("brt%d" % i, [128, 8, 128], BF16) for i in range(2)]
            gt = [S.sbuf("gt%d" % i, [128, 2048], F32) for i in range(2)]
            acc = [S.sbuf("acc%d" % i, [128, 2048], F32) for i in range(2)]
            pm = [S.psum("pm%d" % i, [128, 512], F32) for i in range(4)]
            ev = 0
            for t in range(NTILE):
                bt, g, a = brt[t % 2], gt[t % 2], acc[t % 2]
                S.dma("sp", lambda e: e.dma_start(out=bt[:], in_=brT[b][:, t * 128:(t + 1) * 128].rearrange("(k p) t -> p k t", p=128)),
                      reads=[brT[b]], writes=[bt])
                S.dma("sp", lambda e: e.dma_start(out=g[:], in_=P_T[t * 128:(t + 1) * 128, ag + b * 2048:ag + (b + 1) * 2048]),
                      reads=[P_T], writes=[g])
                if b > 0:
                    S.dma("sp", lambda e: e.dma_start(out=a[:], in_=mrg[t * 128:(t + 1) * 128, :]), reads=[mrg], writes=[a])
                S.op("act", lambda e: e.activation(out=g[:], in_=g[:], func=AF.Sigmoid), reads=[g], writes=[g])
                for nb in range(4):
                    ps = pm[ev % 4]
                    ev += 1
                    for kc in range(8):
                        S.op("pe", lambda e: e.matmul(ps[:, :], lhsT=bt[:, kc, :], rhs=wbr[:, kc, nb * 512:(nb + 1) * 512],
                                                      start=(kc == 0), stop=(kc == 7)), reads=[bt, wbr], writes=[ps])
                    S.op("dve", lambda e: e.tensor_tensor(out=g[:, nb * 512:(nb + 1) * 512], in0=ps[:, :],
                                                          in1=g[:, nb * 512:(nb + 1) * 512], op=ALU.mult),
                         reads=[ps, g], writes=[g])
                if b > 0:
                    S.op("pool", lambda e: e.tensor_tensor(out=g[:], in0=g[:], in1=a[:], op=ALU.add), reads=[g, a], writes=[g])
                S.dma("sp", lambda e: e.dma_start(out=mrg[t * 128:(t + 1) * 128, :], in_=g[:]), reads=[g], writes=[mrg])
            S.barrier()
    with ExitStack() as sc:
        S.scope = sc
        wout = S.sbuf("wout", [128, 16, 2048], BF16)
        S.dma("pool", lambda e: e.dma_start(out=wout[:], in_=R["w_out"][l].rearrange("(k p) c -> p k c", p=128)),
              reads=[R["w_out"]], writes=[wout])
        g1b = [S.sbuf("g1b%d" % c, [128, 2048], F32) for c in range(2)]
        for c in range(2):
            S.dma("sp", lambda e: e.dma_start(out=g1b[c][:], in_=modd[c, 2 * D:3 * D].partition_broadcast(128)),
                  reads=[modd], writes=[g1b[c]])
        mt = [S.sbuf("mt%d" % i, [128, 2048], F32) for i in range(2)]
        mb = [S.sbuf("mb%d" % i, [128, 2048], BF16) for i in range(2)]
        mT = [S.sbuf("mT%d" % i, [128, 16, 128], BF16) for i in range(2)]
        xt = [S.sbuf("xo%d" % i, [128, 2048], F32) for i in range(2)]
        ptr = [S.psum("optr%d" % i, [128, 8, 128], BF16) for i in range(2)]
        pm = [S.psum("pmo%d" % i, [128, 512], F32) for i in range(4)]
        ev = 0
        for t in range(NTILE):
            c = 0 if t < (NCTX * LC) // 128 else 1
            m, mbb, mTT, x = mt[t % 2], mb[t % 2], mT[t % 2], xt[t % 2]
            S.dma("sp", lambda e: e.dma_start(out=m[:], in_=mrg[t * 128:(t + 1) * 128, :]), reads=[mrg], writes=[m])
            S.dma("sp", lambda e: e.dma_start(out=x[:], in_=x_src[t * 128:(t + 1) * 128, :]), reads=[x_src], writes=[x])
            S.op("pool", lambda e: e.tensor_copy(out=mbb[:], in_=m[:]), reads=[m], writes=[mbb])
            for half in range(2):
                pt = ptr[half]
                for j in range(8):
                    kc = half * 8 + j
                    S.op("pe", lambda e: e.transpose(pt[:, j, :], mbb[:, kc * 128:(kc + 1) * 128], ident[:]),
                         reads=[mbb, ident], writes=[pt])
                _evac(S, half, mTT[:, half * 8:(half + 1) * 8, :], pt[:, :, :], reads=[pt], writes=[mTT])
            for nb in range(4):
                ps = pm[ev % 4]
                ev += 1
                for kc in range(16):
                    S.op("pe", lambda e: e.matmul(ps[:, :], lhsT=mTT[:, kc, :], rhs=wout[:, kc, nb * 512:(nb + 1) * 512],
                                                  start=(kc == 0), stop=(kc == 15)), reads=[mTT, wout], writes=[ps])
                S.op("dve", lambda e: e.tensor_tensor(out=m[:, nb * 512:(nb + 1) * 512], in0=ps[:, :],
                                                      in1=g1b[c][:, nb * 512:(nb + 1) * 512], op=ALU.mult),
                     reads=[ps, g1b[c]], writes=[m])
            S.op("pool", lambda e: e.tensor_tensor(out=x[:], in0=x[:], in1=m[:], op=ALU.add), reads=[x, m], writes=[x])
            S.dma("sp", lambda e: e.dma_start(out=xbuf[t * 128:(t + 1) * 128, :], in_=x[:]), reads=[x], writes=[xbuf])
        S.barrier()
    S.scope = S.es


def stage_peer(S, l, R, last):
    ident = R["ident"]
    xbuf, modd = R["xbuf"], R["modd"]
    with ExitStack() as sc:
        S.scope = sc
        wq = S.sbuf("pwq", [128, 16, 2048], BF16)
        S.dma("pool", lambda e: e.dma_start(out=wq[:], in_=R["peer_wq"][l].rearrange("(k p) c -> p k c", p=128)),
              reads=[R["peer_wq"]], writes=[wq])
        identf = S.sbuf("identf", [128, 128], F32)
        S.dma("sp", lambda e: e.dma_start(out=identf[:], in_=R["ident_in"][:, :]), reads=[R["ident_in"]], writes=[identf])
        keysT = S.sbuf("keysT", [128, 2, 128], F32)
        S.dma("sp", lambda e: e.dma_start(out=keysT[:], in_=R["peer_keysT"][l].rearrange("s c n -> c s n")),
              reads=[R["peer_keysT"]], writes=[keysT])
        gm2 = S.sbuf("gm2", [128, 2048], F32)
        sh2 = S.sbuf("sh2", [128, 2048], F32)
        x1 = S.sbuf("px1", [128, 2048], F32)
        h2 = S.sbuf("ph2", [128, 2048], F32)
        h2b = S.sbuf("ph2b", [128, 2048], BF16)
        h2T = S.sbuf("ph2T", [128, 16, 128], BF16)
        qTf = S.sbuf("pqTf", [128, 16, 128], F32)
        scr = S.sbuf("pscr", [128, 16, 128], F32)
        scw = S.sbuf("pscw", [128, 128], F32)
        vals = S.sbuf("pvals", [128, 16, 16], F32)
        idx = S.sbuf("pidx", [128, 16, 16], U32)
        idxf = S.sbuf("pidxf", [128, 16, 16], F32)
        cand = S.sbuf("pcand", [128, 256], F32)
        candw = S.sbuf("pcandw", [128, 256], F32)
        cid = S.sbuf("pcid", [128, 256], F32)
        bs = S.sbuf("pbs", [128, 8, 16], F32)
        eq = S.sbuf("peq", [128, 8, 256], F32)
        eidf = S.sbuf("peidf", [128, 8, 16], F32)
        eidi = S.sbuf("peidi", [128, 128], I32)
        negb = S.sbuf("pnegb", [128, 8], F32)
        zs = S.sbuf("pzs", [128, 8], F32)
        gat = S.sbuf("pgat", [128, 8, 16], F32)
        actv = S.sbuf("pact", [128, 128], F32)
        coef = S.sbuf("pcoef", [128, 128], F32)
        yacc = S.sbuf("pyacc", [128, 2048], F32)
        qf = yacc
        jk = S.sbuf("pjk", [128, 2048], F32)
        st = S.sbuf("pst", [128, 1], F32)
        ug = [S.sbuf("pug%d" % i, [128, 2048], F32) for i in range(4)]
        ptr = [S.psum("pptr%d" % i, [128, 8, 128], BF16) for i in range(2)]
        pm = [S.psum("ppm%d" % i, [128, 512], F32) for i in range(4)]
        cur_c = -1
        for t in range(NTILE):
            c = 0 if t < (NCTX * LC) // 128 else 1
            if c != cur_c:
                cur_c = c
                S.dma("sp", lambda e: e.dma_start(out=sh2[:], in_=modd[c, 3 * D:4 * D].partition_broadcast(128)),
                      reads=[modd], writes=[sh2])
                S.dma("sp", lambda e: e.dma_start(out=gm2[:], in_=modd[c, 4 * D:5 * D].partition_broadcast(128)),
                      reads=[modd], writes=[gm2])
                S.dma("sp", lambda e: e.dma_start(out=jk[:], in_=R["norm2_g"][l, :].partition_broadcast(128)),
                      reads=[R["norm2_g"]], writes=[jk])
                S.op("dve", lambda e: e.scalar_tensor_tensor(out=gm2[:], in0=gm2[:], scalar=1.0, in1=jk[:],
                                                             op0=ALU.add, op1=ALU.mult), reads=[gm2, jk], writes=[gm2])
            S.dma("sp", lambda e: e.dma_start(out=x1[:], in_=xbuf[t * 128:(t + 1) * 128, :]), reads=[xbuf], writes=[x1])
            S.op("act", lambda e: e.activation(out=jk[:], in_=x1[:], func=AF.Square, accum_out=st[:]),
                 reads=[x1], writes=[jk, st])
            S.op("act", lambda e: e.activation(out=st[:], in_=st[:], func=AF.Sqrt, scale=1.0 / D, bias=EPS),
                 reads=[st], writes=[st])
            S.op("dve", lambda e: e.reciprocal(out=st[:], in_=st[:]), reads=[st], writes=[st])
            S.op("dve", lambda e: e.scalar_tensor_tensor(out=h2[:], in0=x1[:], scalar=st[:, 0:1], in1=gm2[:],
                                                         op0=ALU.mult, op1=ALU.mult), reads=[x1, st, gm2], writes=[h2])
            S.op("dve", lambda e: e.tensor_tensor(out=h2[:], in0=h2[:], in1=sh2[:], op=ALU.add), reads=[h2, sh2], writes=[h2])
            S.op("act", lambda e: e.activation(out=h2b[:], in_=h2[:], func=AF.Copy), reads=[h2], writes=[h2b])
            for half in range(2):
                pt = ptr[half]
                for j in range(8):
                    kc = half * 8 + j
                    S.op("pe", lambda e: e.transpose(pt[:, j, :], h2b[:, kc * 128:(kc + 1) * 128], ident[:]),
                         reads=[h2b, ident], writes=[pt])
                _evac(S, half, h2T[:, half * 8:(half + 1) * 8, :], pt[:, :, :], reads=[pt], writes=[h2T])
            for nb in range(4):
                ps = pm[nb]
                for kc in range(16):
                    S.op("pe", lambda e: e.matmul(ps[:, :], lhsT=h2T[:, kc, :], rhs=wq[:, kc, nb * 512:(nb + 1) * 512],
                                                  start=(kc == 0), stop=(kc == 15)), reads=[h2T, wq], writes=[ps])
                _evac(S, nb, qf[:, nb * 512:(nb + 1) * 512], ps[:, :], reads=[ps], writes=[qf])
            for b4 in range(4):
                ps = pm[b4]
                for j in range(4):
                    ch = b4 * 4 + j
                    S.op("pe", lambda e: e.transpose(ps[:, j * 128:(j + 1) * 128], qf[:, ch * 128:(ch + 1) * 128], identf[:]),
                         reads=[qf, identf], writes=[ps])
                _evac(S, b4, qTf[:, b4 * 4:(b4 + 1) * 4, :], ps[:, :].rearrange("p (a b) -> p a b", a=4), reads=[ps], writes=[qTf])
            for b4 in range(4):
                ps = pm[b4]
                for j in range(4):
                    ch = b4 * 4 + j
                    S.op("pe", lambda e: e.matmul(ps[:, j * 128:(j + 1) * 128], lhsT=qTf[:, ch, :], rhs=keysT[:, ch % 2, :],
                                                  start=True, stop=True), reads=[qTf, keysT], writes=[ps])
                _evac(S, b4 + 1, scr[:, b4 * 4:(b4 + 1) * 4, :], ps[:, :].rearrange("p (a b) -> p a b", a=4), reads=[ps], writes=[scr])
            for ch in range(16):
                S.op("dve", lambda e: e.max(out=vals[:, ch, 0:8], in_=scr[:, ch, :]), reads=[scr], writes=[vals])
                S.op("dve", lambda e: e.match_replace(out=scw[:, :], in_to_replace=vals[:, ch, 0:8], in_values=scr[:, ch, :],
                                                      imm_value=-1e30), reads=[vals, scr], writes=[scw])
                S.op("dve", lambda e: e.max(out=vals[:, ch, 8:16], in_=scw[:, :]), reads=[scw], writes=[vals])
                S.op("dve", lambda e: e.max_index(out=idx[:, ch, 0:8], in_max=vals[:, ch, 0:8], in_values=scr[:, ch, :]),
                     reads=[vals, scr], writes=[idx])
                S.op("dve", lambda e: e.max_index(out=idx[:, ch, 8:16], in_max=vals[:, ch, 8:16], in_values=scr[:, ch, :]),
                     reads=[vals, scr], writes=[idx])
            S.op("dve", lambda e: e.tensor_copy(out=idxf[:], in_=idx[:]), reads=[idx], writes=[idxf])
            for h in range(8):
                c3 = cand[:, :].rearrange("p (a b) -> p a b", a=16)
                i3 = cid[:, :].rearrange("p (a b) -> p a b", a=16)
                S.op("dve", lambda e: e.tensor_tensor(out=c3, in0=vals[:, 2 * h, :].unsqueeze(2).to_broadcast([128, 16, 16]),
                                                      in1=vals[:, 2 * h + 1, :].unsqueeze(1).to_broadcast([128, 16, 16]),
                                                      op=ALU.add), reads=[vals], writes=[cand])
                S.op("dve", lambda e: e.scalar_tensor_tensor(out=i3, in0=idxf[:, 2 * h, :].unsqueeze(2).to_broadcast([128, 16, 16]),
                                                             scalar=128.0,
                                                             in1=idxf[:, 2 * h + 1, :].unsqueeze(1).to_broadcast([128, 16, 16]),
                                                             op0=ALU.mult, op1=ALU.add), reads=[idxf], writes=[cid])
                S.op("dve", lambda e: e.max(out=bs[:, h, 0:8], in_=cand[:, :]), reads=[cand], writes=[bs])
                S.op("dve", lambda e: e.match_replace(out=candw[:, :], in_to_replace=bs[:, h, 0:8], in_values=cand[:, :],
                                                      imm_value=-1e30), reads=[bs, cand], writes=[candw])
                S.op("dve", lambda e: e.max(out=bs[:, h, 8:16], in_=candw[:, :]), reads=[candw], writes=[bs])
                for hf in range(2):
                    S.op("dve", lambda e: e.tensor_tensor(out=eq[:], in0=cand[:, :].unsqueeze(1).to_broadcast([128, 8, 256]),
                                                          in1=bs[:, h, hf * 8:(hf + 1) * 8].unsqueeze(2).to_broadcast([128, 8, 256]),
                                                          op=ALU.is_equal), reads=[cand, bs], writes=[eq])
                    S.op("dve", lambda e: e.tensor_tensor(out=eq[:], in0=eq[:],
                                                          in1=cid[:, :].unsqueeze(1).to_broadcast([128, 8, 256]),
                                                          op=ALU.mult), reads=[eq, cid], writes=[eq])
                    S.op("dve", lambda e: e.tensor_reduce(out=eidf[:, h, hf * 8:(hf + 1) * 8], in_=eq[:], axis=AX.X, op=ALU.add),
                         reads=[eq], writes=[eidf])
            S.op("dve", lambda e: e.tensor_copy(out=eidi[:, :].rearrange("p (a b) -> p a b", a=8), in_=eidf[:]),
                 reads=[eidf], writes=[eidi])
            S.op("dve", lambda e: e.tensor_scalar(out=negb[:], in0=bs[:, :, 0], scalar1=-1.0, scalar2=None, op0=ALU.mult),
                 reads=[bs], writes=[negb])
            for h in range(8):
                S.op("act", lambda e: e.activation(out=gat[:, h, :], in_=bs[:, h, :], func=AF.Exp, bias=negb[:, h:h + 1],
                                                   accum_out=zs[:, h:h + 1]), reads=[bs, negb], writes=[gat, zs])
            S.op("dve", lambda e: e.reciprocal(out=zs[:], in_=zs[:]), reads=[zs], writes=[zs])
            S.op("dve", lambda e: e.tensor_tensor(out=gat[:], in0=gat[:], in1=zs[:, :].unsqueeze(2).to_broadcast([128, 8, 16]),
                                                  op=ALU.mult), reads=[gat, zs], writes=[gat])
            for s in range(128):
                u = ug[s % 4]
                S.dma("pool", lambda e: e.indirect_dma_start(
                    out=u[:], out_offset=None, in_=R["peer_u"][l][:, :],
                    in_offset=bass.IndirectOffsetOnAxis(ap=eidi[:, s:s + 1], axis=0)),
                    reads=[R["peer_u"][l], eidi], writes=[u])
                S.op("dve", lambda e: e.scalar_tensor_tensor(out=jk[:], in0=u[:], scalar=1.0, in1=h2[:],
                                                             op0=ALU.mult, op1=ALU.mult, accum_out=actv[:, s:s + 1]),
                     reads=[u, h2], writes=[jk, actv])
            S.op("act", lambda e: e.activation(out=actv[:], in_=actv[:], func=AF.Gelu), reads=[actv], writes=[actv])
            S.op("dve", lambda e: e.tensor_tensor(out=coef[:], in0=actv[:], in1=gat[:, :, :].rearrange("p a b -> p (a b)"),
                                                  op=ALU.mult), reads=[actv, gat], writes=[coef])
            for s in range(128):
                u = ug[s % 4]
                S.dma("pool", lambda e: e.indirect_dma_start(
                    out=u[:], out_offset=None, in_=R["peer_v"][l][:, :],
                    in_offset=bass.IndirectOffsetOnAxis(ap=eidi[:, s:s + 1], axis=0)),
                    reads=[R["peer_v"][l], eidi], writes=[u])
                if s == 0:
                    S.op("dve", lambda e: e.tensor_scalar(out=yacc[:], in0=u[:], scalar1=coef[:, 0:1], scalar2=None,
                                                          op0=ALU.mult), reads=[u, coef], writes=[yacc])
                else:
                    S.op("dve", lambda e: e.scalar_tensor_tensor(out=yacc[:], in0=u[:], scalar=coef[:, s:s + 1], in1=yacc[:],
                                                                 op0=ALU.mult, op1=ALU.add), reads=[u, coef, yacc], writes=[yacc])
            S.dma("sp", lambda e: e.dma_start(out=jk[:], in_=modd[c, 5 * D:6 * D].partition_broadcast(128)),
                  reads=[modd], writes=[jk])
            S.op("dve", lambda e: e.tensor_tensor(out=yacc[:], in0=yacc[:], in1=jk[:], op=ALU.mult), reads=[yacc, jk], writes=[yacc])
            S.op("dve", lambda e: e.tensor_tensor(out=x1[:], in0=x1[:], in1=yacc[:], op=ALU.add), reads=[x1, yacc], writes=[x1])
            if not last:
                S.dma("sp", lambda e: e.dma_start(out=xbuf[t * 128:(t + 1) * 128, :], in_=x1[:]), reads=[x1], writes=[xbuf])
            else:
                S.op("act", lambda e: e.activation(out=jk[:], in_=x1[:], func=AF.Square, accum_out=st[:]),
                     reads=[x1], writes=[jk, st])
                S.op("act", lambda e: e.activation(out=st[:], in_=st[:], func=AF.Sqrt, scale=1.0 / D, bias=EPS),
                     reads=[st], writes=[st])
                S.op("dve", lambda e: e.reciprocal(out=st[:], in_=st[:]), reads=[st], writes=[st])
                S.dma("sp", lambda e: e.dma_start(out=jk[:], in_=R["final_g"][0, :].partition_broadcast(128)),
                      reads=[R["final_g"]], writes=[jk])
                S.op("dve", lambda e: e.scalar_tensor_tensor(out=x1[:], in0=x1[:], scalar=st[:, 0:1], in1=jk[:],
                                                             op0=ALU.mult, op1=ALU.mult), reads=[x1, st, jk], writes=[x1])
                S.dma("sp", lambda e: e.dma_start(out=R["y_out"][t * 128:(t + 1) * 128, :], in_=x1[:]), reads=[x1], writes=[R["y_out"]])
        S.barrier()
    S.scope = S.es


def stage_dn(S, l, R):
    P_T, P_F, brT = R["P_T"], R["P_F"], R["brT"]
    fdn, _ = F_ROWS["dnT"]
    az, _ = T_COLS["z"]
    aa, _ = T_COLS["a"]
    with ExitStack() as sc:
        S.scope = sc
        cst = S.sbuf("dncst", [128, 10, 128], F32)
        S.dma("sp", lambda e: e.dma_start(out=cst[:], in_=R["dn_consts"][:, :, :].rearrange("m p q -> p m q")),
              reads=[R["dn_consts"]], writes=[cst])
        onesf = S.sbuf("dnones", [128, 128], F32)
        S.op("dve", lambda e: e.memset(onesf[:], 1.0), writes=[onesf])
        identf = S.sbuf("dnidentf", [128, 128], F32)
        S.dma("sp", lambda e: e.dma_start(out=identf[:], in_=R["dn_consts"][2]), reads=[R["dn_consts"]], writes=[identf])
        identb = R["ident"]
        cw = S.sbuf("dncw", [128, 24, 5], F32)
        S.dma("sp", lambda e: e.dma_start(out=cw[:], in_=R["dn_convT"][l]), reads=[R["dn_convT"]], writes=[cw])
        alog = S.sbuf("dnalog", [128, 16], F32)
        dtb = S.sbuf("dndtb", [128, 16], F32)
        S.dma("sp", lambda e: e.dma_start(out=alog[:], in_=R["dn_a_log"][l, :].partition_broadcast(128)),
              reads=[R["dn_a_log"]], writes=[alog])
        S.dma("sp", lambda e: e.dma_start(out=dtb[:], in_=R["dn_dt_bias"][l, :].partition_broadcast(128)),
              reads=[R["dn_dt_bias"]], writes=[dtb])
        S.op("act", lambda e: e.activation(out=alog[:], in_=alog[:], func=AF.Exp), reads=[alog], writes=[alog])
        gon = S.sbuf("dngon", [128, 128], F32)
        S.dma("sp", lambda e: e.dma_start(out=gon[:], in_=R["dn_out_norm"][l, :].partition_broadcast(128)),
              reads=[R["dn_out_norm"]], writes=[gon])
        NCH = LL // 128
        xin = S.sbuf("dnxin", [128, LL + 4], F32)
        acc = S.sbuf("dnacc", [128, LL], F32)
        qT = S.sbuf("dnqT", [128, LL], F32)
        kT = S.sbuf("dnkT", [128, LL], F32)
        vT = S.sbuf("dnvT", [128, LL], F32)
        ktok = S.sbuf("dnktok", [128, NCH, 128], F32)
        vtok = S.sbuf("dnvtok", [128, NCH, 128], F32)
        oall = S.sbuf("dnoall", [128, NCH, 1024], F32)
        gt = S.sbuf("dng", [128, NCH, 16], F32)
        bt = S.sbuf("dnb", [128, NCH, 16], F32)
        gc = S.sbuf("dngc", [128, NCH, 16], F32)
        egc = S.sbuf("dnegc", [128, NCH, 16], F32)
        bg = S.sbuf("dnbg", [128, NCH, 16], F32)
        egl = S.sbuf("dnegl", [128, NCH, 16], F32)
        edl = S.sbuf("dnedl", [128, NCH, 16], F32)
        ab = S.sbuf("dnab", [128, 32], F32)
        sq = S.sbuf("dnsq", [128, 512], F32)
        rn = S.sbuf("dnrn", [128, 512], F32)
        Sst = S.sbuf("dnS", [128, 128], F32)
        Bsets = []
        for j_ in range(3):
            B = {"j": j_}
            for nm_ in ("Gm", "EA", "ET", "Am", "Ad", "Aoff", "Boff", "qkT", "wT", "kd"):
                B[nm_] = S.sbuf("dn%s%d" % (nm_, j_), [128, 128], F32)
            B["Bk"] = [S.sbuf("dnB%d_%d" % (i, j_), [128, 128], F32) for i in range(6)]
            B["Ck"] = [S.sbuf("dnC%d_%d" % (i, j_), [128, 128], F32) for i in range(2)]
            B["X"] = S.sbuf("dnX%d" % j_, [128, 256], F32)
            B["Zb"] = S.sbuf("dnZb%d" % j_, [128, 256], F32)
            Bsets.append(B)
        vnew = S.sbuf("dnvnew", [128, 128], F32)
        tmp = S.sbuf("dntmp", [128, 128], F32)
        zt = S.sbuf("dnz", [128, 1024], F32)
        ob = S.sbuf("dnob", [128, 1024], BF16)
        obT = S.sbuf("dnobT", [128, 8, 128], BF16)
        ssq = S.sbuf("dnssq", [128, 8], F32)
        pqb = [S.psum("dnpqb%d" % i, [128, 512], F32) for i in range(7)]
        pbig = pqb[0:2]
        pq = [Buf(pqb[i].t[:, 0:128], "dnpq%d" % i, root=pqb[i]) for i in range(7)]
        pxs = [Buf(pqb[i].t[:, 0:256], "dnpx%d" % i, root=pqb[i]) for i in range(7)]
        ptr = S.psum("dnptr", [128, 8, 128], BF16)
        npq = [0]

        def PQ():
            npq[0] += 1
            return pq[npq[0] % 7]

        def PX():
            npq[0] += 1
            return pxs[npq[0] % 7]

        slot = [0, 0, 0]

        def PQs(j, wide=False):
            slot[j] += 1
            bank = pqb[3 * j + slot[j] % 3]
            w_ = 256 if wide else 128
            return Buf(bank.t[:, 0:w_], "dnps", root=bank)

        seqs = [(s * LC, LC, False) for s in range(NCTX)] + [(NCTX * LC, LL, True)]
        seqs = seqs[:DNSEQ]
        for si, (tok0, L, latent) in enumerate(seqs):
            nch = L // 128
            for n in range(nch):
                g0 = tok0 + n * 128
                S.dma("sp", lambda e: e.dma_start(out=ab[:], in_=P_T[g0:g0 + 128, aa:aa + 32]), reads=[P_T], writes=[ab])
                S.op("dve", lambda e: e.tensor_tensor(out=gt[:, n, :], in0=ab[:, 0:16], in1=dtb[:], op=ALU.add),
                     reads=[ab, dtb], writes=[gt])
                S.op("act", lambda e: e.activation(out=gt[:, n, :], in_=gt[:, n, :], func=AF.Exp), reads=[gt], writes=[gt])
                S.op("act", lambda e: e.activation(out=gt[:, n, :], in_=gt[:, n, :], func=AF.Ln, bias=1.0), reads=[gt], writes=[gt])
                S.op("dve", lambda e: e.scalar_tensor_tensor(out=gt[:, n, :], in0=gt[:, n, :], scalar=-1.0, in1=alog[:],
                                                             op0=ALU.mult, op1=ALU.mult), reads=[gt, alog], writes=[gt])
                S.op("act", lambda e: e.activation(out=bt[:, n, :], in_=ab[:, 16:32], func=AF.Sigmoid), reads=[ab], writes=[bt])
                p1 = PQ()
                for d in range(2):
                    S.op("pe", lambda e: e.matmul(p1[:, d * 8:(d + 1) * 8], lhsT=cst[:, d, :], rhs=gt[:, n, d * 8:(d + 1) * 8],
                                                  start=True, stop=True), reads=[cst, gt], writes=[p1])
                S.op("pe", lambda e: e.matmul(p1[:, 16:32], lhsT=onesf[:, :], rhs=gt[:, n, :], start=True, stop=True),
                     reads=[onesf, gt], writes=[p1])
                S.op("dve", lambda e: e.tensor_copy(out=gc[:, n, :], in_=p1[:, 0:16]), reads=[p1], writes=[gc])
                S.op("act", lambda e: e.activation(out=egc[:, n, :], in_=p1[:, 0:16], func=AF.Exp), reads=[p1], writes=[egc])
                S.op("act", lambda e: e.activation(out=egl[:, n, :], in_=p1[:, 16:32], func=AF.Exp), reads=[p1], writes=[egl])
                S.op("dve", lambda e: e.tensor_tensor(out=edl[:, n, :], in0=p1[:, 16:32], in1=gc[:, n, :], op=ALU.subtract),
                     reads=[p1, gc], writes=[edl])
                S.op("act", lambda e: e.activation(out=edl[:, n, :], in_=edl[:, n, :], func=AF.Exp), reads=[edl], writes=[edl])
                S.op("dve", lambda e: e.tensor_tensor(out=bg[:, n, :], in0=bt[:, n, :], in1=egc[:, n, :], op=ALU.mult),
                     reads=[bt, egc], writes=[bg])
            for h in range(8):
                if DNSTOP < 2:
                    continue
                for which, dst in ((0, qT), (1, kT), (2, vT)):
                    ch = which * 8 + h
                    r0 = fdn + ch * 128
                    S.op("pool", lambda e: e.memset(xin[:, 0:2], 0.0), writes=[xin])
                    S.op("pool", lambda e: e.memset(xin[:, L + 2:L + 4], 0.0), writes=[xin])
                    S.dma("sp", lambda e: e.dma_start(out=xin[:, 2:L + 2], in_=P_F[r0:r0 + 128, tok0:tok0 + L]),
                          reads=[P_F], writes=[xin])
                    S.op("dve", lambda e: e.tensor_scalar(out=acc[:, 0:L], in0=xin[:, 0:L], scalar1=cw[:, ch, 0:1], scalar2=None,
                                                          op0=ALU.mult), reads=[xin, cw], writes=[acc])
                    for kk in range(1, 5):
                        S.op("dve", lambda e: e.scalar_tensor_tensor(out=acc[:, 0:L], in0=xin[:, kk:kk + L],
                                                                     scalar=cw[:, ch, kk:kk + 1], in1=acc[:, 0:L],
                                                                     op0=ALU.mult, op1=ALU.add), reads=[xin, cw, acc], writes=[acc])
                    S.op("act", lambda e: e.activation(out=dst[:, 0:L], in_=acc[:, 0:L], func=AF.Silu), reads=[acc], writes=[dst])
                    if which < 2:
                        for g in range((L + 511) // 512):
                            w = min(512, L - g * 512)
                            pb = pbig[g % 2]
                            S.op("act", lambda e: e.activation(out=sq[:, 0:w], in_=dst[:, g * 512:g * 512 + w], func=AF.Square),
                                 reads=[dst], writes=[sq])
                            S.op("pe", lambda e: e.matmul(pb[:, 0:w], lhsT=onesf[:, :], rhs=sq[:, 0:w], start=True, stop=True),
                                 reads=[onesf, sq], writes=[pb])
                            sc_ = 128.0 if which == 0 else 1.0
                            S.op("act", lambda e: e.activation(out=rn[:, 0:w], in_=pb[:, 0:w], func=AF.Sqrt, scale=sc_,
                                                               bias=sc_ * EPS), reads=[pb], writes=[rn])
                            S.op("dve", lambda e: e.reciprocal(out=rn[:, 0:w], in_=rn[:, 0:w]), reads=[rn], writes=[rn])
                            S.op("dve", lambda e: e.tensor_tensor(out=dst[:, g * 512:g * 512 + w], in0=dst[:, g * 512:g * 512 + w],
                                                                  in1=rn[:, 0:w], op=ALU.mult), reads=[dst, rn], writes=[dst])
                if DNSTOP < 3:
                    continue
                for n in range(nch):
                    for src, dd in ((kT, ktok), (vT, vtok)):
                        p1 = PQ()
                        S.op("pe", lambda e: e.transpose(p1[:, 0:128], src[:, n * 128:(n + 1) * 128], identf[:]),
                             reads=[src, identf], writes=[p1])
                        _evac(S, n, dd[:, n, :], p1[:, 0:128], reads=[p1], writes=[dd])
                for d in range(2):
                    if DNSTOP < 4:
                        continue
                    dh = d * 8 + h
                    if latent:
                        S.dma("sp", lambda e: e.dma_start(out=Sst[:], in_=R["dn_state"][d][l, h, :, :]),
                              reads=[R["dn_state"][d]], writes=[Sst])
                    else:
                        S.op("dve", lambda e: e.memset(Sst[:], 0.0), writes=[Sst])
                    order = list(range(nch)) if d == 0 else list(range(nch - 1, -1, -1))

                    def prep(n, B, d=d, dh=dh):
                        c0 = n * 128
                        Gm, EA, ET, Am, Ad, Aoff, Boff, Bk, Ck, qkT, X, Zb = (B["Gm"], B["EA"], B["ET"], B["Am"], B["Ad"], B["Aoff"],
                                                                             B["Boff"], B["Bk"], B["Ck"], B["qkT"], B["X"], B["Zb"])
                        S.op("dve", lambda e: e.tensor_scalar(out=Gm[:], in0=cst[:, 3 + d, :], scalar1=gt[:, n, dh:dh + 1],
                                                              scalar2=None, op0=ALU.mult), reads=[cst, gt], writes=[Gm])
                        yield
                        J = B["j"]
                        pA, pT_ = PQs(J), PQs(J)
                        S.op("pe", lambda e: e.matmul(pA[:, :], lhsT=cst[:, d, :], rhs=Gm[:, :], start=True, stop=False),
                             reads=[cst, Gm], writes=[pA])
                        S.op("pe", lambda e: e.matmul(pA[:, :], lhsT=cst[:, 2, :], rhs=cst[:, 5 + d, :], start=False, stop=True),
                             reads=[cst], writes=[pA])
                        S.op("pe", lambda e: e.matmul(pT_[:, :], lhsT=Gm[:, :], rhs=cst[:, d, :], start=True, stop=False),
                             reads=[cst, Gm], writes=[pT_])
                        S.op("pe", lambda e: e.matmul(pT_[:, :], lhsT=cst[:, 2, :], rhs=cst[:, 7 + d, :], start=False, stop=True),
                             reads=[cst], writes=[pT_])
                        yield
                        S.op("act", lambda e: e.activation(out=EA[:], in_=pA[:, :], func=AF.Exp), reads=[pA], writes=[EA])
                        S.op("act", lambda e: e.activation(out=ET[:], in_=pT_[:, :], func=AF.Exp), reads=[pT_], writes=[ET])
                        yield
                        pkk, pkq = PQs(J), PQs(J)
                        S.op("pe", lambda e: e.matmul(pkk[:, :], lhsT=kT[:, c0:c0 + 128], rhs=kT[:, c0:c0 + 128], start=True,
                                                      stop=True), reads=[kT], writes=[pkk])
                        S.op("pe", lambda e: e.matmul(pkq[:, :], lhsT=kT[:, c0:c0 + 128], rhs=qT[:, c0:c0 + 128], start=True,
                                                      stop=True), reads=[kT, qT], writes=[pkq])
                        yield
                        S.op("dve", lambda e: e.scalar_tensor_tensor(out=Am[:], in0=pkk[:, :], scalar=bt[:, n, dh:dh + 1], in1=EA[:],
                                                                     op0=ALU.mult, op1=ALU.mult), reads=[pkk, bt, EA], writes=[Am])
                        S.op("dve", lambda e: e.tensor_tensor(out=qkT[:], in0=pkq[:, :], in1=ET[:], op=ALU.mult),
                             reads=[pkq, ET], writes=[qkT])
                        S.op("pool", lambda e: e.tensor_scalar(out=X[:, 0:128], in0=vtok[:, n, :], scalar1=bt[:, n, dh:dh + 1],
                                                               scalar2=None, op0=ALU.mult), reads=[vtok, bt], writes=[X])
                        S.op("pool", lambda e: e.tensor_scalar(out=X[:, 128:256], in0=ktok[:, n, :], scalar1=bg[:, n, dh:dh + 1],
                                                               scalar2=None, op0=ALU.mult), reads=[ktok, bg], writes=[X])
                        yield
                        if DNSTOP < 5:
                            return
                        S.op("dve", lambda e: e.tensor_tensor(out=Ad[:], in0=Am[:], in1=cst[:, 9, :], op=ALU.mult),
                             reads=[Am, cst], writes=[Ad])
                        S.op("pool", lambda e: e.tensor_tensor(out=Aoff[:], in0=Am[:], in1=Ad[:], op=ALU.subtract),
                             reads=[Am, Ad], writes=[Aoff])
                        yield
                        pt1, pt2 = PQs(J), PQs(J)
                        S.op("pe", lambda e: e.transpose(pt1[:, :], Ad[:, :], identf[:]), reads=[Ad, identf], writes=[pt1])
                        S.op("pe", lambda e: e.transpose(pt2[:, :], Aoff[:, :], identf[:]), reads=[Aoff, identf], writes=[pt2])
                        yield
                        S.op("act", lambda e: e.activation(out=Bk[0][:], in_=pt1[:, :], func=AF.Copy), reads=[pt1], writes=[Bk[0]])
                        S.op("act", lambda e: e.activation(out=Boff[:], in_=pt2[:, :], func=AF.Copy), reads=[pt2], writes=[Boff])
                        yield
                        Cc = Ad
                        for k_ in range(6):
                            Bc = Bk[k_]
                            pxx = PQs(J, True)
                            S.op("pe", lambda e: e.matmul(pxx[:, :], lhsT=Bc[:, :], rhs=X[:, :], start=True, stop=True),
                                 reads=[Bc, X], writes=[pxx])
                            if k_ < 5:
                                Bn, Cn = Bk[k_ + 1], Ck[(k_ + 1) % 2]
                                pb_, pc_ = PQs(J), PQs(J)
                                S.op("pe", lambda e: e.matmul(pb_[:, :], lhsT=Cc[:, :], rhs=Bc[:, :], start=True, stop=True),
                                     reads=[Cc, Bc], writes=[pb_])
                                S.op("pe", lambda e: e.matmul(pc_[:, :], lhsT=Bc[:, :], rhs=Cc[:, :], start=True, stop=True),
                                     reads=[Cc, Bc], writes=[pc_])
                            yield
                            S.op("dve", lambda e: e.tensor_tensor(out=X[:], in0=X[:], in1=pxx[:, :],
                                                                  op=(ALU.subtract if k_ == 0 else ALU.add)),
                                 reads=[X, pxx], writes=[X])
                            if k_ < 5:
                                S.op("act", lambda e: e.activation(out=Bn[:], in_=pb_[:, :], func=AF.Copy), reads=[pb_], writes=[Bn])
                                S.op("dve", lambda e: e.tensor_copy(out=Cn[:], in_=pc_[:, :]), reads=[pc_], writes=[Cn])
                                Cc = Cn
                            yield
                        pz = PQs(J, True)
                        S.op("pe", lambda e: e.matmul(pz[:, :], lhsT=Boff[:, :], rhs=X[:, :], start=True, stop=True),
                             reads=[Boff, X], writes=[pz])
                        yield
                        S.op("act", lambda e: e.activation(out=Zb[:], in_=pz[:, :], func=AF.Copy), reads=[pz], writes=[Zb])
                        yield
                        for k_ in range(6):
                            pxx = PQs(J, True)
                            S.op("pe", lambda e: e.matmul(pxx[:, :], lhsT=Bk[k_][:, :], rhs=Zb[:, :], start=True, stop=True),
                                 reads=[Bk[k_], Zb], writes=[pxx])
                            yield
                            S.op("dve", lambda e: e.tensor_tensor(out=Zb[:], in0=Zb[:], in1=pxx[:, :],
                                                                  op=(ALU.subtract if k_ == 0 else ALU.add)),
                                 reads=[Zb, pxx], writes=[Zb])
                            yield
                        S.op("dve", lambda e: e.tensor_tensor(out=X[:], in0=X[:], in1=Zb[:], op=ALU.subtract),
                             reads=[X, Zb], writes=[X])
                        yield
                        pw = PQs(J)
                        S.op("pe", lambda e: e.transpose(pw[:, :], X[:, 128:256], identf[:]), reads=[X, identf], writes=[pw])
                        yield
                        S.op("act", lambda e: e.activation(out=B["wT"][:], in_=pw[:, :], func=AF.Copy), reads=[pw], writes=[B["wT"]])
                        S.op("pool", lambda e: e.tensor_scalar(out=B["kd"][:], in0=ktok[:, n, :], scalar1=edl[:, n, dh:dh + 1],
                                                               scalar2=None, op0=ALU.mult), reads=[ktok, edl], writes=[B["kd"]])

                    def scan(n, B, d=d, dh=dh):
                        c0 = n * 128
                        X, qkT, wT, kd = B["X"], B["qkT"], B["wT"], B["kd"]
                        p1, p2, p3, p4 = PQ(), PQ(), PQ(), PQ()
                        S.op("pe", lambda e: e.matmul(p1[:, :], lhsT=wT[:, :], rhs=Sst[:, :], start=True, stop=True),
                             reads=[wT, Sst], writes=[p1])
                        S.op("pe", lambda e: e.matmul(p2[:, :], lhsT=qT[:, c0:c0 + 128], rhs=Sst[:, :], start=True, stop=True),
                             reads=[qT, Sst], writes=[p2])
                        S.op("dve", lambda e: e.tensor_tensor(out=vnew[:], in0=X[:, 0:128], in1=p1[:, :], op=ALU.subtract),
                             reads=[X, p1], writes=[vnew])
                        S.op("pe", lambda e: e.matmul(p3[:, :], lhsT=qkT[:, :], rhs=vnew[:, :], start=True, stop=True),
                             reads=[qkT, vnew], writes=[p3])
                        S.op("pe", lambda e: e.matmul(p4[:, :], lhsT=kd[:, :], rhs=vnew[:, :], start=True, stop=True),
                             reads=[kd, vnew], writes=[p4])
                        S.op("dve", lambda e: e.scalar_tensor_tensor(out=Sst[:], in0=Sst[:], scalar=egl[:, n, dh:dh + 1], in1=p4[:, :],
                                                                     op0=ALU.mult, op1=ALU.add), reads=[Sst, egl, p4], writes=[Sst])
                        S.op("dve", lambda e: e.tensor_scalar(out=tmp[:], in0=p2[:, :], scalar1=egc[:, n, dh:dh + 1], scalar2=None,
                                                              op0=ALU.mult), reads=[p2, egc], writes=[tmp])
                        osl = oall[:, n, h * 128:(h + 1) * 128]
                        if d == 0:
                            S.op("pool" if False else "dve", lambda e: e.tensor_tensor(out=osl, in0=tmp[:], in1=p3[:, :], op=ALU.add),
                                 reads=[tmp, p3], writes=[oall])
                        else:
                            S.op("dve", lambda e: e.tensor_tensor(out=tmp[:], in0=tmp[:], in1=p3[:, :], op=ALU.add),
                                 reads=[tmp, p3], writes=[tmp])
                            S.op("pool", lambda e: e.tensor_tensor(out=osl, in0=osl, in1=tmp[:], op=ALU.add),
                                 reads=[tmp, oall], writes=[oall])

                    NG = 2
                    for g0 in range(0, nch, NG):
                        grp = order[g0:g0 + NG]
                        gens = [prep(n, Bsets[j]) for j, n in enumerate(grp)]
                        alive = list(gens)
                        while alive:
                            for g_ in list(alive):
                                try:
                                    next(g_)
                                except StopIteration:
                                    alive.remove(g_)
                        if DNSTOP < 6:
                            continue
                        for j, n in enumerate(grp):
                            scan(n, Bsets[j])
                    if not latent:
                        ost = R["o_dn"][d]
                        S.dma("sp", lambda e: e.dma_start(out=ost[si, l, h, :, :], in_=Sst[:]), reads=[Sst], writes=[ost])
            for n in range(nch):
                if DNSTOP < 7:
                    continue
                g0 = tok0 + n * 128
                S.dma("sp", lambda e: e.dma_start(out=zt[:], in_=P_T[g0:g0 + 128, az:az + 1024]), reads=[P_T], writes=[zt])
                S.op("act", lambda e: e.activation(out=zt[:], in_=zt[:], func=AF.Silu), reads=[zt], writes=[zt])
                o3 = oall[:, n, :].rearrange("p (h v) -> p h v", h=8)
                for h in range(8):
                    S.op("act", lambda e: e.activation(out=tmp[:], in_=oall[:, n, h * 128:(h + 1) * 128], func=AF.Square,
                                                       accum_out=ssq[:, h:h + 1]), reads=[oall], writes=[tmp, ssq])
                S.op("act", lambda e: e.activation(out=ssq[:], in_=ssq[:], func=AF.Sqrt, scale=1.0 / 128, bias=EPS),
                     reads=[ssq], writes=[ssq])
                S.op("dve", lambda e: e.reciprocal(out=ssq[:], in_=ssq[:]), reads=[ssq], writes=[ssq])
                S.op("dve", lambda e: e.tensor_tensor(out=o3, in0=o3, in1=ssq[:, :].unsqueeze(2).to_broadcast([128, 8, 128]),
                                                      op=ALU.mult), reads=[oall, ssq], writes=[oall])
                S.op("dve", lambda e: e.tensor_tensor(out=o3, in0=o3, in1=gon[:, :].unsqueeze(1).to_broadcast([128, 8, 128]),
                                                      op=ALU.mult), reads=[oall, gon], writes=[oall])
                S.op("dve", lambda e: e.tensor_tensor(out=ob[:], in0=oall[:, n, :], in1=zt[:], op=ALU.mult),
                     reads=[oall, zt], writes=[ob])
                for h in range(8):
                    S.op("pe", lambda e: e.transpose(ptr[:, h, :], ob[:, h * 128:(h + 1) * 128], identb[:]),
                         reads=[ob, identb], writes=[ptr])
                _evac(S, n, obT[:], ptr[:, :, :], reads=[ptr], writes=[obT])
                S.dma("sp", lambda e: e.dma_start(out=brT[1][:, g0:g0 + 128].rearrange("(h p) t -> p h t", p=128), in_=obT[:]),
                      reads=[obT], writes=[brT[1]])
        S.barrier()
    S.scope = S.es


def _hy_geom(L):
    nf = L + 1
    KT = (nf + 127) // 128
    NB = (nf + 511) // 512
    return nf, KT, NB


def stage_hyena(S, l, R):
    P_F, brT = R["P_F"], R["brT"]
    ident = R["ident"]
    fhy, _ = F_ROWS["hyT"]
    hyZ, hyY1, hyU, hyPQ, hyYS = R["hyZ"], R["hyY1"], R["hyU"], R["hyPQ"], R["hyYS"]
    PI = float(np.pi)
    seqs = [(s * LC, LC, False) for s in range(NCTX)] + [(NCTX * LC, LL, True)]
    for (Lx, tabi, seq_list) in ((LC, 0, seqs[:NCTX]), (LL, 1, seqs[NCTX:])):
        L = Lx
        nf, KT, NB = _hy_geom(L)
        KU = L // 128
        tab = R["hy_tab"][tabi]
        with ExitStack() as sc:
            S.scope = sc
            w1 = S.sbuf("hyw1", [33, 64], F32)
            w2 = S.sbuf("hyw2", [64, 64], F32)
            w3 = S.sbuf("hyw3", [64, 4096], F32)
            b12 = S.sbuf("hyb12", [64, 2], F32)
            S.dma("sp", lambda e: e.dma_start(out=w1[:], in_=R["hy_w1"][l]), reads=[R["hy_w1"]], writes=[w1])
            S.dma("sp", lambda e: e.dma_start(out=w2[:], in_=R["hy_w2"][l]), reads=[R["hy_w2"]], writes=[w2])
            S.dma("sp", lambda e: e.dma_start(out=w3[:], in_=R["hy_w3"][l]), reads=[R["hy_w3"]], writes=[w3])
            S.dma("sp", lambda e: e.dma_start(out=b12[:], in_=R["hy_b12"][l]), reads=[R["hy_b12"]], writes=[b12])
            zT = S.sbuf("hyzT", [33, L], F32)
            S.dma("sp", lambda e: e.dma_start(out=zT[:], in_=R["hy_zemb"][tabi][:, :]), reads=[R["hy_zemb"][tabi]], writes=[zT])
            wfs = S.sbuf("hywf", [128, KT], F32)
            S.dma("sp", lambda e: e.dma_start(out=wfs[:], in_=R["hy_wf"][tabi][:, :]), reads=[R["hy_wf"][tabi]], writes=[wfs])
            h1 = S.sbuf("hyh1", [64, L], F32)
            h2 = S.sbuf("hyh2", [64, L], F32)
            xa = S.sbuf("hyxa", [64, 512], F32)
            xb_ = S.sbuf("hyxb", [64, 512], F32)
            xc = S.sbuf("hyxc", [64, 512], F32)
            win = S.sbuf("hywin", [128, 1024], F32)
            hs = S.sbuf("hyhs", [128, KU, 1024], BF16)
            hd = S.sbuf("hyhd", [128, KU, 1024], BF16)
            tf = [S.sbuf("hytf%d" % i, [128, 512], F32) for i in range(2)]
            slab = [S.sbuf("hyslab%d" % i, [128, KT, 512], BF16) for i in range(2)]
            pst = S.sbuf("hypst", [128, 512], F32)
            pm = [S.psum("hypm%d" % i, [128, 512], F32) for i in range(4)]
            npm = [0]

            def PM():
                npm[0] += 1
                return pm[npm[0] % 4]

            def sin_layer(dst, wmat, kdim, src, bcol):
                for g in range((L + 511) // 512):
                    w = min(512, L - g * 512)
                    ps = PM()
                    S.op("pe", lambda e: e.matmul(ps[0:64, 0:w], lhsT=wmat[0:kdim, :], rhs=src[0:kdim, g * 512:g * 512 + w],
                                                  start=True, stop=True), reads=[wmat, src], writes=[ps])
                    S.op("dve", lambda e: e.tensor_scalar(out=xa[:, 0:w], in0=ps[0:64, 0:w], scalar1=b12[:, bcol:bcol + 1],
                                                          scalar2=None, op0=ALU.add), reads=[ps, b12], writes=[xa])
                    S.op("dve", lambda e: e.tensor_scalar(out=xb_[:, 0:w], in0=xa[:, 0:w], scalar1=PI, scalar2=-2 * PI,
                                                          op0=ALU.is_gt, op1=ALU.mult), reads=[xa], writes=[xb_])
                    S.op("dve", lambda e: e.tensor_scalar(out=xc[:, 0:w], in0=xa[:, 0:w], scalar1=-PI, scalar2=2 * PI,
                                                          op0=ALU.is_lt, op1=ALU.mult), reads=[xa], writes=[xc])
                    S.op("dve", lambda e: e.tensor_tensor(out=xa[:, 0:w], in0=xa[:, 0:w], in1=xb_[:, 0:w], op=ALU.add),
                         reads=[xa, xb_], writes=[xa])
                    S.op("dve", lambda e: e.tensor_tensor(out=xa[:, 0:w], in0=xa[:, 0:w], in1=xc[:, 0:w], op=ALU.add),
                         reads=[xa, xc], writes=[xa])
                    S.op("act", lambda e: e.activation(out=dst[:, g * 512:g * 512 + w], in_=xa[:, 0:w], func=AF.Sin),
                         reads=[xa], writes=[dst])

            sin_layer(h1, w1, 33, zT, 0)
            sin_layer(h2, w2, 64, h1, 1)
            for o in range(2):
                for t in range(KU):
                    S.dma("sp", lambda e: e.dma_start(out=win[:], in_=R["hy_win"][tabi][t * 128:(t + 1) * 128, :]),
                          reads=[R["hy_win"][tabi]], writes=[win])
                    for cb in range(2):
                        pf, pb = PM(), PM()
                        cf = (o * 2 + 0) * 1024 + cb * 512
                        cbk = (o * 2 + 1) * 1024 + cb * 512
                        S.op("pe", lambda e: e.matmul(pf[:, :], lhsT=h2[:, t * 128:(t + 1) * 128], rhs=w3[:, cf:cf + 512],
                                                      start=True, stop=True), reads=[h2, w3], writes=[pf])
                        S.op("pe", lambda e: e.matmul(pb[:, :], lhsT=h2[:, t * 128:(t + 1) * 128], rhs=w3[:, cbk:cbk + 512],
                                                      start=True, stop=True), reads=[h2, w3], writes=[pb])
                        S.op("dve", lambda e: e.tensor_tensor(out=tf[0][:], in0=pf[:, :], in1=win[:, cb * 512:(cb + 1) * 512],
                                                              op=ALU.mult), reads=[pf, win], writes=[tf[0]])
                        S.op("dve", lambda e: e.tensor_tensor(out=tf[1][:], in0=pb[:, :], in1=win[:, cb * 512:(cb + 1) * 512],
                                                              op=ALU.mult), reads=[pb, win], writes=[tf[1]])
                        if t == 0:
                            S.op("dve", lambda e: e.memset(tf[1][0:1, :], 0.0), writes=[tf[1]])
                        S.op("dve", lambda e: e.tensor_tensor(out=hs[:, t, cb * 512:(cb + 1) * 512], in0=tf[0][:], in1=tf[1][:],
                                                              op=ALU.add), reads=[tf[0], tf[1]], writes=[hs])
                        S.op("pool", lambda e: e.tensor_tensor(out=hd[:, t, cb * 512:(cb + 1) * 512], in0=tf[0][:], in1=tf[1][:],
                                                               op=ALU.subtract), reads=[tf[0], tf[1]], writes=[hd])
                for fb in range(NB):
                    for cs in range(2):
                        S.dma("sp", lambda e: e.dma_start(out=slab[cs][:], in_=tab[cs, fb]), reads=[tab], writes=[slab[cs]])
                    for j in range(4):
                        m = fb * 4 + j
                        if m >= KT:
                            break
                        fm = min(128, nf - m * 128)
                        for cs, src in ((0, hs), (1, hd)):
                            for cb in range(2):
                                ps = PM()
                                for kt in range(KU):
                                    S.op("pe", lambda e: e.matmul(ps[0:fm, :], lhsT=slab[cs][:, kt, j * 128:j * 128 + fm],
                                                                  rhs=src[:, kt, cb * 512:(cb + 1) * 512], start=(kt == 0),
                                                                  stop=(kt == KU - 1)), reads=[slab[cs], src], writes=[ps])
                                S.op("dve", lambda e: e.tensor_scalar(out=pst[0:fm, :], in0=ps[0:fm, :], scalar1=wfs[0:fm, m:m + 1],
                                                                      scalar2=None, op0=ALU.mult), reads=[ps, wfs], writes=[pst])
                                S.dma("sp", lambda e: e.dma_start(
                                    out=hyPQ[tabi][o, cs, m * 128:m * 128 + fm, cb * 512:(cb + 1) * 512], in_=pst[0:fm, :]),
                                    reads=[pst], writes=[hyPQ[tabi]])
            S.barrier()
        for (tok0, L_, latent) in seq_list:
            si = tok0 // LC if not latent else NCTX
            with ExitStack() as sc:
                S.scope = sc
                cw = S.sbuf("hycw", [128, 24, 3], F32)
                S.dma("sp", lambda e: e.dma_start(out=cw[:], in_=R["hy_convT"][l]), reads=[R["hy_convT"]], writes=[cw])
                xin = [S.sbuf("hyxin%d" % i, [128, L + 2], F32) for i in range(2)]
                acc = [S.sbuf("hyacc%d" % i, [128, L], F32) for i in range(2)]
                accb = S.sbuf("hyaccb", [128, L], BF16)
                ut = S.sbuf("hyut", [128, KU, 128], BF16)
                ptr = [S.psum("hyptr%d" % i, [128, 8, 128], BF16) for i in range(2)]
                for ch in range(24):
                    xi, ac = xin[ch % 2], acc[ch % 2]
                    r0 = fhy + ch * 128
                    S.op("pool", lambda e: e.memset(xi[:, 0:1], 0.0), writes=[xi])
                    S.op("pool", lambda e: e.memset(xi[:, L + 1:L + 2], 0.0), writes=[xi])
                    S.dma("sp", lambda e: e.dma_start(out=xi[:, 1:L + 1], in_=P_F[r0:r0 + 128, tok0:tok0 + L]), reads=[P_F], writes=[xi])
                    S.op("dve", lambda e: e.tensor_scalar(out=ac[:], in0=xi[:, 0:L], scalar1=cw[:, ch, 0:1], scalar2=None, op0=ALU.mult),
                         reads=[xi, cw], writes=[ac])
                    for kk in (1, 2):
                        S.op("dve", lambda e: e.scalar_tensor_tensor(out=ac[:], in0=xi[:, kk:kk + L], scalar=cw[:, ch, kk:kk + 1], in1=ac[:],
                                                                     op0=ALU.mult, op1=ALU.add), reads=[xi, cw, ac], writes=[ac])
                    S.dma("sp", lambda e: e.dma_start(out=hyZ[ch * 128:(ch + 1) * 128, tok0:tok0 + L], in_=ac[:]), reads=[ac], writes=[hyZ])
                    if ch < 8:
                        S.op("act", lambda e: e.activation(out=accb[:], in_=ac[:], func=AF.Copy), reads=[ac], writes=[accb])
                        for t0 in range(0, KU, 8):
                            pt = ptr[(t0 // 8) % 2]
                            nn = min(8, KU - t0)
                            for j in range(nn):
                                S.op("pe", lambda e: e.transpose(pt[:, j, :], accb[:, (t0 + j) * 128:(t0 + j + 1) * 128], ident[:]),
                                     reads=[accb, ident], writes=[pt])
                            _evac(S, t0 // 8, ut[:, t0:t0 + nn, :], pt[:, 0:nn, :], reads=[pt], writes=[ut])
                        S.dma("sp", lambda e: e.dma_start(
                            out=hyU[tok0:tok0 + L, ch * 128:(ch + 1) * 128].rearrange("(k p) c -> p k c", p=128), in_=ut[:]),
                            reads=[ut], writes=[hyU])
                S.barrier()
            for o in range(2):
                with ExitStack() as sc:
                    S.scope = sc
                    u = S.sbuf("hyu", [128, KU, 1024], BF16)
                    S.dma("sp", lambda e: e.dma_start(out=u[:], in_=hyU[tok0:tok0 + L, :].rearrange("(k p) c -> p k c", p=128)),
                          reads=[hyU], writes=[u])
                    slab = [S.sbuf("hyslabf%d" % i, [128, KT, 512], BF16) for i in range(2)]
                    PQt = [S.sbuf("hyPQt%d" % i, [128, 1024], F32) for i in range(2)]
                    ta = S.sbuf("hyta", [128, 512], F32)
                    tb_ = S.sbuf("hytb", [128, 512], F32)
                    yc = S.sbuf("hyyc", [128, 512], BF16)
                    ys = S.sbuf("hyys", [128, 512], BF16)
                    pm = [S.psum("hypmf%d" % i, [128, 512], F32) for i in range(4)]
                    for fb in range(NB):
                        for cs in range(2):
                            S.dma("sp", lambda e: e.dma_start(out=slab[cs][:], in_=tab[cs, fb]), reads=[tab], writes=[slab[cs]])
                        for j in range(4):
                            m = fb * 4 + j
                            if m >= KT:
                                break
                            fm = min(128, nf - m * 128)
                            for cs in range(2):
                                S.dma("sp", lambda e: e.dma_start(out=PQt[cs][0:fm, :], in_=hyPQ[tabi][o, cs, m * 128:m * 128 + fm, :]),
                                      reads=[hyPQ[tabi]], writes=[PQt[cs]])
                            for cb in range(2):
                                pa, pb = pm[(2 * cb) % 4], pm[(2 * cb + 1) % 4]
                                for kt in range(KU):
                                    S.op("pe", lambda e: e.matmul(pa[0:fm, :], lhsT=slab[0][:, kt, j * 128:j * 128 + fm],
                                                                  rhs=u[:, kt, cb * 512:(cb + 1) * 512], start=(kt == 0), stop=(kt == KU - 1)),
                                         reads=[slab[0], u], writes=[pa])
                                for kt in range(KU):
                                    S.op("pe", lambda e: e.matmul(pb[0:fm, :], lhsT=slab[1][:, kt, j * 128:j * 128 + fm],
                                                                  rhs=u[:, kt, cb * 512:(cb + 1) * 512], start=(kt == 0), stop=(kt == KU - 1)),
                                         reads=[slab[1], u], writes=[pb])
                                Pc = PQt[0][0:fm, cb * 512:(cb + 1) * 512]
                                Qc = PQt[1][0:fm, cb * 512:(cb + 1) * 512]
                                S.op("dve", lambda e: e.tensor_tensor(out=ta[0:fm, :], in0=pa[0:fm, :], in1=Pc, op=ALU.mult),
                                     reads=[pa, PQt[0]], writes=[ta])
                                S.op("dve", lambda e: e.tensor_tensor(out=tb_[0:fm, :], in0=pb[0:fm, :], in1=Qc, op=ALU.mult),
                                     reads=[pb, PQt[1]], writes=[tb_])
                                S.op("pool", lambda e: e.tensor_tensor(out=yc[0:fm, :], in0=ta[0:fm, :], in1=tb_[0:fm, :], op=ALU.subtract),
                                     reads=[ta, tb_], writes=[yc])
                                S.op("dve", lambda e: e.tensor_tensor(out=ta[0:fm, :], in0=pa[0:fm, :], in1=Qc, op=ALU.mult),
                                     reads=[pa, PQt[1]], writes=[ta])
                                S.op("dve", lambda e: e.tensor_tensor(out=tb_[0:fm, :], in0=pb[0:fm, :], in1=Pc, op=ALU.mult),
                                     reads=[pb, PQt[0]], writes=[tb_])
                                S.op("pool", lambda e: e.tensor_tensor(out=ys[0:fm, :], in0=ta[0:fm, :], in1=tb_[0:fm, :], op=ALU.add),
                                     reads=[ta, tb_], writes=[ys])
                                S.dma("sp", lambda e: e.dma_start(out=hyYS[0, m * 128:m * 128 + fm, cb * 512:(cb + 1) * 512], in_=yc[0:fm, :]),
                                      reads=[yc], writes=[hyYS])
                                S.dma("sp", lambda e: e.dma_start(out=hyYS[1, m * 128:m * 128 + fm, cb * 512:(cb + 1) * 512], in_=ys[0:fm, :]),
                                      reads=[ys], writes=[hyYS])
                    S.barrier()
                with ExitStack() as sc:
                    S.scope = sc
                    ycs = [S.sbuf("hyYc%d" % i, [128, KT, 1024], BF16) for i in range(2)]
                    for cs in range(2):
                        S.op("dve", lambda e: e.memset(ycs[cs][:, KT - 1, :], 0.0), writes=[ycs[cs]])
                        S.dma("sp", lambda e: e.dma_start(
                            out=ycs[cs][:, 0:KT - 1, :], in_=hyYS[cs, 0:(KT - 1) * 128, :].rearrange("(k p) c -> p k c", p=128)),
                            reads=[hyYS], writes=[ycs[cs]])
                        S.dma("sp", lambda e: e.dma_start(out=ycs[cs][0:1, KT - 1, :], in_=hyYS[cs, (KT - 1) * 128:(KT - 1) * 128 + 1, :]),
                              reads=[hyYS], writes=[ycs[cs]])
                    slab = [S.sbuf("hyslabi%d" % i, [128, KT, 512], BF16) for i in range(2)]
                    bia = S.sbuf("hybia", [128, 8], F32)
                    S.dma("sp", lambda e: e.dma_start(out=bia[:], in_=R["hy_biasT"][l, o]), reads=[R["hy_biasT"]], writes=[bia])
                    uT = [S.sbuf("hyuT%d" % i, [128, 512], F32) for i in range(2)]
                    gT = [S.sbuf("hygT%d" % i, [128, 512], F32) for i in range(2)]
                    yo = [S.sbuf("hyyo%d" % i, [128, 512], F32) for i in range(2)]
                    yb = [S.sbuf("hyyb%d" % i, [128, 512], BF16) for i in range(2)]
                    ut = [S.sbuf("hyuti%d" % i, [128, 4, 128], BF16) for i in range(2)]
                    pm = [S.psum("hypmi%d" % i, [128, 512], F32) for i in range(4)]
                    ptr = [S.psum("hyptri%d" % i, [128, 8, 128], BF16) for i in range(2)]
                    it = 0
                    for tb in range((L + 511) // 512):
                        tw = min(512, L - tb * 512)
                        for cs in range(2):
                            S.dma("sp", lambda e: e.dma_start(out=slab[cs][:], in_=tab[cs, tb]), reads=[tab], writes=[slab[cs]])
                        for cc in range(8):
                            ps = pm[it % 4]
                            u_, g_, y_, yb_, ut_ = uT[it % 2], gT[it % 2], yo[it % 2], yb[it % 2], ut[it % 2]
                            it += 1
                            usrc = hyZ if o == 0 else hyY1
                            S.dma("sp", lambda e: e.dma_start(out=u_[:, 0:tw], in_=usrc[cc * 128:(cc + 1) * 128, tok0 + tb * 512:tok0 + tb * 512 + tw]),
                                  reads=[usrc], writes=[u_])
                            gr = (1 + o) * 1024 + cc * 128
                            S.dma("sp", lambda e: e.dma_start(out=g_[:, 0:tw], in_=hyZ[gr:gr + 128, tok0 + tb * 512:tok0 + tb * 512 + tw]),
                                  reads=[hyZ], writes=[g_])
                            n_mm = 2 * KT
                            i_mm = 0
                            for cs in range(2):
                                for kt in range(KT):
                                    kp = 128 if kt < KT - 1 else 1
                                    S.op("pe", lambda e: e.matmul(ps[:, 0:tw], lhsT=ycs[cs][0:kp, kt, cc * 128:(cc + 1) * 128],
                                                                  rhs=slab[cs][0:kp, kt, 0:tw], start=(i_mm == 0), stop=(i_mm == n_mm - 1)),
                                         reads=[ycs[cs], slab[cs]], writes=[ps])
                                    i_mm += 1
                            S.op("dve", lambda e: e.scalar_tensor_tensor(out=y_[:, 0:tw], in0=u_[:, 0:tw], scalar=bia[:, cc:cc + 1],
                                                                         in1=ps[:, 0:tw], op0=ALU.mult, op1=ALU.add),
                                 reads=[u_, bia, ps], writes=[y_])
                            if o == 0:
                                S.op("pool", lambda e: e.tensor_tensor(out=y_[:, 0:tw], in0=y_[:, 0:tw], in1=g_[:, 0:tw], op=ALU.mult),
                                     reads=[y_, g_], writes=[y_])
                                S.dma("sp", lambda e: e.dma_start(out=hyY1[cc * 128:(cc + 1) * 128, tok0 + tb * 512:tok0 + tb * 512 + tw],
                                                                  in_=y_[:, 0:tw]), reads=[y_], writes=[hyY1])
                                S.op("act", lambda e: e.activation(out=yb_[:, 0:tw], in_=y_[:, 0:tw], func=AF.Copy), reads=[y_], writes=[yb_])
                                pt = ptr[it % 2]
                                nt = tw // 128
                                for j in range(nt):
                                    S.op("pe", lambda e: e.transpose(pt[:, j, :], yb_[:, j * 128:(j + 1) * 128], ident[:]),
                                         reads=[yb_, ident], writes=[pt])
                                _evac(S, it, ut_[:, 0:nt, :], pt[:, 0:nt, :], reads=[pt], writes=[ut_])
                                S.dma("sp", lambda e: e.dma_start(
                                    out=hyU[tok0 + tb * 512:tok0 + tb * 512 + tw, cc * 128:(cc + 1) * 128].rearrange("(k p) c -> p k c", p=128),
                                    in_=ut_[:, 0:nt, :]), reads=[ut_], writes=[hyU])
                            else:
                                S.op("pool", lambda e: e.tensor_tensor(out=yb_[:, 0:tw], in0=y_[:, 0:tw], in1=g_[:, 0:tw], op=ALU.mult),
                                     reads=[y_, g_], writes=[yb_])
                                S.dma("sp", lambda e: e.dma_start(out=brT[2][cc * 128:(cc + 1) * 128, tok0 + tb * 512:tok0 + tb * 512 + tw],
                                                                  in_=yb_[:, 0:tw]), reads=[yb_], writes=[brT[2]])
                    S.barrier()
    S.scope = S.es


def build_program(dbg=False):
    nc = bass.Bass("TRN2", target_bir_lowering=False)
    k = K()
    k.nc = nc

    def ein(name, shape, dtype=F32):
        return Buf(nc.dram_tensor(name, list(shape), dtype, kind="ExternalInput").ap(), name)

    def eout(name, shape, dtype=F32):
        return Buf(nc.dram_tensor(name, list(shape), dtype, kind="ExternalOutput").ap(), name)

    x_in = ein("x_in", [TT, D])
    c2T = ein("c2T", [128, 16, 2])
    ident_in = ein("ident", [128, 128])
    norm1_g = ein("norm1_g", [DEPTH, D])
    w_ada = ein("w_ada", [DEPTH, D, 6 * D])
    b_ada = ein("b_ada", [DEPTH, 6 * D])
    w_in_T = ein("w_in_T", [DEPTH, D, NT_COLS])
    w_in_F = ein("w_in_F", [DEPTH, D, NF_ROWS])
    mla_kv_norm = ein("mla_kv_norm", [DEPTH, 256])
    R = {}
    R["ident_in"] = ident_in
    R["mla_kv_norm"] = mla_kv_norm
    R["mla_q_norm"] = ein("mla_q_norm", [DEPTH, 512])
    R["w_qb"] = ein("w_qb_p", [DEPTH, 512, 2048])
    R["w_kvb"] = ein("w_kvb_p", [DEPTH, 256, 2048])
    R["cache_ckv"] = ein("cache_ckv", [DEPTH, PAST, 256])
    R["cache_kr"] = ein("cache_kr", [DEPTH, PAST, 64])
    R["cache_nak"] = ein("cache_nak", [DEPTH, PAST, 1024])
    R["cache_nav"] = ein("cache_nav", [DEPTH, PAST, 1024])
    R["ropeT"] = ein("ropeT", [2, 64, LL])
    R["na_ctab"] = ein("na_ctab", [DEPTH, 8, 64, 31, 64])
    R["na_mask"] = ein("na_mask", [NA_NMASK, 128, 512])
    R["w_branch"] = ein("w_branch", [DEPTH, 4, 1024, 2048])
    R["w_out"] = ein("w_out", [DEPTH, D, D])
    R["norm2_g"] = ein("norm2_g", [DEPTH, D])
    R["peer_wq"] = ein("peer_wq", [DEPTH, D, D])
    R["peer_keysT"] = ein("peer_keysT", [DEPTH, 2, 128, 128])
    R["peer_u"] = [ein("peer_u%d" % i, [16384, D]) for i in range(DEPTH)]
    R["peer_v"] = [ein("peer_v%d" % i, [16384, D]) for i in range(DEPTH)]
    R["final_g"] = ein("final_g", [1, D])
    _hy_inputs(R, ein)
    R["dn_consts"] = ein("dn_consts", [10, 128, 128])
    R["dn_convT"] = ein("dn_convT", [DEPTH, 128, 24, 5])
    R["dn_a_log"] = ein("dn_a_log", [DEPTH, 16])
    R["dn_dt_bias"] = ein("dn_dt_bias", [DEPTH, 16])
    R["dn_out_norm"] = ein("dn_out_norm", [DEPTH, 128])
    R["dn_state"] = [ein("dn_state%d" % i, [DEPTH, 8, 128, 128]) for i in range(2)]
    if DBG_BR:
        R["dbg_br"] = ein("dbg_br", [DEPTH, 2, 1024, TT], BF16)

    o_ckv = eout("o_ckv", [NCTX, DEPTH, LC, 256])
    o_kr = eout("o_kr", [NCTX, DEPTH, LC, 64])
    o_nak = eout("o_nak", [NCTX, DEPTH, LC, 1024])
    o_nav = eout("o_nav", [NCTX, DEPTH, LC, 1024])
    y_out = eout("y_out", [TT, D])
    o_dn = [eout("o_dn%d" % i, [NCTX, DEPTH, 8, 128, 128]) for i in range(2)]
    R["o_dn"] = o_dn
    outs = [o_ckv, o_kr, o_nak, o_nav, y_out] + o_dn
    R["o_ckv"] = o_ckv
    R["y_out"] = y_out

    with ExitStack() as es:
        S = Sched(nc, es)
        k.S = S
        modd = S.dram("modd", [2, 6 * D])
        P_T = S.dram("P_T", [TT, NT_COLS])
        P_F = S.dram("P_F", [NF_ROWS, TT])
        brT = [S.dram("brT%d" % b_, [1024, TT], BF16) for b_ in range(4)]
        R.update(P_T=P_T, P_F=P_F, brT=brT, modd=modd)
        R["mrg"] = S.dram("mrg", [TT, D])
        R["xbuf"] = S.dram("xbuf", [TT, D])
        _hy_scratch(S, R)

        ident = S.sbuf("identb", [128, 128], BF16)
        S.dma("pool", lambda e: e.dma_start(out=ident[:], in_=ident_in[:, :]), reads=[ident_in], writes=[ident])
        R["ident"] = ident

        for l in range(1 if DBG_BR else DEPTH):
            with ExitStack() as sc:
                S.scope = sc
                cT = S.sbuf("cT", [128, 16, 2], F32)
                sT = S.sbuf("sT", [128, 16, 2], BF16)
                S.dma("sp", lambda e: e.dma_start(out=cT[:], in_=c2T[:, :, :]), reads=[c2T], writes=[cT])
                S.op("act", lambda e: e.activation(out=sT[:], in_=cT[:], func=AF.Silu), reads=[cT], writes=[sT])
                wts = [S.sbuf("adaw%d" % i, [128, 16, 512], BF16) for i in range(2)]
                bts = [S.sbuf("adab%d" % i, [2, 512], F32) for i in range(2)]
                mts = [S.sbuf("adam%d" % i, [2, 512], F32) for i in range(2)]
                pss = [S.psum("adap%d" % i, [2, 512], F32) for i in range(2)]
                for cb in range(24):
                    wt, bt, mt, ps = wts[cb % 2], bts[cb % 2], mts[cb % 2], pss[cb % 2]
                    c0 = cb * 512
                    S.dma("pool", lambda e: e.dma_start(
                        out=wt[:], in_=w_ada[l, :, c0:c0 + 512].rearrange("(k p) c -> p k c", p=128)),
                        reads=[w_ada], writes=[wt])
                    S.dma("sp", lambda e: e.dma_start(out=bt[:], in_=b_ada[l, c0:c0 + 512].partition_broadcast(2)),
                          reads=[b_ada], writes=[bt])
                    for kc in range(16):
                        S.op("pe", lambda e: e.matmul(ps[:, :], lhsT=sT[:, kc, :], rhs=wt[:, kc, :],
                                                      start=(kc == 0), stop=(kc == 15)),
                             reads=[sT, wt], writes=[ps])
                    S.op("dve", lambda e: e.tensor_tensor(out=mt[:], in0=ps[:], in1=bt[:], op=ALU.add),
                         reads=[ps, bt], writes=[mt])
                    S.dma("sp", lambda e: e.dma_start(out=modd[:, c0:c0 + 512], in_=mt[:]), reads=[mt], writes=[modd])
            S.barrier()

            x_cur = x_in if l == 0 else R["xbuf"]
            with ExitStack() as sc:
                S.scope = sc
                hT = S.sbuf("hT", [128, 16, TT], BF16)
                with ExitStack() as sc2:
                    S.scope = sc2
                    gm = [S.sbuf("gm%d" % c, [128, D], F32) for c in range(2)]
                    sh = [S.sbuf("sh%d" % c, [128, D], F32) for c in range(2)]
                    gt = S.sbuf("gt", [128, D], F32)
                    S.dma("sp", lambda e: e.dma_start(out=gt[:], in_=norm1_g[l, :].partition_broadcast(128)),
                          reads=[norm1_g], writes=[gt])
                    for c in range(2):
                        S.dma("sp", lambda e: e.dma_start(out=sh[c][:], in_=modd[c, 0:D].partition_broadcast(128)),
                              reads=[modd], writes=[sh[c]])
                        S.dma("sp", lambda e: e.dma_start(out=gm[c][:], in_=modd[c, D:2 * D].partition_broadcast(128)),
                              reads=[modd], writes=[gm[c]])
                        S.op("dve", lambda e: e.scalar_tensor_tensor(out=gm[c][:], in0=gm[c][:], scalar=1.0, in1=gt[:],
                                                                     op0=ALU.add, op1=ALU.mult),
                             reads=[gm[c], gt], writes=[gm[c]])
                    xts = [S.sbuf("xt%d" % i, [128, D], F32) for i in range(2)]
                    junk = S.sbuf("junk", [128, D], F32)
                    hb = [S.sbuf("hb%d" % i, [128, D], BF16) for i in range(2)]
                    ss = [S.sbuf("ss%d" % i, [128, 1], F32) for i in range(2)]
                    rs = [S.sbuf("rs%d" % i, [128, 1], F32) for i in range(2)]
                    ptr = [S.psum("ptr%d" % i, [128, 8, 128], BF16) for i in range(2)]
                    for t in range(NTILE):
                        c = 0 if t < (NCTX * LC) // 128 else 1
                        xt, hbt, sst, rst = xts[t % 2], hb[t % 2], ss[t % 2], rs[t % 2]
                        S.dma("sp", lambda e: e.dma_start(out=xt[:], in_=x_cur[t * 128:(t + 1) * 128, :]),
                              reads=[x_cur], writes=[xt])
                        S.op("act", lambda e: e.activation(out=junk[:], in_=xt[:], func=AF.Square, accum_out=sst[:]),
                             reads=[xt], writes=[junk, sst])
                        S.op("act", lambda e: e.activation(out=rst[:], in_=sst[:], func=AF.Sqrt, scale=1.0 / D, bias=EPS),
                             reads=[sst], writes=[rst])
                        S.op("dve", lambda e: e.reciprocal(out=rst[:], in_=rst[:]), reads=[rst], writes=[rst])
                        S.op("dve", lambda e: e.scalar_tensor_tensor(out=xt[:], in0=xt[:], scalar=rst[:, 0:1], in1=gm[c][:],
                                                                     op0=ALU.mult, op1=ALU.mult),
                             reads=[xt, rst, gm[c]], writes=[xt])
                        S.op("pool", lambda e: e.tensor_tensor(out=hbt[:], in0=xt[:], in1=sh[c][:], op=ALU.add),
                             reads=[xt, sh[c]], writes=[hbt])
                        for half in range(2):
                            pt = ptr[half]
                            for j in range(8):
                                kc = half * 8 + j
                                S.op("pe", lambda e: e.transpose(pt[:, j, :], hbt[:, kc * 128:(kc + 1) * 128], ident[:]),
                                     reads=[hbt, ident], writes=[pt])
                            _evac(S, half, hT[:, half * 8:(half + 1) * 8, t * 128:(t + 1) * 128], pt[:, :, :],
                                  reads=[pt], writes=[hT])
                    S.barrier()
                S.scope = sc
                wts = [S.sbuf("wint%d" % i, [128, 16, 512], BF16) for i in range(2)]
                stg = [S.sbuf("stg%d" % i, [128, 512], F32) for i in range(4)]
                pmm = [S.psum("pmm%d" % i, [128, 512], F32) for i in range(4)]
                nblk = (NT_COLS + 511) // 512
                ev = 0
                for cb in range(nblk):
                    c0 = cb * 512
                    cw = min(512, NT_COLS - c0)
                    wt = wts[cb % 2]
                    S.dma("pool", lambda e: e.dma_start(
                        out=wt[:, :, 0:cw], in_=w_in_T[l, :, c0:c0 + cw].rearrange("(k p) c -> p k c", p=128)),
                        reads=[w_in_T], writes=[wt])
                    for t in range(NTILE):
                        ps, st = pmm[ev % 4], stg[ev % 4]
                        for kc in range(16):
                            S.op("pe", lambda e: e.matmul(ps[:, 0:cw], lhsT=hT[:, kc, t * 128:(t + 1) * 128],
                                                          rhs=wt[:, kc, 0:cw], start=(kc == 0), stop=(kc == 15)),
                                 reads=[hT, wt], writes=[ps])
                        _evac(S, ev, st[:, 0:cw], ps[:, 0:cw], reads=[ps], writes=[st])
                        S.dma("sp", lambda e: e.dma_start(out=P_T[t * 128:(t + 1) * 128, c0:c0 + cw], in_=st[:, 0:cw]),
                              reads=[st], writes=[P_T])
                        ev += 1
                nblk = (NF_ROWS + 511) // 512
                for cb in range(nblk):
                    c0 = cb * 512
                    cw = min(512, NF_ROWS - c0)
                    wt = wts[cb % 2]
                    S.dma("pool", lambda e: e.dma_start(
                        out=wt[:, :, 0:cw], in_=w_in_F[l, :, c0:c0 + cw].rearrange("(k p) c -> p k c", p=128)),
                        reads=[w_in_F], writes=[wt])
                    for sb in range((cw + 127) // 128):
                        r0 = c0 + sb * 128
                        rw = min(128, NF_ROWS - r0)
                        for g in range(TT // 512):
                            ps, st = pmm[ev % 4], stg[ev % 4]
                            for kc in range(16):
                                S.op("pe", lambda e: e.matmul(ps[0:rw, :], lhsT=wt[:, kc, sb * 128:sb * 128 + rw],
                                                              rhs=hT[:, kc, g * 512:(g + 1) * 512],
                                                              start=(kc == 0), stop=(kc == 15)),
                                     reads=[hT, wt], writes=[ps])
                            _evac(S, ev, st[0:rw, :], ps[0:rw, :], reads=[ps], writes=[st])
                            S.dma("sp", lambda e: e.dma_start(out=P_F[r0:r0 + rw, g * 512:(g + 1) * 512], in_=st[0:rw, :]),
                                  reads=[st], writes=[P_F])
                            ev += 1
            S.barrier()
            S.scope = es

            for s_ in range(NCTX):
                for (nm, ob) in (("kr", o_kr), ("nak", o_nak), ("nav", o_nav)):
                    a0, aw = T_COLS[nm]
                    S.dma("sp", lambda e: e.dma_start(out=ob[s_, l, :, :], in_=P_T[s_ * LC:(s_ + 1) * LC, a0:a0 + aw]),
                          reads=[P_T], writes=[ob])
            if DBG_BR:
                for b_ in DBG_FILL:
                    S.dma("sp", lambda e: e.dma_start(out=brT[b_][:, :], in_=R["dbg_br"][l, b_ - 1]),
                          reads=[R["dbg_br"]], writes=[brT[b_]])
            stage_dn(S, l, R)
            if not DBG_BR:
                stage_hyena(S, l, R)
                stage_mla(S, l, R)
                stage_na(S, l, R)
            if DBG_BR:
                for nm_, b_ in (("d_brT1", 1),):
                    db = eout(nm_, [1024, TT], BF16)
                    outs.append(db)
                    S.dma("sp", lambda e: e.dma_start(out=db[:, :], in_=brT[b_][:, :]), reads=[brT[b_]], writes=[db])
            if not DBG_BR:
                stage_merge(S, l, R, x_in if l == 0 else R["xbuf"])
                stage_peer(S, l, R, last=(l == DEPTH - 1))

        for b in outs:
            if b.last_w is not None:
                S._wait("sp", b.last_w[0], b.last_w[1])
        S.barrier()
        k.ninstr = S.ninstr
    return nc, k


def _prep_weights(inp):
    w_in = inp["w_in"]
    tcols = np.concatenate([
        np.arange(O_CQ, O_CQ + 512), np.arange(O_CKV, O_CKV + 256), np.arange(O_KR, O_KR + 64),
        np.arange(O_Z, O_Z + 1024), np.arange(O_A, O_A + 16), np.arange(O_B, O_B + 16),
        np.arange(O_NA + 1024, O_NA + 2048), np.arange(O_NA + 2048, O_NA + 3072),
        np.arange(O_GATE, O_GATE + 8192)])
    sw = np.concatenate([np.arange(16, 32), np.arange(0, 16), np.arange(48, 64), np.arange(32, 48)])
    fcols = np.concatenate([
        np.arange(O_KR, O_KR + 64), O_KR + sw, np.arange(O_DN, O_DN + 3072), np.arange(O_HY, O_HY + 3072),
        np.arange(O_NA, O_NA + 1024), np.arange(O_NA + 1024, O_NA + 2048)])
    assert len(tcols) == NT_COLS and len(fcols) == NF_ROWS
    return np.ascontiguousarray(w_in[:, :, tcols]), np.ascontiguousarray(w_in[:, :, fcols])


def _prep_consts(inp):
    C = {}
    sw = np.concatenate([np.arange(16, 32), np.arange(0, 16), np.arange(48, 64), np.arange(32, 48)])
    nope = np.concatenate([np.arange(h * 192, h * 192 + 128) for h in range(8)])
    rope = np.concatenate([np.arange(h * 192 + 128, h * 192 + 192) for h in range(8)])
    ropesw = np.concatenate([h * 192 + 128 + sw for h in range(8)])
    C["w_qb_p"] = np.ascontiguousarray(inp["mla_w_qb"][:, :, np.concatenate([nope, rope, ropesw])])
    kn = np.concatenate([np.arange(h * 256, h * 256 + 128) for h in range(8)])
    vv = np.concatenate([np.arange(h * 256 + 128, h * 256 + 256) for h in range(8)])
    C["w_kvb_p"] = np.ascontiguousarray(inp["mla_w_kvb"][:, :, np.concatenate([kn, vv])])
    t = np.arange(LL)
    pos = [t // 64, t % 64]
    inv = 10000.0 ** (-np.arange(0, 32, 2, dtype=np.float32) / 32.0)
    cosT = np.zeros((64, LL), np.float32)
    sinT = np.zeros((64, LL), np.float32)
    for d in range(64):
        half, j = d // 32, d % 32
        ang = pos[half].astype(np.float32) * inv[j % 16]
        cosT[d] = np.cos(ang)
        sinT[d] = (-np.sin(ang)) if j < 16 else np.sin(ang)
    C["ropeT"] = np.stack([cosT, sinT], 0).astype(np.float32)
    rpb = inp["na_rpb"]
    NEG = np.float32(-30000.0)
    cp = np.arange(64)[:, None]
    c = np.arange(64)[None, :]
    cstart = np.clip(c - 8, 0, 48)
    cvalid = (cp >= cstart) & (cp < cstart + 16)
    dcol = np.clip(cp - c + 15, 0, 30)
    ctab = np.full((DEPTH, 8, 64, 31, 64), NEG, np.float32)
    for mm in range(31):
        dr = 22 - mm
        if 0 <= dr <= 14:
            g = rpb[:, :, dr, :][:, :, dcol]
            ctab[:, :, :, mm, :] = np.where(cvalid[None, None], g, NEG)
    C["na_ctab"] = ctab
    mask = np.full((NA_NMASK, 128, 512), NEG, np.float32)
    for (qb, kt), mi in NA_MASK_IDX.items():
        for rr in range(2):
            rp = 2 * kt + rr
            for j in range(8):
                r = 8 * qb + j
                st_ = min(max(r - 4, 0), 24)
                if st_ <= rp < st_ + 8:
                    mask[mi, rr * 64:(rr + 1) * 64, j * 64:(j + 1) * 64] = 0.0
    C["na_mask"] = mask
    C["peer_keysT"] = np.ascontiguousarray(inp["peer_keys"].transpose(0, 1, 3, 2))
    ii = np.arange(128)[:, None]
    jj = np.arange(128)[None, :]
    NEGM = np.float32(-30000.0)
    dc = np.zeros((10, 128, 128), np.float32)
    dc[9] = ((ii // 64) == (jj // 64))
    dc[0] = (ii <= jj)
    dc[1] = (ii >= jj)
    dc[2] = np.eye(128)
    dc[3] = (ii > jj)
    dc[4] = (ii < jj)
    dc[5] = np.where(ii > jj, 0.0, NEGM)
    dc[6] = np.where(ii < jj, 0.0, NEGM)
    dc[7] = np.where(jj >= ii, 0.0, NEGM)
    dc[8] = np.where(jj <= ii, 0.0, NEGM)
    C["dn_consts"] = dc
    C["dn_convT"] = np.ascontiguousarray(inp["dn_conv"].reshape(DEPTH, 5, 24, 128).transpose(0, 3, 2, 1))
    return C


_CACHE = {}
_DBG = {}


def kernel(**inp):
    inp = {k_: np.asarray(v) for k_, v in inp.items()}
    if "prog" not in _CACHE:
        _CACHE["prog"] = build_program()
    nc, kk = _CACHE["prog"]
    w_in_T, w_in_F = _prep_weights(inp)
    C = _prep_consts(inp)
    H = _hy_host(inp)
    ident = np.eye(128, dtype=np.float32)
    in_maps = []
    for i in range(NCORE):
        b = i // 2
        x_in = np.concatenate([inp["x_prompt"][2 * i].reshape(LC, D), inp["x_prompt"][2 * i + 1].reshape(LC, D),
                               inp["x_sample"][b].reshape(LL, D)], axis=0)
        c2 = np.stack([inp["c_ctx"], inp["c"][b]], axis=0)
        c2T = np.ascontiguousarray(c2.reshape(2, 16, 128).transpose(2, 1, 0))
        in_maps.append({
            "x_in": np.ascontiguousarray(x_in), "c2T": c2T, "ident": ident,
            "norm1_g": inp["norm1_g"], "w_ada": inp["w_ada"], "b_ada": inp["b_ada"],
            "w_in_T": w_in_T, "w_in_F": w_in_F, "mla_kv_norm": inp["mla_kv_norm"],
            "mla_q_norm": inp["mla_q_norm"], "w_qb_p": C["w_qb_p"], "w_kvb_p": C["w_kvb_p"],
            "cache_ckv": inp["cache_mla_ckv"][b], "cache_kr": inp["cache_mla_krope"][b],
            "cache_nak": np.ascontiguousarray(inp["cache_na_k"][b].reshape(DEPTH, PAST, 1024)),
            "cache_nav": np.ascontiguousarray(inp["cache_na_v"][b].reshape(DEPTH, PAST, 1024)),
            "ropeT": C["ropeT"], "na_ctab": C["na_ctab"], "na_mask": C["na_mask"],
            "w_branch": inp["w_branch"], "w_out": inp["w_out"], "norm2_g": inp["norm2_g"],
            "peer_wq": inp["peer_wq"], "peer_keysT": C["peer_keysT"],
            "peer_u0": inp["peer_u"][0], "peer_u1": inp["peer_u"][1],
            "peer_v0": inp["peer_v"][0], "peer_v1": inp["peer_v"][1],
            "final_g": inp["final_g"].reshape(1, D),
            "dn_consts": C["dn_consts"], "dn_convT": C["dn_convT"],
            "dn_a_log": inp["dn_a_log"].reshape(DEPTH, 16), "dn_dt_bias": inp["dn_dt_bias"].reshape(DEPTH, 16),
            "dn_out_norm": inp["dn_out_norm"],
            "dn_state0": inp["state_dn_fwd"][b], "dn_state1": inp["state_dn_bwd"][b],
        })
        in_maps[-1].update(H)
        if DBG_BR:
            in_maps[-1]["dbg_br"] = _DBG["br"]
    import os
    ndev = int(os.environ.get("KDEV_CORES", NCORE))
    res = run_bass_kernel_spmd(nc, in_maps[:ndev], core_ids=list(range(ndev)))
    r = list(res.results) + [res.results[0]] * (NCORE - ndev)
    _DBG["res"] = res.results[0]
    B = 16
    y_prompt = np.concatenate([r[i]["y_out"][:NCTX * LC].reshape(NCTX, LC, D) for i in range(NCORE)], axis=0)
    y_sample = np.stack([r[2 * j]["y_out"][NCTX * LC:] for j in range(4)], axis=0)
    new_ckv = np.concatenate([r[i]["o_ckv"] for i in range(NCORE)], axis=0)
    new_kr = np.concatenate([r[i]["o_kr"] for i in range(NCORE)], axis=0)
    new_nak = np.concatenate([r[i]["o_nak"] for i in range(NCORE)], axis=0).reshape(B, DEPTH, LC, 8, 128)
    new_nav = np.concatenate([r[i]["o_nav"] for i in range(NCORE)], axis=0).reshape(B, DEPTH, LC, 8, 128)
    new_dnf = np.concatenate([r[i]["o_dn0"] for i in range(NCORE)], axis=0)
    new_dnb = np.concatenate([r[i]["o_dn1"] for i in range(NCORE)], axis=0)
    return (y_prompt, y_sample, new_ckv, new_kr, new_nak, new_nav, new_dnf, new_dnb)
```

```python
import numpy as np
from contextlib import ExitStack
import concourse.bass as bass
import concourse.mybir as mybir
from concourse.bass_utils import run_bass_kernel_spmd

F32 = mybir.dt.float32
BF16 = mybir.dt.bfloat16
I32 = mybir.dt.int32
U32 = mybir.dt.uint32
U16 = mybir.dt.uint16
AF = mybir.ActivationFunctionType
ALU = mybir.AluOpType
AX = mybir.AxisListType

D = 2048
DEPTH = 2
NCORE = 8
LC = 256
LL = 2048
PAST = 512
NCTX = 2
TT = NCTX * LC + LL
NTILE = TT // 128
EPS = 1e-6

T_COLS = {}
_o = 0
for _n, _s in (("cq", 512), ("ckv", 256), ("kr", 64), ("z", 1024), ("a", 16), ("b", 16),
               ("nak", 1024), ("nav", 1024), ("gate", 8192)):
    T_COLS[_n] = (_o, _s)
    _o += _s
NT_COLS = _o
F_ROWS = {}
_o = 0
for _n, _s in (("krT", 64), ("krswT", 64), ("dnT", 3072), ("hyT", 3072), ("naqT", 1024), ("nakT", 1024)):
    F_ROWS[_n] = (_o, _s)
    _o += _s
NF_ROWS = _o

IN_SIZES = (512, 256, 64, 3072, 1024, 16, 16, 3072, 3072, 8192)
IN_OFF = np.concatenate([[0], np.cumsum(IN_SIZES)]).astype(int)
(O_CQ, O_CKV, O_KR, O_DN, O_Z, O_A, O_B, O_HY, O_NA, O_GATE) = [int(v) for v in IN_OFF[:-1]]


class Buf:
    __slots__ = ("t", "last_w", "readers", "name", "root")

    def __init__(self, t, name, root=None):
        self.t = t
        self.name = name
        self.last_w = None
        self.readers = {}
        self.root = root if root is not None else self

    def __getitem__(self, idx):
        return self.t[idx]


class Sched:
    ENG = ("pe", "act", "dve", "pool", "sp")
    NDMA = 10

    def __init__(self, nc, es):
        self.nc = nc
        self.es = es
        self.eng = {"pe": nc.tensor, "act": nc.scalar, "dve": nc.vector, "pool": nc.gpsimd, "sp": nc.sync}
        self.sem = {}
        self.cnt = {}
        for e in self.ENG:
            self.sem[e] = es.enter_context(nc.semaphore("sem_" + e))
            self.cnt[e] = 0
        self.dq = {}
        for q in ("sp", "pool"):
            sl = []
            for i in range(self.NDMA):
                key = ("dma", q, i)
                self.sem[key] = es.enter_context(nc.semaphore("dsem_%s_%d" % (q, i)))
                self.cnt[key] = 0
                sl.append(key)
            self.dq[q] = [sl, 0]
        self.seen = {e: {} for e in self.ENG}
        self.ninstr = 0
        self.scope = es

    def _nm(self, name):
        self.nid = getattr(self, "nid", 0) + 1
        return "%s_%d" % (name, self.nid)

    def sbuf(self, name, shape, dtype=F32):
        name = self._nm(name)
        return Buf(self.scope.enter_context(self.nc.sbuf_tensor(name, list(shape), dtype)), name)

    def psum(self, name, shape, dtype=F32):
        name = self._nm(name)
        return Buf(self.scope.enter_context(self.nc.psum_tensor(name, list(shape), dtype)), name)

    def dram(self, name, shape, dtype=F32, kind="Internal"):
        t = self.nc.dram_tensor(name, list(shape), dtype, kind=kind)
        return Buf(t.ap(), name)

    def _wait(self, e, key, val):
        if self.seen[e].get(key, 0) >= val:
            return
        self.eng[e].wait_ge(self.sem[key], val)
        self.seen[e][key] = val
        self.ninstr += 1

    def _deps(self, e, reads, writes):
        reads = [b.root for b in reads]
        writes = [b.root for b in writes]
        need = {}
        for b in list(reads) + list(writes):
            if b.last_w is not None:
                k, v = b.last_w
                if need.get(k, 0) < v:
                    need[k] = v
        for b in writes:
            for k, v in b.readers.items():
                if need.get(k, 0) < v:
                    need[k] = v
        for k, v in need.items():
            if k == "pe" and e == "pe":
                continue
            self._wait(e, k, v)

    def _mark(self, ev, reads, writes):
        reads = [b.root for b in reads]
        writes = [b.root for b in writes]
        k, v = ev
        for b in writes:
            b.last_w = ev
            b.readers = {}
        for b in reads:
            if b.readers.get(k, 0) < v:
                b.readers[k] = v

    def op(self, e, fn, reads=(), writes=()):
        self._deps(e, reads, writes)
        ins = fn(self.eng[e])
        self.cnt[e] += 1
        ins.then_inc(self.sem[e], 1)
        self._mark((e, self.cnt[e]), reads, writes)
        self.ninstr += 1
        return ins

    def dma(self, q, fn, reads=(), writes=()):
        sl, i = self.dq[q]
        key = sl[i % self.NDMA]
        self.dq[q][1] = i + 1
        if self.cnt[key] > 0:
            self._wait(q, key, self.cnt[key])
        self._deps(q, reads, writes)
        ins = fn(self.eng[q])
        self.cnt[key] += 16
        ins.then_inc(self.sem[key], 16)
        self._mark((key, self.cnt[key]), reads, writes)
        self.ninstr += 1
        return ins

    def barrier(self):
        for e in self.ENG:
            for key, v in self.cnt.items():
                if v > 0:
                    self._wait(e, key, v)


import os as _os
DBG_BR = bool(int(_os.environ.get("KDBG_BR", "0")))
DBG_FILL = (2,)
NTILE_PEER = 0
DNSTOP = int(_os.environ.get("KDN_STOP", "9"))
DNVAR = int(_os.environ.get("KDN_VAR", "0"))
DNSEQ = int(_os.environ.get("KDN_SEQ", "3"))


def _hy_inputs(R, ein):
    R["hy_tab"] = []
    R["hy_zemb"] = []
    R["hy_wf"] = []
    R["hy_win"] = []
    for i, L in enumerate((LC, LL)):
        nf, KT, NB = (L + 1), (L + 1 + 127) // 128, (L + 1 + 511) // 512
        R["hy_tab"].append(ein("hy_tab%d" % i, [2, NB, 128, KT, 512], BF16))
        R["hy_zemb"].append(ein("hy_zemb%d" % i, [33, L]))
        R["hy_wf"].append(ein("hy_wf%d" % i, [128, KT]))
        R["hy_win"].append(ein("hy_win%d" % i, [L, 1024]))
    R["hy_w1"] = ein("hy_w1", [DEPTH, 33, 64])
    R["hy_w2"] = ein("hy_w2", [DEPTH, 64, 64])
    R["hy_w3"] = ein("hy_w3", [DEPTH, 64, 4096])
    R["hy_b12"] = ein("hy_b12", [DEPTH, 64, 2])
    R["hy_convT"] = ein("hy_convT", [DEPTH, 128, 24, 3])
    R["hy_biasT"] = ein("hy_biasT", [DEPTH, 2, 128, 8])


def _hy_scratch(S, R):
    R["hyZ"] = S.dram("hyZ", [3072, TT])
    R["hyY1"] = S.dram("hyY1", [1024, TT])
    R["hyU"] = S.dram("hyU", [TT, 1024], BF16)
    R["hyPQ"] = [S.dram("hyPQ%d" % i, [2, 2, ((L + 1 + 127) // 128) * 128, 1024]) for i, L in enumerate((LC, LL))]
    R["hyYS"] = S.dram("hyYS", [2, ((LL + 1 + 127) // 128) * 128, 1024], BF16)


def _hy_host(inp):
    import ml_dtypes
    H = {}
    for i, L in enumerate((LC, LL)):
        nf, KT, NB = (L + 1), (L + 1 + 127) // 128, (L + 1 + 511) // 512
        r = np.arange(KT * 128, dtype=np.int64)
        q = np.arange(NB * 512, dtype=np.int64)
        prod = (r[:, None] * q[None, :]) % (2 * L)
        ang = np.pi * prod.astype(np.float64) / L
        valid = (r[:, None] <= L) & (q[None, :] <= L)
        tabs = []
        for fn in (np.cos, np.sin):
            T = np.where(valid, fn(ang), 0.0).astype(np.float32)
            T = T.reshape(KT, 128, NB, 512).transpose(2, 1, 0, 3)
            tabs.append(T)
        H["hy_tab%d" % i] = np.ascontiguousarray(np.stack(tabs, 0)).astype(ml_dtypes.bfloat16)
        f = np.arange(KT * 128)
        wf = np.where((f == 0) | (f == L), 1.0, np.where(f < L, 2.0, 0.0)) / (2.0 * L)
        H["hy_wf%d" % i] = np.ascontiguousarray(wf.reshape(KT, 128).T).astype(np.float32)
        t01 = np.linspace(0.0, 1.0, L, dtype=np.float32)[:, None]
        w = (np.float32(2.0 * np.pi) * np.arange(L, dtype=np.float32)[:, None] / np.float32(L)).astype(np.float32)
        fr = np.linspace(1e-4, 15.0, 16, dtype=np.float32)[None, :]
        z = np.concatenate([t01, np.cos(fr * w), -np.sin(fr * w)], axis=-1).astype(np.float32)
        H["hy_zemb%d" % i] = np.ascontiguousarray(z.T)
        max_decay = np.log(1e-2) / 0.3
        min_decay = np.log(1e-2) / 1.5
        deltas = np.linspace(min_decay, max_decay, 1024, dtype=np.float32)
        H["hy_win%d" % i] = np.exp(-t01 * np.abs(deltas)[None, :]).astype(np.float32)
    H["hy_w1"] = inp["hy_w1"]
    H["hy_w2"] = inp["hy_w2"]
    H["hy_w3"] = inp["hy_w3"]
    H["hy_b12"] = np.ascontiguousarray(np.stack([inp["hy_b1"], inp["hy_b2"]], axis=-1))
    H["hy_convT"] = np.ascontiguousarray(inp["hy_conv"].reshape(DEPTH, 3, 24, 128).transpose(0, 3, 2, 1))
    H["hy_biasT"] = np.ascontiguousarray(inp["hy_bias"].reshape(DEPTH, 2, 8, 128).transpose(0, 1, 3, 2))
    return H


class K:
    pass


def _evac(S, i, out_ap, in_ap, reads, writes):
    if i % 2 == 0:
        S.op("act", lambda e: e.activation(out=out_ap, in_=in_ap, func=AF.Copy), reads=reads, writes=writes)
    else:
        S.op("dve", lambda e: e.tensor_copy(out=out_ap, in_=in_ap), reads=reads, writes=writes)


def _rmsnorm_rows(S, xt, g, n, jk, st):
    S.op("act", lambda e: e.activation(out=jk[:, 0:n], in_=xt[:, 0:n], func=AF.Square, accum_out=st[:]),
         reads=[xt], writes=[jk, st])
    S.op("act", lambda e: e.activation(out=st[:], in_=st[:], func=AF.Sqrt, scale=1.0 / n, bias=EPS),
         reads=[st], writes=[st])
    S.op("dve", lambda e: e.reciprocal(out=st[:], in_=st[:]), reads=[st], writes=[st])
    S.op("dve", lambda e: e.scalar_tensor_tensor(out=xt[:, 0:n], in0=xt[:, 0:n], scalar=st[:, 0:1], in1=g[:, 0:n],
                                                 op0=ALU.mult, op1=ALU.mult),
         reads=[xt, st, g], writes=[xt])


def _attn(S, A, qparts, kparts, v_ap, ktl, q0, QB, scale, bias_fn, out_dst):
    psO, psD = A["psO"][A["n"] % 2], A["psD"][A["n"] % 2]
    A["n"] += 1
    n = len(ktl)
    for i, kt in enumerate(ktl):
        ps = A["psS"][A["ns"] % 2]
        pT = A["pT"][A["ns"] % 3]
        A["ns"] += 1
        for pi in range(len(qparts)):
            kb, kf = kparts[pi]
            qb_, qf = qparts[pi]
            S.op("pe", lambda e: e.matmul(ps[:, 0:QB], lhsT=kf(kt), rhs=qf(q0, QB), start=(pi == 0),
                                          stop=(pi == len(qparts) - 1)), reads=[kb, qb_], writes=[ps])
        bb = bias_fn(kt) if bias_fn is not None else None
        if bb is not None:
            tf = A["tf"][A["ns"] % 2]
            S.op("dve", lambda e: e.scalar_tensor_tensor(out=tf[:, 0:QB], in0=ps[:, 0:QB], scalar=float(scale),
                                                         in1=bb[:, 0:QB], op0=ALU.mult, op1=ALU.add),
                 reads=[ps, bb], writes=[tf])
            S.op("act", lambda e: e.activation(out=pT[:, 0:QB], in_=tf[:, 0:QB], func=AF.Exp), reads=[tf], writes=[pT])
        else:
            S.op("act", lambda e: e.activation(out=pT[:, 0:QB], in_=ps[:, 0:QB], func=AF.Exp, scale=float(scale)),
                 reads=[ps], writes=[pT])
        vb, va = v_ap(kt)
        S.op("pe", lambda e: e.matmul(psO[:, 0:QB], lhsT=va, rhs=pT[:, 0:QB], start=(i == 0), stop=(i == n - 1)),
             reads=[vb, pT], writes=[psO])
        S.op("pe", lambda e: e.matmul(psD[:, 0:QB], lhsT=A["ones"][:, :], rhs=pT[:, 0:QB], start=(i == 0),
                                      stop=(i == n - 1)), reads=[A["ones"], pT], writes=[psD])
    rd = A["rd"]
    ob = A["ob"][A["n"] % 2]
    S.op("dve", lambda e: e.reciprocal(out=rd[:, 0:QB], in_=psD[:, 0:QB]), reads=[psD], writes=[rd])
    S.op("dve", lambda e: e.tensor_tensor(out=ob[:, 0:QB], in0=psO[:, 0:QB], in1=rd[:, 0:QB], op=ALU.mult),
         reads=[psO, rd], writes=[ob])
    dbuf, dap = out_dst
    S.dma("sp", lambda e: e.dma_start(out=dap, in_=ob[:, 0:QB]), reads=[ob], writes=[dbuf])


def _attn_res(S):
    A = {"n": 0, "ns": 0}
    A["psS"] = [S.psum("psS%d" % i, [128, 512], F32) for i in range(2)]
    A["psO"] = [S.psum("psO%d" % i, [128, 512], F32) for i in range(2)]
    A["psD"] = [S.psum("psD%d" % i, [128, 512], F32) for i in range(2)]
    A["pT"] = [S.sbuf("pT%d" % i, [128, 512], BF16) for i in range(3)]
    A["tf"] = [S.sbuf("tf%d" % i, [128, 512], F32) for i in range(2)]
    A["rd"] = S.sbuf("rd", [128, 512], F32)
    A["ob"] = [S.sbuf("ob%d" % i, [128, 512], BF16) for i in range(2)]
    ones = S.sbuf("onesb", [128, 128], BF16)
    S.op("dve", lambda e: e.memset(ones[:], 1.0), writes=[ones])
    A["ones"] = ones
    return A


def _transpose_rows(S, src, ncol, dst, dst_fn, ident, ptr, ev0=0):
    nch = ncol // 128
    for c0 in range(0, nch, 8):
        pt = ptr[(ev0 + c0 // 8) % 2]
        nn = min(8, nch - c0)
        for j in range(nn):
            S.op("pe", lambda e: e.transpose(pt[:, j, :], src[:, (c0 + j) * 128:(c0 + j + 1) * 128], ident[:]),
                 reads=[src, ident], writes=[pt])
        for j in range(nn):
            _evac(S, j, dst_fn(c0 + j), pt[:, j, :], reads=[pt], writes=[dst])


def stage_mla(S, l, R):
    ident = R["ident"]
    P_T, P_F, brT = R["P_T"], R["P_F"], R["brT"]
    with ExitStack() as sc:
        S.scope = sc
        A = _attn_res(S)
        psP = [S.psum("psP%d" % i, [128, 512], F32) for i in range(2)]
        wqb = S.sbuf("wqb", [128, 4, 2048], BF16)
        wkvb = S.sbuf("wkvb", [128, 2, 2048], BF16)
        S.dma("pool", lambda e: e.dma_start(out=wqb[:], in_=R["w_qb"][l].rearrange("(k p) c -> p k c", p=128)),
              reads=[R["w_qb"]], writes=[wqb])
        S.dma("pool", lambda e: e.dma_start(out=wkvb[:], in_=R["w_kvb"][l].rearrange("(k p) c -> p k c", p=128)),
              reads=[R["w_kvb"]], writes=[wkvb])
        gq = S.sbuf("gq", [128, 512], F32)
        gk = S.sbuf("gk", [128, 256], F32)
        S.dma("sp", lambda e: e.dma_start(out=gq[:], in_=R["mla_q_norm"][l, :].partition_broadcast(128)),
              reads=[R["mla_q_norm"]], writes=[gq])
        S.dma("sp", lambda e: e.dma_start(out=gk[:], in_=R["mla_kv_norm"][l, :].partition_broadcast(128)),
              reads=[R["mla_kv_norm"]], writes=[gk])
        ropc = S.sbuf("ropc", [64, LL], F32)
        rops = S.sbuf("rops", [64, LL], F32)
        S.dma("sp", lambda e: e.dma_start(out=ropc[:], in_=R["ropeT"][0]), reads=[R["ropeT"]], writes=[ropc])
        S.dma("sp", lambda e: e.dma_start(out=rops[:], in_=R["ropeT"][1]), reads=[R["ropeT"]], writes=[rops])
        cqT = S.sbuf("cqT", [128, 4, LL], BF16)
        ckvT = S.sbuf("ckvT", [128, 2, LL + PAST], BF16)
        krT = S.sbuf("krT", [128, LL + PAST], BF16)
        vall = S.sbuf("vall", [128, (LL + PAST) // 128, 1024], BF16)
        knT = S.sbuf("knT", [128, LL + PAST], BF16)
        qnT = S.sbuf("qnT", [128, LL], BF16)
        qrT = S.sbuf("qrT", [128, LL], BF16)
        S.op("dve", lambda e: e.memset(krT[64:128, :], 0.0), writes=[krT])
        S.op("dve", lambda e: e.memset(qrT[64:128, :], 0.0), writes=[qrT])
        xt = [S.sbuf("mx%d" % i, [128, 512], F32) for i in range(2)]
        xb = [S.sbuf("mxb%d" % i, [128, 512], BF16) for i in range(2)]
        jk = S.sbuf("mjk", [128, 512], F32)
        st = [S.sbuf("mst%d" % i, [128, 1], F32) for i in range(2)]
        r1 = S.sbuf("mr1", [64, 512], F32)
        r2 = S.sbuf("mr2", [64, 512], F32)
        ptr = [S.psum("mptr%d" % i, [128, 8, 128], BF16) for i in range(0)]
        aq, _ = T_COLS["cq"]
        ak, _ = T_COLS["ckv"]
        akr, _ = T_COLS["kr"]
        fkr, _ = F_ROWS["krT"]
        fks, _ = F_ROWS["krswT"]

        def tr_bf(src, ncol, dst, dst_fn, ev):
            nch = ncol // 128
            pt = psP[ev % 2]
            ptv = pt[:, :].bitcast(BF16)
            for j in range(nch):
                S.op("pe", lambda e: e.transpose(ptv[:, j * 128:(j + 1) * 128], src[:, j * 128:(j + 1) * 128], ident[:]),
                     reads=[src, ident], writes=[pt])
            for j in range(nch):
                _evac(S, j, dst_fn(j), ptv[:, j * 128:(j + 1) * 128], reads=[pt], writes=[dst])

        seqs = [(s * LC, LC, False) for s in range(NCTX)] + [(NCTX * LC, LL, True)]
        for si, (tok0, L, latent) in enumerate(seqs):
            Lk = L + (PAST if latent else 0)
            nkt = Lk // 128
            QB = 512 if latent else 256
            for t in range(L // 128):
                g0 = tok0 + t * 128
                x1, x1b, s1 = xt[t % 2], xb[t % 2], st[t % 2]
                S.dma("sp", lambda e: e.dma_start(out=x1[:, 0:512], in_=P_T[g0:g0 + 128, aq:aq + 512]),
                      reads=[P_T], writes=[x1])
                _rmsnorm_rows(S, x1, gq, 512, jk, s1)
                S.op("pool", lambda e: e.tensor_copy(out=x1b[:, 0:512], in_=x1[:, 0:512]), reads=[x1], writes=[x1b])
                tr_bf(x1b, 512, cqT, lambda j: cqT[:, j, t * 128:(t + 1) * 128], t)
            for t in range(nkt):
                x1, x1b, s1 = xt[t % 2], xb[t % 2], st[t % 2]
                if t < L // 128:
                    g0 = tok0 + t * 128
                    S.dma("sp", lambda e: e.dma_start(out=x1[:, 0:256], in_=P_T[g0:g0 + 128, ak:ak + 256]),
                          reads=[P_T], writes=[x1])
                    _rmsnorm_rows(S, x1, gk, 256, jk, s1)
                    if not latent:
                        S.dma("sp", lambda e: e.dma_start(out=R["o_ckv"][si, l, t * 128:(t + 1) * 128, :], in_=x1[:, 0:256]),
                              reads=[x1], writes=[R["o_ckv"]])
                else:
                    p0 = (t - L // 128) * 128
                    S.dma("sp", lambda e: e.dma_start(out=x1[:, 0:256], in_=R["cache_ckv"][l, p0:p0 + 128, :]),
                          reads=[R["cache_ckv"]], writes=[x1])
                S.op("pool", lambda e: e.tensor_copy(out=x1b[:, 0:256], in_=x1[:, 0:256]), reads=[x1], writes=[x1b])
                tr_bf(x1b, 256, ckvT, lambda j: ckvT[:, j, t * 128:(t + 1) * 128], t)
                if t >= L // 128:
                    p0 = (t - L // 128) * 128
                    S.dma("sp", lambda e: e.dma_start(out=x1[:, 256:320], in_=R["cache_kr"][l, p0:p0 + 128, :]),
                          reads=[R["cache_kr"]], writes=[x1])
                    S.op("pool", lambda e: e.tensor_copy(out=x1b[:, 256:320], in_=x1[:, 256:320]), reads=[x1], writes=[x1b])
                    pt = psP[(t + 1) % 2]
                    ptv = pt[:, :].bitcast(BF16)
                    S.op("pe", lambda e: e.transpose(ptv[0:64, 0:128], x1b[:, 256:320], ident[:]),
                         reads=[x1b, ident], writes=[pt])
                    _evac(S, t, krT[0:64, t * 128:(t + 1) * 128], ptv[0:64, 0:128], reads=[pt], writes=[krT])
            for g in range(L // QB):
                g0 = tok0 + g * QB
                S.dma("sp", lambda e: e.dma_start(out=r1[:, 0:QB], in_=P_F[fkr:fkr + 64, g0:g0 + QB]), reads=[P_F], writes=[r1])
                if latent:
                    S.dma("sp", lambda e: e.dma_start(out=r2[:, 0:QB], in_=P_F[fks:fks + 64, g0:g0 + QB]),
                          reads=[P_F], writes=[r2])
                    S.op("dve", lambda e: e.tensor_tensor(out=r1[:, 0:QB], in0=r1[:, 0:QB], in1=ropc[:, g * QB:(g + 1) * QB],
                                                          op=ALU.mult), reads=[r1, ropc], writes=[r1])
                    S.op("dve", lambda e: e.tensor_tensor(out=r2[:, 0:QB], in0=r2[:, 0:QB], in1=rops[:, g * QB:(g + 1) * QB],
                                                          op=ALU.mult), reads=[r2, rops], writes=[r2])
                    S.op("dve", lambda e: e.tensor_tensor(out=krT[0:64, g * QB:(g + 1) * QB], in0=r1[:, 0:QB], in1=r2[:, 0:QB],
                                                          op=ALU.add), reads=[r1, r2], writes=[krT])
                else:
                    S.op("dve", lambda e: e.tensor_copy(out=krT[0:64, g * QB:(g + 1) * QB], in_=r1[:, 0:QB]),
                         reads=[r1], writes=[krT])
            ev = 0
            for t in range(nkt):
                for hb in range(2):
                    ps = psP[ev % 2]
                    for kc in range(2):
                        S.op("pe", lambda e: e.matmul(ps[:, :], lhsT=ckvT[:, kc, t * 128:(t + 1) * 128],
                                                      rhs=wkvb[:, kc, 1024 + hb * 512:1024 + (hb + 1) * 512],
                                                      start=(kc == 0), stop=(kc == 1)), reads=[ckvT, wkvb], writes=[ps])
                    _evac(S, ev, vall[:, t, hb * 512:(hb + 1) * 512], ps[:, :], reads=[ps], writes=[vall])
                    ev += 1
            for h in range(8):
                for g in range((Lk + 511) // 512):
                    w = min(512, Lk - g * 512)
                    ps = psP[ev % 2]
                    for kc in range(2):
                        S.op("pe", lambda e: e.matmul(ps[:, 0:w], lhsT=wkvb[:, kc, h * 128:(h + 1) * 128],
                                                      rhs=ckvT[:, kc, g * 512:g * 512 + w], start=(kc == 0), stop=(kc == 1)),
                             reads=[ckvT, wkvb], writes=[ps])
                    _evac(S, ev, knT[:, g * 512:g * 512 + w], ps[:, 0:w], reads=[ps], writes=[knT])
                    ev += 1
                for g in range(L // QB):
                    ps = psP[ev % 2]
                    for kc in range(4):
                        S.op("pe", lambda e: e.matmul(ps[:, 0:QB], lhsT=wqb[:, kc, h * 128:(h + 1) * 128],
                                                      rhs=cqT[:, kc, g * QB:(g + 1) * QB], start=(kc == 0), stop=(kc == 3)),
                             reads=[cqT, wqb], writes=[ps])
                    _evac(S, ev, qnT[:, g * QB:(g + 1) * QB], ps[:, 0:QB], reads=[ps], writes=[qnT])
                    ev += 1
                    ps = psP[ev % 2]
                    for kc in range(4):
                        S.op("pe", lambda e: e.matmul(ps[0:64, 0:QB], lhsT=wqb[:, kc, 1024 + h * 64:1024 + (h + 1) * 64],
                                                      rhs=cqT[:, kc, g * QB:(g + 1) * QB], start=(kc == 0), stop=(kc == 3)),
                             reads=[cqT, wqb], writes=[ps])
                    if latent:
                        ps2 = psP[(ev + 1) % 2]
                        for kc in range(4):
                            S.op("pe", lambda e: e.matmul(ps2[0:64, 0:QB], lhsT=wqb[:, kc, 1536 + h * 64:1536 + (h + 1) * 64],
                                                          rhs=cqT[:, kc, g * QB:(g + 1) * QB], start=(kc == 0), stop=(kc == 3)),
                                 reads=[cqT, wqb], writes=[ps2])
                        S.op("dve", lambda e: e.tensor_tensor(out=r1[:, 0:QB], in0=ps[0:64, 0:QB],
                                                              in1=ropc[:, g * QB:(g + 1) * QB], op=ALU.mult),
                             reads=[ps, ropc], writes=[r1])
                        S.op("dve", lambda e: e.tensor_tensor(out=r2[:, 0:QB], in0=ps2[0:64, 0:QB],
                                                              in1=rops[:, g * QB:(g + 1) * QB], op=ALU.mult),
                             reads=[ps2, rops], writes=[r2])
                        S.op("dve", lambda e: e.tensor_tensor(out=qrT[0:64, g * QB:(g + 1) * QB], in0=r1[:, 0:QB],
                                                              in1=r2[:, 0:QB], op=ALU.add), reads=[r1, r2], writes=[qrT])
                        ev += 2
                    else:
                        _evac(S, ev, qrT[0:64, g * QB:(g + 1) * QB], ps[0:64, 0:QB], reads=[ps], writes=[qrT])
                        ev += 1
                for g in range(L // QB):
                    _attn(S, A,
                          qparts=[(qnT, lambda q0, n: qnT[:, q0:q0 + n]), (qrT, lambda q0, n: qrT[:, q0:q0 + n])],
                          kparts=[(knT, lambda kt: knT[:, kt * 128:(kt + 1) * 128]),
                                  (krT, lambda kt: krT[:, kt * 128:(kt + 1) * 128])],
                          v_ap=lambda kt: (vall, vall[:, kt, h * 128:(h + 1) * 128]),
                          ktl=list(range(nkt)), q0=g * QB, QB=QB, scale=192 ** -0.5, bias_fn=None,
                          out_dst=(brT[0], brT[0][h * 128:(h + 1) * 128, tok0 + g * QB:tok0 + (g + 1) * QB]))
        S.barrier()
    S.scope = S.es


NA_QB_TILES = {0: list(range(0, 6)), 1: list(range(2, 10)), 2: list(range(6, 14)), 3: list(range(10, 16))}
NA_MASK_IDX = {}
_i = 0
for _qb in range(4):
    for _kt in NA_QB_TILES[_qb]:
        NA_MASK_IDX[(_qb, _kt)] = _i
        _i += 1
NA_NMASK = _i


def stage_na(S, l, R):
    ident = R["ident"]
    P_T, P_F, brT = R["P_T"], R["P_F"], R["brT"]
    with ExitStack() as sc:
        S.scope = sc
        A = _attn_res(S)
        psP = [S.psum("psP%d" % i, [128, 512], F32) for i in range(2)]
        qT = S.sbuf("naqT", [128, LL], BF16)
        kT = S.sbuf("nakT", [128, LL + PAST], BF16)
        vall = S.sbuf("navall", [128, (LL + PAST) // 128, 1024], BF16)
        kc_f = [S.sbuf("nakc%d" % i, [128, 1024], F32) for i in range(2)]
        kc_b = S.sbuf("nakcb", [128, 4, 1024], BF16)
        bias = [S.sbuf("nabias%d" % i, [128, 512], F32) for i in range(2)]
        mask = [S.sbuf("namask%d" % i, [128, 512], F32) for i in range(2)]
        fq, _ = F_ROWS["naqT"]
        fk, _ = F_ROWS["nakT"]
        av, _ = T_COLS["nav"]
        seqs = [(s * LC, LC, False) for s in range(NCTX)] + [(NCTX * LC, LL, True)]
        nb = 0
        for si, (tok0, L, latent) in enumerate(seqs):
            Lk = L + (PAST if latent else 0)
            nkt = Lk // 128
            QB = 512 if latent else 256
            for t in range(L // 128):
                g0 = tok0 + t * 128
                S.dma("pool", lambda e: e.dma_start(out=vall[:, t, :], in_=P_T[g0:g0 + 128, av:av + 1024]),
                      reads=[P_T], writes=[vall])
            if latent:
                for t in range(4):
                    S.dma("pool", lambda e: e.dma_start(out=vall[:, L // 128 + t, :],
                                                        in_=R["cache_nav"][l, t * 128:(t + 1) * 128, :]),
                          reads=[R["cache_nav"]], writes=[vall])
                    S.dma("pool", lambda e: e.dma_start(out=kc_b[:, t, :], in_=R["cache_nak"][l, t * 128:(t + 1) * 128, :]),
                          reads=[R["cache_nak"]], writes=[kc_b])
            for h in range(8):
                S.dma("pool", lambda e: e.dma_start(out=qT[:, 0:L], in_=P_F[fq + h * 128:fq + (h + 1) * 128, tok0:tok0 + L]),
                      reads=[P_F], writes=[qT])
                S.dma("pool", lambda e: e.dma_start(out=kT[:, 0:L], in_=P_F[fk + h * 128:fk + (h + 1) * 128, tok0:tok0 + L]),
                      reads=[P_F], writes=[kT])
                if latent:
                    pt = psP[h % 2]
                    ptv = pt[:, :].bitcast(BF16)
                    for t in range(4):
                        S.op("pe", lambda e: e.transpose(ptv[:, t * 128:(t + 1) * 128], kc_b[:, t, h * 128:(h + 1) * 128],
                                                         ident[:]), reads=[kc_b, ident], writes=[pt])
                    _evac(S, h, kT[:, L:L + 512], ptv[:, 0:512], reads=[pt], writes=[kT])
                for g in range(L // QB):
                    if latent:
                        ktl = NA_QB_TILES[g] + [16, 17, 18, 19]

                        def bias_fn(kt, g=g, h=h):
                            if kt >= 16:
                                return None
                            bb, mm = bias[bias_fn.n % 2], mask[bias_fn.n % 2]
                            bias_fn.n += 1
                            for rr in range(2):
                                rp = 2 * kt + rr
                                mm0 = 15 - rp + 8 * g
                                S.dma("sp", lambda e: e.dma_start(
                                    out=bb[rr * 64:(rr + 1) * 64, :],
                                    in_=R["na_ctab"][l, h, :, mm0:mm0 + 8, :].rearrange("c m q -> c (m q)")),
                                    reads=[R["na_ctab"]], writes=[bb])
                            mi = NA_MASK_IDX[(g, kt)]
                            S.dma("sp", lambda e: e.dma_start(out=mm[:], in_=R["na_mask"][mi]), reads=[R["na_mask"]], writes=[mm])
                            S.op("pool", lambda e: e.tensor_tensor(out=bb[:], in0=bb[:], in1=mm[:], op=ALU.add),
                                 reads=[bb, mm], writes=[bb])
                            return bb
                        bias_fn.n = nb
                    else:
                        ktl = list(range(nkt))
                        bias_fn = None
                    _attn(S, A,
                          qparts=[(qT, lambda q0, n: qT[:, q0:q0 + n])],
                          kparts=[(kT, lambda kt: kT[:, kt * 128:(kt + 1) * 128])],
                          v_ap=lambda kt: (vall, vall[:, kt, h * 128:(h + 1) * 128]),
                          ktl=ktl, q0=g * QB, QB=QB, scale=128 ** -0.5, bias_fn=bias_fn,
                          out_dst=(brT[3], brT[3][h * 128:(h + 1) * 128, tok0 + g * QB:tok0 + (g + 1) * QB]))
                    if latent:
                        nb = bias_fn.n
        S.barrier()
    S.scope = S.es


def stage_merge(S, l, R, x_src):
    ident = R["ident"]
    P_T, brT, mrg, xbuf, modd = R["P_T"], R["brT"], R["mrg"], R["xbuf"], R["modd"]
    ag, _ = T_COLS["gate"]
    for b in range(4):
        with ExitStack() as sc:
            S.scope = sc
            wbr = S.sbuf("wbr", [128, 8, 2048], BF16)
            S.dma("pool", lambda e: e.dma_start(out=wbr[:], in_=R["w_branch"][l, b].rearrange("(k p) c -> p k c", p=128)),
                  reads=[R["w_branch"]], writes=[wbr])
            brt = [S.sbuf("brt%d" % i, [128, 8, 128], BF16) for i in range(2)]
            gt = [S.sbuf("gt%d" % i, [128, 2048], F32) for i in range(2)]
            acc = [S.sbuf("acc%d" % i, [128, 2048], F32) for i in range(2)]
            pm = [S.psum("pm%d" % i, [128, 512], F32) for i in range(4)]
            ev = 0
            for t in range(NTILE):
                bt, g, a = brt[t % 2], gt[t % 2], acc[t % 2]
                S.dma("sp", lambda e: e.dma_start(out=bt[:], in_=brT[b][:, t * 128:(t + 1) * 128].rearrange("(k p) t -> p k t", p=128)),
                      reads=[brT[b]], writes=[bt])
                S.dma("sp", lambda e: e.dma_start(out=g[:], in_=P_T[t * 128:(t + 1) * 128, ag + b * 2048:ag + (b + 1) * 2048]),
                      reads=[P_T], writes=[g])
                if b > 0:
                    S.dma("sp", lambda e: e.dma_start(out=a[:], in_=mrg[t * 128:(t + 1) * 128, :]), reads=[mrg], writes=[a])
                S.op("act", lambda e: e.activation(out=g[:], in_=g[:], func=AF.Sigmoid), reads=[g], writes=[g])
                for nb in range(4):
                    ps = pm[ev % 4]
                    ev += 1
                    for kc in range(8):
                        S.op("pe", lambda e: e.matmul(ps[:, :], lhsT=bt[:, kc, :], rhs=wbr[:, kc, nb * 512:(nb + 1) * 512],
                                                      start=(kc == 0), stop=(kc == 7)), reads=[bt, wbr], writes=[ps])
                    S.op("dve", lambda e: e.tensor_tensor(out=g[:, nb * 512:(nb + 1) * 512], in0=ps[:, :],
                                                          in1=g[:, nb * 512:(nb + 1) * 512], op=ALU.mult),
                         reads=[ps, g], writes=[g])
                if b > 0:
                    S.op("pool", lambda e: e.tensor_tensor(out=g[:], in0=g[:], in1=a[:], op=ALU.add), reads=[g, a], writes=[g])
                S.dma("sp", lambda e: e.dma_start(out=mrg[t * 128:(t + 1) * 128, :], in_=g[:]), reads=[g], writes=[mrg])
            S.barrier()
    with ExitStack() as sc:
        S.scope = sc
        wout = S.sbuf("wout", [128, 16, 2048], BF16)
        S.dma("pool", lambda e: e.dma_start(out=wout[:], in_=R["w_out"][l].rearrange("(k p) c -> p k c", p=128)),
              reads=[R["w_out"]], writes=[wout])
        g1b = [S.sbuf("g1b%d" % c, [128, 2048], F32) for c in range(2)]
        for c in range(2):
            S.dma("sp", lambda e: e.dma_start(out=g1b[c][:], in_=modd[c, 2 * D:3 * D].partition_broadcast(128)),
                  reads=[modd], writes=[g1b[c]])
        mt = [S.sbuf("mt%d" % i, [128, 2048], F32) for i in range(2)]
        mb = [S.sbuf("mb%d" % i, [128, 2048], BF16) for i in range(2)]
        mT = [S.sbuf("mT%d" % i, [128, 16, 128], BF16) for i in range(2)]
        xt = [S.sbuf("xo%d" % i, [128, 2048], F32) for i in range(2)]
        ptr = [S.psum("optr%d" % i, [128, 8, 128], BF16) for i in range(2)]
        pm = [S.psum("pmo%d" % i, [128, 512], F32) for i in range(4)]
        ev = 0
        for t in range(NTILE):
            c = 0 if t < (NCTX * LC) // 128 else 1
            m, mbb, mTT, x = mt[t % 2], mb[t % 2], mT[t % 2], xt[t % 2]
            S.dma("sp", lambda e: e.dma_start(out=m[:], in_=mrg[t * 128:(t + 1) * 128, :]), reads=[mrg], writes=[m])
            S.dma("sp", lambda e: e.dma_start(out=x[:], in_=x_src[t * 128:(t + 1) * 128, :]), reads=[x_src], writes=[x])
            S.op("pool", lambda e: e.tensor_copy(out=mbb[:], in_=m[:]), reads=[m], writes=[mbb])
            for half in range(2):
                pt = ptr[half]
                for j in range(8):
                    kc = half * 8 + j
                    S.op("pe", lambda e: e.transpose(pt[:, j, :], mbb[:, kc * 128:(kc + 1) * 128], ident[:]),
                         reads=[mbb, ident], writes=[pt])
                _evac(S, half, mTT[:, half * 8:(half + 1) * 8, :], pt[:, :, :], reads=[pt], writes=[mTT])
            for nb in range(4):
                ps = pm[ev % 4]
                ev += 1
                for kc in range(16):
                    S.op("pe", lambda e: e.matmul(ps[:, :], lhsT=mTT[:, kc, :], rhs=wout[:, kc, nb * 512:(nb + 1) * 512],
                                                  start=(kc == 0), stop=(kc == 15)), reads=[mTT, wout], writes=[ps])
                S.op("dve", lambda e: e.tensor_tensor(out=m[:, nb * 512:(nb + 1) * 512], in0=ps[:, :],
                                                      in1=g1b[c][:, nb * 512:(nb + 1) * 512], op=ALU.mult),
                     reads=[ps, g1b[c]], writes=[m])
            S.op("pool", lambda e: e.tensor_tensor(out=x[:], in0=x[:], in1=m[:], op=ALU.add), reads=[x, m], writes=[x])
            S.dma("sp", lambda e: e.dma_start(out=xbuf[t * 128:(t + 1) * 128, :], in_=x[:]), reads=[x], writes=[xbuf])
        S.barrier()
    S.scope = S.es


def stage_peer(S, l, R, last):
    ident = R["ident"]
    xbuf, modd = R["xbuf"], R["modd"]
    with ExitStack() as sc:
        S.scope = sc
        wq = S.sbuf("pwq", [128, 16, 2048], BF16)
        S.dma("pool", lambda e: e.dma_start(out=wq[:], in_=R["peer_wq"][l].rearrange("(k p) c -> p k c", p=128)),
              reads=[R["peer_wq"]], writes=[wq])
        identf = S.sbuf("identf", [128, 128], F32)
        S.dma("sp", lambda e: e.dma_start(out=identf[:], in_=R["ident_in"][:, :]), reads=[R["ident_in"]], writes=[identf])
        keysT = S.sbuf("keysT", [128, 2, 128], F32)
        S.dma("sp", lambda e: e.dma_start(out=keysT[:], in_=R["peer_keysT"][l].rearrange("s c n -> c s n")),
              reads=[R["peer_keysT"]], writes=[keysT])
        gm2 = S.sbuf("gm2", [128, 2048], F32)
        sh2 = S.sbuf("sh2", [128, 2048], F32)
        x1 = S.sbuf("px1", [128, 2048], F32)
        h2 = S.sbuf("ph2", [128, 2048], F32)
        h2b = S.sbuf("ph2b", [128, 2048], BF16)
        h2T = S.sbuf("ph2T", [128, 16, 128], BF16)
        qTf = S.sbuf("pqTf", [128, 16, 128], F32)
        scr = S.sbuf("pscr", [128, 16, 128], F32)
        scw = S.sbuf("pscw", [128, 128], F32)
        vals = S.sbuf("pvals", [128, 16, 16], F32)
        idx = S.sbuf("pidx", [128, 16, 16], U32)
        idxf = S.sbuf("pidxf", [128, 16, 16], F32)
        cand = S.sbuf("pcand", [128, 256], F32)
        candw = S.sbuf("pcandw", [128, 256], F32)
        cid = S.sbuf("pcid", [128, 256], F32)
        bs = S.sbuf("pbs", [128, 8, 16], F32)
        bj = S.sbuf("pbj", [128, 16], U32)
        bjf = S.sbuf("pbjf", [128, 16], F32)
        iota = S.sbuf("piota", [128, 256], F32)
        S.dma("sp", lambda e: e.dma_start(out=iota[:], in_=R["iota256"][0, :].partition_broadcast(128)),
              reads=[R["iota256"]], writes=[iota])
        eq = S.sbuf("peq", [128, 8, 256], F32)
        eidf = S.sbuf("peidf", [128, 8, 16], F32)
        eidi = S.sbuf("peidi", [128, 128], I32)
        negb = S.sbuf("pnegb", [128, 8], F32)
        zs = S.sbuf("pzs", [128, 8], F32)
        gat = S.sbuf("pgat", [128, 8, 16], F32)
        actv = S.sbuf("pact", [128, 128], F32)
        coef = S.sbuf("pcoef", [128, 128], F32)
        yacc = S.sbuf("pyacc", [128, 2048], F32)
        qf = yacc
        jk = S.sbuf("pjk", [128, 2048], F32)
        st = S.sbuf("pst", [128, 1], F32)
        ug = [S.sbuf("pug%d" % i, [128, 2048], BF16) for i in range(8)]
        ptr = [S.psum("pptr%d" % i, [128, 8, 128], BF16) for i in range(2)]
        pm = [S.psum("ppm%d" % i, [128, 512], F32) for i in range(4)]
        cur_c = -1
        for t in range(NTILE_PEER if NTILE_PEER else NTILE):
            c = 0 if t < (NCTX * LC) // 128 else 1
            if c != cur_c:
                cur_c = c
                S.dma("sp", lambda e: e.dma_start(out=sh2[:], in_=modd[c, 3 * D:4 * D].partition_broadcast(128)),
                      reads=[modd], writes=[sh2])
                S.dma("sp", lambda e: e.dma_start(out=gm2[:], in_=modd[c, 4 * D:5 * D].partition_broadcast(128)),
                      reads=[modd], writes=[gm2])
                S.dma("sp", lambda e: e.dma_start(out=jk[:], in_=R["norm2_g"][l, :].partition_broadcast(128)),
                      reads=[R["norm2_g"]], writes=[jk])
                S.op("dve", lambda e: e.scalar_tensor_tensor(out=gm2[:], in0=gm2[:], scalar=1.0, in1=jk[:],
                                                             op0=ALU.add, op1=ALU.mult), reads=[gm2, jk], writes=[gm2])
            S.dma("sp", lambda e: e.dma_start(out=x1[:], in_=xbuf[t * 128:(t + 1) * 128, :]), reads=[xbuf], writes=[x1])
            S.op("act", lambda e: e.activation(out=jk[:], in_=x1[:], func=AF.Square, accum_out=st[:]),
                 reads=[x1], writes=[jk, st])
            S.op("act", lambda e: e.activation(out=st[:], in_=st[:], func=AF.Sqrt, scale=1.0 / D, bias=EPS),
                 reads=[st], writes=[st])
            S.op("dve", lambda e: e.reciprocal(out=st[:], in_=st[:]), reads=[st], writes=[st])
            S.op("dve", lambda e: e.scalar_tensor_tensor(out=h2[:], in0=x1[:], scalar=st[:, 0:1], in1=gm2[:],
                                                         op0=ALU.mult, op1=ALU.mult), reads=[x1, st, gm2], writes=[h2])
            S.op("dve", lambda e: e.tensor_tensor(out=h2[:], in0=h2[:], in1=sh2[:], op=ALU.add), reads=[h2, sh2], writes=[h2])
            S.op("act", lambda e: e.activation(out=h2b[:], in_=h2[:], func=AF.Copy), reads=[h2], writes=[h2b])
            for half in range(2):
                pt = ptr[half]
                for j in range(8):
                    kc = half * 8 + j
                    S.op("pe", lambda e: e.transpose(pt[:, j, :], h2b[:, kc * 128:(kc + 1) * 128], ident[:]),
                         reads=[h2b, ident], writes=[pt])
                _evac(S, half, h2T[:, half * 8:(half + 1) * 8, :], pt[:, :, :], reads=[pt], writes=[h2T])
            for nb in range(4):
                ps = pm[nb]
                for kc in range(16):
                    S.op("pe", lambda e: e.matmul(ps[:, :], lhsT=h2T[:, kc, :], rhs=wq[:, kc, nb * 512:(nb + 1) * 512],
                                                  start=(kc == 0), stop=(kc == 15)), reads=[h2T, wq], writes=[ps])
                _evac(S, nb, qf[:, nb * 512:(nb + 1) * 512], ps[:, :], reads=[ps], writes=[qf])
            for b4 in range(4):
                ps = pm[b4]
                for j in range(4):
                    ch = b4 * 4 + j
                    S.op("pe", lambda e: e.transpose(ps[:, j * 128:(j + 1) * 128], qf[:, ch * 128:(ch + 1) * 128], identf[:]),
                         reads=[qf, identf], writes=[ps])
                _evac(S, b4, qTf[:, b4 * 4:(b4 + 1) * 4, :], ps[:, :].rearrange("p (a b) -> p a b", a=4), reads=[ps], writes=[qTf])
            for b4 in range(4):
                ps = pm[b4]
                for j in range(4):
                    ch = b4 * 4 + j
                    S.op("pe", lambda e: e.matmul(ps[:, j * 128:(j + 1) * 128], lhsT=qTf[:, ch, :], rhs=keysT[:, ch % 2, :],
                                                  start=True, stop=True), reads=[qTf, keysT], writes=[ps])
                _evac(S, b4 + 1, scr[:, b4 * 4:(b4 + 1) * 4, :], ps[:, :].rearrange("p (a b) -> p a b", a=4), reads=[ps], writes=[scr])
            for ch in range(16):
                S.op("dve", lambda e: e.max(out=vals[:, ch, 0:8], in_=scr[:, ch, :]), reads=[scr], writes=[vals])
                S.op("dve", lambda e: e.match_replace(out=scw[:, :], in_to_replace=vals[:, ch, 0:8], in_values=scr[:, ch, :],
                                                      imm_value=-1e30), reads=[vals, scr], writes=[scw])
                S.op("dve", lambda e: e.max(out=vals[:, ch, 8:16], in_=scw[:, :]), reads=[scw], writes=[vals])
                S.op("dve", lambda e: e.max_index(out=idx[:, ch, 0:8], in_max=vals[:, ch, 0:8], in_values=scr[:, ch, :]),
                     reads=[vals, scr], writes=[idx])
                S.op("dve", lambda e: e.max_index(out=idx[:, ch, 8:16], in_max=vals[:, ch, 8:16], in_values=scw[:, :]),
                     reads=[vals, scw], writes=[idx])
            S.op("dve", lambda e: e.tensor_copy(out=idxf[:], in_=idx[:]), reads=[idx], writes=[idxf])
            for h in range(8):
                c3 = cand[:, :].rearrange("p (a b) -> p a b", a=16)
                i3 = cid[:, :].rearrange("p (a b) -> p a b", a=16)
                S.op("dve", lambda e: e.tensor_tensor(out=c3, in0=vals[:, 2 * h, :].unsqueeze(2).to_broadcast([128, 16, 16]),
                                                      in1=vals[:, 2 * h + 1, :].unsqueeze(1).to_broadcast([128, 16, 16]),
                                                      op=ALU.add), reads=[vals], writes=[cand])
                S.op("dve", lambda e: e.scalar_tensor_tensor(out=i3, in0=idxf[:, 2 * h, :].unsqueeze(2).to_broadcast([128, 16, 16]),
                                                             scalar=128.0,
                                                             in1=idxf[:, 2 * h + 1, :].unsqueeze(1).to_broadcast([128, 16, 16]),
                                                             op0=ALU.mult, op1=ALU.add), reads=[idxf], writes=[cid])
                S.op("dve", lambda e: e.max(out=bs[:, h, 0:8], in_=cand[:, :]), reads=[cand], writes=[bs])
                S.op("dve", lambda e: e.match_replace(out=candw[:, :], in_to_replace=bs[:, h, 0:8], in_values=cand[:, :],
                                                      imm_value=-1e30), reads=[bs, cand], writes=[candw])
                S.op("dve", lambda e: e.max(out=bs[:, h, 8:16], in_=candw[:, :]), reads=[candw], writes=[bs])
                S.op("dve", lambda e: e.max_index(out=bj[:, 0:8], in_max=bs[:, h, 0:8], in_values=cand[:, :]),
                     reads=[bs, cand], writes=[bj])
                S.op("dve", lambda e: e.max_index(out=bj[:, 8:16], in_max=bs[:, h, 8:16], in_values=candw[:, :]),
                     reads=[bs, candw], writes=[bj])
                S.op("dve", lambda e: e.tensor_copy(out=bjf[:], in_=bj[:]), reads=[bj], writes=[bjf])
                for hf in range(2):
                    S.op("dve", lambda e: e.tensor_tensor(out=eq[:], in0=iota[:, :].unsqueeze(1).to_broadcast([128, 8, 256]),
                                                          in1=bjf[:, hf * 8:(hf + 1) * 8].unsqueeze(2).to_broadcast([128, 8, 256]),
                                                          op=ALU.is_equal), reads=[iota, bjf], writes=[eq])
                    S.op("dve", lambda e: e.tensor_tensor(out=eq[:], in0=eq[:],
                                                          in1=cid[:, :].unsqueeze(1).to_broadcast([128, 8, 256]),
                                                          op=ALU.mult), reads=[eq, cid], writes=[eq])
                    S.op("dve", lambda e: e.tensor_reduce(out=eidf[:, h, hf * 8:(hf + 1) * 8], in_=eq[:], axis=AX.X, op=ALU.add),
                         reads=[eq], writes=[eidf])
            S.op("dve", lambda e: e.tensor_copy(out=eidi[:, :].rearrange("p (a b) -> p a b", a=8), in_=eidf[:]),
                 reads=[eidf], writes=[eidi])
            S.op("dve", lambda e: e.tensor_scalar(out=negb[:], in0=bs[:, :, 0], scalar1=-1.0, scalar2=None, op0=ALU.mult),
                 reads=[bs], writes=[negb])
            for h in range(8):
                S.op("act", lambda e: e.activation(out=gat[:, h, :], in_=bs[:, h, :], func=AF.Exp, bias=negb[:, h:h + 1],
                                                   accum_out=zs[:, h:h + 1]), reads=[bs, negb], writes=[gat, zs])
            S.op("dve", lambda e: e.reciprocal(out=zs[:], in_=zs[:]), reads=[zs], writes=[zs])
            S.op("dve", lambda e: e.tensor_tensor(out=gat[:], in0=gat[:], in1=zs[:, :].unsqueeze(2).to_broadcast([128, 8, 16]),
                                                  op=ALU.mult), reads=[gat, zs], writes=[gat])
            for s in range(128):
                u = ug[s % 8]
                S.dma("pool", lambda e: e.indirect_dma_start(
                    out=u[:], out_offset=None, in_=R["peer_ub"][l][:, :],
                    in_offset=bass.IndirectOffsetOnAxis(ap=eidi[:, s:s + 1], axis=0)),
                    reads=[R["peer_ub"][l], eidi], writes=[u])
                S.op("dve", lambda e: e.scalar_tensor_tensor(out=jk[:], in0=u[:], scalar=1.0, in1=h2[:],
                                                             op0=ALU.mult, op1=ALU.mult, accum_out=actv[:, s:s + 1]),
                     reads=[u, h2], writes=[jk, actv])
            S.op("act", lambda e: e.activation(out=actv[:], in_=actv[:], func=AF.Gelu), reads=[actv], writes=[actv])
            S.op("dve", lambda e: e.tensor_tensor(out=coef[:], in0=actv[:], in1=gat[:, :, :].rearrange("p a b -> p (a b)"),
                                                  op=ALU.mult), reads=[actv, gat], writes=[coef])
            for s in range(128):
                u = ug[s % 8]
                S.dma("pool", lambda e: e.indirect_dma_start(
                    out=u[:], out_offset=None, in_=R["peer_vb"][l][:, :],
                    in_offset=bass.IndirectOffsetOnAxis(ap=eidi[:, s:s + 1], axis=0)),
                    reads=[R["peer_vb"][l], eidi], writes=[u])
                if s == 0:
                    S.op("dve", lambda e: e.tensor_scalar(out=yacc[:], in0=u[:], scalar1=coef[:, 0:1], scalar2=None,
                                                          op0=ALU.mult), reads=[u, coef], writes=[yacc])
                else:
                    S.op("dve", lambda e: e.scalar_tensor_tensor(out=yacc[:], in0=u[:], scalar=coef[:, s:s + 1], in1=yacc[:],
                                                                 op0=ALU.mult, op1=ALU.add), reads=[u, coef, yacc], writes=[yacc])
            S.dma("sp", lambda e: e.dma_start(out=jk[:], in_=modd[c, 5 * D:6 * D].partition_broadcast(128)),
                  reads=[modd], writes=[jk])
            S.op("dve", lambda e: e.tensor_tensor(out=yacc[:], in0=yacc[:], in1=jk[:], op=ALU.mult), reads=[yacc, jk], writes=[yacc])
            S.op("dve", lambda e: e.tensor_tensor(out=x1[:], in0=x1[:], in1=yacc[:], op=ALU.add), reads=[x1, yacc], writes=[x1])
            if not last:
                S.dma("sp", lambda e: e.dma_start(out=xbuf[t * 128:(t + 1) * 128, :], in_=x1[:]), reads=[x1], writes=[xbuf])
            else:
                S.op("act", lambda e: e.activation(out=jk[:], in_=x1[:], func=AF.Square, accum_out=st[:]),
                     reads=[x1], writes=[jk, st])
                S.op("act", lambda e: e.activation(out=st[:], in_=st[:], func=AF.Sqrt, scale=1.0 / D, bias=EPS),
                     reads=[st], writes=[st])
                S.op("dve", lambda e: e.reciprocal(out=st[:], in_=st[:]), reads=[st], writes=[st])
                S.dma("sp", lambda e: e.dma_start(out=jk[:], in_=R["final_g"][0, :].partition_broadcast(128)),
                      reads=[R["final_g"]], writes=[jk])
                S.op("dve", lambda e: e.scalar_tensor_tensor(out=x1[:], in0=x1[:], scalar=st[:, 0:1], in1=jk[:],
                                                             op0=ALU.mult, op1=ALU.mult), reads=[x1, st, jk], writes=[x1])
                S.dma("sp", lambda e: e.dma_start(out=R["y_out"][t * 128:(t + 1) * 128, :], in_=x1[:]), reads=[x1], writes=[R["y_out"]])
        S.barrier()
    S.scope = S.es


def stage_dn(S, l, R):
    P_T, P_F, brT = R["P_T"], R["P_F"], R["brT"]
    fdn, _ = F_ROWS["dnT"]
    az, _ = T_COLS["z"]
    aa, _ = T_COLS["a"]
    with ExitStack() as sc:
        S.scope = sc
        cst = S.sbuf("dncst", [128, 10, 128], F32)
        S.dma("sp", lambda e: e.dma_start(out=cst[:], in_=R["dn_consts"][:, :, :].rearrange("m p q -> p m q")),
              reads=[R["dn_consts"]], writes=[cst])
        onesf = S.sbuf("dnones", [128, 128], F32)
        S.op("dve", lambda e: e.memset(onesf[:], 1.0), writes=[onesf])
        identf = S.sbuf("dnidentf", [128, 128], F32)
        S.dma("sp", lambda e: e.dma_start(out=identf[:], in_=R["dn_consts"][2]), reads=[R["dn_consts"]], writes=[identf])
        identb = R["ident"]
        cw = S.sbuf("dncw", [128, 24, 5], F32)
        S.dma("sp", lambda e: e.dma_start(out=cw[:], in_=R["dn_convT"][l]), reads=[R["dn_convT"]], writes=[cw])
        alog = S.sbuf("dnalog", [128, 16], F32)
        dtb = S.sbuf("dndtb", [128, 16], F32)
        S.dma("sp", lambda e: e.dma_start(out=alog[:], in_=R["dn_a_log"][l, :].partition_broadcast(128)),
              reads=[R["dn_a_log"]], writes=[alog])
        S.dma("sp", lambda e: e.dma_start(out=dtb[:], in_=R["dn_dt_bias"][l, :].partition_broadcast(128)),
              reads=[R["dn_dt_bias"]], writes=[dtb])
        S.op("act", lambda e: e.activation(out=alog[:], in_=alog[:], func=AF.Exp), reads=[alog], writes=[alog])
        gon = S.sbuf("dngon", [128, 128], F32)
        S.dma("sp", lambda e: e.dma_start(out=gon[:], in_=R["dn_out_norm"][l, :].partition_broadcast(128)),
              reads=[R["dn_out_norm"]], writes=[gon])
        NCH = LL // 128
        xin = S.sbuf("dnxin", [128, LL + 4], F32)
        acc = S.sbuf("dnacc", [128, LL], F32)
        qT = S.sbuf("dnqT", [128, LL], F32)
        kT = S.sbuf("dnkT", [128, LL], F32)
        vT = S.sbuf("dnvT", [128, LL], F32)
        ktok = S.sbuf("dnktok", [128, NCH, 128], F32)
        vtok = S.sbuf("dnvtok", [128, NCH, 128], F32)
        oall = S.sbuf("dnoall", [128, NCH, 1024], F32)
        gt = S.sbuf("dng", [128, NCH, 16], F32)
        bt = S.sbuf("dnb", [128, NCH, 16], F32)
        gc = S.sbuf("dngc", [128, NCH, 16], F32)
        egc = S.sbuf("dnegc", [128, NCH, 16], F32)
        bg = S.sbuf("dnbg", [128, NCH, 16], F32)
        egl = S.sbuf("dnegl", [128, NCH, 16], F32)
        edl = S.sbuf("dnedl", [128, NCH, 16], F32)
        ab = S.sbuf("dnab", [128, 32], F32)
        sq = S.sbuf("dnsq", [128, 512], F32)
        rn = S.sbuf("dnrn", [128, 512], F32)
        Sst = S.sbuf("dnS", [128, 128], F32)
        Bsets = []
        for j_ in range(3):
            B = {"j": j_}
            for nm_ in ("Gm", "EA", "ET", "Am", "Ad", "Aoff", "Boff", "qkT", "wT", "kd"):
                B[nm_] = S.sbuf("dn%s%d" % (nm_, j_), [128, 128], F32)
            B["Bk"] = [S.sbuf("dnB%d_%d" % (i, j_), [128, 128], F32) for i in range(6)]
            B["Ck"] = [S.sbuf("dnC%d_%d" % (i, j_), [128, 128], F32) for i in range(2)]
            B["X"] = S.sbuf("dnX%d" % j_, [128, 256], F32)
            B["Zb"] = S.sbuf("dnZb%d" % j_, [128, 256], F32)
            Bsets.append(B)
        vnew = S.sbuf("dnvnew", [128, 128], F32)
        tmp = S.sbuf("dntmp", [128, 128], F32)
        zt = S.sbuf("dnz", [128, 1024], F32)
        ob = S.sbuf("dnob", [128, 1024], BF16)
        obT = S.sbuf("dnobT", [128, 8, 128], BF16)
        ssq = S.sbuf("dnssq", [128, 8], F32)
        pqb = [S.psum("dnpqb%d" % i, [128, 512], F32) for i in range(7)]
        pbig = pqb[0:2]
        pq = [Buf(pqb[i].t[:, 0:128], "dnpq%d" % i, root=pqb[i]) for i in range(7)]
        pxs = [Buf(pqb[i].t[:, 0:256], "dnpx%d" % i, root=pqb[i]) for i in range(7)]
        ptr = S.psum("dnptr", [128, 8, 128], BF16)
        npq = [0]

        def PQ():
            npq[0] += 1
            return pq[npq[0] % 7]

        def PX():
            npq[0] += 1
            return pxs[npq[0] % 7]

        slot = [0, 0, 0]

        def PQs(j, wide=False):
            slot[j] += 1
            bank = pqb[3 * j + slot[j] % 3]
            w_ = 256 if wide else 128
            return Buf(bank.t[:, 0:w_], "dnps", root=bank)

        seqs = [(s * LC, LC, False) for s in range(NCTX)] + [(NCTX * LC, LL, True)]
        seqs = seqs[:DNSEQ]
        for si, (tok0, L, latent) in enumerate(seqs):
            nch = L // 128
            for n in range(nch):
                g0 = tok0 + n * 128
                S.dma("sp", lambda e: e.dma_start(out=ab[:], in_=P_T[g0:g0 + 128, aa:aa + 32]), reads=[P_T], writes=[ab])
                S.op("dve", lambda e: e.tensor_tensor(out=gt[:, n, :], in0=ab[:, 0:16], in1=dtb[:], op=ALU.add),
                     reads=[ab, dtb], writes=[gt])
                S.op("act", lambda e: e.activation(out=gt[:, n, :], in_=gt[:, n, :], func=AF.Exp), reads=[gt], writes=[gt])
                S.op("act", lambda e: e.activation(out=gt[:, n, :], in_=gt[:, n, :], func=AF.Ln, bias=1.0), reads=[gt], writes=[gt])
                S.op("dve", lambda e: e.scalar_tensor_tensor(out=gt[:, n, :], in0=gt[:, n, :], scalar=-1.0, in1=alog[:],
                                                             op0=ALU.mult, op1=ALU.mult), reads=[gt, alog], writes=[gt])
                S.op("act", lambda e: e.activation(out=bt[:, n, :], in_=ab[:, 16:32], func=AF.Sigmoid), reads=[ab], writes=[bt])
                p1 = PQ()
                for d in range(2):
                    S.op("pe", lambda e: e.matmul(p1[:, d * 8:(d + 1) * 8], lhsT=cst[:, d, :], rhs=gt[:, n, d * 8:(d + 1) * 8],
                                                  start=True, stop=True), reads=[cst, gt], writes=[p1])
                S.op("pe", lambda e: e.matmul(p1[:, 16:32], lhsT=onesf[:, :], rhs=gt[:, n, :], start=True, stop=True),
                     reads=[onesf, gt], writes=[p1])
                S.op("dve", lambda e: e.tensor_copy(out=gc[:, n, :], in_=p1[:, 0:16]), reads=[p1], writes=[gc])
                S.op("act", lambda e: e.activation(out=egc[:, n, :], in_=p1[:, 0:16], func=AF.Exp), reads=[p1], writes=[egc])
                S.op("act", lambda e: e.activation(out=egl[:, n, :], in_=p1[:, 16:32], func=AF.Exp), reads=[p1], writes=[egl])
                S.op("dve", lambda e: e.tensor_tensor(out=edl[:, n, :], in0=p1[:, 16:32], in1=gc[:, n, :], op=ALU.subtract),
                     reads=[p1, gc], writes=[edl])
                S.op("act", lambda e: e.activation(out=edl[:, n, :], in_=edl[:, n, :], func=AF.Exp), reads=[edl], writes=[edl])
                S.op("dve", lambda e: e.tensor_tensor(out=bg[:, n, :], in0=bt[:, n, :], in1=egc[:, n, :], op=ALU.mult),
                     reads=[bt, egc], writes=[bg])
            for h in range(8):
                if DNSTOP < 2:
                    continue
                for which, dst in ((0, qT), (1, kT), (2, vT)):
                    ch = which * 8 + h
                    r0 = fdn + ch * 128
                    S.op("pool", lambda e: e.memset(xin[:, 0:2], 0.0), writes=[xin])
                    S.op("pool", lambda e: e.memset(xin[:, L + 2:L + 4], 0.0), writes=[xin])
                    S.dma("sp", lambda e: e.dma_start(out=xin[:, 2:L + 2], in_=P_F[r0:r0 + 128, tok0:tok0 + L]),
                          reads=[P_F], writes=[xin])
                    S.op("dve", lambda e: e.tensor_scalar(out=acc[:, 0:L], in0=xin[:, 0:L], scalar1=cw[:, ch, 0:1], scalar2=None,
                                                          op0=ALU.mult), reads=[xin, cw], writes=[acc])
                    for kk in range(1, 5):
                        S.op("dve", lambda e: e.scalar_tensor_tensor(out=acc[:, 0:L], in0=xin[:, kk:kk + L],
                                                                     scalar=cw[:, ch, kk:kk + 1], in1=acc[:, 0:L],
                                                                     op0=ALU.mult, op1=ALU.add), reads=[xin, cw, acc], writes=[acc])
                    S.op("act", lambda e: e.activation(out=dst[:, 0:L], in_=acc[:, 0:L], func=AF.Silu), reads=[acc], writes=[dst])
                    if which < 2:
                        for g in range((L + 511) // 512):
                            w = min(512, L - g * 512)
                            pb = pbig[g % 2]
                            S.op("act", lambda e: e.activation(out=sq[:, 0:w], in_=dst[:, g * 512:g * 512 + w], func=AF.Square),
                                 reads=[dst], writes=[sq])
                            S.op("pe", lambda e: e.matmul(pb[:, 0:w], lhsT=onesf[:, :], rhs=sq[:, 0:w], start=True, stop=True),
                                 reads=[onesf, sq], writes=[pb])
                            sc_ = 128.0 if which == 0 else 1.0
                            S.op("act", lambda e: e.activation(out=rn[:, 0:w], in_=pb[:, 0:w], func=AF.Sqrt, scale=sc_,
                                                               bias=sc_ * EPS), reads=[pb], writes=[rn])
                            S.op("dve", lambda e: e.reciprocal(out=rn[:, 0:w], in_=rn[:, 0:w]), reads=[rn], writes=[rn])
                            S.op("dve", lambda e: e.tensor_tensor(out=dst[:, g * 512:g * 512 + w], in0=dst[:, g * 512:g * 512 + w],
                                                                  in1=rn[:, 0:w], op=ALU.mult), reads=[dst, rn], writes=[dst])
                if DNSTOP < 3:
                    continue
                for n in range(nch):
                    for src, dd in ((kT, ktok), (vT, vtok)):
                        p1 = PQ()
                        S.op("pe", lambda e: e.transpose(p1[:, 0:128], src[:, n * 128:(n + 1) * 128], identf[:]),
                             reads=[src, identf], writes=[p1])
                        _evac(S, n, dd[:, n, :], p1[:, 0:128], reads=[p1], writes=[dd])
                for d in range(2):
                    if DNSTOP < 4:
                        continue
                    dh = d * 8 + h
                    if latent:
                        S.dma("sp", lambda e: e.dma_start(out=Sst[:], in_=R["dn_state"][d][l, h, :, :]),
                              reads=[R["dn_state"][d]], writes=[Sst])
                    else:
                        S.op("dve", lambda e: e.memset(Sst[:], 0.0), writes=[Sst])
                    order = list(range(nch)) if d == 0 else list(range(nch - 1, -1, -1))

                    def prep(n, B, d=d, dh=dh):
                        c0 = n * 128
                        Gm, EA, ET, Am, Ad, Aoff, Boff, Bk, Ck, qkT, X, Zb = (B["Gm"], B["EA"], B["ET"], B["Am"], B["Ad"], B["Aoff"],
                                                                             B["Boff"], B["Bk"], B["Ck"], B["qkT"], B["X"], B["Zb"])
                        S.op("dve", lambda e: e.tensor_scalar(out=Gm[:], in0=cst[:, 3 + d, :], scalar1=gt[:, n, dh:dh + 1],
                                                              scalar2=None, op0=ALU.mult), reads=[cst, gt], writes=[Gm])
                        yield
                        J = B["j"]
                        pA, pT_ = PQs(J), PQs(J)
                        S.op("pe", lambda e: e.matmul(pA[:, :], lhsT=cst[:, d, :], rhs=Gm[:, :], start=True, stop=False),
                             reads=[cst, Gm], writes=[pA])
                        S.op("pe", lambda e: e.matmul(pA[:, :], lhsT=cst[:, 2, :], rhs=cst[:, 5 + d, :], start=False, stop=True),
                             reads=[cst], writes=[pA])
                        S.op("pe", lambda e: e.matmul(pT_[:, :], lhsT=Gm[:, :], rhs=cst[:, d, :], start=True, stop=False),
                             reads=[cst, Gm], writes=[pT_])
                        S.op("pe", lambda e: e.matmul(pT_[:, :], lhsT=cst[:, 2, :], rhs=cst[:, 7 + d, :], start=False, stop=True),
                             reads=[cst], writes=[pT_])
                        yield
                        S.op("act", lambda e: e.activation(out=EA[:], in_=pA[:, :], func=AF.Exp), reads=[pA], writes=[EA])
                        S.op("act", lambda e: e.activation(out=ET[:], in_=pT_[:, :], func=AF.Exp), reads=[pT_], writes=[ET])
                        yield
                        pkk, pkq = PQs(J), PQs(J)
                        S.op("pe", lambda e: e.matmul(pkk[:, :], lhsT=kT[:, c0:c0 + 128], rhs=kT[:, c0:c0 + 128], start=True,
                                                      stop=True), reads=[kT], writes=[pkk])
                        S.op("pe", lambda e: e.matmul(pkq[:, :], lhsT=kT[:, c0:c0 + 128], rhs=qT[:, c0:c0 + 128], start=True,
                                                      stop=True), reads=[kT, qT], writes=[pkq])
                        yield
                        S.op("dve", lambda e: e.scalar_tensor_tensor(out=Am[:], in0=pkk[:, :], scalar=bt[:, n, dh:dh + 1], in1=EA[:],
                                                                     op0=ALU.mult, op1=ALU.mult), reads=[pkk, bt, EA], writes=[Am])
                        S.op("dve", lambda e: e.tensor_tensor(out=qkT[:], in0=pkq[:, :], in1=ET[:], op=ALU.mult),
                             reads=[pkq, ET], writes=[qkT])
                        S.op("pool", lambda e: e.tensor_scalar(out=X[:, 0:128], in0=vtok[:, n, :], scalar1=bt[:, n, dh:dh + 1],
                                                               scalar2=None, op0=ALU.mult), reads=[vtok, bt], writes=[X])
                        S.op("pool", lambda e: e.tensor_scalar(out=X[:, 128:256], in0=ktok[:, n, :], scalar1=bg[:, n, dh:dh + 1],
                                                               scalar2=None, op0=ALU.mult), reads=[ktok, bg], writes=[X])
                        yield
                        if DNSTOP < 5:
                            return
                        S.op("dve", lambda e: e.tensor_tensor(out=Ad[:], in0=Am[:], in1=cst[:, 9, :], op=ALU.mult),
                             reads=[Am, cst], writes=[Ad])
                        S.op("pool", lambda e: e.tensor_tensor(out=Aoff[:], in0=Am[:], in1=Ad[:], op=ALU.subtract),
                             reads=[Am, Ad], writes=[Aoff])
                        yield
                        pt1, pt2 = PQs(J), PQs(J)
                        S.op("pe", lambda e: e.transpose(pt1[:, :], Ad[:, :], identf[:]), reads=[Ad, identf], writes=[pt1])
                        S.op("pe", lambda e: e.transpose(pt2[:, :], Aoff[:, :], identf[:]), reads=[Aoff, identf], writes=[pt2])
                        yield
                        S.op("act", lambda e: e.activation(out=Bk[0][:], in_=pt1[:, :], func=AF.Copy), reads=[pt1], writes=[Bk[0]])
                        S.op("act", lambda e: e.activation(out=Boff[:], in_=pt2[:, :], func=AF.Copy), reads=[pt2], writes=[Boff])
                        yield
                        Cc = Ad
                        for k_ in range(6):
                            Bc = Bk[k_]
                            pxx = PQs(J, True)
                            S.op("pe", lambda e: e.matmul(pxx[:, :], lhsT=Bc[:, :], rhs=X[:, :], start=True, stop=True),
                                 reads=[Bc, X], writes=[pxx])
                            if k_ < 5:
                                Bn, Cn = Bk[k_ + 1], Ck[(k_ + 1) % 2]
                                pb_, pc_ = PQs(J), PQs(J)
                                S.op("pe", lambda e: e.matmul(pb_[:, :], lhsT=Cc[:, :], rhs=Bc[:, :], start=True, stop=True),
                                     reads=[Cc, Bc], writes=[pb_])
                                S.op("pe", lambda e: e.matmul(pc_[:, :], lhsT=Bc[:, :], rhs=Cc[:, :], start=True, stop=True),
                                     reads=[Cc, Bc], writes=[pc_])
                            yield
                            S.op("dve", lambda e: e.tensor_tensor(out=X[:], in0=X[:], in1=pxx[:, :],
                                                                  op=(ALU.subtract if k_ == 0 else ALU.add)),
                                 reads=[X, pxx], writes=[X])
                            if k_ < 5:
                                S.op("act", lambda e: e.activation(out=Bn[:], in_=pb_[:, :], func=AF.Copy), reads=[pb_], writes=[Bn])
                                S.op("dve", lambda e: e.tensor_copy(out=Cn[:], in_=pc_[:, :]), reads=[pc_], writes=[Cn])
                                Cc = Cn
                            yield
                        pz = PQs(J, True)
                        S.op("pe", lambda e: e.matmul(pz[:, :], lhsT=Boff[:, :], rhs=X[:, :], start=True, stop=True),
                             reads=[Boff, X], writes=[pz])
                        yield
                        S.op("act", lambda e: e.activation(out=Zb[:], in_=pz[:, :], func=AF.Copy), reads=[pz], writes=[Zb])
                        yield
                        for k_ in range(6):
                            pxx = PQs(J, True)
                            S.op("pe", lambda e: e.matmul(pxx[:, :], lhsT=Bk[k_][:, :], rhs=Zb[:, :], start=True, stop=True),
                                 reads=[Bk[k_], Zb], writes=[pxx])
                            yield
                            S.op("dve", lambda e: e.tensor_tensor(out=Zb[:], in0=Zb[:], in1=pxx[:, :],
                                                                  op=(ALU.subtract if k_ == 0 else ALU.add)),
                                 reads=[Zb, pxx], writes=[Zb])
                            yield
                        S.op("dve", lambda e: e.tensor_tensor(out=X[:], in0=X[:], in1=Zb[:], op=ALU.subtract),
                             reads=[X, Zb], writes=[X])
                        yield
                        pw = PQs(J)
                        S.op("pe", lambda e: e.transpose(pw[:, :], X[:, 128:256], identf[:]), reads=[X, identf], writes=[pw])
                        yield
                        S.op("act", lambda e: e.activation(out=B["wT"][:], in_=pw[:, :], func=AF.Copy), reads=[pw], writes=[B["wT"]])
                        S.op("pool", lambda e: e.tensor_scalar(out=B["kd"][:], in0=ktok[:, n, :], scalar1=edl[:, n, dh:dh + 1],
                                                               scalar2=None, op0=ALU.mult), reads=[ktok, edl], writes=[B["kd"]])

                    def scan(n, B, d=d, dh=dh):
                        c0 = n * 128
                        X, qkT, wT, kd = B["X"], B["qkT"], B["wT"], B["kd"]
                        p1, p2, p3, p4 = PQ(), PQ(), PQ(), PQ()
                        S.op("pe", lambda e: e.matmul(p1[:, :], lhsT=wT[:, :], rhs=Sst[:, :], start=True, stop=True),
                             reads=[wT, Sst], writes=[p1])
                        S.op("pe", lambda e: e.matmul(p2[:, :], lhsT=qT[:, c0:c0 + 128], rhs=Sst[:, :], start=True, stop=True),
                             reads=[qT, Sst], writes=[p2])
                        S.op("dve", lambda e: e.tensor_tensor(out=vnew[:], in0=X[:, 0:128], in1=p1[:, :], op=ALU.subtract),
                             reads=[X, p1], writes=[vnew])
                        S.op("pe", lambda e: e.matmul(p3[:, :], lhsT=qkT[:, :], rhs=vnew[:, :], start=True, stop=True),
                             reads=[qkT, vnew], writes=[p3])
                        S.op("pe", lambda e: e.matmul(p4[:, :], lhsT=kd[:, :], rhs=vnew[:, :], start=True, stop=True),
                             reads=[kd, vnew], writes=[p4])
                        S.op("dve", lambda e: e.scalar_tensor_tensor(out=Sst[:], in0=Sst[:], scalar=egl[:, n, dh:dh + 1], in1=p4[:, :],
                                                                     op0=ALU.mult, op1=ALU.add), reads=[Sst, egl, p4], writes=[Sst])
                        S.op("dve", lambda e: e.tensor_scalar(out=tmp[:], in0=p2[:, :], scalar1=egc[:, n, dh:dh + 1], scalar2=None,
                                                              op0=ALU.mult), reads=[p2, egc], writes=[tmp])
                        osl = oall[:, n, h * 128:(h + 1) * 128]
                        if d == 0:
                            S.op("pool" if False else "dve", lambda e: e.tensor_tensor(out=osl, in0=tmp[:], in1=p3[:, :], op=ALU.add),
                                 reads=[tmp, p3], writes=[oall])
                        else:
                            S.op("dve", lambda e: e.tensor_tensor(out=tmp[:], in0=tmp[:], in1=p3[:, :], op=ALU.add),
                                 reads=[tmp, p3], writes=[tmp])
                            S.op("pool", lambda e: e.tensor_tensor(out=osl, in0=osl, in1=tmp[:], op=ALU.add),
                                 reads=[tmp, oall], writes=[oall])

                    NG = 2
                    for g0 in range(0, nch, NG):
                        grp = order[g0:g0 + NG]
                        gens = [prep(n, Bsets[j]) for j, n in enumerate(grp)]
                        alive = list(gens)
                        while alive:
                            for g_ in list(alive):
                                try:
                                    next(g_)
                                except StopIteration:
                                    alive.remove(g_)
                        if DNSTOP < 6:
                            continue
                        for j, n in enumerate(grp):
                            scan(n, Bsets[j])
                    if not latent:
                        ost = R["o_dn"][d]
                        S.dma("sp", lambda e: e.dma_start(out=ost[si, l, h, :, :], in_=Sst[:]), reads=[Sst], writes=[ost])
            for n in range(nch):
                if DNSTOP < 7:
                    continue
                g0 = tok0 + n * 128
                S.dma("sp", lambda e: e.dma_start(out=zt[:], in_=P_T[g0:g0 + 128, az:az + 1024]), reads=[P_T], writes=[zt])
                S.op("act", lambda e: e.activation(out=zt[:], in_=zt[:], func=AF.Silu), reads=[zt], writes=[zt])
                o3 = oall[:, n, :].rearrange("p (h v) -> p h v", h=8)
                for h in range(8):
                    S.op("act", lambda e: e.activation(out=tmp[:], in_=oall[:, n, h * 128:(h + 1) * 128], func=AF.Square,
                                                       accum_out=ssq[:, h:h + 1]), reads=[oall], writes=[tmp, ssq])
                S.op("act", lambda e: e.activation(out=ssq[:], in_=ssq[:], func=AF.Sqrt, scale=1.0 / 128, bias=EPS),
                     reads=[ssq], writes=[ssq])
                S.op("dve", lambda e: e.reciprocal(out=ssq[:], in_=ssq[:]), reads=[ssq], writes=[ssq])
                S.op("dve", lambda e: e.tensor_tensor(out=o3, in0=o3, in1=ssq[:, :].unsqueeze(2).to_broadcast([128, 8, 128]),
                                                      op=ALU.mult), reads=[oall, ssq], writes=[oall])
                S.op("dve", lambda e: e.tensor_tensor(out=o3, in0=o3, in1=gon[:, :].unsqueeze(1).to_broadcast([128, 8, 128]),
                                                      op=ALU.mult), reads=[oall, gon], writes=[oall])
                S.op("dve", lambda e: e.tensor_tensor(out=ob[:], in0=oall[:, n, :], in1=zt[:], op=ALU.mult),
                     reads=[oall, zt], writes=[ob])
                for h in range(8):
                    S.op("pe", lambda e: e.transpose(ptr[:, h, :], ob[:, h * 128:(h + 1) * 128], identb[:]),
                         reads=[ob, identb], writes=[ptr])
                _evac(S, n, obT[:], ptr[:, :, :], reads=[ptr], writes=[obT])
                S.dma("sp", lambda e: e.dma_start(out=brT[1][:, g0:g0 + 128].rearrange("(h p) t -> p h t", p=128), in_=obT[:]),
                      reads=[obT], writes=[brT[1]])
        S.barrier()
    S.scope = S.es


def _hy_geom(L):
    nf = L + 1
    KT = (nf + 127) // 128
    NB = (nf + 511) // 512
    return nf, KT, NB


def stage_hyena(S, l, R):
    P_F, brT = R["P_F"], R["brT"]
    ident = R["ident"]
    fhy, _ = F_ROWS["hyT"]
    hyZ, hyY1, hyU, hyPQ, hyYS = R["hyZ"], R["hyY1"], R["hyU"], R["hyPQ"], R["hyYS"]
    PI = float(np.pi)
    seqs = [(s * LC, LC, False) for s in range(NCTX)] + [(NCTX * LC, LL, True)]
    for (Lx, tabi, seq_list) in ((LC, 0, seqs[:NCTX]), (LL, 1, seqs[NCTX:])):
        L = Lx
        nf, KT, NB = _hy_geom(L)
        KU = L // 128
        tab = R["hy_tab"][tabi]
        with ExitStack() as sc:
            S.scope = sc
            w1 = S.sbuf("hyw1", [33, 64], F32)
            w2 = S.sbuf("hyw2", [64, 64], F32)
            w3 = S.sbuf("hyw3", [64, 4096], F32)
            b12 = S.sbuf("hyb12", [64, 2], F32)
            S.dma("sp", lambda e: e.dma_start(out=w1[:], in_=R["hy_w1"][l]), reads=[R["hy_w1"]], writes=[w1])
            S.dma("sp", lambda e: e.dma_start(out=w2[:], in_=R["hy_w2"][l]), reads=[R["hy_w2"]], writes=[w2])
            S.dma("sp", lambda e: e.dma_start(out=w3[:], in_=R["hy_w3"][l]), reads=[R["hy_w3"]], writes=[w3])
            S.dma("sp", lambda e: e.dma_start(out=b12[:], in_=R["hy_b12"][l]), reads=[R["hy_b12"]], writes=[b12])
            zT = S.sbuf("hyzT", [33, L], F32)
            S.dma("sp", lambda e: e.dma_start(out=zT[:], in_=R["hy_zemb"][tabi][:, :]), reads=[R["hy_zemb"][tabi]], writes=[zT])
            wfs = S.sbuf("hywf", [128, KT], F32)
            S.dma("sp", lambda e: e.dma_start(out=wfs[:], in_=R["hy_wf"][tabi][:, :]), reads=[R["hy_wf"][tabi]], writes=[wfs])
            h1 = S.sbuf("hyh1", [64, L], F32)
            h2 = S.sbuf("hyh2", [64, L], F32)
            xa = S.sbuf("hyxa", [64, 512], F32)
            xb_ = S.sbuf("hyxb", [64, 512], F32)
            xc = S.sbuf("hyxc", [64, 512], F32)
            win = S.sbuf("hywin", [128, 1024], F32)
            hs = S.sbuf("hyhs", [128, KU, 1024], BF16)
            hd = S.sbuf("hyhd", [128, KU, 1024], BF16)
            tf = [S.sbuf("hytf%d" % i, [128, 512], F32) for i in range(2)]
            slab = [S.sbuf("hyslab%d" % i, [128, KT, 512], BF16) for i in range(2)]
            pst = S.sbuf("hypst", [128, 512], F32)
            pm = [S.psum("hypm%d" % i, [128, 512], F32) for i in range(4)]
            npm = [0]

            def PM():
                npm[0] += 1
                return pm[npm[0] % 4]

            def sin_layer(dst, wmat, kdim, src, bcol):
                for g in range((L + 511) // 512):
                    w = min(512, L - g * 512)
                    ps = PM()
                    S.op("pe", lambda e: e.matmul(ps[0:64, 0:w], lhsT=wmat[0:kdim, :], rhs=src[0:kdim, g * 512:g * 512 + w],
                                                  start=True, stop=True), reads=[wmat, src], writes=[ps])
                    S.op("dve", lambda e: e.tensor_scalar(out=xa[:, 0:w], in0=ps[0:64, 0:w], scalar1=b12[:, bcol:bcol + 1],
                                                          scalar2=None, op0=ALU.add), reads=[ps, b12], writes=[xa])
                    S.op("dve", lambda e: e.tensor_scalar(out=xb_[:, 0:w], in0=xa[:, 0:w], scalar1=PI, scalar2=-2 * PI,
                                                          op0=ALU.is_gt, op1=ALU.mult), reads=[xa], writes=[xb_])
                    S.op("dve", lambda e: e.tensor_scalar(out=xc[:, 0:w], in0=xa[:, 0:w], scalar1=-PI, scalar2=2 * PI,
                                                          op0=ALU.is_lt, op1=ALU.mult), reads=[xa], writes=[xc])
                    S.op("dve", lambda e: e.tensor_tensor(out=xa[:, 0:w], in0=xa[:, 0:w], in1=xb_[:, 0:w], op=ALU.add),
                         reads=[xa, xb_], writes=[xa])
                    S.op("dve", lambda e: e.tensor_tensor(out=xa[:, 0:w], in0=xa[:, 0:w], in1=xc[:, 0:w], op=ALU.add),
                         reads=[xa, xc], writes=[xa])
                    S.op("act", lambda e: e.activation(out=dst[:, g * 512:g * 512 + w], in_=xa[:, 0:w], func=AF.Sin),
                         reads=[xa], writes=[dst])

            sin_layer(h1, w1, 33, zT, 0)
            sin_layer(h2, w2, 64, h1, 1)
            for o in range(2):
                for t in range(KU):
                    S.dma("sp", lambda e: e.dma_start(out=win[:], in_=R["hy_win"][tabi][t * 128:(t + 1) * 128, :]),
                          reads=[R["hy_win"][tabi]], writes=[win])
                    for cb in range(2):
                        pf, pb = PM(), PM()
                        cf = (o * 2 + 0) * 1024 + cb * 512
                        cbk = (o * 2 + 1) * 1024 + cb * 512
                        S.op("pe", lambda e: e.matmul(pf[:, :], lhsT=h2[:, t * 128:(t + 1) * 128], rhs=w3[:, cf:cf + 512],
                                                      start=True, stop=True), reads=[h2, w3], writes=[pf])
                        S.op("pe", lambda e: e.matmul(pb[:, :], lhsT=h2[:, t * 128:(t + 1) * 128], rhs=w3[:, cbk:cbk + 512],
                                                      start=True, stop=True), reads=[h2, w3], writes=[pb])
                        S.op("dve", lambda e: e.tensor_tensor(out=tf[0][:], in0=pf[:, :], in1=win[:, cb * 512:(cb + 1) * 512],
                                                              op=ALU.mult), reads=[pf, win], writes=[tf[0]])
                        S.op("dve", lambda e: e.tensor_tensor(out=tf[1][:], in0=pb[:, :], in1=win[:, cb * 512:(cb + 1) * 512],
                                                              op=ALU.mult), reads=[pb, win], writes=[tf[1]])
                        if t == 0:
                            S.op("dve", lambda e: e.memset(tf[1][0:1, :], 0.0), writes=[tf[1]])
                        S.op("dve", lambda e: e.tensor_tensor(out=hs[:, t, cb * 512:(cb + 1) * 512], in0=tf[0][:], in1=tf[1][:],
                                                              op=ALU.add), reads=[tf[0], tf[1]], writes=[hs])
                        S.op("pool", lambda e: e.tensor_tensor(out=hd[:, t, cb * 512:(cb + 1) * 512], in0=tf[0][:], in1=tf[1][:],
                                                               op=ALU.subtract), reads=[tf[0], tf[1]], writes=[hd])
                for fb in range(NB):
                    for cs in range(2):
                        S.dma("sp", lambda e: e.dma_start(out=slab[cs][:], in_=tab[cs, fb]), reads=[tab], writes=[slab[cs]])
                    for j in range(4):
                        m = fb * 4 + j
                        if m >= KT:
                            break
                        fm = min(128, nf - m * 128)
                        for cs, src in ((0, hs), (1, hd)):
                            for cb in range(2):
                                ps = PM()
                                for kt in range(KU):
                                    S.op("pe", lambda e: e.matmul(ps[0:fm, :], lhsT=slab[cs][:, kt, j * 128:j * 128 + fm],
                                                                  rhs=src[:, kt, cb * 512:(cb + 1) * 512], start=(kt == 0),
                                                                  stop=(kt == KU - 1)), reads=[slab[cs], src], writes=[ps])
                                S.op("dve", lambda e: e.tensor_scalar(out=pst[0:fm, :], in0=ps[0:fm, :], scalar1=wfs[0:fm, m:m + 1],
                                                                      scalar2=None, op0=ALU.mult), reads=[ps, wfs], writes=[pst])
                                S.dma("sp", lambda e: e.dma_start(
                                    out=hyPQ[tabi][o, cs, m * 128:m * 128 + fm, cb * 512:(cb + 1) * 512], in_=pst[0:fm, :]),
                                    reads=[pst], writes=[hyPQ[tabi]])
            S.barrier()
        for (tok0, L_, latent) in seq_list:
            si = tok0 // LC if not latent else NCTX
            with ExitStack() as sc:
                S.scope = sc
                cw = S.sbuf("hycw", [128, 24, 3], F32)
                S.dma("sp", lambda e: e.dma_start(out=cw[:], in_=R["hy_convT"][l]), reads=[R["hy_convT"]], writes=[cw])
                xin = [S.sbuf("hyxin%d" % i, [128, L + 2], F32) for i in range(2)]
                acc = [S.sbuf("hyacc%d" % i, [128, L], F32) for i in range(2)]
                accb = S.sbuf("hyaccb", [128, L], BF16)
                ut = S.sbuf("hyut", [128, KU, 128], BF16)
                ptr = [S.psum("hyptr%d" % i, [128, 8, 128], BF16) for i in range(2)]
                for ch in range(24):
                    xi, ac = xin[ch % 2], acc[ch % 2]
                    r0 = fhy + ch * 128
                    S.op("pool", lambda e: e.memset(xi[:, 0:1], 0.0), writes=[xi])
                    S.op("pool", lambda e: e.memset(xi[:, L + 1:L + 2], 0.0), writes=[xi])
                    S.dma("sp", lambda e: e.dma_start(out=xi[:, 1:L + 1], in_=P_F[r0:r0 + 128, tok0:tok0 + L]), reads=[P_F], writes=[xi])
                    S.op("dve", lambda e: e.tensor_scalar(out=ac[:], in0=xi[:, 0:L], scalar1=cw[:, ch, 0:1], scalar2=None, op0=ALU.mult),
                         reads=[xi, cw], writes=[ac])
                    for kk in (1, 2):
                        S.op("dve", lambda e: e.scalar_tensor_tensor(out=ac[:], in0=xi[:, kk:kk + L], scalar=cw[:, ch, kk:kk + 1], in1=ac[:],
                                                                     op0=ALU.mult, op1=ALU.add), reads=[xi, cw, ac], writes=[ac])
                    S.dma("sp", lambda e: e.dma_start(out=hyZ[ch * 128:(ch + 1) * 128, tok0:tok0 + L], in_=ac[:]), reads=[ac], writes=[hyZ])
                    if ch < 8:
                        S.op("act", lambda e: e.activation(out=accb[:], in_=ac[:], func=AF.Copy), reads=[ac], writes=[accb])
                        for t0 in range(0, KU, 8):
                            pt = ptr[(t0 // 8) % 2]
                            nn = min(8, KU - t0)
                            for j in range(nn):
                                S.op("pe", lambda e: e.transpose(pt[:, j, :], accb[:, (t0 + j) * 128:(t0 + j + 1) * 128], ident[:]),
                                     reads=[accb, ident], writes=[pt])
                            _evac(S, t0 // 8, ut[:, t0:t0 + nn, :], pt[:, 0:nn, :], reads=[pt], writes=[ut])
                        S.dma("sp", lambda e: e.dma_start(
                            out=hyU[tok0:tok0 + L, ch * 128:(ch + 1) * 128].rearrange("(k p) c -> p k c", p=128), in_=ut[:]),
                            reads=[ut], writes=[hyU])
                S.barrier()
            for o in range(2):
                with ExitStack() as sc:
                    S.scope = sc
                    u = S.sbuf("hyu", [128, KU, 1024], BF16)
                    S.dma("sp", lambda e: e.dma_start(out=u[:], in_=hyU[tok0:tok0 + L, :].rearrange("(k p) c -> p k c", p=128)),
                          reads=[hyU], writes=[u])
                    slab = [S.sbuf("hyslabf%d" % i, [128, KT, 512], BF16) for i in range(2)]
                    PQt = [S.sbuf("hyPQt%d" % i, [128, 1024], F32) for i in range(2)]
                    ta = S.sbuf("hyta", [128, 512], F32)
                    tb_ = S.sbuf("hytb", [128, 512], F32)
                    yc = S.sbuf("hyyc", [128, 512], BF16)
                    ys = S.sbuf("hyys", [128, 512], BF16)
                    pm = [S.psum("hypmf%d" % i, [128, 512], F32) for i in range(4)]
                    for fb in range(NB):
                        for cs in range(2):
                            S.dma("sp", lambda e: e.dma_start(out=slab[cs][:], in_=tab[cs, fb]), reads=[tab], writes=[slab[cs]])
                        for j in range(4):
                            m = fb * 4 + j
                            if m >= KT:
                                break
                            fm = min(128, nf - m * 128)
                            for cs in range(2):
                                S.dma("sp", lambda e: e.dma_start(out=PQt[cs][0:fm, :], in_=hyPQ[tabi][o, cs, m * 128:m * 128 + fm, :]),
                                      reads=[hyPQ[tabi]], writes=[PQt[cs]])
                            for cb in range(2):
                                pa, pb = pm[(2 * cb) % 4], pm[(2 * cb + 1) % 4]
                                for kt in range(KU):
                                    S.op("pe", lambda e: e.matmul(pa[0:fm, :], lhsT=slab[0][:, kt, j * 128:j * 128 + fm],
                                                                  rhs=u[:, kt, cb * 512:(cb + 1) * 512], start=(kt == 0), stop=(kt == KU - 1)),
                                         reads=[slab[0], u], writes=[pa])
                                for kt in range(KU):
                                    S.op("pe", lambda e: e.matmul(pb[0:fm, :], lhsT=slab[1][:, kt, j * 128:j * 128 + fm],
                                                                  rhs=u[:, kt, cb * 512:(cb + 1) * 512], start=(kt == 0), stop=(kt == KU - 1)),
                                         reads=[slab[1], u], writes=[pb])
                                Pc = PQt[0][0:fm, cb * 512:(cb + 1) * 512]
                                Qc = PQt[1][0:fm, cb * 512:(cb + 1) * 512]
                                S.op("dve", lambda e: e.tensor_tensor(out=ta[0:fm, :], in0=pa[0:fm, :], in1=Pc, op=ALU.mult),
                                     reads=[pa, PQt[0]], writes=[ta])
                                S.op("dve", lambda e: e.tensor_tensor(out=tb_[0:fm, :], in0=pb[0:fm, :], in1=Qc, op=ALU.mult),
                                     reads=[pb, PQt[1]], writes=[tb_])
                                S.op("pool", lambda e: e.tensor_tensor(out=yc[0:fm, :], in0=ta[0:fm, :], in1=tb_[0:fm, :], op=ALU.subtract),
                                     reads=[ta, tb_], writes=[yc])
                                S.op("dve", lambda e: e.tensor_tensor(out=ta[0:fm, :], in0=pa[0:fm, :], in1=Qc, op=ALU.mult),
                                     reads=[pa, PQt[1]], writes=[ta])
                                S.op("dve", lambda e: e.tensor_tensor(out=tb_[0:fm, :], in0=pb[0:fm, :], in1=Pc, op=ALU.mult),
                                     reads=[pb, PQt[0]], writes=[tb_])
                                S.op("pool", lambda e: e.tensor_tensor(out=ys[0:fm, :], in0=ta[0:fm, :], in1=tb_[0:fm, :], op=ALU.add),
                                     reads=[ta, tb_], writes=[ys])
                                S.dma("sp", lambda e: e.dma_start(out=hyYS[0, m * 128:m * 128 + fm, cb * 512:(cb + 1) * 512], in_=yc[0:fm, :]),
                                      reads=[yc], writes=[hyYS])
                                S.dma("sp", lambda e: e.dma_start(out=hyYS[1, m * 128:m * 128 + fm, cb * 512:(cb + 1) * 512], in_=ys[0:fm, :]),
                                      reads=[ys], writes=[hyYS])
                    S.barrier()
                with ExitStack() as sc:
                    S.scope = sc
                    ycs = [S.sbuf("hyYc%d" % i, [128, KT, 1024], BF16) for i in range(2)]
                    for cs in range(2):
                        S.op("dve", lambda e: e.memset(ycs[cs][:, KT - 1, :], 0.0), writes=[ycs[cs]])
                        S.dma("sp", lambda e: e.dma_start(
                            out=ycs[cs][:, 0:KT - 1, :], in_=hyYS[cs, 0:(KT - 1) * 128, :].rearrange("(k p) c -> p k c", p=128)),
                            reads=[hyYS], writes=[ycs[cs]])
                        S.dma("sp", lambda e: e.dma_start(out=ycs[cs][0:1, KT - 1, :], in_=hyYS[cs, (KT - 1) * 128:(KT - 1) * 128 + 1, :]),
                              reads=[hyYS], writes=[ycs[cs]])
                    slab = [S.sbuf("hyslabi%d" % i, [128, KT, 512], BF16) for i in range(2)]
                    bia = S.sbuf("hybia", [128, 8], F32)
                    S.dma("sp", lambda e: e.dma_start(out=bia[:], in_=R["hy_biasT"][l, o]), reads=[R["hy_biasT"]], writes=[bia])
                    uT = [S.sbuf("hyuT%d" % i, [128, 512], F32) for i in range(2)]
                    gT = [S.sbuf("hygT%d" % i, [128, 512], F32) for i in range(2)]
                    yo = [S.sbuf("hyyo%d" % i, [128, 512], F32) for i in range(2)]
                    yb = [S.sbuf("hyyb%d" % i, [128, 512], BF16) for i in range(2)]
                    ut = [S.sbuf("hyuti%d" % i, [128, 4, 128], BF16) for i in range(2)]
                    pm = [S.psum("hypmi%d" % i, [128, 512], F32) for i in range(4)]
                    ptr = [S.psum("hyptri%d" % i, [128, 8, 128], BF16) for i in range(2)]
                    it = 0
                    for tb in range((L + 511) // 512):
                        tw = min(512, L - tb * 512)
                        for cs in range(2):
                            S.dma("sp", lambda e: e.dma_start(out=slab[cs][:], in_=tab[cs, tb]), reads=[tab], writes=[slab[cs]])
                        for cc in range(8):
                            ps = pm[it % 4]
                            u_, g_, y_, yb_, ut_ = uT[it % 2], gT[it % 2], yo[it % 2], yb[it % 2], ut[it % 2]
                            it += 1
                            usrc = hyZ if o == 0 else hyY1
                            S.dma("sp", lambda e: e.dma_start(out=u_[:, 0:tw], in_=usrc[cc * 128:(cc + 1) * 128, tok0 + tb * 512:tok0 + tb * 512 + tw]),
                                  reads=[usrc], writes=[u_])
                            gr = (1 + o) * 1024 + cc * 128
                            S.dma("sp", lambda e: e.dma_start(out=g_[:, 0:tw], in_=hyZ[gr:gr + 128, tok0 + tb * 512:tok0 + tb * 512 + tw]),
                                  reads=[hyZ], writes=[g_])
                            n_mm = 2 * KT
                            i_mm = 0
                            for cs in range(2):
                                for kt in range(KT):
                                    kp = 128 if kt < KT - 1 else 1
                                    S.op("pe", lambda e: e.matmul(ps[:, 0:tw], lhsT=ycs[cs][0:kp, kt, cc * 128:(cc + 1) * 128],
                                                                  rhs=slab[cs][0:kp, kt, 0:tw], start=(i_mm == 0), stop=(i_mm == n_mm - 1)),
                                         reads=[ycs[cs], slab[cs]], writes=[ps])
                                    i_mm += 1
                            S.op("dve", lambda e: e.scalar_tensor_tensor(out=y_[:, 0:tw], in0=u_[:, 0:tw], scalar=bia[:, cc:cc + 1],
                                                                         in1=ps[:, 0:tw], op0=ALU.mult, op1=ALU.add),
                                 reads=[u_, bia, ps], writes=[y_])
                            if o == 0:
                                S.op("pool", lambda e: e.tensor_tensor(out=y_[:, 0:tw], in0=y_[:, 0:tw], in1=g_[:, 0:tw], op=ALU.mult),
                                     reads=[y_, g_], writes=[y_])
                                S.dma("sp", lambda e: e.dma_start(out=hyY1[cc * 128:(cc + 1) * 128, tok0 + tb * 512:tok0 + tb * 512 + tw],
                                                                  in_=y_[:, 0:tw]), reads=[y_], writes=[hyY1])
                                S.op("act", lambda e: e.activation(out=yb_[:, 0:tw], in_=y_[:, 0:tw], func=AF.Copy), reads=[y_], writes=[yb_])
                                pt = ptr[it % 2]
                                nt = tw // 128
                                for j in range(nt):
                                    S.op("pe", lambda e: e.transpose(pt[:, j, :], yb_[:, j * 128:(j + 1) * 128], ident[:]),
                                         reads=[yb_, ident], writes=[pt])
                                _evac(S, it, ut_[:, 0:nt, :], pt[:, 0:nt, :], reads=[pt], writes=[ut_])
                                S.dma("sp", lambda e: e.dma_start(
                                    out=hyU[tok0 + tb * 512:tok0 + tb * 512 + tw, cc * 128:(cc + 1) * 128].rearrange("(k p) c -> p k c", p=128),
                                    in_=ut_[:, 0:nt, :]), reads=[ut_], writes=[hyU])
                            else:
                                S.op("pool", lambda e: e.tensor_tensor(out=yb_[:, 0:tw], in0=y_[:, 0:tw], in1=g_[:, 0:tw], op=ALU.mult),
                                     reads=[y_, g_], writes=[yb_])
                                S.dma("sp", lambda e: e.dma_start(out=brT[2][cc * 128:(cc + 1) * 128, tok0 + tb * 512:tok0 + tb * 512 + tw],
                                                                  in_=yb_[:, 0:tw]), reads=[yb_], writes=[brT[2]])
                    S.barrier()
    S.scope = S.es


def build_program(dbg=False):
    nc = bass.Bass("TRN2", target_bir_lowering=False)
    k = K()
    k.nc = nc

    def ein(name, shape, dtype=F32):
        return Buf(nc.dram_tensor(name, list(shape), dtype, kind="ExternalInput").ap(), name)

    def eout(name, shape, dtype=F32):
        return Buf(nc.dram_tensor(name, list(shape), dtype, kind="ExternalOutput").ap(), name)

    x_in = ein("x_in", [TT, D])
    c2T = ein("c2T", [128, 16, 2])
    ident_in = ein("ident", [128, 128])
    norm1_g = ein("norm1_g", [DEPTH, D])
    w_ada = ein("w_ada", [DEPTH, D, 6 * D])
    b_ada = ein("b_ada", [DEPTH, 6 * D])
    w_in_T = ein("w_in_T", [DEPTH, D, NT_COLS])
    w_in_F = ein("w_in_F", [DEPTH, D, NF_ROWS])
    mla_kv_norm = ein("mla_kv_norm", [DEPTH, 256])
    R = {}
    R["ident_in"] = ident_in
    R["mla_kv_norm"] = mla_kv_norm
    R["mla_q_norm"] = ein("mla_q_norm", [DEPTH, 512])
    R["w_qb"] = ein("w_qb_p", [DEPTH, 512, 2048])
    R["w_kvb"] = ein("w_kvb_p", [DEPTH, 256, 2048])
    R["cache_ckv"] = ein("cache_ckv", [DEPTH, PAST, 256])
    R["cache_kr"] = ein("cache_kr", [DEPTH, PAST, 64])
    R["cache_nak"] = ein("cache_nak", [DEPTH, PAST, 1024])
    R["cache_nav"] = ein("cache_nav", [DEPTH, PAST, 1024])
    R["ropeT"] = ein("ropeT", [2, 64, LL])
    R["na_ctab"] = ein("na_ctab", [DEPTH, 8, 64, 31, 64])
    R["na_mask"] = ein("na_mask", [NA_NMASK, 128, 512])
    R["w_branch"] = ein("w_branch", [DEPTH, 4, 1024, 2048])
    R["w_out"] = ein("w_out", [DEPTH, D, D])
    R["norm2_g"] = ein("norm2_g", [DEPTH, D])
    R["peer_wq"] = ein("peer_wq", [DEPTH, D, D])
    R["peer_keysT"] = ein("peer_keysT", [DEPTH, 2, 128, 128])
    R["peer_u"] = [ein("peer_u%d" % i, [16384, D]) for i in range(DEPTH)]
    R["peer_v"] = [ein("peer_v%d" % i, [16384, D]) for i in range(DEPTH)]
    R["final_g"] = ein("final_g", [1, D])
    R["iota256"] = ein("iota256", [1, 256])
    _hy_inputs(R, ein)
    R["dn_consts"] = ein("dn_consts", [10, 128, 128])
    R["dn_convT"] = ein("dn_convT", [DEPTH, 128, 24, 5])
    R["dn_a_log"] = ein("dn_a_log", [DEPTH, 16])
    R["dn_dt_bias"] = ein("dn_dt_bias", [DEPTH, 16])
    R["dn_out_norm"] = ein("dn_out_norm", [DEPTH, 128])
    R["dn_state"] = [ein("dn_state%d" % i, [DEPTH, 8, 128, 128]) for i in range(2)]
    if DBG_BR:
        R["dbg_br"] = ein("dbg_br", [DEPTH, 2, 1024, TT], BF16)

    o_ckv = eout("o_ckv", [NCTX, DEPTH, LC, 256])
    o_kr = eout("o_kr", [NCTX, DEPTH, LC, 64])
    o_nak = eout("o_nak", [NCTX, DEPTH, LC, 1024])
    o_nav = eout("o_nav", [NCTX, DEPTH, LC, 1024])
    y_out = eout("y_out", [TT, D])
    o_dn = [eout("o_dn%d" % i, [NCTX, DEPTH, 8, 128, 128]) for i in range(2)]
    R["o_dn"] = o_dn
    outs = [o_ckv, o_kr, o_nak, o_nav, y_out] + o_dn
    R["o_ckv"] = o_ckv
    R["y_out"] = y_out

    with ExitStack() as es:
        S = Sched(nc, es)
        k.S = S
        modd = S.dram("modd", [2, 6 * D])
        P_T = S.dram("P_T", [TT, NT_COLS])
        P_F = S.dram("P_F", [NF_ROWS, TT])
        brT = [S.dram("brT%d" % b_, [1024, TT], BF16) for b_ in range(4)]
        R.update(P_T=P_T, P_F=P_F, brT=brT, modd=modd)
        R["mrg"] = S.dram("mrg", [TT, D])
        R["xbuf"] = S.dram("xbuf", [TT, D])
        _hy_scratch(S, R)
        R["peer_ub"] = [S.dram("peer_ub%d" % i, [16384, D], BF16) for i in range(DEPTH)]
        R["peer_vb"] = [S.dram("peer_vb%d" % i, [16384, D], BF16) for i in range(DEPTH)]

        ident = S.sbuf("identb", [128, 128], BF16)
        S.dma("pool", lambda e: e.dma_start(out=ident[:], in_=ident_in[:, :]), reads=[ident_in], writes=[ident])
        R["ident"] = ident

        for l in range(1 if DBG_BR else DEPTH):
            with ExitStack() as sc:
                S.scope = sc
                cT = S.sbuf("cT", [128, 16, 2], F32)
                sT = S.sbuf("sT", [128, 16, 2], BF16)
                S.dma("sp", lambda e: e.dma_start(out=cT[:], in_=c2T[:, :, :]), reads=[c2T], writes=[cT])
                S.op("act", lambda e: e.activation(out=sT[:], in_=cT[:], func=AF.Silu), reads=[cT], writes=[sT])
                wts = [S.sbuf("adaw%d" % i, [128, 16, 512], BF16) for i in range(2)]
                bts = [S.sbuf("adab%d" % i, [2, 512], F32) for i in range(2)]
                mts = [S.sbuf("adam%d" % i, [2, 512], F32) for i in range(2)]
                pss = [S.psum("adap%d" % i, [2, 512], F32) for i in range(2)]
                for cb in range(24):
                    wt, bt, mt, ps = wts[cb % 2], bts[cb % 2], mts[cb % 2], pss[cb % 2]
                    c0 = cb * 512
                    S.dma("pool", lambda e: e.dma_start(
                        out=wt[:], in_=w_ada[l, :, c0:c0 + 512].rearrange("(k p) c -> p k c", p=128)),
                        reads=[w_ada], writes=[wt])
                    S.dma("sp", lambda e: e.dma_start(out=bt[:], in_=b_ada[l, c0:c0 + 512].partition_broadcast(2)),
                          reads=[b_ada], writes=[bt])
                    for kc in range(16):
                        S.op("pe", lambda e: e.matmul(ps[:, :], lhsT=sT[:, kc, :], rhs=wt[:, kc, :],
                                                      start=(kc == 0), stop=(kc == 15)),
                             reads=[sT, wt], writes=[ps])
                    S.op("dve", lambda e: e.tensor_tensor(out=mt[:], in0=ps[:], in1=bt[:], op=ALU.add),
                         reads=[ps, bt], writes=[mt])
                    S.dma("sp", lambda e: e.dma_start(out=modd[:, c0:c0 + 512], in_=mt[:]), reads=[mt], writes=[modd])
            S.barrier()

            x_cur = x_in if l == 0 else R["xbuf"]
            with ExitStack() as sc:
                S.scope = sc
                hT = S.sbuf("hT", [128, 16, TT], BF16)
                with ExitStack() as sc2:
                    S.scope = sc2
                    gm = [S.sbuf("gm%d" % c, [128, D], F32) for c in range(2)]
                    sh = [S.sbuf("sh%d" % c, [128, D], F32) for c in range(2)]
                    gt = S.sbuf("gt", [128, D], F32)
                    S.dma("sp", lambda e: e.dma_start(out=gt[:], in_=norm1_g[l, :].partition_broadcast(128)),
                          reads=[norm1_g], writes=[gt])
                    for c in range(2):
                        S.dma("sp", lambda e: e.dma_start(out=sh[c][:], in_=modd[c, 0:D].partition_broadcast(128)),
                              reads=[modd], writes=[sh[c]])
                        S.dma("sp", lambda e: e.dma_start(out=gm[c][:], in_=modd[c, D:2 * D].partition_broadcast(128)),
                              reads=[modd], writes=[gm[c]])
                        S.op("dve", lambda e: e.scalar_tensor_tensor(out=gm[c][:], in0=gm[c][:], scalar=1.0, in1=gt[:],
                                                                     op0=ALU.add, op1=ALU.mult),
                             reads=[gm[c], gt], writes=[gm[c]])
                    xts = [S.sbuf("xt%d" % i, [128, D], F32) for i in range(2)]
                    junk = S.sbuf("junk", [128, D], F32)
                    hb = [S.sbuf("hb%d" % i, [128, D], BF16) for i in range(2)]
                    ss = [S.sbuf("ss%d" % i, [128, 1], F32) for i in range(2)]
                    rs = [S.sbuf("rs%d" % i, [128, 1], F32) for i in range(2)]
                    ptr = [S.psum("ptr%d" % i, [128, 8, 128], BF16) for i in range(2)]
                    for t in range(NTILE):
                        c = 0 if t < (NCTX * LC) // 128 else 1
                        xt, hbt, sst, rst = xts[t % 2], hb[t % 2], ss[t % 2], rs[t % 2]
                        S.dma("sp", lambda e: e.dma_start(out=xt[:], in_=x_cur[t * 128:(t + 1) * 128, :]),
                              reads=[x_cur], writes=[xt])
                        S.op("act", lambda e: e.activation(out=junk[:], in_=xt[:], func=AF.Square, accum_out=sst[:]),
                             reads=[xt], writes=[junk, sst])
                        S.op("act", lambda e: e.activation(out=rst[:], in_=sst[:], func=AF.Sqrt, scale=1.0 / D, bias=EPS),
                             reads=[sst], writes=[rst])
                        S.op("dve", lambda e: e.reciprocal(out=rst[:], in_=rst[:]), reads=[rst], writes=[rst])
                        S.op("dve", lambda e: e.scalar_tensor_tensor(out=xt[:], in0=xt[:], scalar=rst[:, 0:1], in1=gm[c][:],
                                                                     op0=ALU.mult, op1=ALU.mult),
                             reads=[xt, rst, gm[c]], writes=[xt])
                        S.op("pool", lambda e: e.tensor_tensor(out=hbt[:], in0=xt[:], in1=sh[c][:], op=ALU.add),
                             reads=[xt, sh[c]], writes=[hbt])
                        for half in range(2):
                            pt = ptr[half]
                            for j in range(8):
                                kc = half * 8 + j
                                S.op("pe", lambda e: e.transpose(pt[:, j, :], hbt[:, kc * 128:(kc + 1) * 128], ident[:]),
                                     reads=[hbt, ident], writes=[pt])
                            _evac(S, half, hT[:, half * 8:(half + 1) * 8, t * 128:(t + 1) * 128], pt[:, :, :],
                                  reads=[pt], writes=[hT])
                    S.barrier()
                S.scope = sc
                wts = [S.sbuf("wint%d" % i, [128, 16, 512], BF16) for i in range(2)]
                stg = [S.sbuf("stg%d" % i, [128, 512], F32) for i in range(4)]
                pmm = [S.psum("pmm%d" % i, [128, 512], F32) for i in range(4)]
                nblk = (NT_COLS + 511) // 512
                ev = 0
                for cb in range(nblk):
                    c0 = cb * 512
                    cw = min(512, NT_COLS - c0)
                    wt = wts[cb % 2]
                    S.dma("pool", lambda e: e.dma_start(
                        out=wt[:, :, 0:cw], in_=w_in_T[l, :, c0:c0 + cw].rearrange("(k p) c -> p k c", p=128)),
                        reads=[w_in_T], writes=[wt])
                    for t in range(NTILE):
                        ps, st = pmm[ev % 4], stg[ev % 4]
                        for kc in range(16):
                            S.op("pe", lambda e: e.matmul(ps[:, 0:cw], lhsT=hT[:, kc, t * 128:(t + 1) * 128],
                                                          rhs=wt[:, kc, 0:cw], start=(kc == 0), stop=(kc == 15)),
                                 reads=[hT, wt], writes=[ps])
                        _evac(S, ev, st[:, 0:cw], ps[:, 0:cw], reads=[ps], writes=[st])
                        S.dma("sp", lambda e: e.dma_start(out=P_T[t * 128:(t + 1) * 128, c0:c0 + cw], in_=st[:, 0:cw]),
                              reads=[st], writes=[P_T])
                        ev += 1
                nblk = (NF_ROWS + 511) // 512
                for cb in range(nblk):
                    c0 = cb * 512
                    cw = min(512, NF_ROWS - c0)
                    wt = wts[cb % 2]
                    S.dma("pool", lambda e: e.dma_start(
                        out=wt[:, :, 0:cw], in_=w_in_F[l, :, c0:c0 + cw].rearrange("(k p) c -> p k c", p=128)),
                        reads=[w_in_F], writes=[wt])
                    for sb in range((cw + 127) // 128):
                        r0 = c0 + sb * 128
                        rw = min(128, NF_ROWS - r0)
                        for g in range(TT // 512):
                            ps, st = pmm[ev % 4], stg[ev % 4]
                            for kc in range(16):
                                S.op("pe", lambda e: e.matmul(ps[0:rw, :], lhsT=wt[:, kc, sb * 128:sb * 128 + rw],
                                                              rhs=hT[:, kc, g * 512:(g + 1) * 512],
                                                              start=(kc == 0), stop=(kc == 15)),
                                     reads=[hT, wt], writes=[ps])
                            _evac(S, ev, st[0:rw, :], ps[0:rw, :], reads=[ps], writes=[st])
                            S.dma("sp", lambda e: e.dma_start(out=P_F[r0:r0 + rw, g * 512:(g + 1) * 512], in_=st[0:rw, :]),
                                  reads=[st], writes=[P_F])
                            ev += 1
            S.barrier()
            S.scope = es

            for s_ in range(NCTX):
                for (nm, ob) in (("kr", o_kr), ("nak", o_nak), ("nav", o_nav)):
                    a0, aw = T_COLS[nm]
                    S.dma("sp", lambda e: e.dma_start(out=ob[s_, l, :, :], in_=P_T[s_ * LC:(s_ + 1) * LC, a0:a0 + aw]),
                          reads=[P_T], writes=[ob])
            if DBG_BR:
                for b_ in DBG_FILL:
                    S.dma("sp", lambda e: e.dma_start(out=brT[b_][:, :], in_=R["dbg_br"][l, b_ - 1]),
                          reads=[R["dbg_br"]], writes=[brT[b_]])
            if not DBG_BR:
                for src_, dst_ in ((R["peer_u"][l], R["peer_ub"][l]), (R["peer_v"][l], R["peer_vb"][l])):
                    for r_ in range(0, 16384, 1024):
                        S.dma("pool", lambda e: e.dma_start(out=dst_[r_:r_ + 1024, :], in_=src_[r_:r_ + 1024, :]),
                              reads=[src_], writes=[dst_])
            stage_dn(S, l, R)
            if not DBG_BR:
                stage_hyena(S, l, R)
                stage_mla(S, l, R)
                stage_na(S, l, R)
            if DBG_BR:
                for nm_, b_ in (("d_brT1", 1),):
                    db = eout(nm_, [1024, TT], BF16)
                    outs.append(db)
                    S.dma("sp", lambda e: e.dma_start(out=db[:, :], in_=brT[b_][:, :]), reads=[brT[b_]], writes=[db])
            if not DBG_BR:
                stage_merge(S, l, R, x_in if l == 0 else R["xbuf"])
                stage_peer(S, l, R, last=(l == DEPTH - 1))

        for b in outs:
            if b.last_w is not None:
                S._wait("sp", b.last_w[0], b.last_w[1])
        S.barrier()
        k.ninstr = S.ninstr
    return nc, k


def _prep_weights(inp):
    w_in = inp["w_in"]
    tcols = np.concatenate([
        np.arange(O_CQ, O_CQ + 512), np.arange(O_CKV, O_CKV + 256), np.arange(O_KR, O_KR + 64),
        np.arange(O_Z, O_Z + 1024), np.arange(O_A, O_A + 16), np.arange(O_B, O_B + 16),
        np.arange(O_NA + 1024, O_NA + 2048), np.arange(O_NA + 2048, O_NA + 3072),
        np.arange(O_GATE, O_GATE + 8192)])
    sw = np.concatenate([np.arange(16, 32), np.arange(0, 16), np.arange(48, 64), np.arange(32, 48)])
    fcols = np.concatenate([
        np.arange(O_KR, O_KR + 64), O_KR + sw, np.arange(O_DN, O_DN + 3072), np.arange(O_HY, O_HY + 3072),
        np.arange(O_NA, O_NA + 1024), np.arange(O_NA + 1024, O_NA + 2048)])
    assert len(tcols) == NT_COLS and len(fcols) == NF_ROWS
    return np.ascontiguousarray(w_in[:, :, tcols]), np.ascontiguousarray(w_in[:, :, fcols])


def _prep_consts(inp):
    C = {}
    sw = np.concatenate([np.arange(16, 32), np.arange(0, 16), np.arange(48, 64), np.arange(32, 48)])
    nope = np.concatenate([np.arange(h * 192, h * 192 + 128) for h in range(8)])
    rope = np.concatenate([np.arange(h * 192 + 128, h * 192 + 192) for h in range(8)])
    ropesw = np.concatenate([h * 192 + 128 + sw for h in range(8)])
    C["w_qb_p"] = np.ascontiguousarray(inp["mla_w_qb"][:, :, np.concatenate([nope, rope, ropesw])])
    kn = np.concatenate([np.arange(h * 256, h * 256 + 128) for h in range(8)])
    vv = np.concatenate([np.arange(h * 256 + 128, h * 256 + 256) for h in range(8)])
    C["w_kvb_p"] = np.ascontiguousarray(inp["mla_w_kvb"][:, :, np.concatenate([kn, vv])])
    t = np.arange(LL)
    pos = [t // 64, t % 64]
    inv = 10000.0 ** (-np.arange(0, 32, 2, dtype=np.float32) / 32.0)
    cosT = np.zeros((64, LL), np.float32)
    sinT = np.zeros((64, LL), np.float32)
    for d in range(64):
        half, j = d // 32, d % 32
        ang = pos[half].astype(np.float32) * inv[j % 16]
        cosT[d] = np.cos(ang)
        sinT[d] = (-np.sin(ang)) if j < 16 else np.sin(ang)
    C["ropeT"] = np.stack([cosT, sinT], 0).astype(np.float32)
    rpb = inp["na_rpb"]
    NEG = np.float32(-30000.0)
    cp = np.arange(64)[:, None]
    c = np.arange(64)[None, :]
    cstart = np.clip(c - 8, 0, 48)
    cvalid = (cp >= cstart) & (cp < cstart + 16)
    dcol = np.clip(cp - c + 15, 0, 30)
    ctab = np.full((DEPTH, 8, 64, 31, 64), NEG, np.float32)
    for mm in range(31):
        dr = 22 - mm
        if 0 <= dr <= 14:
            g = rpb[:, :, dr, :][:, :, dcol]
            ctab[:, :, :, mm, :] = np.where(cvalid[None, None], g, NEG)
    C["na_ctab"] = ctab
    mask = np.full((NA_NMASK, 128, 512), NEG, np.float32)
    for (qb, kt), mi in NA_MASK_IDX.items():
        for rr in range(2):
            rp = 2 * kt + rr
            for j in range(8):
                r = 8 * qb + j
                st_ = min(max(r - 4, 0), 24)
                if st_ <= rp < st_ + 8:
                    mask[mi, rr * 64:(rr + 1) * 64, j * 64:(j + 1) * 64] = 0.0
    C["na_mask"] = mask
    C["peer_keysT"] = np.ascontiguousarray(inp["peer_keys"].transpose(0, 1, 3, 2))
    ii = np.arange(128)[:, None]
    jj = np.arange(128)[None, :]
    NEGM = np.float32(-30000.0)
    dc = np.zeros((10, 128, 128), np.float32)
    dc[9] = ((ii // 64) == (jj // 64))
    dc[0] = (ii <= jj)
    dc[1] = (ii >= jj)
    dc[2] = np.eye(128)
    dc[3] = (ii > jj)
    dc[4] = (ii < jj)
    dc[5] = np.where(ii > jj, 0.0, NEGM)
    dc[6] = np.where(ii < jj, 0.0, NEGM)
    dc[7] = np.where(jj >= ii, 0.0, NEGM)
    dc[8] = np.where(jj <= ii, 0.0, NEGM)
    C["dn_consts"] = dc
    C["dn_convT"] = np.ascontiguousarray(inp["dn_conv"].reshape(DEPTH, 5, 24, 128).transpose(0, 3, 2, 1))
    return C


_CACHE = {}
_DBG = {}


def kernel(**inp):
    inp = {k_: np.asarray(v) for k_, v in inp.items()}
    if "prog" not in _CACHE:
        _CACHE["prog"] = build_program()
    nc, kk = _CACHE["prog"]
    w_in_T, w_in_F = _prep_weights(inp)
    C = _prep_consts(inp)
    H = _hy_host(inp)
    ident = np.eye(128, dtype=np.float32)
    in_maps = []
    for i in range(NCORE):
        b = i // 2
        x_in = np.concatenate([inp["x_prompt"][2 * i].reshape(LC, D), inp["x_prompt"][2 * i + 1].reshape(LC, D),
                               inp["x_sample"][b].reshape(LL, D)], axis=0)
        c2 = np.stack([inp["c_ctx"], inp["c"][b]], axis=0)
        c2T = np.ascontiguousarray(c2.reshape(2, 16, 128).transpose(2, 1, 0))
        in_maps.append({
            "x_in": np.ascontiguousarray(x_in), "c2T": c2T, "ident": ident,
            "norm1_g": inp["norm1_g"], "w_ada": inp["w_ada"], "b_ada": inp["b_ada"],
            "w_in_T": w_in_T, "w_in_F": w_in_F, "mla_kv_norm": inp["mla_kv_norm"],
            "mla_q_norm": inp["mla_q_norm"], "w_qb_p": C["w_qb_p"], "w_kvb_p": C["w_kvb_p"],
            "cache_ckv": inp["cache_mla_ckv"][b], "cache_kr": inp["cache_mla_krope"][b],
            "cache_nak": np.ascontiguousarray(inp["cache_na_k"][b].reshape(DEPTH, PAST, 1024)),
            "cache_nav": np.ascontiguousarray(inp["cache_na_v"][b].reshape(DEPTH, PAST, 1024)),
            "ropeT": C["ropeT"], "na_ctab": C["na_ctab"], "na_mask": C["na_mask"],
            "w_branch": inp["w_branch"], "w_out": inp["w_out"], "norm2_g": inp["norm2_g"],
            "peer_wq": inp["peer_wq"], "peer_keysT": C["peer_keysT"],
            "peer_u0": inp["peer_u"][0], "peer_u1": inp["peer_u"][1],
            "peer_v0": inp["peer_v"][0], "peer_v1": inp["peer_v"][1],
            "final_g": inp["final_g"].reshape(1, D), "iota256": np.arange(256, dtype=np.float32).reshape(1, 256),
            "dn_consts": C["dn_consts"], "dn_convT": C["dn_convT"],
            "dn_a_log": inp["dn_a_log"].reshape(DEPTH, 16), "dn_dt_bias": inp["dn_dt_bias"].reshape(DEPTH, 16),
            "dn_out_norm": inp["dn_out_norm"],
            "dn_state0": inp["state_dn_fwd"][b], "dn_state1": inp["state_dn_bwd"][b],
        })
        in_maps[-1].update(H)
        if DBG_BR:
            in_maps[-1]["dbg_br"] = _DBG["br"]
    import os
    ndev = int(os.environ.get("KDEV_CORES", NCORE))
    res = run_bass_kernel_spmd(nc, in_maps[:ndev], core_ids=list(range(ndev)))
    r = list(res.results) + [res.results[0]] * (NCORE - ndev)
    _DBG["res"] = res.results[0]
    B = 16
    y_prompt = np.concatenate([r[i]["y_out"][:NCTX * LC].reshape(NCTX, LC, D) for i in range(NCORE)], axis=0)
    y_sample = np.stack([r[2 * j]["y_out"][NCTX * LC:] for j in range(4)], axis=0)
    new_ckv = np.concatenate([r[i]["o_ckv"] for i in range(NCORE)], axis=0)
    new_kr = np.concatenate([r[i]["o_kr"] for i in range(NCORE)], axis=0)
    new_nak = np.concatenate([r[i]["o_nak"] for i in range(NCORE)], axis=0).reshape(B, DEPTH, LC, 8, 128)
    new_nav = np.concatenate([r[i]["o_nav"] for i in range(NCORE)], axis=0).reshape(B, DEPTH, LC, 8, 128)
    new_dnf = np.concatenate([r[i]["o_dn0"] for i in range(NCORE)], axis=0)
    new_dnb = np.concatenate([r[i]["o_dn1"] for i in range(NCORE)], axis=0)
    return (y_prompt, y_sample, new_ckv, new_kr, new_nak, new_nav, new_dnf, new_dnb)
```

```python
import numpy as np
from contextlib import ExitStack
import concourse.bass as bass
import concourse.mybir as mybir
from concourse.bass_utils import run_bass_kernel_spmd

F32 = mybir.dt.float32
BF16 = mybir.dt.bfloat16
I32 = mybir.dt.int32
U32 = mybir.dt.uint32
U16 = mybir.dt.uint16
AF = mybir.ActivationFunctionType
ALU = mybir.AluOpType
AX = mybir.AxisListType

D = 2048
DEPTH = 2
NCORE = 8
LC = 256
LL = 2048
PAST = 512
NCTX = 2
TT = NCTX * LC + LL
NTILE = TT // 128
EPS = 1e-6

T_COLS = {}
_o = 0
for _n, _s in (("cq", 512), ("ckv", 256), ("kr", 64), ("z", 1024), ("a", 16), ("b", 16),
               ("nak", 1024), ("nav", 1024), ("gate", 8192)):
    T_COLS[_n] = (_o, _s)
    _o += _s
NT_COLS = _o
F_ROWS = {}
_o = 0
for _n, _s in (("krT", 64), ("krswT", 64), ("dnT", 3072), ("hyT", 3072), ("naqT", 1024), ("nakT", 1024)):
    F_ROWS[_n] = (_o, _s)
    _o += _s
NF_ROWS = _o

IN_SIZES = (512, 256, 64, 3072, 1024, 16, 16, 3072, 3072, 8192)
IN_OFF = np.concatenate([[0], np.cumsum(IN_SIZES)]).astype(int)
(O_CQ, O_CKV, O_KR, O_DN, O_Z, O_A, O_B, O_HY, O_NA, O_GATE) = [int(v) for v in IN_OFF[:-1]]


class Buf:
    __slots__ = ("t", "last_w", "readers", "name", "root")

    def __init__(self, t, name, root=None):
        self.t = t
        self.name = name
        self.last_w = None
        self.readers = {}
        self.root = root if root is not None else self

    def __getitem__(self, idx):
        return self.t[idx]


class Sched:
    ENG = ("pe", "act", "dve", "pool", "sp")
    NDMA = 10

    def __init__(self, nc, es):
        self.nc = nc
        self.es = es
        self.eng = {"pe": nc.tensor, "act": nc.scalar, "dve": nc.vector, "pool": nc.gpsimd, "sp": nc.sync}
        self.sem = {}
        self.cnt = {}
        for e in self.ENG:
            self.sem[e] = es.enter_context(nc.semaphore("sem_" + e))
            self.cnt[e] = 0
        self.dq = {}
        for q in ("sp", "pool"):
            sl = []
            for i in range(self.NDMA):
                key = ("dma", q, i)
                self.sem[key] = es.enter_context(nc.semaphore("dsem_%s_%d" % (q, i)))
                self.cnt[key] = 0
                sl.append(key)
            self.dq[q] = [sl, 0]
        self.seen = {e: {} for e in self.ENG}
        self.ninstr = 0
        self.scope = es

    def _nm(self, name):
        self.nid = getattr(self, "nid", 0) + 1
        return "%s_%d" % (name, self.nid)

    def sbuf(self, name, shape, dtype=F32):
        name = self._nm(name)
        return Buf(self.scope.enter_context(self.nc.sbuf_tensor(name, list(shape), dtype)), name)

    def psum(self, name, shape, dtype=F32):
        name = self._nm(name)
        return Buf(self.scope.enter_context(self.nc.psum_tensor(name, list(shape), dtype)), name)

    def dram(self, name, shape, dtype=F32, kind="Internal"):
        t = self.nc.dram_tensor(name, list(shape), dtype, kind=kind)
        return Buf(t.ap(), name)

    def _wait(self, e, key, val):
        if self.seen[e].get(key, 0) >= val:
            return
        self.eng[e].wait_ge(self.sem[key], val)
        self.seen[e][key] = val
        self.ninstr += 1

    def _deps(self, e, reads, writes):
        reads = [b.root for b in reads]
        writes = [b.root for b in writes]
        need = {}
        for b in list(reads) + list(writes):
            if b.last_w is not None:
                k, v = b.last_w
                if need.get(k, 0) < v:
                    need[k] = v
        for b in writes:
            for k, v in b.readers.items():
                if need.get(k, 0) < v:
                    need[k] = v
        for k, v in need.items():
            if k == "pe" and e == "pe":
                continue
            self._wait(e, k, v)

    def _mark(self, ev, reads, writes):
        reads = [b.root for b in reads]
        writes = [b.root for b in writes]
        k, v = ev
        for b in writes:
            b.last_w = ev
            b.readers = {}
        for b in reads:
            if b.readers.get(k, 0) < v:
                b.readers[k] = v

    def op(self, e, fn, reads=(), writes=()):
        self._deps(e, reads, writes)
        ins = fn(self.eng[e])
        self.cnt[e] += 1
        ins.then_inc(self.sem[e], 1)
        self._mark((e, self.cnt[e]), reads, writes)
        self.ninstr += 1
        return ins

    def dma(self, q, fn, reads=(), writes=()):
        sl, i = self.dq[q]
        key = sl[i % self.NDMA]
        self.dq[q][1] = i + 1
        if self.cnt[key] > 0:
            self._wait(q, key, self.cnt[key])
        self._deps(q, reads, writes)
        ins = fn(self.eng[q])
        self.cnt[key] += 16
        ins.then_inc(self.sem[key], 16)
        self._mark((key, self.cnt[key]), reads, writes)
        self.ninstr += 1
        return ins

    def barrier(self):
        for e in self.ENG:
            for key, v in self.cnt.items():
                if v > 0:
                    self._wait(e, key, v)


import os as _os
DBG_BR = bool(int(_os.environ.get("KDBG_BR", "0")))
DBG_FILL = (2,)
NTILE_PEER = 0
DNSTOP = int(_os.environ.get("KDN_STOP", "9"))
DNVAR = int(_os.environ.get("KDN_VAR", "0"))
DNSEQ = int(_os.environ.get("KDN_SEQ", "3"))


def _hy_inputs(R, ein):
    R["hy_tab"] = []
    R["hy_zemb"] = []
    R["hy_wf"] = []
    R["hy_win"] = []
    for i, L in enumerate((LC, LL)):
        nf, KT, NB = (L + 1), (L + 1 + 127) // 128, (L + 1 + 511) // 512
        R["hy_tab"].append(ein("hy_tab%d" % i, [2, NB, 128, KT, 512], BF16))
        R["hy_zemb"].append(ein("hy_zemb%d" % i, [33, L]))
        R["hy_wf"].append(ein("hy_wf%d" % i, [128, KT]))
        R["hy_win"].append(ein("hy_win%d" % i, [L, 1024]))
    R["hy_w1"] = ein("hy_w1", [DEPTH, 33, 64])
    R["hy_w2"] = ein("hy_w2", [DEPTH, 64, 64])
    R["hy_w3"] = ein("hy_w3", [DEPTH, 64, 4096])
    R["hy_b12"] = ein("hy_b12", [DEPTH, 64, 2])
    R["hy_convT"] = ein("hy_convT", [DEPTH, 128, 24, 3])
    R["hy_biasT"] = ein("hy_biasT", [DEPTH, 2, 128, 8])


def _hy_scratch(S, R):
    R["hyZ"] = S.dram("hyZ", [3072, TT])
    R["hyY1"] = S.dram("hyY1", [1024, TT])
    R["hyU"] = S.dram("hyU", [TT, 1024], BF16)
    R["hyPQ"] = [S.dram("hyPQ%d" % i, [2, 2, ((L + 1 + 127) // 128) * 128, 1024]) for i, L in enumerate((LC, LL))]
    R["hyYS"] = S.dram("hyYS", [2, ((LL + 1 + 127) // 128) * 128, 1024], BF16)


def _hy_host(inp):
    import ml_dtypes
    H = {}
    for i, L in enumerate((LC, LL)):
        nf, KT, NB = (L + 1), (L + 1 + 127) // 128, (L + 1 + 511) // 512
        r = np.arange(KT * 128, dtype=np.int64)
        q = np.arange(NB * 512, dtype=np.int64)
        prod = (r[:, None] * q[None, :]) % (2 * L)
        ang = np.pi * prod.astype(np.float64) / L
        valid = (r[:, None] <= L) & (q[None, :] <= L)
        tabs = []
        for fn in (np.cos, np.sin):
            T = np.where(valid, fn(ang), 0.0).astype(np.float32)
            T = T.reshape(KT, 128, NB, 512).transpose(2, 1, 0, 3)
            tabs.append(T)
        H["hy_tab%d" % i] = np.ascontiguousarray(np.stack(tabs, 0)).astype(ml_dtypes.bfloat16)
        f = np.arange(KT * 128)
        wf = np.where((f == 0) | (f == L), 1.0, np.where(f < L, 2.0, 0.0)) / (2.0 * L)
        H["hy_wf%d" % i] = np.ascontiguousarray(wf.reshape(KT, 128).T).astype(np.float32)
        t01 = np.linspace(0.0, 1.0, L, dtype=np.float32)[:, None]
        w = (np.float32(2.0 * np.pi) * np.arange(L, dtype=np.float32)[:, None] / np.float32(L)).astype(np.float32)
        fr = np.linspace(1e-4, 15.0, 16, dtype=np.float32)[None, :]
        z = np.concatenate([t01, np.cos(fr * w), -np.sin(fr * w)], axis=-1).astype(np.float32)
        H["hy_zemb%d" % i] = np.ascontiguousarray(z.T)
        max_decay = np.log(1e-2) / 0.3
        min_decay = np.log(1e-2) / 1.5
        deltas = np.linspace(min_decay, max_decay, 1024, dtype=np.float32)
        H["hy_win%d" % i] = np.exp(-t01 * np.abs(deltas)[None, :]).astype(np.float32)
    H["hy_w1"] = inp["hy_w1"]
    H["hy_w2"] = inp["hy_w2"]
    H["hy_w3"] = inp["hy_w3"]
    H["hy_b12"] = np.ascontiguousarray(np.stack([inp["hy_b1"], inp["hy_b2"]], axis=-1))
    H["hy_convT"] = np.ascontiguousarray(inp["hy_conv"].reshape(DEPTH, 3, 24, 128).transpose(0, 3, 2, 1))
    H["hy_biasT"] = np.ascontiguousarray(inp["hy_bias"].reshape(DEPTH, 2, 8, 128).transpose(0, 1, 3, 2))
    return H


class K:
    pass


def _evac(S, i, out_ap, in_ap, reads, writes):
    if i % 2 == 0:
        S.op("act", lambda e: e.activation(out=out_ap, in_=in_ap, func=AF.Copy), reads=reads, writes=writes)
    else:
        S.op("dve", lambda e: e.tensor_copy(out=out_ap, in_=in_ap), reads=reads, writes=writes)


def _rmsnorm_rows(S, xt, g, n, jk, st):
    S.op("act", lambda e: e.activation(out=jk[:, 0:n], in_=xt[:, 0:n], func=AF.Square, accum_out=st[:]),
         reads=[xt], writes=[jk, st])
    S.op("act", lambda e: e.activation(out=st[:], in_=st[:], func=AF.Sqrt, scale=1.0 / n, bias=EPS),
         reads=[st], writes=[st])
    S.op("dve", lambda e: e.reciprocal(out=st[:], in_=st[:]), reads=[st], writes=[st])
    S.op("dve", lambda e: e.scalar_tensor_tensor(out=xt[:, 0:n], in0=xt[:, 0:n], scalar=st[:, 0:1], in1=g[:, 0:n],
                                                 op0=ALU.mult, op1=ALU.mult),
         reads=[xt, st, g], writes=[xt])


def _attn(S, A, qparts, kparts, v_ap, ktl, q0, QB, scale, bias_fn, out_dst):
    psO, psD = A["psO"][A["n"] % 2], A["psD"][A["n"] % 2]
    A["n"] += 1
    n = len(ktl)
    for i, kt in enumerate(ktl):
        ps = A["psS"][A["ns"] % 2]
        pT = A["pT"][A["ns"] % 3]
        A["ns"] += 1
        for pi in range(len(qparts)):
            kb, kf = kparts[pi]
            qb_, qf = qparts[pi]
            S.op("pe", lambda e: e.matmul(ps[:, 0:QB], lhsT=kf(kt), rhs=qf(q0, QB), start=(pi == 0),
                                          stop=(pi == len(qparts) - 1)), reads=[kb, qb_], writes=[ps])
        bb = bias_fn(kt) if bias_fn is not None else None
        if bb is not None:
            tf = A["tf"][A["ns"] % 2]
            S.op("dve", lambda e: e.scalar_tensor_tensor(out=tf[:, 0:QB], in0=ps[:, 0:QB], scalar=float(scale),
                                                         in1=bb[:, 0:QB], op0=ALU.mult, op1=ALU.add),
                 reads=[ps, bb], writes=[tf])
            S.op("act", lambda e: e.activation(out=pT[:, 0:QB], in_=tf[:, 0:QB], func=AF.Exp), reads=[tf], writes=[pT])
        else:
            S.op("act", lambda e: e.activation(out=pT[:, 0:QB], in_=ps[:, 0:QB], func=AF.Exp, scale=float(scale)),
                 reads=[ps], writes=[pT])
        vb, va = v_ap(kt)
        S.op("pe", lambda e: e.matmul(psO[:, 0:QB], lhsT=va, rhs=pT[:, 0:QB], start=(i == 0), stop=(i == n - 1)),
             reads=[vb, pT], writes=[psO])
        S.op("pe", lambda e: e.matmul(psD[:, 0:QB], lhsT=A["ones"][:, :], rhs=pT[:, 0:QB], start=(i == 0),
                                      stop=(i == n - 1)), reads=[A["ones"], pT], writes=[psD])
    rd = A["rd"]
    ob = A["ob"][A["n"] % 2]
    S.op("dve", lambda e: e.reciprocal(out=rd[:, 0:QB], in_=psD[:, 0:QB]), reads=[psD], writes=[rd])
    S.op("dve", lambda e: e.tensor_tensor(out=ob[:, 0:QB], in0=psO[:, 0:QB], in1=rd[:, 0:QB], op=ALU.mult),
         reads=[psO, rd], writes=[ob])
    dbuf, dap = out_dst
    S.dma("sp", lambda e: e.dma_start(out=dap, in_=ob[:, 0:QB]), reads=[ob], writes=[])


def _attn_res(S):
    A = {"n": 0, "ns": 0}
    A["psS"] = [S.psum("psS%d" % i, [128, 512], F32) for i in range(2)]
    A["psO"] = [S.psum("psO%d" % i, [128, 512], F32) for i in range(2)]
    A["psD"] = [S.psum("psD%d" % i, [128, 512], F32) for i in range(2)]
    A["pT"] = [S.sbuf("pT%d" % i, [128, 512], BF16) for i in range(3)]
    A["tf"] = [S.sbuf("tf%d" % i, [128, 512], F32) for i in range(2)]
    A["rd"] = S.sbuf("rd", [128, 512], F32)
    A["ob"] = [S.sbuf("ob%d" % i, [128, 512], BF16) for i in range(2)]
    ones = S.sbuf("onesb", [128, 128], BF16)
    S.op("dve", lambda e: e.memset(ones[:], 1.0), writes=[ones])
    A["ones"] = ones
    return A


def _transpose_rows(S, src, ncol, dst, dst_fn, ident, ptr, ev0=0):
    nch = ncol // 128
    for c0 in range(0, nch, 8):
        pt = ptr[(ev0 + c0 // 8) % 2]
        nn = min(8, nch - c0)
        for j in range(nn):
            S.op("pe", lambda e: e.transpose(pt[:, j, :], src[:, (c0 + j) * 128:(c0 + j + 1) * 128], ident[:]),
                 reads=[src, ident], writes=[pt])
        for j in range(nn):
            _evac(S, j, dst_fn(c0 + j), pt[:, j, :], reads=[pt], writes=[dst])


def stage_mla(S, l, R):
    ident = R["ident"]
    P_T, P_F, brT = R["P_T"], R["P_F"], R["brT"]
    with ExitStack() as sc:
        S.scope = sc
        A = _attn_res(S)
        psP = [S.psum("psP%d" % i, [128, 512], F32) for i in range(2)]
        wqb = S.sbuf("wqb", [128, 4, 2048], BF16)
        wkvb = S.sbuf("wkvb", [128, 2, 2048], BF16)
        S.dma("pool", lambda e: e.dma_start(out=wqb[:], in_=R["w_qb"][l].rearrange("(k p) c -> p k c", p=128)),
              reads=[R["w_qb"]], writes=[wqb])
        S.dma("pool", lambda e: e.dma_start(out=wkvb[:], in_=R["w_kvb"][l].rearrange("(k p) c -> p k c", p=128)),
              reads=[R["w_kvb"]], writes=[wkvb])
        gq = S.sbuf("gq", [128, 512], F32)
        gk = S.sbuf("gk", [128, 256], F32)
        S.dma("sp", lambda e: e.dma_start(out=gq[:], in_=R["mla_q_norm"][l, :].partition_broadcast(128)),
              reads=[R["mla_q_norm"]], writes=[gq])
        S.dma("sp", lambda e: e.dma_start(out=gk[:], in_=R["mla_kv_norm"][l, :].partition_broadcast(128)),
              reads=[R["mla_kv_norm"]], writes=[gk])
        ropc = S.sbuf("ropc", [64, LL], F32)
        rops = S.sbuf("rops", [64, LL], F32)
        S.dma("sp", lambda e: e.dma_start(out=ropc[:], in_=R["ropeT"][0]), reads=[R["ropeT"]], writes=[ropc])
        S.dma("sp", lambda e: e.dma_start(out=rops[:], in_=R["ropeT"][1]), reads=[R["ropeT"]], writes=[rops])
        cqT = S.sbuf("cqT", [128, 4, LL], BF16)
        ckvT = S.sbuf("ckvT", [128, 2, LL + PAST], BF16)
        krT = S.sbuf("krT", [128, LL + PAST], BF16)
        vall = S.sbuf("vall", [128, (LL + PAST) // 128, 1024], BF16)
        knT = S.sbuf("knT", [128, LL + PAST], BF16)
        qnT = S.sbuf("qnT", [128, LL], BF16)
        qrT = S.sbuf("qrT", [128, LL], BF16)
        S.op("dve", lambda e: e.memset(krT[64:128, :], 0.0), writes=[krT])
        S.op("dve", lambda e: e.memset(qrT[64:128, :], 0.0), writes=[qrT])
        xt = [S.sbuf("mx%d" % i, [128, 512], F32) for i in range(2)]
        xb = [S.sbuf("mxb%d" % i, [128, 512], BF16) for i in range(2)]
        jk = S.sbuf("mjk", [128, 512], F32)
        st = [S.sbuf("mst%d" % i, [128, 1], F32) for i in range(2)]
        r1 = S.sbuf("mr1", [64, 512], F32)
        r2 = S.sbuf("mr2", [64, 512], F32)
        ptr = [S.psum("mptr%d" % i, [128, 8, 128], BF16) for i in range(0)]
        aq, _ = T_COLS["cq"]
        ak, _ = T_COLS["ckv"]
        akr, _ = T_COLS["kr"]
        fkr, _ = F_ROWS["krT"]
        fks, _ = F_ROWS["krswT"]

        def tr_bf(src, ncol, dst, dst_fn, ev):
            nch = ncol // 128
            pt = psP[ev % 2]
            ptv = pt[:, :].bitcast(BF16)
            for j in range(nch):
                S.op("pe", lambda e: e.transpose(ptv[:, j * 128:(j + 1) * 128], src[:, j * 128:(j + 1) * 128], ident[:]),
                     reads=[src, ident], writes=[pt])
            for j in range(nch):
                _evac(S, j, dst_fn(j), ptv[:, j * 128:(j + 1) * 128], reads=[pt], writes=[dst])

        seqs = [(s * LC, LC, False) for s in range(NCTX)] + [(NCTX * LC, LL, True)]
        for si, (tok0, L, latent) in enumerate(seqs):
            Lk = L + (PAST if latent else 0)
            nkt = Lk // 128
            QB = 512 if latent else 256
            for t in range(L // 128):
                g0 = tok0 + t * 128
                x1, x1b, s1 = xt[t % 2], xb[t % 2], st[t % 2]
                S.dma("sp", lambda e: e.dma_start(out=x1[:, 0:512], in_=P_T[g0:g0 + 128, aq:aq + 512]),
                      reads=[P_T], writes=[x1])
                _rmsnorm_rows(S, x1, gq, 512, jk, s1)
                S.op("pool", lambda e: e.tensor_copy(out=x1b[:, 0:512], in_=x1[:, 0:512]), reads=[x1], writes=[x1b])
                tr_bf(x1b, 512, cqT, lambda j: cqT[:, j, t * 128:(t + 1) * 128], t)
            for t in range(nkt):
                x1, x1b, s1 = xt[t % 2], xb[t % 2], st[t % 2]
                if t < L // 128:
                    g0 = tok0 + t * 128
                    S.dma("sp", lambda e: e.dma_start(out=x1[:, 0:256], in_=P_T[g0:g0 + 128, ak:ak + 256]),
                          reads=[P_T], writes=[x1])
                    _rmsnorm_rows(S, x1, gk, 256, jk, s1)
                    if not latent:
                        S.dma("sp", lambda e: e.dma_start(out=R["o_ckv"][si, l, t * 128:(t + 1) * 128, :], in_=x1[:, 0:256]),
                              reads=[x1], writes=[R["o_ckv"]])
                else:
                    p0 = (t - L // 128) * 128
                    S.dma("sp", lambda e: e.dma_start(out=x1[:, 0:256], in_=R["cache_ckv"][l, p0:p0 + 128, :]),
                          reads=[R["cache_ckv"]], writes=[x1])
                S.op("pool", lambda e: e.tensor_copy(out=x1b[:, 0:256], in_=x1[:, 0:256]), reads=[x1], writes=[x1b])
                tr_bf(x1b, 256, ckvT, lambda j: ckvT[:, j, t * 128:(t + 1) * 128], t)
                if t >= L // 128:
                    p0 = (t - L // 128) * 128
                    S.dma("sp", lambda e: e.dma_start(out=x1[:, 256:320], in_=R["cache_kr"][l, p0:p0 + 128, :]),
                          reads=[R["cache_kr"]], writes=[x1])
                    S.op("pool", lambda e: e.tensor_copy(out=x1b[:, 256:320], in_=x1[:, 256:320]), reads=[x1], writes=[x1b])
                    pt = psP[(t + 1) % 2]
                    ptv = pt[:, :].bitcast(BF16)
                    S.op("pe", lambda e: e.transpose(ptv[0:64, 0:128], x1b[:, 256:320], ident[:]),
                         reads=[x1b, ident], writes=[pt])
                    _evac(S, t, krT[0:64, t * 128:(t + 1) * 128], ptv[0:64, 0:128], reads=[pt], writes=[krT])
            for g in range(L // QB):
                g0 = tok0 + g * QB
                S.dma("sp", lambda e: e.dma_start(out=r1[:, 0:QB], in_=P_F[fkr:fkr + 64, g0:g0 + QB]), reads=[P_F], writes=[r1])
                if latent:
                    S.dma("sp", lambda e: e.dma_start(out=r2[:, 0:QB], in_=P_F[fks:fks + 64, g0:g0 + QB]),
                          reads=[P_F], writes=[r2])
                    S.op("dve", lambda e: e.tensor_tensor(out=r1[:, 0:QB], in0=r1[:, 0:QB], in1=ropc[:, g * QB:(g + 1) * QB],
                                                          op=ALU.mult), reads=[r1, ropc], writes=[r1])
                    S.op("dve", lambda e: e.tensor_tensor(out=r2[:, 0:QB], in0=r2[:, 0:QB], in1=rops[:, g * QB:(g + 1) * QB],
                                                          op=ALU.mult), reads=[r2, rops], writes=[r2])
                    S.op("dve", lambda e: e.tensor_tensor(out=krT[0:64, g * QB:(g + 1) * QB], in0=r1[:, 0:QB], in1=r2[:, 0:QB],
                                                          op=ALU.add), reads=[r1, r2], writes=[krT])
                else:
                    S.op("dve", lambda e: e.tensor_copy(out=krT[0:64, g * QB:(g + 1) * QB], in_=r1[:, 0:QB]),
                         reads=[r1], writes=[krT])
            ev = 0
            for t in range(nkt):
                for hb in range(2):
                    ps = psP[ev % 2]
                    for kc in range(2):
                        S.op("pe", lambda e: e.matmul(ps[:, :], lhsT=ckvT[:, kc, t * 128:(t + 1) * 128],
                                                      rhs=wkvb[:, kc, 1024 + hb * 512:1024 + (hb + 1) * 512],
                                                      start=(kc == 0), stop=(kc == 1)), reads=[ckvT, wkvb], writes=[ps])
                    _evac(S, ev, vall[:, t, hb * 512:(hb + 1) * 512], ps[:, :], reads=[ps], writes=[vall])
                    ev += 1
            for h in range(8):
                for g in range((Lk + 511) // 512):
                    w = min(512, Lk - g * 512)
                    ps = psP[ev % 2]
                    for kc in range(2):
                        S.op("pe", lambda e: e.matmul(ps[:, 0:w], lhsT=wkvb[:, kc, h * 128:(h + 1) * 128],
                                                      rhs=ckvT[:, kc, g * 512:g * 512 + w], start=(kc == 0), stop=(kc == 1)),
                             reads=[ckvT, wkvb], writes=[ps])
                    _evac(S, ev, knT[:, g * 512:g * 512 + w], ps[:, 0:w], reads=[ps], writes=[knT])
                    ev += 1
                for g in range(L // QB):
                    ps = psP[ev % 2]
                    for kc in range(4):
                        S.op("pe", lambda e: e.matmul(ps[:, 0:QB], lhsT=wqb[:, kc, h * 128:(h + 1) * 128],
                                                      rhs=cqT[:, kc, g * QB:(g + 1) * QB], start=(kc == 0), stop=(kc == 3)),
                             reads=[cqT, wqb], writes=[ps])
                    _evac(S, ev, qnT[:, g * QB:(g + 1) * QB], ps[:, 0:QB], reads=[ps], writes=[qnT])
                    ev += 1
                    ps = psP[ev % 2]
                    for kc in range(4):
                        S.op("pe", lambda e: e.matmul(ps[0:64, 0:QB], lhsT=wqb[:, kc, 1024 + h * 64:1024 + (h + 1) * 64],
                                                      rhs=cqT[:, kc, g * QB:(g + 1) * QB], start=(kc == 0), stop=(kc == 3)),
                             reads=[cqT, wqb], writes=[ps])
                    if latent:
                        ps2 = psP[(ev + 1) % 2]
                        for kc in range(4):
                            S.op("pe", lambda e: e.matmul(ps2[0:64, 0:QB], lhsT=wqb[:, kc, 1536 + h * 64:1536 + (h + 1) * 64],
                                                          rhs=cqT[:, kc, g * QB:(g + 1) * QB], start=(kc == 0), stop=(kc == 3)),
                                 reads=[cqT, wqb], writes=[ps2])
                        S.op("dve", lambda e: e.tensor_tensor(out=r1[:, 0:QB], in0=ps[0:64, 0:QB],
                                                              in1=ropc[:, g * QB:(g + 1) * QB], op=ALU.mult),
                             reads=[ps, ropc], writes=[r1])
                        S.op("dve", lambda e: e.tensor_tensor(out=r2[:, 0:QB], in0=ps2[0:64, 0:QB],
                                                              in1=rops[:, g * QB:(g + 1) * QB], op=ALU.mult),
                             reads=[ps2, rops], writes=[r2])
                        S.op("dve", lambda e: e.tensor_tensor(out=qrT[0:64, g * QB:(g + 1) * QB], in0=r1[:, 0:QB],
                                                              in1=r2[:, 0:QB], op=ALU.add), reads=[r1, r2], writes=[qrT])
                        ev += 2
                    else:
                        _evac(S, ev, qrT[0:64, g * QB:(g + 1) * QB], ps[0:64, 0:QB], reads=[ps], writes=[qrT])
                        ev += 1
                for g in range(L // QB):
                    _attn(S, A,
                          qparts=[(qnT, lambda q0, n: qnT[:, q0:q0 + n]), (qrT, lambda q0, n: qrT[:, q0:q0 + n])],
                          kparts=[(knT, lambda kt: knT[:, kt * 128:(kt + 1) * 128]),
                                  (krT, lambda kt: krT[:, kt * 128:(kt + 1) * 128])],
                          v_ap=lambda kt: (vall, vall[:, kt, h * 128:(h + 1) * 128]),
                          ktl=list(range(nkt)), q0=g * QB, QB=QB, scale=192 ** -0.5, bias_fn=None,
                          out_dst=(brT[0], brT[0][h * 128:(h + 1) * 128, tok0 + g * QB:tok0 + (g + 1) * QB]))
        S.barrier()
    S.scope = S.es


NA_QB_TILES = {0: list(range(0, 6)), 1: list(range(2, 10)), 2: list(range(6, 14)), 3: list(range(10, 16))}
NA_MASK_IDX = {}
_i = 0
for _qb in range(4):
    for _kt in NA_QB_TILES[_qb]:
        NA_MASK_IDX[(_qb, _kt)] = _i
        _i += 1
NA_NMASK = _i


def stage_na(S, l, R):
    ident = R["ident"]
    P_T, P_F, brT = R["P_T"], R["P_F"], R["brT"]
    with ExitStack() as sc:
        S.scope = sc
        A = _attn_res(S)
        psP = [S.psum("psP%d" % i, [128, 512], F32) for i in range(2)]
        qT = S.sbuf("naqT", [128, LL], BF16)
        kT = S.sbuf("nakT", [128, LL + PAST], BF16)
        vall = S.sbuf("navall", [128, (LL + PAST) // 128, 1024], BF16)
        kc_f = [S.sbuf("nakc%d" % i, [128, 1024], F32) for i in range(2)]
        kc_b = S.sbuf("nakcb", [128, 4, 1024], BF16)
        bias = [S.sbuf("nabias%d" % i, [128, 512], F32) for i in range(2)]
        mask = [S.sbuf("namask%d" % i, [128, 512], F32) for i in range(2)]
        fq, _ = F_ROWS["naqT"]
        fk, _ = F_ROWS["nakT"]
        av, _ = T_COLS["nav"]
        seqs = [(s * LC, LC, False) for s in range(NCTX)] + [(NCTX * LC, LL, True)]
        nb = 0
        for si, (tok0, L, latent) in enumerate(seqs):
            Lk = L + (PAST if latent else 0)
            nkt = Lk // 128
            QB = 512 if latent else 256
            for t in range(L // 128):
                g0 = tok0 + t * 128
                S.dma("pool", lambda e: e.dma_start(out=vall[:, t, :], in_=P_T[g0:g0 + 128, av:av + 1024]),
                      reads=[P_T], writes=[vall])
            if latent:
                for t in range(4):
                    S.dma("pool", lambda e: e.dma_start(out=vall[:, L // 128 + t, :],
                                                        in_=R["cache_nav"][l, t * 128:(t + 1) * 128, :]),
                          reads=[R["cache_nav"]], writes=[vall])
                    S.dma("pool", lambda e: e.dma_start(out=kc_b[:, t, :], in_=R["cache_nak"][l, t * 128:(t + 1) * 128, :]),
                          reads=[R["cache_nak"]], writes=[kc_b])
            for h in range(8):
                S.dma("pool", lambda e: e.dma_start(out=qT[:, 0:L], in_=P_F[fq + h * 128:fq + (h + 1) * 128, tok0:tok0 + L]),
                      reads=[P_F], writes=[qT])
                S.dma("pool", lambda e: e.dma_start(out=kT[:, 0:L], in_=P_F[fk + h * 128:fk + (h + 1) * 128, tok0:tok0 + L]),
                      reads=[P_F], writes=[kT])
                if latent:
                    pt = psP[h % 2]
                    ptv = pt[:, :].bitcast(BF16)
                    for t in range(4):
                        S.op("pe", lambda e: e.transpose(ptv[:, t * 128:(t + 1) * 128], kc_b[:, t, h * 128:(h + 1) * 128],
                                                         ident[:]), reads=[kc_b, ident], writes=[pt])
                    _evac(S, h, kT[:, L:L + 512], ptv[:, 0:512], reads=[pt], writes=[kT])
                for g in range(L // QB):
                    if latent:
                        ktl = NA_QB_TILES[g] + [16, 17, 18, 19]

                        def bias_fn(kt, g=g, h=h):
                            if kt >= 16:
                                return None
                            bb, mm = bias[bias_fn.n % 2], mask[bias_fn.n % 2]
                            bias_fn.n += 1
                            for rr in range(2):
                                rp = 2 * kt + rr
                                mm0 = 15 - rp + 8 * g
                                S.dma("sp", lambda e: e.dma_start(
                                    out=bb[rr * 64:(rr + 1) * 64, :],
                                    in_=R["na_ctab"][l, h, :, mm0:mm0 + 8, :].rearrange("c m q -> c (m q)")),
                                    reads=[R["na_ctab"]], writes=[bb])
                            mi = NA_MASK_IDX[(g, kt)]
                            S.dma("sp", lambda e: e.dma_start(out=mm[:], in_=R["na_mask"][mi]), reads=[R["na_mask"]], writes=[mm])
                            S.op("pool", lambda e: e.tensor_tensor(out=bb[:], in0=bb[:], in1=mm[:], op=ALU.add),
                                 reads=[bb, mm], writes=[bb])
                            return bb
                        bias_fn.n = nb
                    else:
                        ktl = list(range(nkt))
                        bias_fn = None
                    _attn(S, A,
                          qparts=[(qT, lambda q0, n: qT[:, q0:q0 + n])],
                          kparts=[(kT, lambda kt: kT[:, kt * 128:(kt + 1) * 128])],
                          v_ap=lambda kt: (vall, vall[:, kt, h * 128:(h + 1) * 128]),
                          ktl=ktl, q0=g * QB, QB=QB, scale=128 ** -0.5, bias_fn=bias_fn,
                          out_dst=(brT[3], brT[3][h * 128:(h + 1) * 128, tok0 + g * QB:tok0 + (g + 1) * QB]))
                    if latent:
                        nb = bias_fn.n
        S.barrier()
    S.scope = S.es


def stage_merge(S, l, R, x_src):
    ident = R["ident"]
    P_T, brT, mrg, xbuf, modd = R["P_T"], R["brT"], R["mrg"], R["xbuf"], R["modd"]
    ag, _ = T_COLS["gate"]
    for b in range(4):
        with ExitStack() as sc:
            S.scope = sc
            wbr = S.sbuf("wbr", [128, 8, 2048], BF16)
            S.dma("pool", lambda e: e.dma_start(out=wbr[:], in_=R["w_branch"][l, b].rearrange("(k p) c -> p k c", p=128)),
                  reads=[R["w_branch"]], writes=[wbr])
            brt = [S.sbuf("brt%d" % i, [128, 8, 128], BF16) for i in range(2)]
            gt = [S.sbuf("gt%d" % i, [128, 2048], F32) for i in range(2)]
            acc = [S.sbuf("acc%d" % i, [128, 2048], F32) for i in range(2)]
            pm = [S.psum("pm%d" % i, [128, 512], F32) for i in range(4)]
            ev = 0
            for t in range(NTILE):
                bt, g, a = brt[t % 2], gt[t % 2], acc[t % 2]
                S.dma("sp", lambda e: e.dma_start(out=bt[:], in_=brT[b][:, t * 128:(t + 1) * 128].rearrange("(k p) t -> p k t", p=128)),
                      reads=[brT[b]], writes=[bt])
                S.dma("sp", lambda e: e.dma_start(out=g[:], in_=P_T[t * 128:(t + 1) * 128, ag + b * 2048:ag + (b + 1) * 2048]),
                      reads=[P_T], writes=[g])
                if b > 0:
                    S.dma("sp", lambda e: e.dma_start(out=a[:], in_=mrg[t * 128:(t + 1) * 128, :]), reads=[R["mrgt"][t]], writes=[a])
                S.op("act", lambda e: e.activation(out=g[:], in_=g[:], func=AF.Sigmoid), reads=[g], writes=[g])
                for nb in range(4):
                    ps = pm[ev % 4]
                    ev += 1
                    for kc in range(8):
                        S.op("pe", lambda e: e.matmul(ps[:, :], lhsT=bt[:, kc, :], rhs=wbr[:, kc, nb * 512:(nb + 1) * 512],
                                                      start=(kc == 0), stop=(kc == 7)), reads=[bt, wbr], writes=[ps])
                    S.op("dve", lambda e: e.tensor_tensor(out=g[:, nb * 512:(nb + 1) * 512], in0=ps[:, :],
                                                          in1=g[:, nb * 512:(nb + 1) * 512], op=ALU.mult),
                         reads=[ps, g], writes=[g])
                if b > 0:
                    S.op("pool", lambda e: e.tensor_tensor(out=g[:], in0=g[:], in1=a[:], op=ALU.add), reads=[g, a], writes=[g])
                S.dma("sp", lambda e: e.dma_start(out=mrg[t * 128:(t + 1) * 128, :], in_=g[:]), reads=[g], writes=[R["mrgt"][t]])
            S.barrier()
    with ExitStack() as sc:
        S.scope = sc
        wout = S.sbuf("wout", [128, 16, 2048], BF16)
        S.dma("pool", lambda e: e.dma_start(out=wout[:], in_=R["w_out"][l].rearrange("(k p) c -> p k c", p=128)),
              reads=[R["w_out"]], writes=[wout])
        g1b = [S.sbuf("g1b%d" % c, [128, 2048], F32) for c in range(2)]
        for c in range(2):
            S.dma("sp", lambda e: e.dma_start(out=g1b[c][:], in_=modd[c, 2 * D:3 * D].partition_broadcast(128)),
                  reads=[modd], writes=[g1b[c]])
        mt = [S.sbuf("mt%d" % i, [128, 2048], F32) for i in range(2)]
        mb = [S.sbuf("mb%d" % i, [128, 2048], BF16) for i in range(2)]
        mT = [S.sbuf("mT%d" % i, [128, 16, 128], BF16) for i in range(2)]
        xt = [S.sbuf("xo%d" % i, [128, 2048], F32) for i in range(2)]
        ptr = [S.psum("optr%d" % i, [128, 8, 128], BF16) for i in range(2)]
        pm = [S.psum("pmo%d" % i, [128, 512], F32) for i in range(4)]
        ev = 0
        for t in range(NTILE):
            c = 0 if t < (NCTX * LC) // 128 else 1
            m, mbb, mTT, x = mt[t % 2], mb[t % 2], mT[t % 2], xt[t % 2]
            S.dma("sp", lambda e: e.dma_start(out=m[:], in_=mrg[t * 128:(t + 1) * 128, :]), reads=[R["mrgt"][t]], writes=[m])
            S.dma("sp", lambda e: e.dma_start(out=x[:], in_=x_src[t * 128:(t + 1) * 128, :]), reads=[R["xbt"][t]], writes=[x])
            S.op("pool", lambda e: e.tensor_copy(out=mbb[:], in_=m[:]), reads=[m], writes=[mbb])
            for half in range(2):
                pt = ptr[half]
                for j in range(8):
                    kc = half * 8 + j
                    S.op("pe", lambda e: e.transpose(pt[:, j, :], mbb[:, kc * 128:(kc + 1) * 128], ident[:]),
                         reads=[mbb, ident], writes=[pt])
                _evac(S, half, mTT[:, half * 8:(half + 1) * 8, :], pt[:, :, :], reads=[pt], writes=[mTT])
            for nb in range(4):
                ps = pm[ev % 4]
                ev += 1
                for kc in range(16):
                    S.op("pe", lambda e: e.matmul(ps[:, :], lhsT=mTT[:, kc, :], rhs=wout[:, kc, nb * 512:(nb + 1) * 512],
                                                  start=(kc == 0), stop=(kc == 15)), reads=[mTT, wout], writes=[ps])
                S.op("dve", lambda e: e.tensor_tensor(out=m[:, nb * 512:(nb + 1) * 512], in0=ps[:, :],
                                                      in1=g1b[c][:, nb * 512:(nb + 1) * 512], op=ALU.mult),
                     reads=[ps, g1b[c]], writes=[m])
            S.op("pool", lambda e: e.tensor_tensor(out=x[:], in0=x[:], in1=m[:], op=ALU.add), reads=[x, m], writes=[x])
            S.dma("sp", lambda e: e.dma_start(out=xbuf[t * 128:(t + 1) * 128, :], in_=x[:]), reads=[x], writes=[R["xbt"][t]])
        S.barrier()
    S.scope = S.es


def stage_peer(S, l, R, last):
    ident = R["ident"]
    xbuf, modd = R["xbuf"], R["modd"]
    with ExitStack() as sc:
        S.scope = sc
        wq = S.sbuf("pwq", [128, 16, 2048], BF16)
        S.dma("pool", lambda e: e.dma_start(out=wq[:], in_=R["peer_wq"][l].rearrange("(k p) c -> p k c", p=128)),
              reads=[R["peer_wq"]], writes=[wq])
        identf = S.sbuf("identf", [128, 128], F32)
        S.dma("sp", lambda e: e.dma_start(out=identf[:], in_=R["ident_in"][:, :]), reads=[R["ident_in"]], writes=[identf])
        keysT = S.sbuf("keysT", [128, 2, 128], F32)
        S.dma("sp", lambda e: e.dma_start(out=keysT[:], in_=R["peer_keysT"][l].rearrange("s c n -> c s n")),
              reads=[R["peer_keysT"]], writes=[keysT])
        gm2 = S.sbuf("gm2", [128, 2048], F32)
        sh2 = S.sbuf("sh2", [128, 2048], F32)
        x1 = S.sbuf("px1", [128, 2048], F32)
        h2 = S.sbuf("ph2", [128, 2048], F32)
        h2b = S.sbuf("ph2b", [128, 2048], BF16)
        h2T = S.sbuf("ph2T", [128, 16, 128], BF16)
        qTf = S.sbuf("pqTf", [128, 16, 128], F32)
        scr = S.sbuf("pscr", [128, 16, 128], F32)
        scw = S.sbuf("pscw", [128, 128], F32)
        vals = S.sbuf("pvals", [128, 16, 16], F32)
        idx = S.sbuf("pidx", [128, 16, 16], U32)
        idxf = S.sbuf("pidxf", [128, 16, 16], F32)
        cand = S.sbuf("pcand", [128, 256], F32)
        candw = S.sbuf("pcandw", [128, 256], F32)
        cid = S.sbuf("pcid", [128, 256], F32)
        bs = S.sbuf("pbs", [128, 8, 16], F32)
        bj = S.sbuf("pbj", [128, 16], U32)
        bjf = S.sbuf("pbjf", [128, 2, 16], F32)
        bja = S.sbuf("pbja", [128, 16], U32)
        bjb = S.sbuf("pbjb", [128, 16], U32)
        eq16 = S.sbuf("peq16", [128, 16, 16], F32)
        sel = S.sbuf("psel", [128, 2, 16], F32)
        iota = S.sbuf("piota", [128, 256], F32)
        S.dma("sp", lambda e: e.dma_start(out=iota[:], in_=R["iota256"][0, :].partition_broadcast(128)),
              reads=[R["iota256"]], writes=[iota])
        eq = S.sbuf("peq", [128, 8, 256], F32)
        eidf = S.sbuf("peidf", [128, 8, 16], F32)
        eidi = S.sbuf("peidi", [128, 128], I32)
        negb = S.sbuf("pnegb", [128, 8], F32)
        zs = S.sbuf("pzs", [128, 8], F32)
        gat = S.sbuf("pgat", [128, 8, 16], F32)
        actv = S.sbuf("pact", [128, 128], F32)
        coef = S.sbuf("pcoef", [128, 128], F32)
        yacc = S.sbuf("pyacc", [128, 2048], F32)
        qf = yacc
        jk = S.sbuf("pjk", [128, 2048], F32)
        st = S.sbuf("pst", [128, 1], F32)
        ug = [S.sbuf("pug%d" % i, [128, 2048], BF16) for i in range(8)]
        ptr = [S.psum("pptr%d" % i, [128, 8, 128], BF16) for i in range(2)]
        pm = [S.psum("ppm%d" % i, [128, 512], F32) for i in range(4)]
        cur_c = -1
        for t in range(NTILE_PEER if NTILE_PEER else NTILE):
            c = 0 if t < (NCTX * LC) // 128 else 1
            if c != cur_c:
                cur_c = c
                S.dma("sp", lambda e: e.dma_start(out=sh2[:], in_=modd[c, 3 * D:4 * D].partition_broadcast(128)),
                      reads=[modd], writes=[sh2])
                S.dma("sp", lambda e: e.dma_start(out=gm2[:], in_=modd[c, 4 * D:5 * D].partition_broadcast(128)),
                      reads=[modd], writes=[gm2])
                S.dma("sp", lambda e: e.dma_start(out=jk[:], in_=R["norm2_g"][l, :].partition_broadcast(128)),
                      reads=[R["norm2_g"]], writes=[jk])
                S.op("dve", lambda e: e.scalar_tensor_tensor(out=gm2[:], in0=gm2[:], scalar=1.0, in1=jk[:],
                                                             op0=ALU.add, op1=ALU.mult), reads=[gm2, jk], writes=[gm2])
            S.dma("sp", lambda e: e.dma_start(out=x1[:], in_=xbuf[t * 128:(t + 1) * 128, :]), reads=[R["xbt"][t]], writes=[x1])
            S.op("act", lambda e: e.activation(out=jk[:], in_=x1[:], func=AF.Square, accum_out=st[:]),
                 reads=[x1], writes=[jk, st])
            S.op("act", lambda e: e.activation(out=st[:], in_=st[:], func=AF.Sqrt, scale=1.0 / D, bias=EPS),
                 reads=[st], writes=[st])
            S.op("dve", lambda e: e.reciprocal(out=st[:], in_=st[:]), reads=[st], writes=[st])
            S.op("dve", lambda e: e.scalar_tensor_tensor(out=h2[:], in0=x1[:], scalar=st[:, 0:1], in1=gm2[:],
                                                         op0=ALU.mult, op1=ALU.mult), reads=[x1, st, gm2], writes=[h2])
            S.op("dve", lambda e: e.tensor_tensor(out=h2[:], in0=h2[:], in1=sh2[:], op=ALU.add), reads=[h2, sh2], writes=[h2])
            S.op("act", lambda e: e.activation(out=h2b[:], in_=h2[:], func=AF.Copy), reads=[h2], writes=[h2b])
            for half in range(2):
                pt = ptr[half]
                for j in range(8):
                    kc = half * 8 + j
                    S.op("pe", lambda e: e.transpose(pt[:, j, :], h2b[:, kc * 128:(kc + 1) * 128], ident[:]),
                         reads=[h2b, ident], writes=[pt])
                _evac(S, half, h2T[:, half * 8:(half + 1) * 8, :], pt[:, :, :], reads=[pt], writes=[h2T])
            for nb in range(4):
                ps = pm[nb]
                for kc in range(16):
                    S.op("pe", lambda e: e.matmul(ps[:, :], lhsT=h2T[:, kc, :], rhs=wq[:, kc, nb * 512:(nb + 1) * 512],
                                                  start=(kc == 0), stop=(kc == 15)), reads=[h2T, wq], writes=[ps])
                _evac(S, nb, qf[:, nb * 512:(nb + 1) * 512], ps[:, :], reads=[ps], writes=[qf])
            for b4 in range(4):
                ps = pm[b4]
                for j in range(4):
                    ch = b4 * 4 + j
                    S.op("pe", lambda e: e.transpose(ps[:, j * 128:(j + 1) * 128], qf[:, ch * 128:(ch + 1) * 128], identf[:]),
                         reads=[qf, identf], writes=[ps])
                _evac(S, b4, qTf[:, b4 * 4:(b4 + 1) * 4, :], ps[:, :].rearrange("p (a b) -> p a b", a=4), reads=[ps], writes=[qTf])
            for b4 in range(4):
                ps = pm[b4]
                for j in range(4):
                    ch = b4 * 4 + j
                    S.op("pe", lambda e: e.matmul(ps[:, j * 128:(j + 1) * 128], lhsT=qTf[:, ch, :], rhs=keysT[:, ch % 2, :],
                                                  start=True, stop=True), reads=[qTf, keysT], writes=[ps])
                _evac(S, b4 + 1, scr[:, b4 * 4:(b4 + 1) * 4, :], ps[:, :].rearrange("p (a b) -> p a b", a=4), reads=[ps], writes=[scr])
            for ch in range(16):
                S.op("dve", lambda e: e.max(out=vals[:, ch, 0:8], in_=scr[:, ch, :]), reads=[scr], writes=[vals])
                S.op("dve", lambda e: e.match_replace(out=scw[:, :], in_to_replace=vals[:, ch, 0:8], in_values=scr[:, ch, :],
                                                      imm_value=-1e30), reads=[vals, scr], writes=[scw])
                S.op("dve", lambda e: e.max(out=vals[:, ch, 8:16], in_=scw[:, :]), reads=[scw], writes=[vals])
                S.op("dve", lambda e: e.max_index(out=idx[:, ch, 0:8], in_max=vals[:, ch, 0:8], in_values=scr[:, ch, :]),
                     reads=[vals, scr], writes=[idx])
                S.op("dve", lambda e: e.max_index(out=idx[:, ch, 8:16], in_max=vals[:, ch, 8:16], in_values=scw[:, :]),
                     reads=[vals, scw], writes=[idx])
            S.op("dve", lambda e: e.tensor_copy(out=idxf[:], in_=idx[:]), reads=[idx], writes=[idxf])
            for h in range(8):
                c3 = cand[:, :].rearrange("p (a b) -> p a b", a=16)
                i3 = cid[:, :].rearrange("p (a b) -> p a b", a=16)
                S.op("dve", lambda e: e.tensor_tensor(out=c3, in0=vals[:, 2 * h, :].unsqueeze(2).to_broadcast([128, 16, 16]),
                                                      in1=vals[:, 2 * h + 1, :].unsqueeze(1).to_broadcast([128, 16, 16]),
                                                      op=ALU.add), reads=[vals], writes=[cand])
                S.op("dve", lambda e: e.scalar_tensor_tensor(out=i3, in0=idxf[:, 2 * h, :].unsqueeze(2).to_broadcast([128, 16, 16]),
                                                             scalar=128.0,
                                                             in1=idxf[:, 2 * h + 1, :].unsqueeze(1).to_broadcast([128, 16, 16]),
                                                             op0=ALU.mult, op1=ALU.add), reads=[idxf], writes=[cid])
                S.op("dve", lambda e: e.max(out=bs[:, h, 0:8], in_=cand[:, :]), reads=[cand], writes=[bs])
                S.op("dve", lambda e: e.match_replace(out=candw[:, :], in_to_replace=bs[:, h, 0:8], in_values=cand[:, :],
                                                      imm_value=-1e30), reads=[bs, cand], writes=[candw])
                S.op("dve", lambda e: e.max(out=bs[:, h, 8:16], in_=candw[:, :]), reads=[candw], writes=[bs])
                S.op("dve", lambda e: e.max_index(out=bj[:, 0:8], in_max=bs[:, h, 0:8], in_values=cand[:, :]),
                     reads=[bs, cand], writes=[bj])
                S.op("dve", lambda e: e.max_index(out=bj[:, 8:16], in_max=bs[:, h, 8:16], in_values=candw[:, :]),
                     reads=[bs, candw], writes=[bj])
                S.op("dve", lambda e: e.tensor_single_scalar(out=bja[:], in_=bj[:], scalar=4, op=ALU.logical_shift_right),
                     reads=[bj], writes=[bja])
                S.op("dve", lambda e: e.tensor_single_scalar(out=bjb[:], in_=bj[:], scalar=15, op=ALU.bitwise_and),
                     reads=[bj], writes=[bjb])
                S.op("dve", lambda e: e.tensor_copy(out=bjf[:, 0, :], in_=bja[:]), reads=[bja], writes=[bjf])
                S.op("dve", lambda e: e.tensor_copy(out=bjf[:, 1, :], in_=bjb[:]), reads=[bjb], writes=[bjf])
                for ab in range(2):
                    S.op("dve", lambda e: e.tensor_tensor(out=eq16[:], in0=iota[:, 0:16].unsqueeze(1).to_broadcast([128, 16, 16]),
                                                          in1=bjf[:, ab, :].unsqueeze(2).to_broadcast([128, 16, 16]),
                                                          op=ALU.is_equal), reads=[iota, bjf], writes=[eq16])
                    S.op("dve", lambda e: e.tensor_tensor(out=eq16[:], in0=eq16[:],
                                                          in1=idxf[:, 2 * h + ab, :].unsqueeze(1).to_broadcast([128, 16, 16]),
                                                          op=ALU.mult), reads=[eq16, idxf], writes=[eq16])
                    S.op("dve", lambda e: e.tensor_reduce(out=sel[:, ab, :], in_=eq16[:], axis=AX.X, op=ALU.add),
                         reads=[eq16], writes=[sel])
                S.op("dve", lambda e: e.scalar_tensor_tensor(out=eidf[:, h, :], in0=sel[:, 0, :], scalar=128.0, in1=sel[:, 1, :],
                                                             op0=ALU.mult, op1=ALU.add), reads=[sel], writes=[eidf])
            S.op("dve", lambda e: e.tensor_copy(out=eidi[:, :].rearrange("p (a b) -> p a b", a=8), in_=eidf[:]),
                 reads=[eidf], writes=[eidi])
            S.op("dve", lambda e: e.tensor_scalar(out=negb[:], in0=bs[:, :, 0], scalar1=-1.0, scalar2=None, op0=ALU.mult),
                 reads=[bs], writes=[negb])
            for h in range(8):
                S.op("act", lambda e: e.activation(out=gat[:, h, :], in_=bs[:, h, :], func=AF.Exp, bias=negb[:, h:h + 1],
                                                   accum_out=zs[:, h:h + 1]), reads=[bs, negb], writes=[gat, zs])
            S.op("dve", lambda e: e.reciprocal(out=zs[:], in_=zs[:]), reads=[zs], writes=[zs])
            S.op("dve", lambda e: e.tensor_tensor(out=gat[:], in0=gat[:], in1=zs[:, :].unsqueeze(2).to_broadcast([128, 8, 16]),
                                                  op=ALU.mult), reads=[gat, zs], writes=[gat])
            for s in range(128):
                u = ug[s % 8]
                S.dma("pool", lambda e: e.indirect_dma_start(
                    out=u[:], out_offset=None, in_=R["peer_ub"][l][:, :],
                    in_offset=bass.IndirectOffsetOnAxis(ap=eidi[:, s:s + 1], axis=0)),
                    reads=[R["peer_ub"][l], eidi], writes=[u])
                S.op("dve", lambda e: e.scalar_tensor_tensor(out=jk[:], in0=u[:], scalar=1.0, in1=h2[:],
                                                             op0=ALU.mult, op1=ALU.mult, accum_out=actv[:, s:s + 1]),
                     reads=[u, h2], writes=[jk, actv])
            S.op("act", lambda e: e.activation(out=actv[:], in_=actv[:], func=AF.Gelu), reads=[actv], writes=[actv])
            S.op("dve", lambda e: e.tensor_tensor(out=coef[:], in0=actv[:], in1=gat[:, :, :].rearrange("p a b -> p (a b)"),
                                                  op=ALU.mult), reads=[actv, gat], writes=[coef])
            for s in range(128):
                u = ug[s % 8]
                S.dma("pool", lambda e: e.indirect_dma_start(
                    out=u[:], out_offset=None, in_=R["peer_vb"][l][:, :],
                    in_offset=bass.IndirectOffsetOnAxis(ap=eidi[:, s:s + 1], axis=0)),
                    reads=[R["peer_vb"][l], eidi], writes=[u])
                if s == 0:
                    S.op("dve", lambda e: e.tensor_scalar(out=yacc[:], in0=u[:], scalar1=coef[:, 0:1], scalar2=None,
                                                          op0=ALU.mult), reads=[u, coef], writes=[yacc])
                else:
                    S.op("dve", lambda e: e.scalar_tensor_tensor(out=yacc[:], in0=u[:], scalar=coef[:, s:s + 1], in1=yacc[:],
                                                                 op0=ALU.mult, op1=ALU.add), reads=[u, coef, yacc], writes=[yacc])
            S.dma("sp", lambda e: e.dma_start(out=jk[:], in_=modd[c, 5 * D:6 * D].partition_broadcast(128)),
                  reads=[modd], writes=[jk])
            S.op("dve", lambda e: e.tensor_tensor(out=yacc[:], in0=yacc[:], in1=jk[:], op=ALU.mult), reads=[yacc, jk], writes=[yacc])
            S.op("dve", lambda e: e.tensor_tensor(out=x1[:], in0=x1[:], in1=yacc[:], op=ALU.add), reads=[x1, yacc], writes=[x1])
            if not last:
                S.dma("sp", lambda e: e.dma_start(out=xbuf[t * 128:(t + 1) * 128, :], in_=x1[:]), reads=[x1], writes=[R["xbt"][t]])
            else:
                S.op("act", lambda e: e.activation(out=jk[:], in_=x1[:], func=AF.Square, accum_out=st[:]),
                     reads=[x1], writes=[jk, st])
                S.op("act", lambda e: e.activation(out=st[:], in_=st[:], func=AF.Sqrt, scale=1.0 / D, bias=EPS),
                     reads=[st], writes=[st])
                S.op("dve", lambda e: e.reciprocal(out=st[:], in_=st[:]), reads=[st], writes=[st])
                S.dma("sp", lambda e: e.dma_start(out=jk[:], in_=R["final_g"][0, :].partition_broadcast(128)),
                      reads=[R["final_g"]], writes=[jk])
                S.op("dve", lambda e: e.scalar_tensor_tensor(out=x1[:], in0=x1[:], scalar=st[:, 0:1], in1=jk[:],
                                                             op0=ALU.mult, op1=ALU.mult), reads=[x1, st, jk], writes=[x1])
                S.dma("sp", lambda e: e.dma_start(out=R["y_out"][t * 128:(t + 1) * 128, :], in_=x1[:]), reads=[x1], writes=[R["y_out"]])
        S.barrier()
    S.scope = S.es


def stage_dn(S, l, R):
    P_T, P_F, brT = R["P_T"], R["P_F"], R["brT"]
    fdn, _ = F_ROWS["dnT"]
    az, _ = T_COLS["z"]
    aa, _ = T_COLS["a"]
    with ExitStack() as sc:
        S.scope = sc
        cst = S.sbuf("dncst", [128, 10, 128], F32)
        S.dma("sp", lambda e: e.dma_start(out=cst[:], in_=R["dn_consts"][:, :, :].rearrange("m p q -> p m q")),
              reads=[R["dn_consts"]], writes=[cst])
        onesf = S.sbuf("dnones", [128, 128], F32)
        S.op("dve", lambda e: e.memset(onesf[:], 1.0), writes=[onesf])
        identf = S.sbuf("dnidentf", [128, 128], F32)
        S.dma("sp", lambda e: e.dma_start(out=identf[:], in_=R["dn_consts"][2]), reads=[R["dn_consts"]], writes=[identf])
        identb = R["ident"]
        cw = S.sbuf("dncw", [128, 24, 5], F32)
        S.dma("sp", lambda e: e.dma_start(out=cw[:], in_=R["dn_convT"][l]), reads=[R["dn_convT"]], writes=[cw])
        alog = S.sbuf("dnalog", [128, 16], F32)
        dtb = S.sbuf("dndtb", [128, 16], F32)
        S.dma("sp", lambda e: e.dma_start(out=alog[:], in_=R["dn_a_log"][l, :].partition_broadcast(128)),
              reads=[R["dn_a_log"]], writes=[alog])
        S.dma("sp", lambda e: e.dma_start(out=dtb[:], in_=R["dn_dt_bias"][l, :].partition_broadcast(128)),
              reads=[R["dn_dt_bias"]], writes=[dtb])
        S.op("act", lambda e: e.activation(out=alog[:], in_=alog[:], func=AF.Exp), reads=[alog], writes=[alog])
        gon = S.sbuf("dngon", [128, 128], F32)
        S.dma("sp", lambda e: e.dma_start(out=gon[:], in_=R["dn_out_norm"][l, :].partition_broadcast(128)),
              reads=[R["dn_out_norm"]], writes=[gon])
        NCH = LL // 128
        xin = S.sbuf("dnxin", [128, LL + 4], F32)
        acc = S.sbuf("dnacc", [128, LL], F32)
        qT = S.sbuf("dnqT", [128, LL], F32)
        kT = S.sbuf("dnkT", [128, LL], F32)
        vT = S.sbuf("dnvT", [128, LL], F32)
        ktok = S.sbuf("dnktok", [128, NCH, 128], F32)
        vtok = S.sbuf("dnvtok", [128, NCH, 128], F32)
        oall = S.sbuf("dnoall", [128, NCH, 1024], F32)
        gt = S.sbuf("dng", [128, NCH, 16], F32)
        bt = S.sbuf("dnb", [128, NCH, 16], F32)
        gc = S.sbuf("dngc", [128, NCH, 16], F32)
        egc = S.sbuf("dnegc", [128, NCH, 16], F32)
        bg = S.sbuf("dnbg", [128, NCH, 16], F32)
        egl = S.sbuf("dnegl", [128, NCH, 16], F32)
        edl = S.sbuf("dnedl", [128, NCH, 16], F32)
        ab = S.sbuf("dnab", [128, 32], F32)
        sq = S.sbuf("dnsq", [128, 512], F32)
        rn = S.sbuf("dnrn", [128, 512], F32)
        Sst = S.sbuf("dnS", [128, 128], F32)
        Bsets = []
        for j_ in range(3):
            B = {"j": j_}
            for nm_ in ("Gm", "EA", "ET", "Am", "Ad", "Aoff", "Boff", "qkT", "wT", "kd"):
                B[nm_] = S.sbuf("dn%s%d" % (nm_, j_), [128, 128], F32)
            B["Bk"] = [S.sbuf("dnB%d_%d" % (i, j_), [128, 128], F32) for i in range(6)]
            B["Ck"] = [S.sbuf("dnC%d_%d" % (i, j_), [128, 128], F32) for i in range(2)]
            B["X"] = S.sbuf("dnX%d" % j_, [128, 256], F32)
            B["Zb"] = S.sbuf("dnZb%d" % j_, [128, 256], F32)
            Bsets.append(B)
        vnew = S.sbuf("dnvnew", [128, 128], F32)
        tmp = S.sbuf("dntmp", [128, 128], F32)
        zt = S.sbuf("dnz", [128, 1024], F32)
        ob = S.sbuf("dnob", [128, 1024], BF16)
        obT = S.sbuf("dnobT", [128, 8, 128], BF16)
        ssq = S.sbuf("dnssq", [128, 8], F32)
        pqb = [S.psum("dnpqb%d" % i, [128, 512], F32) for i in range(7)]
        pbig = pqb[0:2]
        pq = [Buf(pqb[i].t[:, 0:128], "dnpq%d" % i, root=pqb[i]) for i in range(7)]
        pxs = [Buf(pqb[i].t[:, 0:256], "dnpx%d" % i, root=pqb[i]) for i in range(7)]
        ptr = S.psum("dnptr", [128, 8, 128], BF16)
        npq = [0]

        def PQ():
            npq[0] += 1
            return pq[npq[0] % 7]

        def PX():
            npq[0] += 1
            return pxs[npq[0] % 7]

        slot = [0, 0, 0]

        def PQs(j, wide=False):
            slot[j] += 1
            bank = pqb[3 * j + slot[j] % 3]
            w_ = 256 if wide else 128
            return Buf(bank.t[:, 0:w_], "dnps", root=bank)

        seqs = [(s * LC, LC, False) for s in range(NCTX)] + [(NCTX * LC, LL, True)]
        seqs = seqs[:DNSEQ]
        for si, (tok0, L, latent) in enumerate(seqs):
            nch = L // 128
            for n in range(nch):
                g0 = tok0 + n * 128
                S.dma("sp", lambda e: e.dma_start(out=ab[:], in_=P_T[g0:g0 + 128, aa:aa + 32]), reads=[P_T], writes=[ab])
                S.op("dve", lambda e: e.tensor_tensor(out=gt[:, n, :], in0=ab[:, 0:16], in1=dtb[:], op=ALU.add),
                     reads=[ab, dtb], writes=[gt])
                S.op("act", lambda e: e.activation(out=gt[:, n, :], in_=gt[:, n, :], func=AF.Exp), reads=[gt], writes=[gt])
                S.op("act", lambda e: e.activation(out=gt[:, n, :], in_=gt[:, n, :], func=AF.Ln, bias=1.0), reads=[gt], writes=[gt])
                S.op("dve", lambda e: e.scalar_tensor_tensor(out=gt[:, n, :], in0=gt[:, n, :], scalar=-1.0, in1=alog[:],
                                                             op0=ALU.mult, op1=ALU.mult), reads=[gt, alog], writes=[gt])
                S.op("act", lambda e: e.activation(out=bt[:, n, :], in_=ab[:, 16:32], func=AF.Sigmoid), reads=[ab], writes=[bt])
                p1 = PQ()
                for d in range(2):
                    S.op("pe", lambda e: e.matmul(p1[:, d * 8:(d + 1) * 8], lhsT=cst[:, d, :], rhs=gt[:, n, d * 8:(d + 1) * 8],
                                                  start=True, stop=True), reads=[cst, gt], writes=[p1])
                S.op("pe", lambda e: e.matmul(p1[:, 16:32], lhsT=onesf[:, :], rhs=gt[:, n, :], start=True, stop=True),
                     reads=[onesf, gt], writes=[p1])
                S.op("dve", lambda e: e.tensor_copy(out=gc[:, n, :], in_=p1[:, 0:16]), reads=[p1], writes=[gc])
                S.op("act", lambda e: e.activation(out=egc[:, n, :], in_=p1[:, 0:16], func=AF.Exp), reads=[p1], writes=[egc])
                S.op("act", lambda e: e.activation(out=egl[:, n, :], in_=p1[:, 16:32], func=AF.Exp), reads=[p1], writes=[egl])
                S.op("dve", lambda e: e.tensor_tensor(out=edl[:, n, :], in0=p1[:, 16:32], in1=gc[:, n, :], op=ALU.subtract),
                     reads=[p1, gc], writes=[edl])
                S.op("act", lambda e: e.activation(out=edl[:, n, :], in_=edl[:, n, :], func=AF.Exp), reads=[edl], writes=[edl])
                S.op("dve", lambda e: e.tensor_tensor(out=bg[:, n, :], in0=bt[:, n, :], in1=egc[:, n, :], op=ALU.mult),
                     reads=[bt, egc], writes=[bg])
            for h in range(8):
                if DNSTOP < 2:
                    continue
                for which, dst in ((0, qT), (1, kT), (2, vT)):
                    ch = which * 8 + h
                    r0 = fdn + ch * 128
                    S.op("pool", lambda e: e.memset(xin[:, 0:2], 0.0), writes=[xin])
                    S.op("pool", lambda e: e.memset(xin[:, L + 2:L + 4], 0.0), writes=[xin])
                    S.dma("sp", lambda e: e.dma_start(out=xin[:, 2:L + 2], in_=P_F[r0:r0 + 128, tok0:tok0 + L]),
                          reads=[P_F], writes=[xin])
                    S.op("dve", lambda e: e.tensor_scalar(out=acc[:, 0:L], in0=xin[:, 0:L], scalar1=cw[:, ch, 0:1], scalar2=None,
                                                          op0=ALU.mult), reads=[xin, cw], writes=[acc])
                    for kk in range(1, 5):
                        S.op("dve", lambda e: e.scalar_tensor_tensor(out=acc[:, 0:L], in0=xin[:, kk:kk + L],
                                                                     scalar=cw[:, ch, kk:kk + 1], in1=acc[:, 0:L],
                                                                     op0=ALU.mult, op1=ALU.add), reads=[xin, cw, acc], writes=[acc])
                    S.op("act", lambda e: e.activation(out=dst[:, 0:L], in_=acc[:, 0:L], func=AF.Silu), reads=[acc], writes=[dst])
                    if which < 2:
                        for g in range((L + 511) // 512):
                            w = min(512, L - g * 512)
                            pb = pbig[g % 2]
                            S.op("act", lambda e: e.activation(out=sq[:, 0:w], in_=dst[:, g * 512:g * 512 + w], func=AF.Square),
                                 reads=[dst], writes=[sq])
                            S.op("pe", lambda e: e.matmul(pb[:, 0:w], lhsT=onesf[:, :], rhs=sq[:, 0:w], start=True, stop=True),
                                 reads=[onesf, sq], writes=[pb])
                            sc_ = 128.0 if which == 0 else 1.0
                            S.op("act", lambda e: e.activation(out=rn[:, 0:w], in_=pb[:, 0:w], func=AF.Sqrt, scale=sc_,
                                                               bias=sc_ * EPS), reads=[pb], writes=[rn])
                            S.op("dve", lambda e: e.reciprocal(out=rn[:, 0:w], in_=rn[:, 0:w]), reads=[rn], writes=[rn])
                            S.op("dve", lambda e: e.tensor_tensor(out=dst[:, g * 512:g * 512 + w], in0=dst[:, g * 512:g * 512 + w],
                                                                  in1=rn[:, 0:w], op=ALU.mult), reads=[dst, rn], writes=[dst])
                if DNSTOP < 3:
                    continue
                for n in range(nch):
                    for src, dd in ((kT, ktok), (vT, vtok)):
                        p1 = PQ()
                        S.op("pe", lambda e: e.transpose(p1[:, 0:128], src[:, n * 128:(n + 1) * 128], identf[:]),
                             reads=[src, identf], writes=[p1])
                        _evac(S, n, dd[:, n, :], p1[:, 0:128], reads=[p1], writes=[dd])
                for d in range(2):
                    if DNSTOP < 4:
                        continue
                    dh = d * 8 + h
                    if latent:
                        S.dma("sp", lambda e: e.dma_start(out=Sst[:], in_=R["dn_state"][d][l, h, :, :]),
                              reads=[R["dn_state"][d]], writes=[Sst])
                    else:
                        S.op("dve", lambda e: e.memset(Sst[:], 0.0), writes=[Sst])
                    order = list(range(nch)) if d == 0 else list(range(nch - 1, -1, -1))

                    def prep(n, B, d=d, dh=dh):
                        c0 = n * 128
                        Gm, EA, ET, Am, Ad, Aoff, Boff, Bk, Ck, qkT, X, Zb = (B["Gm"], B["EA"], B["ET"], B["Am"], B["Ad"], B["Aoff"],
                                                                             B["Boff"], B["Bk"], B["Ck"], B["qkT"], B["X"], B["Zb"])
                        S.op("dve", lambda e: e.tensor_scalar(out=Gm[:], in0=cst[:, 3 + d, :], scalar1=gt[:, n, dh:dh + 1],
                                                              scalar2=None, op0=ALU.mult), reads=[cst, gt], writes=[Gm])
                        yield
                        J = B["j"]
                        pA, pT_ = PQs(J), PQs(J)
                        S.op("pe", lambda e: e.matmul(pA[:, :], lhsT=cst[:, d, :], rhs=Gm[:, :], start=True, stop=False),
                             reads=[cst, Gm], writes=[pA])
                        S.op("pe", lambda e: e.matmul(pA[:, :], lhsT=cst[:, 2, :], rhs=cst[:, 5 + d, :], start=False, stop=True),
                             reads=[cst], writes=[pA])
                        S.op("pe", lambda e: e.matmul(pT_[:, :], lhsT=Gm[:, :], rhs=cst[:, d, :], start=True, stop=False),
                             reads=[cst, Gm], writes=[pT_])
                        S.op("pe", lambda e: e.matmul(pT_[:, :], lhsT=cst[:, 2, :], rhs=cst[:, 7 + d, :], start=False, stop=True),
                             reads=[cst], writes=[pT_])
                        yield
                        S.op("act", lambda e: e.activation(out=EA[:], in_=pA[:, :], func=AF.Exp), reads=[pA], writes=[EA])
                        S.op("act", lambda e: e.activation(out=ET[:], in_=pT_[:, :], func=AF.Exp), reads=[pT_], writes=[ET])
                        yield
                        pkk, pkq = PQs(J), PQs(J)
                        S.op("pe", lambda e: e.matmul(pkk[:, :], lhsT=kT[:, c0:c0 + 128], rhs=kT[:, c0:c0 + 128], start=True,
                                                      stop=True), reads=[kT], writes=[pkk])
                        S.op("pe", lambda e: e.matmul(pkq[:, :], lhsT=kT[:, c0:c0 + 128], rhs=qT[:, c0:c0 + 128], start=True,
                                                      stop=True), reads=[kT, qT], writes=[pkq])
                        yield
                        S.op("dve", lambda e: e.scalar_tensor_tensor(out=Am[:], in0=pkk[:, :], scalar=bt[:, n, dh:dh + 1], in1=EA[:],
                                                                     op0=ALU.mult, op1=ALU.mult), reads=[pkk, bt, EA], writes=[Am])
                        S.op("dve", lambda e: e.tensor_tensor(out=qkT[:], in0=pkq[:, :], in1=ET[:], op=ALU.mult),
                             reads=[pkq, ET], writes=[qkT])
                        S.op("pool", lambda e: e.tensor_scalar(out=X[:, 0:128], in0=vtok[:, n, :], scalar1=bt[:, n, dh:dh + 1],
                                                               scalar2=None, op0=ALU.mult), reads=[vtok, bt], writes=[X])
                        S.op("pool", lambda e: e.tensor_scalar(out=X[:, 128:256], in0=ktok[:, n, :], scalar1=bg[:, n, dh:dh + 1],
                                                               scalar2=None, op0=ALU.mult), reads=[ktok, bg], writes=[X])
                        yield
                        if DNSTOP < 5:
                            return
                        S.op("dve", lambda e: e.tensor_tensor(out=Ad[:], in0=Am[:], in1=cst[:, 9, :], op=ALU.mult),
                             reads=[Am, cst], writes=[Ad])
                        S.op("pool", lambda e: e.tensor_tensor(out=Aoff[:], in0=Am[:], in1=Ad[:], op=ALU.subtract),
                             reads=[Am, Ad], writes=[Aoff])
                        yield
                        pt1, pt2 = PQs(J), PQs(J)
                        S.op("pe", lambda e: e.transpose(pt1[:, :], Ad[:, :], identf[:]), reads=[Ad, identf], writes=[pt1])
                        S.op("pe", lambda e: e.transpose(pt2[:, :], Aoff[:, :], identf[:]), reads=[Aoff, identf], writes=[pt2])
                        yield
                        S.op("act", lambda e: e.activation(out=Bk[0][:], in_=pt1[:, :], func=AF.Copy), reads=[pt1], writes=[Bk[0]])
                        S.op("act", lambda e: e.activation(out=Boff[:], in_=pt2[:, :], func=AF.Copy), reads=[pt2], writes=[Boff])
                        yield
                        Cc = Ad
                        for k_ in range(6):
                            Bc = Bk[k_]
                            pxx = PQs(J, True)
                            S.op("pe", lambda e: e.matmul(pxx[:, :], lhsT=Bc[:, :], rhs=X[:, :], start=True, stop=True),
                                 reads=[Bc, X], writes=[pxx])
                            if k_ < 5:
                                Bn, Cn = Bk[k_ + 1], Ck[(k_ + 1) % 2]
                                pb_, pc_ = PQs(J), PQs(J)
                                S.op("pe", lambda e: e.matmul(pb_[:, :], lhsT=Cc[:, :], rhs=Bc[:, :], start=True, stop=True),
                                     reads=[Cc, Bc], writes=[pb_])
                                S.op("pe", lambda e: e.matmul(pc_[:, :], lhsT=Bc[:, :], rhs=Cc[:, :], start=True, stop=True),
                                     reads=[Cc, Bc], writes=[pc_])
                            yield
                            S.op("dve", lambda e: e.tensor_tensor(out=X[:], in0=X[:], in1=pxx[:, :],
                                                                  op=(ALU.subtract if k_ == 0 else ALU.add)),
                                 reads=[X, pxx], writes=[X])
                            if k_ < 5:
                                S.op("act", lambda e: e.activation(out=Bn[:], in_=pb_[:, :], func=AF.Copy), reads=[pb_], writes=[Bn])
                                S.op("dve", lambda e: e.tensor_copy(out=Cn[:], in_=pc_[:, :]), reads=[pc_], writes=[Cn])
                                Cc = Cn
                            yield
                        pz = PQs(J, True)
                        S.op("pe", lambda e: e.matmul(pz[:, :], lhsT=Boff[:, :], rhs=X[:, :], start=True, stop=True),
                             reads=[Boff, X], writes=[pz])
                        yield
                        S.op("act", lambda e: e.activation(out=Zb[:], in_=pz[:, :], func=AF.Copy), reads=[pz], writes=[Zb])
                        yield
                        for k_ in range(6):
                            pxx = PQs(J, True)
                            S.op("pe", lambda e: e.matmul(pxx[:, :], lhsT=Bk[k_][:, :], rhs=Zb[:, :], start=True, stop=True),
                                 reads=[Bk[k_], Zb], writes=[pxx])
                            yield
                            S.op("dve", lambda e: e.tensor_tensor(out=Zb[:], in0=Zb[:], in1=pxx[:, :],
                                                                  op=(ALU.subtract if k_ == 0 else ALU.add)),
                                 reads=[Zb, pxx], writes=[Zb])
                            yield
                        S.op("dve", lambda e: e.tensor_tensor(out=X[:], in0=X[:], in1=Zb[:], op=ALU.subtract),
                             reads=[X, Zb], writes=[X])
                        yield
                        pw = PQs(J)
                        S.op("pe", lambda e: e.transpose(pw[:, :], X[:, 128:256], identf[:]), reads=[X, identf], writes=[pw])
                        yield
                        S.op("act", lambda e: e.activation(out=B["wT"][:], in_=pw[:, :], func=AF.Copy), reads=[pw], writes=[B["wT"]])
                        S.op("pool", lambda e: e.tensor_scalar(out=B["kd"][:], in0=ktok[:, n, :], scalar1=edl[:, n, dh:dh + 1],
                                                               scalar2=None, op0=ALU.mult), reads=[ktok, edl], writes=[B["kd"]])

                    def scan(n, B, d=d, dh=dh):
                        c0 = n * 128
                        X, qkT, wT, kd = B["X"], B["qkT"], B["wT"], B["kd"]
                        p1, p2, p3, p4 = PQ(), PQ(), PQ(), PQ()
                        S.op("pe", lambda e: e.matmul(p1[:, :], lhsT=wT[:, :], rhs=Sst[:, :], start=True, stop=True),
                             reads=[wT, Sst], writes=[p1])
                        S.op("pe", lambda e: e.matmul(p2[:, :], lhsT=qT[:, c0:c0 + 128], rhs=Sst[:, :], start=True, stop=True),
                             reads=[qT, Sst], writes=[p2])
                        S.op("dve", lambda e: e.tensor_tensor(out=vnew[:], in0=X[:, 0:128], in1=p1[:, :], op=ALU.subtract),
                             reads=[X, p1], writes=[vnew])
                        S.op("pe", lambda e: e.matmul(p3[:, :], lhsT=qkT[:, :], rhs=vnew[:, :], start=True, stop=True),
                             reads=[qkT, vnew], writes=[p3])
                        S.op("pe", lambda e: e.matmul(p4[:, :], lhsT=kd[:, :], rhs=vnew[:, :], start=True, stop=True),
                             reads=[kd, vnew], writes=[p4])
                        S.op("dve", lambda e: e.scalar_tensor_tensor(out=Sst[:], in0=Sst[:], scalar=egl[:, n, dh:dh + 1], in1=p4[:, :],
                                                                     op0=ALU.mult, op1=ALU.add), reads=[Sst, egl, p4], writes=[Sst])
                        S.op("dve", lambda e: e.tensor_scalar(out=tmp[:], in0=p2[:, :], scalar1=egc[:, n, dh:dh + 1], scalar2=None,
                                                              op0=ALU.mult), reads=[p2, egc], writes=[tmp])
                        osl = oall[:, n, h * 128:(h + 1) * 128]
                        if d == 0:
                            S.op("pool" if False else "dve", lambda e: e.tensor_tensor(out=osl, in0=tmp[:], in1=p3[:, :], op=ALU.add),
                                 reads=[tmp, p3], writes=[oall])
                        else:
                            S.op("dve", lambda e: e.tensor_tensor(out=tmp[:], in0=tmp[:], in1=p3[:, :], op=ALU.add),
                                 reads=[tmp, p3], writes=[tmp])
                            S.op("pool", lambda e: e.tensor_tensor(out=osl, in0=osl, in1=tmp[:], op=ALU.add),
                                 reads=[tmp, oall], writes=[oall])

                    NG = 2
                    for g0 in range(0, nch, NG):
                        grp = order[g0:g0 + NG]
                        gens = [prep(n, Bsets[j]) for j, n in enumerate(grp)]
                        alive = list(gens)
                        while alive:
                            for g_ in list(alive):
                                try:
                                    next(g_)
                                except StopIteration:
                                    alive.remove(g_)
                        if DNSTOP < 6:
                            continue
                        for j, n in enumerate(grp):
                            scan(n, Bsets[j])
                    if not latent:
                        ost = R["o_dn"][d]
                        S.dma("sp", lambda e: e.dma_start(out=ost[si, l, h, :, :], in_=Sst[:]), reads=[Sst], writes=[ost])
            for n in range(nch):
                if DNSTOP < 7:
                    continue
                g0 = tok0 + n * 128
                S.dma("sp", lambda e: e.dma_start(out=zt[:], in_=P_T[g0:g0 + 128, az:az + 1024]), reads=[P_T], writes=[zt])
                S.op("act", lambda e: e.activation(out=zt[:], in_=zt[:], func=AF.Silu), reads=[zt], writes=[zt])
                o3 = oall[:, n, :].rearrange("p (h v) -> p h v", h=8)
                for h in range(8):
                    S.op("act", lambda e: e.activation(out=tmp[:], in_=oall[:, n, h * 128:(h + 1) * 128], func=AF.Square,
                                                       accum_out=ssq[:, h:h + 1]), reads=[oall], writes=[tmp, ssq])
                S.op("act", lambda e: e.activation(out=ssq[:], in_=ssq[:], func=AF.Sqrt, scale=1.0 / 128, bias=EPS),
                     reads=[ssq], writes=[ssq])
                S.op("dve", lambda e: e.reciprocal(out=ssq[:], in_=ssq[:]), reads=[ssq], writes=[ssq])
                S.op("dve", lambda e: e.tensor_tensor(out=o3, in0=o3, in1=ssq[:, :].unsqueeze(2).to_broadcast([128, 8, 128]),
                                                      op=ALU.mult), reads=[oall, ssq], writes=[oall])
                S.op("dve", lambda e: e.tensor_tensor(out=o3, in0=o3, in1=gon[:, :].unsqueeze(1).to_broadcast([128, 8, 128]),
                                                      op=ALU.mult), reads=[oall, gon], writes=[oall])
                S.op("dve", lambda e: e.tensor_tensor(out=ob[:], in0=oall[:, n, :], in1=zt[:], op=ALU.mult),
                     reads=[oall, zt], writes=[ob])
                for h in range(8):
                    S.op("pe", lambda e: e.transpose(ptr[:, h, :], ob[:, h * 128:(h + 1) * 128], identb[:]),
                         reads=[ob, identb], writes=[ptr])
                _evac(S, n, obT[:], ptr[:, :, :], reads=[ptr], writes=[obT])
                S.dma("sp", lambda e: e.dma_start(out=brT[1][:, g0:g0 + 128].rearrange("(h p) t -> p h t", p=128), in_=obT[:]),
                      reads=[obT], writes=[])
        S.barrier()
    S.scope = S.es


def _hy_geom(L):
    nf = L + 1
    KT = (nf + 127) // 128
    NB = (nf + 511) // 512
    return nf, KT, NB


def stage_hyena(S, l, R):
    P_F, brT = R["P_F"], R["brT"]
    ident = R["ident"]
    fhy, _ = F_ROWS["hyT"]
    hyZ, hyY1, hyU, hyPQ, hyYS = R["hyZ"], R["hyY1"], R["hyU"], R["hyPQ"], R["hyYS"]
    PI = float(np.pi)
    seqs = [(s * LC, LC, False) for s in range(NCTX)] + [(NCTX * LC, LL, True)]
    for (Lx, tabi, seq_list) in ((LC, 0, seqs[:NCTX]), (LL, 1, seqs[NCTX:])):
        L = Lx
        nf, KT, NB = _hy_geom(L)
        KU = L // 128
        tab = R["hy_tab"][tabi]
        with ExitStack() as sc:
            S.scope = sc
            w1 = S.sbuf("hyw1", [33, 64], F32)
            w2 = S.sbuf("hyw2", [64, 64], F32)
            w3 = S.sbuf("hyw3", [64, 4096], F32)
            b12 = S.sbuf("hyb12", [64, 2], F32)
            S.dma("sp", lambda e: e.dma_start(out=w1[:], in_=R["hy_w1"][l]), reads=[R["hy_w1"]], writes=[w1])
            S.dma("sp", lambda e: e.dma_start(out=w2[:], in_=R["hy_w2"][l]), reads=[R["hy_w2"]], writes=[w2])
            S.dma("sp", lambda e: e.dma_start(out=w3[:], in_=R["hy_w3"][l]), reads=[R["hy_w3"]], writes=[w3])
            S.dma("sp", lambda e: e.dma_start(out=b12[:], in_=R["hy_b12"][l]), reads=[R["hy_b12"]], writes=[b12])
            zT = S.sbuf("hyzT", [33, L], F32)
            S.dma("sp", lambda e: e.dma_start(out=zT[:], in_=R["hy_zemb"][tabi][:, :]), reads=[R["hy_zemb"][tabi]], writes=[zT])
            wfs = S.sbuf("hywf", [128, KT], F32)
            S.dma("sp", lambda e: e.dma_start(out=wfs[:], in_=R["hy_wf"][tabi][:, :]), reads=[R["hy_wf"][tabi]], writes=[wfs])
            h1 = S.sbuf("hyh1", [64, L], F32)
            h2 = S.sbuf("hyh2", [64, L], F32)
            xa = S.sbuf("hyxa", [64, 512], F32)
            xb_ = S.sbuf("hyxb", [64, 512], F32)
            xc = S.sbuf("hyxc", [64, 512], F32)
            win = S.sbuf("hywin", [128, 1024], F32)
            hs = S.sbuf("hyhs", [128, KU, 1024], BF16)
            hd = S.sbuf("hyhd", [128, KU, 1024], BF16)
            tf = [S.sbuf("hytf%d" % i, [128, 512], F32) for i in range(2)]
            slab = [S.sbuf("hyslab%d" % i, [128, KT, 512], BF16) for i in range(2)]
            pst = S.sbuf("hypst", [128, 512], F32)
            pm = [S.psum("hypm%d" % i, [128, 512], F32) for i in range(4)]
            npm = [0]

            def PM():
                npm[0] += 1
                return pm[npm[0] % 4]

            def sin_layer(dst, wmat, kdim, src, bcol):
                for g in range((L + 511) // 512):
                    w = min(512, L - g * 512)
                    ps = PM()
                    S.op("pe", lambda e: e.matmul(ps[0:64, 0:w], lhsT=wmat[0:kdim, :], rhs=src[0:kdim, g * 512:g * 512 + w],
                                                  start=True, stop=True), reads=[wmat, src], writes=[ps])
                    S.op("dve", lambda e: e.tensor_scalar(out=xa[:, 0:w], in0=ps[0:64, 0:w], scalar1=b12[:, bcol:bcol + 1],
                                                          scalar2=None, op0=ALU.add), reads=[ps, b12], writes=[xa])
                    S.op("dve", lambda e: e.tensor_scalar(out=xb_[:, 0:w], in0=xa[:, 0:w], scalar1=PI, scalar2=-2 * PI,
                                                          op0=ALU.is_gt, op1=ALU.mult), reads=[xa], writes=[xb_])
                    S.op("dve", lambda e: e.tensor_scalar(out=xc[:, 0:w], in0=xa[:, 0:w], scalar1=-PI, scalar2=2 * PI,
                                                          op0=ALU.is_lt, op1=ALU.mult), reads=[xa], writes=[xc])
                    S.op("dve", lambda e: e.tensor_tensor(out=xa[:, 0:w], in0=xa[:, 0:w], in1=xb_[:, 0:w], op=ALU.add),
                         reads=[xa, xb_], writes=[xa])
                    S.op("dve", lambda e: e.tensor_tensor(out=xa[:, 0:w], in0=xa[:, 0:w], in1=xc[:, 0:w], op=ALU.add),
                         reads=[xa, xc], writes=[xa])
                    S.op("act", lambda e: e.activation(out=dst[:, g * 512:g * 512 + w], in_=xa[:, 0:w], func=AF.Sin),
                         reads=[xa], writes=[dst])

            sin_layer(h1, w1, 33, zT, 0)
            sin_layer(h2, w2, 64, h1, 1)
            for o in range(2):
                for t in range(KU):
                    S.dma("sp", lambda e: e.dma_start(out=win[:], in_=R["hy_win"][tabi][t * 128:(t + 1) * 128, :]),
                          reads=[R["hy_win"][tabi]], writes=[win])
                    for cb in range(2):
                        pf, pb = PM(), PM()
                        cf = (o * 2 + 0) * 1024 + cb * 512
                        cbk = (o * 2 + 1) * 1024 + cb * 512
                        S.op("pe", lambda e: e.matmul(pf[:, :], lhsT=h2[:, t * 128:(t + 1) * 128], rhs=w3[:, cf:cf + 512],
                                                      start=True, stop=True), reads=[h2, w3], writes=[pf])
                        S.op("pe", lambda e: e.matmul(pb[:, :], lhsT=h2[:, t * 128:(t + 1) * 128], rhs=w3[:, cbk:cbk + 512],
                                                      start=True, stop=True), reads=[h2, w3], writes=[pb])
                        S.op("dve", lambda e: e.tensor_tensor(out=tf[0][:], in0=pf[:, :], in1=win[:, cb * 512:(cb + 1) * 512],
                                                              op=ALU.mult), reads=[pf, win], writes=[tf[0]])
                        S.op("dve", lambda e: e.tensor_tensor(out=tf[1][:], in0=pb[:, :], in1=win[:, cb * 512:(cb + 1) * 512],
                                                              op=ALU.mult), reads=[pb, win], writes=[tf[1]])
                        if t == 0:
                            S.op("dve", lambda e: e.memset(tf[1][0:1, :], 0.0), writes=[tf[1]])
                        S.op("dve", lambda e: e.tensor_tensor(out=hs[:, t, cb * 512:(cb + 1) * 512], in0=tf[0][:], in1=tf[1][:],
                                                              op=ALU.add), reads=[tf[0], tf[1]], writes=[hs])
                        S.op("pool", lambda e: e.tensor_tensor(out=hd[:, t, cb * 512:(cb + 1) * 512], in0=tf[0][:], in1=tf[1][:],
                                                               op=ALU.subtract), reads=[tf[0], tf[1]], writes=[hd])
                for fb in range(NB):
                    for cs in range(2):
                        S.dma("sp", lambda e: e.dma_start(out=slab[cs][:], in_=tab[cs, fb]), reads=[tab], writes=[slab[cs]])
                    for j in range(4):
                        m = fb * 4 + j
                        if m >= KT:
                            break
                        fm = min(128, nf - m * 128)
                        for cs, src in ((0, hs), (1, hd)):
                            for cb in range(2):
                                ps = PM()
                                for kt in range(KU):
                                    S.op("pe", lambda e: e.matmul(ps[0:fm, :], lhsT=slab[cs][:, kt, j * 128:j * 128 + fm],
                                                                  rhs=src[:, kt, cb * 512:(cb + 1) * 512], start=(kt == 0),
                                                                  stop=(kt == KU - 1)), reads=[slab[cs], src], writes=[ps])
                                S.op("dve", lambda e: e.tensor_scalar(out=pst[0:fm, :], in0=ps[0:fm, :], scalar1=wfs[0:fm, m:m + 1],
                                                                      scalar2=None, op0=ALU.mult), reads=[ps, wfs], writes=[pst])
                                S.dma("sp", lambda e: e.dma_start(
                                    out=hyPQ[tabi][o, cs, m * 128:m * 128 + fm, cb * 512:(cb + 1) * 512], in_=pst[0:fm, :]),
                                    reads=[pst], writes=[])
            S.barrier()
        for (tok0, L_, latent) in seq_list:
            si = tok0 // LC if not latent else NCTX
            with ExitStack() as sc:
                S.scope = sc
                cw = S.sbuf("hycw", [128, 24, 3], F32)
                S.dma("sp", lambda e: e.dma_start(out=cw[:], in_=R["hy_convT"][l]), reads=[R["hy_convT"]], writes=[cw])
                xin = [S.sbuf("hyxin%d" % i, [128, L + 2], F32) for i in range(2)]
                acc = [S.sbuf("hyacc%d" % i, [128, L], F32) for i in range(2)]
                accb = S.sbuf("hyaccb", [128, L], BF16)
                ut = S.sbuf("hyut", [128, KU, 128], BF16)
                ptr = [S.psum("hyptr%d" % i, [128, 8, 128], BF16) for i in range(2)]
                for ch in range(24):
                    xi, ac = xin[ch % 2], acc[ch % 2]
                    r0 = fhy + ch * 128
                    S.op("pool", lambda e: e.memset(xi[:, 0:1], 0.0), writes=[xi])
                    S.op("pool", lambda e: e.memset(xi[:, L + 1:L + 2], 0.0), writes=[xi])
                    S.dma("sp", lambda e: e.dma_start(out=xi[:, 1:L + 1], in_=P_F[r0:r0 + 128, tok0:tok0 + L]), reads=[P_F], writes=[xi])
                    S.op("dve", lambda e: e.tensor_scalar(out=ac[:], in0=xi[:, 0:L], scalar1=cw[:, ch, 0:1], scalar2=None, op0=ALU.mult),
                         reads=[xi, cw], writes=[ac])
                    for kk in (1, 2):
                        S.op("dve", lambda e: e.scalar_tensor_tensor(out=ac[:], in0=xi[:, kk:kk + L], scalar=cw[:, ch, kk:kk + 1], in1=ac[:],
                                                                     op0=ALU.mult, op1=ALU.add), reads=[xi, cw, ac], writes=[ac])
                    S.dma("sp", lambda e: e.dma_start(out=hyZ[ch * 128:(ch + 1) * 128, tok0:tok0 + L], in_=ac[:]), reads=[ac], writes=[])
                    if ch < 8:
                        S.op("act", lambda e: e.activation(out=accb[:], in_=ac[:], func=AF.Copy), reads=[ac], writes=[accb])
                        for t0 in range(0, KU, 8):
                            pt = ptr[(t0 // 8) % 2]
                            nn = min(8, KU - t0)
                            for j in range(nn):
                                S.op("pe", lambda e: e.transpose(pt[:, j, :], accb[:, (t0 + j) * 128:(t0 + j + 1) * 128], ident[:]),
                                     reads=[accb, ident], writes=[pt])
                            _evac(S, t0 // 8, ut[:, t0:t0 + nn, :], pt[:, 0:nn, :], reads=[pt], writes=[ut])
                        S.dma("sp", lambda e: e.dma_start(
                            out=hyU[tok0:tok0 + L, ch * 128:(ch + 1) * 128].rearrange("(k p) c -> p k c", p=128), in_=ut[:]),
                            reads=[ut], writes=[])
                S.barrier()
            for o in range(2):
                with ExitStack() as sc:
                    S.scope = sc
                    u = S.sbuf("hyu", [128, KU, 1024], BF16)
                    S.dma("sp", lambda e: e.dma_start(out=u[:], in_=hyU[tok0:tok0 + L, :].rearrange("(k p) c -> p k c", p=128)),
                          reads=[hyU], writes=[u])
                    slab = [S.sbuf("hyslabf%d" % i, [128, KT, 512], BF16) for i in range(2)]
                    PQt = [S.sbuf("hyPQt%d" % i, [128, 1024], F32) for i in range(2)]
                    ta = S.sbuf("hyta", [128, 512], F32)
                    tb_ = S.sbuf("hytb", [128, 512], F32)
                    yc = S.sbuf("hyyc", [128, 512], BF16)
                    ys = S.sbuf("hyys", [128, 512], BF16)
                    pm = [S.psum("hypmf%d" % i, [128, 512], F32) for i in range(4)]
                    for fb in range(NB):
                        for cs in range(2):
                            S.dma("sp", lambda e: e.dma_start(out=slab[cs][:], in_=tab[cs, fb]), reads=[tab], writes=[slab[cs]])
                        for j in range(4):
                            m = fb * 4 + j
                            if m >= KT:
                                break
                            fm = min(128, nf - m * 128)
                            for cs in range(2):
                                S.dma("sp", lambda e: e.dma_start(out=PQt[cs][0:fm, :], in_=hyPQ[tabi][o, cs, m * 128:m * 128 + fm, :]),
                                      reads=[hyPQ[tabi]], writes=[PQt[cs]])
                            for cb in range(2):
                                pa, pb = pm[(2 * cb) % 4], pm[(2 * cb + 1) % 4]
                                for kt in range(KU):
                                    S.op("pe", lambda e: e.matmul(pa[0:fm, :], lhsT=slab[0][:, kt, j * 128:j * 128 + fm],
                                                                  rhs=u[:, kt, cb * 512:(cb + 1) * 512], start=(kt == 0), stop=(kt == KU - 1)),
                                         reads=[slab[0], u], writes=[pa])
                                for kt in range(KU):
                                    S.op("pe", lambda e: e.matmul(pb[0:fm, :], lhsT=slab[1][:, kt, j * 128:j * 128 + fm],
                                                                  rhs=u[:, kt, cb * 512:(cb + 1) * 512], start=(kt == 0), stop=(kt == KU - 1)),
                                         reads=[slab[1], u], writes=[pb])
                                Pc = PQt[0][0:fm, cb * 512:(cb + 1) * 512]
                                Qc = PQt[1][0:fm, cb * 512:(cb + 1) * 512]
                                S.op("dve", lambda e: e.tensor_tensor(out=ta[0:fm, :], in0=pa[0:fm, :], in1=Pc, op=ALU.mult),
                                     reads=[pa, PQt[0]], writes=[ta])
                                S.op("dve", lambda e: e.tensor_tensor(out=tb_[0:fm, :], in0=pb[0:fm, :], in1=Qc, op=ALU.mult),
                                     reads=[pb, PQt[1]], writes=[tb_])
                                S.op("pool", lambda e: e.tensor_tensor(out=yc[0:fm, :], in0=ta[0:fm, :], in1=tb_[0:fm, :], op=ALU.subtract),
                                     reads=[ta, tb_], writes=[yc])
                                S.op("dve", lambda e: e.tensor_tensor(out=ta[0:fm, :], in0=pa[0:fm, :], in1=Qc, op=ALU.mult),
                                     reads=[pa, PQt[1]], writes=[ta])
                                S.op("dve", lambda e: e.tensor_tensor(out=tb_[0:fm, :], in0=pb[0:fm, :], in1=Pc, op=ALU.mult),
                                     reads=[pb, PQt[0]], writes=[tb_])
                                S.op("pool", lambda e: e.tensor_tensor(out=ys[0:fm, :], in0=ta[0:fm, :], in1=tb_[0:fm, :], op=ALU.add),
                                     reads=[ta, tb_], writes=[ys])
                                S.dma("sp", lambda e: e.dma_start(out=hyYS[0, m * 128:m * 128 + fm, cb * 512:(cb + 1) * 512], in_=yc[0:fm, :]),
                                      reads=[yc], writes=[])
                                S.dma("sp", lambda e: e.dma_start(out=hyYS[1, m * 128:m * 128 + fm, cb * 512:(cb + 1) * 512], in_=ys[0:fm, :]),
                                      reads=[ys], writes=[])
                    S.barrier()
                with ExitStack() as sc:
                    S.scope = sc
                    ycs = [S.sbuf("hyYc%d" % i, [128, KT, 1024], BF16) for i in range(2)]
                    for cs in range(2):
                        S.op("dve", lambda e: e.memset(ycs[cs][:, KT - 1, :], 0.0), writes=[ycs[cs]])
                        S.dma("sp", lambda e: e.dma_start(
                            out=ycs[cs][:, 0:KT - 1, :], in_=hyYS[cs, 0:(KT - 1) * 128, :].rearrange("(k p) c -> p k c", p=128)),
                            reads=[hyYS], writes=[ycs[cs]])
                        S.dma("sp", lambda e: e.dma_start(out=ycs[cs][0:1, KT - 1, :], in_=hyYS[cs, (KT - 1) * 128:(KT - 1) * 128 + 1, :]),
                              reads=[hyYS], writes=[ycs[cs]])
                    slab = [S.sbuf("hyslabi%d" % i, [128, KT, 512], BF16) for i in range(2)]
                    bia = S.sbuf("hybia", [128, 8], F32)
                    S.dma("sp", lambda e: e.dma_start(out=bia[:], in_=R["hy_biasT"][l, o]), reads=[R["hy_biasT"]], writes=[bia])
                    uT = [S.sbuf("hyuT%d" % i, [128, 512], F32) for i in range(2)]
                    gT = [S.sbuf("hygT%d" % i, [128, 512], F32) for i in range(2)]
                    yo = [S.sbuf("hyyo%d" % i, [128, 512], F32) for i in range(2)]
                    yb = [S.sbuf("hyyb%d" % i, [128, 512], BF16) for i in range(2)]
                    ut = [S.sbuf("hyuti%d" % i, [128, 4, 128], BF16) for i in range(2)]
                    pm = [S.psum("hypmi%d" % i, [128, 512], F32) for i in range(4)]
                    ptr = [S.psum("hyptri%d" % i, [128, 8, 128], BF16) for i in range(2)]
                    it = 0
                    for tb in range((L + 511) // 512):
                        tw = min(512, L - tb * 512)
                        for cs in range(2):
                            S.dma("sp", lambda e: e.dma_start(out=slab[cs][:], in_=tab[cs, tb]), reads=[tab], writes=[slab[cs]])
                        for cc in range(8):
                            ps = pm[it % 4]
                            u_, g_, y_, yb_, ut_ = uT[it % 2], gT[it % 2], yo[it % 2], yb[it % 2], ut[it % 2]
                            it += 1
                            usrc = hyZ if o == 0 else hyY1
                            S.dma("sp", lambda e: e.dma_start(out=u_[:, 0:tw], in_=usrc[cc * 128:(cc + 1) * 128, tok0 + tb * 512:tok0 + tb * 512 + tw]),
                                  reads=[usrc], writes=[u_])
                            gr = (1 + o) * 1024 + cc * 128
                            S.dma("sp", lambda e: e.dma_start(out=g_[:, 0:tw], in_=hyZ[gr:gr + 128, tok0 + tb * 512:tok0 + tb * 512 + tw]),
                                  reads=[hyZ], writes=[g_])
                            n_mm = 2 * KT
                            i_mm = 0
                            for cs in range(2):
                                for kt in range(KT):
                                    kp = 128 if kt < KT - 1 else 1
                                    S.op("pe", lambda e: e.matmul(ps[:, 0:tw], lhsT=ycs[cs][0:kp, kt, cc * 128:(cc + 1) * 128],
                                                                  rhs=slab[cs][0:kp, kt, 0:tw], start=(i_mm == 0), stop=(i_mm == n_mm - 1)),
                                         reads=[ycs[cs], slab[cs]], writes=[ps])
                                    i_mm += 1
                            S.op("dve", lambda e: e.scalar_tensor_tensor(out=y_[:, 0:tw], in0=u_[:, 0:tw], scalar=bia[:, cc:cc + 1],
                                                                         in1=ps[:, 0:tw], op0=ALU.mult, op1=ALU.add),
                                 reads=[u_, bia, ps], writes=[y_])
                            if o == 0:
                                S.op("pool", lambda e: e.tensor_tensor(out=y_[:, 0:tw], in0=y_[:, 0:tw], in1=g_[:, 0:tw], op=ALU.mult),
                                     reads=[y_, g_], writes=[y_])
                                S.dma("sp", lambda e: e.dma_start(out=hyY1[cc * 128:(cc + 1) * 128, tok0 + tb * 512:tok0 + tb * 512 + tw],
                                                                  in_=y_[:, 0:tw]), reads=[y_], writes=[])
                                S.op("act", lambda e: e.activation(out=yb_[:, 0:tw], in_=y_[:, 0:tw], func=AF.Copy), reads=[y_], writes=[yb_])
                                pt = ptr[it % 2]
                                nt = tw // 128
                                for j in range(nt):
                                    S.op("pe", lambda e: e.transpose(pt[:, j, :], yb_[:, j * 128:(j + 1) * 128], ident[:]),
                                         reads=[yb_, ident], writes=[pt])
                                _evac(S, it, ut_[:, 0:nt, :], pt[:, 0:nt, :], reads=[pt], writes=[ut_])
                                S.dma("sp", lambda e: e.dma_start(
                                    out=hyU[tok0 + tb * 512:tok0 + tb * 512 + tw, cc * 128:(cc + 1) * 128].rearrange("(k p) c -> p k c", p=128),
                                    in_=ut_[:, 0:nt, :]), reads=[ut_], writes=[])
                            else:
                                S.op("pool", lambda e: e.tensor_tensor(out=yb_[:, 0:tw], in0=y_[:, 0:tw], in1=g_[:, 0:tw], op=ALU.mult),
                                     reads=[y_, g_], writes=[yb_])
                                S.dma("sp", lambda e: e.dma_start(out=brT[2][cc * 128:(cc + 1) * 128, tok0 + tb * 512:tok0 + tb * 512 + tw],
                                                                  in_=yb_[:, 0:tw]), reads=[yb_], writes=[])
                    S.barrier()
    S.scope = S.es


def build_program(dbg=False):
    nc = bass.Bass("TRN2", target_bir_lowering=False)
    k = K()
    k.nc = nc

    def ein(name, shape, dtype=F32):
        return Buf(nc.dram_tensor(name, list(shape), dtype, kind="ExternalInput").ap(), name)

    def eout(name, shape, dtype=F32):
        return Buf(nc.dram_tensor(name, list(shape), dtype, kind="ExternalOutput").ap(), name)

    x_in = ein("x_in", [TT, D])
    c2T = ein("c2T", [128, 16, 2])
    ident_in = ein("ident", [128, 128])
    norm1_g = ein("norm1_g", [DEPTH, D])
    w_ada = ein("w_ada", [DEPTH, D, 6 * D])
    b_ada = ein("b_ada", [DEPTH, 6 * D])
    w_in_T = ein("w_in_T", [DEPTH, D, NT_COLS])
    w_in_F = ein("w_in_F", [DEPTH, D, NF_ROWS])
    mla_kv_norm = ein("mla_kv_norm", [DEPTH, 256])
    R = {}
    R["ident_in"] = ident_in
    R["mla_kv_norm"] = mla_kv_norm
    R["mla_q_norm"] = ein("mla_q_norm", [DEPTH, 512])
    R["w_qb"] = ein("w_qb_p", [DEPTH, 512, 2048])
    R["w_kvb"] = ein("w_kvb_p", [DEPTH, 256, 2048])
    R["cache_ckv"] = ein("cache_ckv", [DEPTH, PAST, 256])
    R["cache_kr"] = ein("cache_kr", [DEPTH, PAST, 64])
    R["cache_nak"] = ein("cache_nak", [DEPTH, PAST, 1024])
    R["cache_nav"] = ein("cache_nav", [DEPTH, PAST, 1024])
    R["ropeT"] = ein("ropeT", [2, 64, LL])
    R["na_ctab"] = ein("na_ctab", [DEPTH, 8, 64, 31, 64])
    R["na_mask"] = ein("na_mask", [NA_NMASK, 128, 512])
    R["w_branch"] = ein("w_branch", [DEPTH, 4, 1024, 2048])
    R["w_out"] = ein("w_out", [DEPTH, D, D])
    R["norm2_g"] = ein("norm2_g", [DEPTH, D])
    R["peer_wq"] = ein("peer_wq", [DEPTH, D, D])
    R["peer_keysT"] = ein("peer_keysT", [DEPTH, 2, 128, 128])
    R["peer_u"] = [ein("peer_u%d" % i, [16384, D]) for i in range(DEPTH)]
    R["peer_v"] = [ein("peer_v%d" % i, [16384, D]) for i in range(DEPTH)]
    R["final_g"] = ein("final_g", [1, D])
    R["iota256"] = ein("iota256", [1, 256])
    _hy_inputs(R, ein)
    R["dn_consts"] = ein("dn_consts", [10, 128, 128])
    R["dn_convT"] = ein("dn_convT", [DEPTH, 128, 24, 5])
    R["dn_a_log"] = ein("dn_a_log", [DEPTH, 16])
    R["dn_dt_bias"] = ein("dn_dt_bias", [DEPTH, 16])
    R["dn_out_norm"] = ein("dn_out_norm", [DEPTH, 128])
    R["dn_state"] = [ein("dn_state%d" % i, [DEPTH, 8, 128, 128]) for i in range(2)]
    if DBG_BR:
        R["dbg_br"] = ein("dbg_br", [DEPTH, 2, 1024, TT], BF16)

    o_ckv = eout("o_ckv", [NCTX, DEPTH, LC, 256])
    o_kr = eout("o_kr", [NCTX, DEPTH, LC, 64])
    o_nak = eout("o_nak", [NCTX, DEPTH, LC, 1024])
    o_nav = eout("o_nav", [NCTX, DEPTH, LC, 1024])
    y_out = eout("y_out", [TT, D])
    o_dn = [eout("o_dn%d" % i, [NCTX, DEPTH, 8, 128, 128]) for i in range(2)]
    R["o_dn"] = o_dn
    outs = [o_ckv, o_kr, o_nak, o_nav, y_out] + o_dn
    R["o_ckv"] = o_ckv
    R["y_out"] = y_out

    with ExitStack() as es:
        S = Sched(nc, es)
        k.S = S
        modd = S.dram("modd", [2, 6 * D])
        P_T = S.dram("P_T", [TT, NT_COLS])
        P_F = S.dram("P_F", [NF_ROWS, TT])
        brT = [S.dram("brT%d" % b_, [1024, TT], BF16) for b_ in range(4)]
        R.update(P_T=P_T, P_F=P_F, brT=brT, modd=modd)
        R["mrg"] = S.dram("mrg", [TT, D])
        R["xbuf"] = S.dram("xbuf", [TT, D])
        R["mrgt"] = [Buf(R["mrg"].t[t_ * 128:(t_ + 1) * 128, :], "mrgt%d" % t_) for t_ in range(NTILE)]
        R["xbt"] = [Buf(R["xbuf"].t[t_ * 128:(t_ + 1) * 128, :], "xbt%d" % t_) for t_ in range(NTILE)]
        _hy_scratch(S, R)
        R["peer_ub"] = [S.dram("peer_ub%d" % i, [16384, D], BF16) for i in range(DEPTH)]
        R["peer_vb"] = [S.dram("peer_vb%d" % i, [16384, D], BF16) for i in range(DEPTH)]

        ident = S.sbuf("identb", [128, 128], BF16)
        S.dma("pool", lambda e: e.dma_start(out=ident[:], in_=ident_in[:, :]), reads=[ident_in], writes=[ident])
        R["ident"] = ident

        for l in range(1 if DBG_BR else DEPTH):
            with ExitStack() as sc:
                S.scope = sc
                cT = S.sbuf("cT", [128, 16, 2], F32)
                sT = S.sbuf("sT", [128, 16, 2], BF16)
                S.dma("sp", lambda e: e.dma_start(out=cT[:], in_=c2T[:, :, :]), reads=[c2T], writes=[cT])
                S.op("act", lambda e: e.activation(out=sT[:], in_=cT[:], func=AF.Silu), reads=[cT], writes=[sT])
                wts = [S.sbuf("adaw%d" % i, [128, 16, 512], BF16) for i in range(2)]
                bts = [S.sbuf("adab%d" % i, [2, 512], F32) for i in range(2)]
                mts = [S.sbuf("adam%d" % i, [2, 512], F32) for i in range(2)]
                pss = [S.psum("adap%d" % i, [2, 512], F32) for i in range(2)]
                for cb in range(24):
                    wt, bt, mt, ps = wts[cb % 2], bts[cb % 2], mts[cb % 2], pss[cb % 2]
                    c0 = cb * 512
                    S.dma("pool", lambda e: e.dma_start(
                        out=wt[:], in_=w_ada[l, :, c0:c0 + 512].rearrange("(k p) c -> p k c", p=128)),
                        reads=[w_ada], writes=[wt])
                    S.dma("sp", lambda e: e.dma_start(out=bt[:], in_=b_ada[l, c0:c0 + 512].partition_broadcast(2)),
                          reads=[b_ada], writes=[bt])
                    for kc in range(16):
                        S.op("pe", lambda e: e.matmul(ps[:, :], lhsT=sT[:, kc, :], rhs=wt[:, kc, :],
                                                      start=(kc == 0), stop=(kc == 15)),
                             reads=[sT, wt], writes=[ps])
                    S.op("dve", lambda e: e.tensor_tensor(out=mt[:], in0=ps[:], in1=bt[:], op=ALU.add),
                         reads=[ps, bt], writes=[mt])
                    S.dma("sp", lambda e: e.dma_start(out=modd[:, c0:c0 + 512], in_=mt[:]), reads=[mt], writes=[modd])
            S.barrier()

            x_cur = x_in if l == 0 else R["xbuf"]
            with ExitStack() as sc:
                S.scope = sc
                hT = S.sbuf("hT", [128, 16, TT], BF16)
                with ExitStack() as sc2:
                    S.scope = sc2
                    gm = [S.sbuf("gm%d" % c, [128, D], F32) for c in range(2)]
                    sh = [S.sbuf("sh%d" % c, [128, D], F32) for c in range(2)]
                    gt = S.sbuf("gt", [128, D], F32)
                    S.dma("sp", lambda e: e.dma_start(out=gt[:], in_=norm1_g[l, :].partition_broadcast(128)),
                          reads=[norm1_g], writes=[gt])
                    for c in range(2):
                        S.dma("sp", lambda e: e.dma_start(out=sh[c][:], in_=modd[c, 0:D].partition_broadcast(128)),
                              reads=[modd], writes=[sh[c]])
                        S.dma("sp", lambda e: e.dma_start(out=gm[c][:], in_=modd[c, D:2 * D].partition_broadcast(128)),
                              reads=[modd], writes=[gm[c]])
                        S.op("dve", lambda e: e.scalar_tensor_tensor(out=gm[c][:], in0=gm[c][:], scalar=1.0, in1=gt[:],
                                                                     op0=ALU.add, op1=ALU.mult),
                             reads=[gm[c], gt], writes=[gm[c]])
                    xts = [S.sbuf("xt%d" % i, [128, D], F32) for i in range(2)]
                    junk = S.sbuf("junk", [128, D], F32)
                    hb = [S.sbuf("hb%d" % i, [128, D], BF16) for i in range(2)]
                    ss = [S.sbuf("ss%d" % i, [128, 1], F32) for i in range(2)]
                    rs = [S.sbuf("rs%d" % i, [128, 1], F32) for i in range(2)]
                    ptr = [S.psum("ptr%d" % i, [128, 8, 128], BF16) for i in range(2)]
                    for t in range(NTILE):
                        c = 0 if t < (NCTX * LC) // 128 else 1
                        xt, hbt, sst, rst = xts[t % 2], hb[t % 2], ss[t % 2], rs[t % 2]
                        S.dma("sp", lambda e: e.dma_start(out=xt[:], in_=x_cur[t * 128:(t + 1) * 128, :]),
                              reads=[x_cur], writes=[xt])
                        S.op("act", lambda e: e.activation(out=junk[:], in_=xt[:], func=AF.Square, accum_out=sst[:]),
                             reads=[xt], writes=[junk, sst])
                        S.op("act", lambda e: e.activation(out=rst[:], in_=sst[:], func=AF.Sqrt, scale=1.0 / D, bias=EPS),
                             reads=[sst], writes=[rst])
                        S.op("dve", lambda e: e.reciprocal(out=rst[:], in_=rst[:]), reads=[rst], writes=[rst])
                        S.op("dve", lambda e: e.scalar_tensor_tensor(out=xt[:], in0=xt[:], scalar=rst[:, 0:1], in1=gm[c][:],
                                                                     op0=ALU.mult, op1=ALU.mult),
                             reads=[xt, rst, gm[c]], writes=[xt])
                        S.op("pool", lambda e: e.tensor_tensor(out=hbt[:], in0=xt[:], in1=sh[c][:], op=ALU.add),
                             reads=[xt, sh[c]], writes=[hbt])
                        for half in range(2):
                            pt = ptr[half]
                            for j in range(8):
                                kc = half * 8 + j
                                S.op("pe", lambda e: e.transpose(pt[:, j, :], hbt[:, kc * 128:(kc + 1) * 128], ident[:]),
                                     reads=[hbt, ident], writes=[pt])
                            _evac(S, half, hT[:, half * 8:(half + 1) * 8, t * 128:(t + 1) * 128], pt[:, :, :],
                                  reads=[pt], writes=[hT])
                    S.barrier()
                S.scope = sc
                wts = [S.sbuf("wint%d" % i, [128, 16, 512], BF16) for i in range(2)]
                stg = [S.sbuf("stg%d" % i, [128, 512], F32) for i in range(4)]
                pmm = [S.psum("pmm%d" % i, [128, 512], F32) for i in range(4)]
                nblk = (NT_COLS + 511) // 512
                ev = 0
                for cb in range(nblk):
                    c0 = cb * 512
                    cw = min(512, NT_COLS - c0)
                    wt = wts[cb % 2]
                    S.dma("pool", lambda e: e.dma_start(
                        out=wt[:, :, 0:cw], in_=w_in_T[l, :, c0:c0 + cw].rearrange("(k p) c -> p k c", p=128)),
                        reads=[w_in_T], writes=[wt])
                    for t in range(NTILE):
                        ps, st = pmm[ev % 4], stg[ev % 4]
                        for kc in range(16):
                            S.op("pe", lambda e: e.matmul(ps[:, 0:cw], lhsT=hT[:, kc, t * 128:(t + 1) * 128],
                                                          rhs=wt[:, kc, 0:cw], start=(kc == 0), stop=(kc == 15)),
                                 reads=[hT, wt], writes=[ps])
                        _evac(S, ev, st[:, 0:cw], ps[:, 0:cw], reads=[ps], writes=[st])
                        S.dma("sp", lambda e: e.dma_start(out=P_T[t * 128:(t + 1) * 128, c0:c0 + cw], in_=st[:, 0:cw]),
                              reads=[st], writes=[])
                        ev += 1
                nblk = (NF_ROWS + 511) // 512
                for cb in range(nblk):
                    c0 = cb * 512
                    cw = min(512, NF_ROWS - c0)
                    wt = wts[cb % 2]
                    S.dma("pool", lambda e: e.dma_start(
                        out=wt[:, :, 0:cw], in_=w_in_F[l, :, c0:c0 + cw].rearrange("(k p) c -> p k c", p=128)),
                        reads=[w_in_F], writes=[wt])
                    for sb in range((cw + 127) // 128):
                        r0 = c0 + sb * 128
                        rw = min(128, NF_ROWS - r0)
                        for g in range(TT // 512):
                            ps, st = pmm[ev % 4], stg[ev % 4]
                            for kc in range(16):
                                S.op("pe", lambda e: e.matmul(ps[0:rw, :], lhsT=wt[:, kc, sb * 128:sb * 128 + rw],
                                                              rhs=hT[:, kc, g * 512:(g + 1) * 512],
                                                              start=(kc == 0), stop=(kc == 15)),
                                     reads=[hT, wt], writes=[ps])
                            _evac(S, ev, st[0:rw, :], ps[0:rw, :], reads=[ps], writes=[st])
                            S.dma("sp", lambda e: e.dma_start(out=P_F[r0:r0 + rw, g * 512:(g + 1) * 512], in_=st[0:rw, :]),
                                  reads=[st], writes=[])
                            ev += 1
            S.barrier()
            S.scope = es

            for s_ in range(NCTX):
                for (nm, ob) in (("kr", o_kr), ("nak", o_nak), ("nav", o_nav)):
                    a0, aw = T_COLS[nm]
                    S.dma("sp", lambda e: e.dma_start(out=ob[s_, l, :, :], in_=P_T[s_ * LC:(s_ + 1) * LC, a0:a0 + aw]),
                          reads=[P_T], writes=[ob])
            if DBG_BR:
                for b_ in DBG_FILL:
                    S.dma("sp", lambda e: e.dma_start(out=brT[b_][:, :], in_=R["dbg_br"][l, b_ - 1]),
                          reads=[R["dbg_br"]], writes=[brT[b_]])
            if not DBG_BR:
                for src_, dst_ in ((R["peer_u"][l], R["peer_ub"][l]), (R["peer_v"][l], R["peer_vb"][l])):
                    for r_ in range(0, 16384, 1024):
                        S.dma("pool", lambda e: e.dma_start(out=dst_[r_:r_ + 1024, :], in_=src_[r_:r_ + 1024, :]),
                              reads=[src_], writes=[dst_])
            stage_dn(S, l, R)
            if not DBG_BR:
                stage_hyena(S, l, R)
                stage_mla(S, l, R)
                stage_na(S, l, R)
            if DBG_BR:
                for nm_, b_ in (("d_brT1", 1),):
                    db = eout(nm_, [1024, TT], BF16)
                    outs.append(db)
                    S.dma("sp", lambda e: e.dma_start(out=db[:, :], in_=brT[b_][:, :]), reads=[brT[b_]], writes=[db])
            if not DBG_BR:
                stage_merge(S, l, R, x_in if l == 0 else R["xbuf"])
                stage_peer(S, l, R, last=(l == DEPTH - 1))

        for b in outs:
            if b.last_w is not None:
                S._wait("sp", b.last_w[0], b.last_w[1])
        S.barrier()
        k.ninstr = S.ninstr
    return nc, k


def _prep_weights(inp):
    w_in = inp["w_in"]
    tcols = np.concatenate([
        np.arange(O_CQ, O_CQ + 512), np.arange(O_CKV, O_CKV + 256), np.arange(O_KR, O_KR + 64),
        np.arange(O_Z, O_Z + 1024), np.arange(O_A, O_A + 16), np.arange(O_B, O_B + 16),
        np.arange(O_NA + 1024, O_NA + 2048), np.arange(O_NA + 2048, O_NA + 3072),
        np.arange(O_GATE, O_GATE + 8192)])
    sw = np.concatenate([np.arange(16, 32), np.arange(0, 16), np.arange(48, 64), np.arange(32, 48)])
    fcols = np.concatenate([
        np.arange(O_KR, O_KR + 64), O_KR + sw, np.arange(O_DN, O_DN + 3072), np.arange(O_HY, O_HY + 3072),
        np.arange(O_NA, O_NA + 1024), np.arange(O_NA + 1024, O_NA + 2048)])
    assert len(tcols) == NT_COLS and len(fcols) == NF_ROWS
    return np.ascontiguousarray(w_in[:, :, tcols]), np.ascontiguousarray(w_in[:, :, fcols])


def _prep_consts(inp):
    C = {}
    sw = np.concatenate([np.arange(16, 32), np.arange(0, 16), np.arange(48, 64), np.arange(32, 48)])
    nope = np.concatenate([np.arange(h * 192, h * 192 + 128) for h in range(8)])
    rope = np.concatenate([np.arange(h * 192 + 128, h * 192 + 192) for h in range(8)])
    ropesw = np.concatenate([h * 192 + 128 + sw for h in range(8)])
    C["w_qb_p"] = np.ascontiguousarray(inp["mla_w_qb"][:, :, np.concatenate([nope, rope, ropesw])])
    kn = np.concatenate([np.arange(h * 256, h * 256 + 128) for h in range(8)])
    vv = np.concatenate([np.arange(h * 256 + 128, h * 256 + 256) for h in range(8)])
    C["w_kvb_p"] = np.ascontiguousarray(inp["mla_w_kvb"][:, :, np.concatenate([kn, vv])])
    t = np.arange(LL)
    pos = [t // 64, t % 64]
    inv = 10000.0 ** (-np.arange(0, 32, 2, dtype=np.float32) / 32.0)
    cosT = np.zeros((64, LL), np.float32)
    sinT = np.zeros((64, LL), np.float32)
    for d in range(64):
        half, j = d // 32, d % 32
        ang = pos[half].astype(np.float32) * inv[j % 16]
        cosT[d] = np.cos(ang)
        sinT[d] = (-np.sin(ang)) if j < 16 else np.sin(ang)
    C["ropeT"] = np.stack([cosT, sinT], 0).astype(np.float32)
    rpb = inp["na_rpb"]
    NEG = np.float32(-30000.0)
    cp = np.arange(64)[:, None]
    c = np.arange(64)[None, :]
    cstart = np.clip(c - 8, 0, 48)
    cvalid = (cp >= cstart) & (cp < cstart + 16)
    dcol = np.clip(cp - c + 15, 0, 30)
    ctab = np.full((DEPTH, 8, 64, 31, 64), NEG, np.float32)
    for mm in range(31):
        dr = 22 - mm
        if 0 <= dr <= 14:
            g = rpb[:, :, dr, :][:, :, dcol]
            ctab[:, :, :, mm, :] = np.where(cvalid[None, None], g, NEG)
    C["na_ctab"] = ctab
    mask = np.full((NA_NMASK, 128, 512), NEG, np.float32)
    for (qb, kt), mi in NA_MASK_IDX.items():
        for rr in range(2):
            rp = 2 * kt + rr
            for j in range(8):
                r = 8 * qb + j
                st_ = min(max(r - 4, 0), 24)
                if st_ <= rp < st_ + 8:
                    mask[mi, rr * 64:(rr + 1) * 64, j * 64:(j + 1) * 64] = 0.0
    C["na_mask"] = mask
    C["peer_keysT"] = np.ascontiguousarray(inp["peer_keys"].transpose(0, 1, 3, 2))
    ii = np.arange(128)[:, None]
    jj = np.arange(128)[None, :]
    NEGM = np.float32(-30000.0)
    dc = np.zeros((10, 128, 128), np.float32)
    dc[9] = ((ii // 64) == (jj // 64))
    dc[0] = (ii <= jj)
    dc[1] = (ii >= jj)
    dc[2] = np.eye(128)
    dc[3] = (ii > jj)
    dc[4] = (ii < jj)
    dc[5] = np.where(ii > jj, 0.0, NEGM)
    dc[6] = np.where(ii < jj, 0.0, NEGM)
    dc[7] = np.where(jj >= ii, 0.0, NEGM)
    dc[8] = np.where(jj <= ii, 0.0, NEGM)
    C["dn_consts"] = dc
    C["dn_convT"] = np.ascontiguousarray(inp["dn_conv"].reshape(DEPTH, 5, 24, 128).transpose(0, 3, 2, 1))
    return C


_CACHE = {}
_DBG = {}


def kernel(**inp):
    inp = {k_: np.asarray(v) for k_, v in inp.items()}
    if "prog" not in _CACHE:
        _CACHE["prog"] = build_program()
    nc, kk = _CACHE["prog"]
    w_in_T, w_in_F = _prep_weights(inp)
    C = _prep_consts(inp)
    H = _hy_host(inp)
    ident = np.eye(128, dtype=np.float32)
    in_maps = []
    for i in range(NCORE):
        b = i // 2
        x_in = np.concatenate([inp["x_prompt"][2 * i].reshape(LC, D), inp["x_prompt"][2 * i + 1].reshape(LC, D),
                               inp["x_sample"][b].reshape(LL, D)], axis=0)
        c2 = np.stack([inp["c_ctx"], inp["c"][b]], axis=0)
        c2T = np.ascontiguousarray(c2.reshape(2, 16, 128).transpose(2, 1, 0))
        in_maps.append({
            "x_in": np.ascontiguousarray(x_in), "c2T": c2T, "ident": ident,
            "norm1_g": inp["norm1_g"], "w_ada": inp["w_ada"], "b_ada": inp["b_ada"],
            "w_in_T": w_in_T, "w_in_F": w_in_F, "mla_kv_norm": inp["mla_kv_norm"],
            "mla_q_norm": inp["mla_q_norm"], "w_qb_p": C["w_qb_p"], "w_kvb_p": C["w_kvb_p"],
            "cache_ckv": inp["cache_mla_ckv"][b], "cache_kr": inp["cache_mla_krope"][b],
            "cache_nak": np.ascontiguousarray(inp["cache_na_k"][b].reshape(DEPTH, PAST, 1024)),
            "cache_nav": np.ascontiguousarray(inp["cache_na_v"][b].reshape(DEPTH, PAST, 1024)),
            "ropeT": C["ropeT"], "na_ctab": C["na_ctab"], "na_mask": C["na_mask"],
            "w_branch": inp["w_branch"], "w_out": inp["w_out"], "norm2_g": inp["norm2_g"],
            "peer_wq": inp["peer_wq"], "peer_keysT": C["peer_keysT"],
            "peer_u0": inp["peer_u"][0], "peer_u1": inp["peer_u"][1],
            "peer_v0": inp["peer_v"][0], "peer_v1": inp["peer_v"][1],
            "final_g": inp["final_g"].reshape(1, D), "iota256": np.arange(256, dtype=np.float32).reshape(1, 256),
            "dn_consts": C["dn_consts"], "dn_convT": C["dn_convT"],
            "dn_a_log": inp["dn_a_log"].reshape(DEPTH, 16), "dn_dt_bias": inp["dn_dt_bias"].reshape(DEPTH, 16),
            "dn_out_norm": inp["dn_out_norm"],
            "dn_state0": inp["state_dn_fwd"][b], "dn_state1": inp["state_dn_bwd"][b],
        })
        in_maps[-1].update(H)
        if DBG_BR:
            in_maps[-1]["dbg_br"] = _DBG["br"]
    import os
    ndev = int(os.environ.get("KDEV_CORES", NCORE))
    res = run_bass_kernel_spmd(nc, in_maps[:ndev], core_ids=list(range(ndev)))
    r = list(res.results) + [res.results[0]] * (NCORE - ndev)
    _DBG["res"] = res.results[0]
    B = 16
    y_prompt = np.concatenate([r[i]["y_out"][:NCTX * LC].reshape(NCTX, LC, D) for i in range(NCORE)], axis=0)
    y_sample = np.stack([r[2 * j]["y_out"][NCTX * LC:] for j in range(4)], axis=0)
    new_ckv = np.concatenate([r[i]["o_ckv"] for i in range(NCORE)], axis=0)
    new_kr = np.concatenate([r[i]["o_kr"] for i in range(NCORE)], axis=0)
    new_nak = np.concatenate([r[i]["o_nak"] for i in range(NCORE)], axis=0).reshape(B, DEPTH, LC, 8, 128)
    new_nav = np.concatenate([r[i]["o_nav"] for i in range(NCORE)], axis=0).reshape(B, DEPTH, LC, 8, 128)
    new_dnf = np.concatenate([r[i]["o_dn0"] for i in range(NCORE)], axis=0)
    new_dnb = np.concatenate([r[i]["o_dn1"] for i in range(NCORE)], axis=0)
    return (y_prompt, y_sample, new_ckv, new_kr, new_nak, new_nav, new_dnf, new_dnb)
```

```python
import numpy as np
from contextlib import ExitStack
import concourse.bass as bass
import concourse.mybir as mybir
from concourse.bass_utils import run_bass_kernel_spmd

F32 = mybir.dt.float32
BF16 = mybir.dt.bfloat16
I32 = mybir.dt.int32
U32 = mybir.dt.uint32
U16 = mybir.dt.uint16
AF = mybir.ActivationFunctionType
ALU = mybir.AluOpType
AX = mybir.AxisListType

D = 2048
DEPTH = 2
NCORE = 8
LC = 256
LL = 2048
PAST = 512
NCTX = 2
TT = NCTX * LC + LL
NTILE = TT // 128
EPS = 1e-6

T_COLS = {}
_o = 0
for _n, _s in (("cq", 512), ("ckv", 256), ("kr", 64), ("z", 1024), ("a", 16), ("b", 16),
               ("nak", 1024), ("nav", 1024), ("gate", 8192)):
    T_COLS[_n] = (_o, _s)
    _o += _s
NT_COLS = _o
F_ROWS = {}
_o = 0
for _n, _s in (("krT", 64), ("krswT", 64), ("dnT", 3072), ("hyT", 3072), ("naqT", 1024), ("nakT", 1024)):
    F_ROWS[_n] = (_o, _s)
    _o += _s
NF_ROWS = _o

IN_SIZES = (512, 256, 64, 3072, 1024, 16, 16, 3072, 3072, 8192)
IN_OFF = np.concatenate([[0], np.cumsum(IN_SIZES)]).astype(int)
(O_CQ, O_CKV, O_KR, O_DN, O_Z, O_A, O_B, O_HY, O_NA, O_GATE) = [int(v) for v in IN_OFF[:-1]]


class Buf:
    __slots__ = ("t", "last_w", "readers", "name", "root")

    def __init__(self, t, name, root=None):
        self.t = t
        self.name = name
        self.last_w = None
        self.readers = {}
        self.root = root if root is not None else self

    def __getitem__(self, idx):
        return self.t[idx]


class Sched:
    ENG = ("pe", "act", "dve", "pool", "sp")
    NDMA = 10

    def __init__(self, nc, es):
        self.nc = nc
        self.es = es
        self.eng = {"pe": nc.tensor, "act": nc.scalar, "dve": nc.vector, "pool": nc.gpsimd, "sp": nc.sync}
        self.sem = {}
        self.cnt = {}
        for e in self.ENG:
            self.sem[e] = es.enter_context(nc.semaphore("sem_" + e))
            self.cnt[e] = 0
        self.dq = {}
        for q in ("sp", "pool"):
            sl = []
            for i in range(self.NDMA):
                key = ("dma", q, i)
                self.sem[key] = es.enter_context(nc.semaphore("dsem_%s_%d" % (q, i)))
                self.cnt[key] = 0
                sl.append(key)
            self.dq[q] = [sl, 0]
        self.seen = {e: {} for e in self.ENG}
        self.ninstr = 0
        self.scope = es

    def _nm(self, name):
        self.nid = getattr(self, "nid", 0) + 1
        return "%s_%d" % (name, self.nid)

    def sbuf(self, name, shape, dtype=F32):
        name = self._nm(name)
        return Buf(self.scope.enter_context(self.nc.sbuf_tensor(name, list(shape), dtype)), name)

    def psum(self, name, shape, dtype=F32):
        name = self._nm(name)
        return Buf(self.scope.enter_context(self.nc.psum_tensor(name, list(shape), dtype)), name)

    def dram(self, name, shape, dtype=F32, kind="Internal"):
        t = self.nc.dram_tensor(name, list(shape), dtype, kind=kind)
        return Buf(t.ap(), name)

    def _wait(self, e, key, val):
        if self.seen[e].get(key, 0) >= val:
            return
        self.eng[e].wait_ge(self.sem[key], val)
        self.seen[e][key] = val
        self.ninstr += 1

    def _deps(self, e, reads, writes):
        reads = [b.root for b in reads]
        writes = [b.root for b in writes]
        need = {}
        for b in list(reads) + list(writes):
            if b.last_w is not None:
                k, v = b.last_w
                if need.get(k, 0) < v:
                    need[k] = v
        for b in writes:
            for k, v in b.readers.items():
                if need.get(k, 0) < v:
                    need[k] = v
        for k, v in need.items():
            if k == "pe" and e == "pe":
                continue
            self._wait(e, k, v)

    def _mark(self, ev, reads, writes):
        reads = [b.root for b in reads]
        writes = [b.root for b in writes]
        k, v = ev
        for b in writes:
            b.last_w = ev
            b.readers = {}
        for b in reads:
            if b.readers.get(k, 0) < v:
                b.readers[k] = v

    def op(self, e, fn, reads=(), writes=()):
        self._deps(e, reads, writes)
        ins = fn(self.eng[e])
        self.cnt[e] += 1
        ins.then_inc(self.sem[e], 1)
        self._mark((e, self.cnt[e]), reads, writes)
        self.ninstr += 1
        return ins

    def dma(self, q, fn, reads=(), writes=()):
        sl, i = self.dq[q]
        key = sl[i % self.NDMA]
        self.dq[q][1] = i + 1
        if self.cnt[key] > 0:
            self._wait(q, key, self.cnt[key])
        self._deps(q, reads, writes)
        ins = fn(self.eng[q])
        self.cnt[key] += 16
        ins.then_inc(self.sem[key], 16)
        self._mark((key, self.cnt[key]), reads, writes)
        self.ninstr += 1
        return ins

    def barrier(self):
        for e in self.ENG:
            for key, v in self.cnt.items():
                if v > 0:
                    self._wait(e, key, v)


import os as _os
DBG_BR = bool(int(_os.environ.get("KDBG_BR", "0")))
DBG_FILL = (2,)
NTILE_PEER = 0
DNSTOP = int(_os.environ.get("KDN_STOP", "9"))
DNVAR = int(_os.environ.get("KDN_VAR", "0"))
DNSEQ = int(_os.environ.get("KDN_SEQ", "3"))


def _hy_inputs(R, ein):
    R["hy_tab"] = []
    R["hy_zemb"] = []
    R["hy_wf"] = []
    R["hy_win"] = []
    for i, L in enumerate((LC, LL)):
        nf, KT, NB = (L + 1), (L + 1 + 127) // 128, (L + 1 + 511) // 512
        R["hy_tab"].append(ein("hy_tab%d" % i, [2, NB, 128, KT, 512], BF16))
        R["hy_zemb"].append(ein("hy_zemb%d" % i, [33, L]))
        R["hy_wf"].append(ein("hy_wf%d" % i, [128, KT]))
        R["hy_win"].append(ein("hy_win%d" % i, [L, 1024]))
    R["hy_w1"] = ein("hy_w1", [DEPTH, 33, 64])
    R["hy_w2"] = ein("hy_w2", [DEPTH, 64, 64])
    R["hy_w3"] = ein("hy_w3", [DEPTH, 64, 4096])
    R["hy_b12"] = ein("hy_b12", [DEPTH, 64, 2])
    R["hy_convT"] = ein("hy_convT", [DEPTH, 128, 24, 3])
    R["hy_biasT"] = ein("hy_biasT", [DEPTH, 2, 128, 8])


def _hy_scratch(S, R):
    R["hyZ"] = S.dram("hyZ", [3072, TT])
    R["hyY1"] = S.dram("hyY1", [1024, TT])
    R["hyU"] = S.dram("hyU", [TT, 1024], BF16)
    R["hyPQ"] = [S.dram("hyPQ%d" % i, [2, 2, ((L + 1 + 127) // 128) * 128, 1024]) for i, L in enumerate((LC, LL))]
    R["hyYS"] = S.dram("hyYS", [2, ((LL + 1 + 127) // 128) * 128, 1024], BF16)


def _hy_host(inp):
    import ml_dtypes
    H = {}
    for i, L in enumerate((LC, LL)):
        nf, KT, NB = (L + 1), (L + 1 + 127) // 128, (L + 1 + 511) // 512
        r = np.arange(KT * 128, dtype=np.int64)
        q = np.arange(NB * 512, dtype=np.int64)
        prod = (r[:, None] * q[None, :]) % (2 * L)
        ang = np.pi * prod.astype(np.float64) / L
        valid = (r[:, None] <= L) & (q[None, :] <= L)
        tabs = []
        for fn in (np.cos, np.sin):
            T = np.where(valid, fn(ang), 0.0).astype(np.float32)
            T = T.reshape(KT, 128, NB, 512).transpose(2, 1, 0, 3)
            tabs.append(T)
        H["hy_tab%d" % i] = np.ascontiguousarray(np.stack(tabs, 0)).astype(ml_dtypes.bfloat16)
        f = np.arange(KT * 128)
        wf = np.where((f == 0) | (f == L), 1.0, np.where(f < L, 2.0, 0.0)) / (2.0 * L)
        H["hy_wf%d" % i] = np.ascontiguousarray(wf.reshape(KT, 128).T).astype(np.float32)
        t01 = np.linspace(0.0, 1.0, L, dtype=np.float32)[:, None]
        w = (np.float32(2.0 * np.pi) * np.arange(L, dtype=np.float32)[:, None] / np.float32(L)).astype(np.float32)
        fr = np.linspace(1e-4, 15.0, 16, dtype=np.float32)[None, :]
        z = np.concatenate([t01, np.cos(fr * w), -np.sin(fr * w)], axis=-1).astype(np.float32)
        H["hy_zemb%d" % i] = np.ascontiguousarray(z.T)
        max_decay = np.log(1e-2) / 0.3
        min_decay = np.log(1e-2) / 1.5
        deltas = np.linspace(min_decay, max_decay, 1024, dtype=np.float32)
        H["hy_win%d" % i] = np.exp(-t01 * np.abs(deltas)[None, :]).astype(np.float32)
    H["hy_w1"] = inp["hy_w1"]
    H["hy_w2"] = inp["hy_w2"]
    H["hy_w3"] = inp["hy_w3"]
    H["hy_b12"] = np.ascontiguousarray(np.stack([inp["hy_b1"], inp["hy_b2"]], axis=-1))
    H["hy_convT"] = np.ascontiguousarray(inp["hy_conv"].reshape(DEPTH, 3, 24, 128).transpose(0, 3, 2, 1))
    H["hy_biasT"] = np.ascontiguousarray(inp["hy_bias"].reshape(DEPTH, 2, 8, 128).transpose(0, 1, 3, 2))
    return H


class K:
    pass


def _evac(S, i, out_ap, in_ap, reads, writes):
    if i % 2 == 0:
        S.op("act", lambda e: e.activation(out=out_ap, in_=in_ap, func=AF.Copy), reads=reads, writes=writes)
    else:
        S.op("dve", lambda e: e.tensor_copy(out=out_ap, in_=in_ap), reads=reads, writes=writes)


def _rmsnorm_rows(S, xt, g, n, jk, st):
    S.op("act", lambda e: e.activation(out=jk[:, 0:n], in_=xt[:, 0:n], func=AF.Square, accum_out=st[:]),
         reads=[xt], writes=[jk, st])
    S.op("act", lambda e: e.activation(out=st[:], in_=st[:], func=AF.Sqrt, scale=1.0 / n, bias=EPS),
         reads=[st], writes=[st])
    S.op("dve", lambda e: e.reciprocal(out=st[:], in_=st[:]), reads=[st], writes=[st])
    S.op("dve", lambda e: e.scalar_tensor_tensor(out=xt[:, 0:n], in0=xt[:, 0:n], scalar=st[:, 0:1], in1=g[:, 0:n],
                                                 op0=ALU.mult, op1=ALU.mult),
         reads=[xt, st, g], writes=[xt])


def _attn(S, A, qparts, kparts, v_ap, ktl, q0, QB, scale, bias_fn, out_dst):
    psO, psD = A["psO"][A["n"] % 2], A["psD"][A["n"] % 2]
    A["n"] += 1
    n = len(ktl)
    for i, kt in enumerate(ktl):
        ps = A["psS"][A["ns"] % 2]
        pT = A["pT"][A["ns"] % 3]
        A["ns"] += 1
        for pi in range(len(qparts)):
            kb, kf = kparts[pi]
            qb_, qf = qparts[pi]
            S.op("pe", lambda e: e.matmul(ps[:, 0:QB], lhsT=kf(kt), rhs=qf(q0, QB), start=(pi == 0),
                                          stop=(pi == len(qparts) - 1)), reads=[kb, qb_], writes=[ps])
        bb = bias_fn(kt) if bias_fn is not None else None
        if bb is not None:
            tf = A["tf"][A["ns"] % 2]
            S.op("dve", lambda e: e.scalar_tensor_tensor(out=tf[:, 0:QB], in0=ps[:, 0:QB], scalar=float(scale),
                                                         in1=bb[:, 0:QB], op0=ALU.mult, op1=ALU.add),
                 reads=[ps, bb], writes=[tf])
            S.op("act", lambda e: e.activation(out=pT[:, 0:QB], in_=tf[:, 0:QB], func=AF.Exp), reads=[tf], writes=[pT])
        else:
            S.op("act", lambda e: e.activation(out=pT[:, 0:QB], in_=ps[:, 0:QB], func=AF.Exp, scale=float(scale)),
                 reads=[ps], writes=[pT])
        vb, va = v_ap(kt)
        S.op("pe", lambda e: e.matmul(psO[:, 0:QB], lhsT=va, rhs=pT[:, 0:QB], start=(i == 0), stop=(i == n - 1)),
             reads=[vb, pT], writes=[psO])
        S.op("pe", lambda e: e.matmul(psD[:, 0:QB], lhsT=A["ones"][:, :], rhs=pT[:, 0:QB], start=(i == 0),
                                      stop=(i == n - 1)), reads=[A["ones"], pT], writes=[psD])
    rd = A["rd"]
    ob = A["ob"][A["n"] % 2]
    S.op("dve", lambda e: e.reciprocal(out=rd[:, 0:QB], in_=psD[:, 0:QB]), reads=[psD], writes=[rd])
    S.op("dve", lambda e: e.tensor_tensor(out=ob[:, 0:QB], in0=psO[:, 0:QB], in1=rd[:, 0:QB], op=ALU.mult),
         reads=[psO, rd], writes=[ob])
    dbuf, dap = out_dst
    S.dma("sp", lambda e: e.dma_start(out=dap, in_=ob[:, 0:QB]), reads=[ob], writes=[])


def _attn_res(S):
    A = {"n": 0, "ns": 0}
    A["psS"] = [S.psum("psS%d" % i, [128, 512], F32) for i in range(2)]
    A["psO"] = [S.psum("psO%d" % i, [128, 512], F32) for i in range(2)]
    A["psD"] = [S.psum("psD%d" % i, [128, 512], F32) for i in range(2)]
    A["pT"] = [S.sbuf("pT%d" % i, [128, 512], BF16) for i in range(3)]
    A["tf"] = [S.sbuf("tf%d" % i, [128, 512], F32) for i in range(2)]
    A["rd"] = S.sbuf("rd", [128, 512], F32)
    A["ob"] = [S.sbuf("ob%d" % i, [128, 512], BF16) for i in range(2)]
    ones = S.sbuf("onesb", [128, 128], BF16)
    S.op("dve", lambda e: e.memset(ones[:], 1.0), writes=[ones])
    A["ones"] = ones
    return A


def _transpose_rows(S, src, ncol, dst, dst_fn, ident, ptr, ev0=0):
    nch = ncol // 128
    for c0 in range(0, nch, 8):
        pt = ptr[(ev0 + c0 // 8) % 2]
        nn = min(8, nch - c0)
        for j in range(nn):
            S.op("pe", lambda e: e.transpose(pt[:, j, :], src[:, (c0 + j) * 128:(c0 + j + 1) * 128], ident[:]),
                 reads=[src, ident], writes=[pt])
        for j in range(nn):
            _evac(S, j, dst_fn(c0 + j), pt[:, j, :], reads=[pt], writes=[dst])


def stage_mla(S, l, R):
    ident = R["ident"]
    P_T, P_F, brT = R["P_T"], R["P_F"], R["brT"]
    with ExitStack() as sc:
        S.scope = sc
        A = _attn_res(S)
        psP = [S.psum("psP%d" % i, [128, 512], F32) for i in range(2)]
        wqb = S.sbuf("wqb", [128, 4, 2048], BF16)
        wkvb = S.sbuf("wkvb", [128, 2, 2048], BF16)
        S.dma("pool", lambda e: e.dma_start(out=wqb[:], in_=R["w_qb"][l].rearrange("(k p) c -> p k c", p=128)),
              reads=[R["w_qb"]], writes=[wqb])
        S.dma("pool", lambda e: e.dma_start(out=wkvb[:], in_=R["w_kvb"][l].rearrange("(k p) c -> p k c", p=128)),
              reads=[R["w_kvb"]], writes=[wkvb])
        gq = S.sbuf("gq", [128, 512], F32)
        gk = S.sbuf("gk", [128, 256], F32)
        S.dma("sp", lambda e: e.dma_start(out=gq[:], in_=R["mla_q_norm"][l, :].partition_broadcast(128)),
              reads=[R["mla_q_norm"]], writes=[gq])
        S.dma("sp", lambda e: e.dma_start(out=gk[:], in_=R["mla_kv_norm"][l, :].partition_broadcast(128)),
              reads=[R["mla_kv_norm"]], writes=[gk])
        ropc = S.sbuf("ropc", [64, LL], F32)
        rops = S.sbuf("rops", [64, LL], F32)
        S.dma("sp", lambda e: e.dma_start(out=ropc[:], in_=R["ropeT"][0]), reads=[R["ropeT"]], writes=[ropc])
        S.dma("sp", lambda e: e.dma_start(out=rops[:], in_=R["ropeT"][1]), reads=[R["ropeT"]], writes=[rops])
        cqT = S.sbuf("cqT", [128, 4, LL], BF16)
        ckvT = S.sbuf("ckvT", [128, 2, LL + PAST], BF16)
        krT = S.sbuf("krT", [128, LL + PAST], BF16)
        vall = S.sbuf("vall", [128, (LL + PAST) // 128, 1024], BF16)
        knT = S.sbuf("knT", [128, LL + PAST], BF16)
        qnT = S.sbuf("qnT", [128, LL], BF16)
        qrT = S.sbuf("qrT", [128, LL], BF16)
        S.op("dve", lambda e: e.memset(krT[64:128, :], 0.0), writes=[krT])
        S.op("dve", lambda e: e.memset(qrT[64:128, :], 0.0), writes=[qrT])
        xt = [S.sbuf("mx%d" % i, [128, 512], F32) for i in range(2)]
        xb = [S.sbuf("mxb%d" % i, [128, 512], BF16) for i in range(2)]
        jk = S.sbuf("mjk", [128, 512], F32)
        st = [S.sbuf("mst%d" % i, [128, 1], F32) for i in range(2)]
        r1 = S.sbuf("mr1", [64, 512], F32)
        r2 = S.sbuf("mr2", [64, 512], F32)
        ptr = [S.psum("mptr%d" % i, [128, 8, 128], BF16) for i in range(0)]
        aq, _ = T_COLS["cq"]
        ak, _ = T_COLS["ckv"]
        akr, _ = T_COLS["kr"]
        fkr, _ = F_ROWS["krT"]
        fks, _ = F_ROWS["krswT"]

        def tr_bf(src, ncol, dst, dst_fn, ev):
            nch = ncol // 128
            pt = psP[ev % 2]
            ptv = pt[:, :].bitcast(BF16)
            for j in range(nch):
                S.op("pe", lambda e: e.transpose(ptv[:, j * 128:(j + 1) * 128], src[:, j * 128:(j + 1) * 128], ident[:]),
                     reads=[src, ident], writes=[pt])
            for j in range(nch):
                _evac(S, j, dst_fn(j), ptv[:, j * 128:(j + 1) * 128], reads=[pt], writes=[dst])

        seqs = [(s * LC, LC, False) for s in range(NCTX)] + [(NCTX * LC, LL, True)]
        for si, (tok0, L, latent) in enumerate(seqs):
            Lk = L + (PAST if latent else 0)
            nkt = Lk // 128
            QB = 512 if latent else 256
            for t in range(L // 128):
                g0 = tok0 + t * 128
                x1, x1b, s1 = xt[t % 2], xb[t % 2], st[t % 2]
                S.dma("sp", lambda e: e.dma_start(out=x1[:, 0:512], in_=P_T[g0:g0 + 128, aq:aq + 512]),
                      reads=[P_T], writes=[x1])
                _rmsnorm_rows(S, x1, gq, 512, jk, s1)
                S.op("pool", lambda e: e.tensor_copy(out=x1b[:, 0:512], in_=x1[:, 0:512]), reads=[x1], writes=[x1b])
                tr_bf(x1b, 512, cqT, lambda j: cqT[:, j, t * 128:(t + 1) * 128], t)
            for t in range(nkt):
                x1, x1b, s1 = xt[t % 2], xb[t % 2], st[t % 2]
                if t < L // 128:
                    g0 = tok0 + t * 128
                    S.dma("sp", lambda e: e.dma_start(out=x1[:, 0:256], in_=P_T[g0:g0 + 128, ak:ak + 256]),
                          reads=[P_T], writes=[x1])
                    _rmsnorm_rows(S, x1, gk, 256, jk, s1)
                    if not latent:
                        S.dma("sp", lambda e: e.dma_start(out=R["o_ckv"][si, l, t * 128:(t + 1) * 128, :], in_=x1[:, 0:256]),
                              reads=[x1], writes=[R["o_ckv"]])
                else:
                    p0 = (t - L // 128) * 128
                    S.dma("sp", lambda e: e.dma_start(out=x1[:, 0:256], in_=R["cache_ckv"][l, p0:p0 + 128, :]),
                          reads=[R["cache_ckv"]], writes=[x1])
                S.op("pool", lambda e: e.tensor_copy(out=x1b[:, 0:256], in_=x1[:, 0:256]), reads=[x1], writes=[x1b])
                tr_bf(x1b, 256, ckvT, lambda j: ckvT[:, j, t * 128:(t + 1) * 128], t)
                if t >= L // 128:
                    p0 = (t - L // 128) * 128
                    S.dma("sp", lambda e: e.dma_start(out=x1[:, 256:320], in_=R["cache_kr"][l, p0:p0 + 128, :]),
                          reads=[R["cache_kr"]], writes=[x1])
                    S.op("pool", lambda e: e.tensor_copy(out=x1b[:, 256:320], in_=x1[:, 256:320]), reads=[x1], writes=[x1b])
                    pt = psP[(t + 1) % 2]
                    ptv = pt[:, :].bitcast(BF16)
                    S.op("pe", lambda e: e.transpose(ptv[0:64, 0:128], x1b[:, 256:320], ident[:]),
                         reads=[x1b, ident], writes=[pt])
                    _evac(S, t, krT[0:64, t * 128:(t + 1) * 128], ptv[0:64, 0:128], reads=[pt], writes=[krT])
            for g in range(L // QB):
                g0 = tok0 + g * QB
                S.dma("sp", lambda e: e.dma_start(out=r1[:, 0:QB], in_=P_F[fkr:fkr + 64, g0:g0 + QB]), reads=[P_F], writes=[r1])
                if latent:
                    S.dma("sp", lambda e: e.dma_start(out=r2[:, 0:QB], in_=P_F[fks:fks + 64, g0:g0 + QB]),
                          reads=[P_F], writes=[r2])
                    S.op("dve", lambda e: e.tensor_tensor(out=r1[:, 0:QB], in0=r1[:, 0:QB], in1=ropc[:, g * QB:(g + 1) * QB],
                                                          op=ALU.mult), reads=[r1, ropc], writes=[r1])
                    S.op("dve", lambda e: e.tensor_tensor(out=r2[:, 0:QB], in0=r2[:, 0:QB], in1=rops[:, g * QB:(g + 1) * QB],
                                                          op=ALU.mult), reads=[r2, rops], writes=[r2])
                    S.op("dve", lambda e: e.tensor_tensor(out=krT[0:64, g * QB:(g + 1) * QB], in0=r1[:, 0:QB], in1=r2[:, 0:QB],
                                                          op=ALU.add), reads=[r1, r2], writes=[krT])
                else:
                    S.op("dve", lambda e: e.tensor_copy(out=krT[0:64, g * QB:(g + 1) * QB], in_=r1[:, 0:QB]),
                         reads=[r1], writes=[krT])
            ev = 0
            for t in range(nkt):
                for hb in range(2):
                    ps = psP[ev % 2]
                    for kc in range(2):
                        S.op("pe", lambda e: e.matmul(ps[:, :], lhsT=ckvT[:, kc, t * 128:(t + 1) * 128],
                                                      rhs=wkvb[:, kc, 1024 + hb * 512:1024 + (hb + 1) * 512],
                                                      start=(kc == 0), stop=(kc == 1)), reads=[ckvT, wkvb], writes=[ps])
                    _evac(S, ev, vall[:, t, hb * 512:(hb + 1) * 512], ps[:, :], reads=[ps], writes=[vall])
                    ev += 1
            for h in range(8):
                for g in range((Lk + 511) // 512):
                    w = min(512, Lk - g * 512)
                    ps = psP[ev % 2]
                    for kc in range(2):
                        S.op("pe", lambda e: e.matmul(ps[:, 0:w], lhsT=wkvb[:, kc, h * 128:(h + 1) * 128],
                                                      rhs=ckvT[:, kc, g * 512:g * 512 + w], start=(kc == 0), stop=(kc == 1)),
                             reads=[ckvT, wkvb], writes=[ps])
                    _evac(S, ev, knT[:, g * 512:g * 512 + w], ps[:, 0:w], reads=[ps], writes=[knT])
                    ev += 1
                for g in range(L // QB):
                    ps = psP[ev % 2]
                    for kc in range(4):
                        S.op("pe", lambda e: e.matmul(ps[:, 0:QB], lhsT=wqb[:, kc, h * 128:(h + 1) * 128],
                                                      rhs=cqT[:, kc, g * QB:(g + 1) * QB], start=(kc == 0), stop=(kc == 3)),
                             reads=[cqT, wqb], writes=[ps])
                    _evac(S, ev, qnT[:, g * QB:(g + 1) * QB], ps[:, 0:QB], reads=[ps], writes=[qnT])
                    ev += 1
                    ps = psP[ev % 2]
                    for kc in range(4):
                        S.op("pe", lambda e: e.matmul(ps[0:64, 0:QB], lhsT=wqb[:, kc, 1024 + h * 64:1024 + (h + 1) * 64],
                                                      rhs=cqT[:, kc, g * QB:(g + 1) * QB], start=(kc == 0), stop=(kc == 3)),
                             reads=[cqT, wqb], writes=[ps])
                    if latent:
                        ps2 = psP[(ev + 1) % 2]
                        for kc in range(4):
                            S.op("pe", lambda e: e.matmul(ps2[0:64, 0:QB], lhsT=wqb[:, kc, 1536 + h * 64:1536 + (h + 1) * 64],
                                                          rhs=cqT[:, kc, g * QB:(g + 1) * QB], start=(kc == 0), stop=(kc == 3)),
                                 reads=[cqT, wqb], writes=[ps2])
                        S.op("dve", lambda e: e.tensor_tensor(out=r1[:, 0:QB], in0=ps[0:64, 0:QB],
                                                              in1=ropc[:, g * QB:(g + 1) * QB], op=ALU.mult),
                             reads=[ps, ropc], writes=[r1])
                        S.op("dve", lambda e: e.tensor_tensor(out=r2[:, 0:QB], in0=ps2[0:64, 0:QB],
                                                              in1=rops[:, g * QB:(g + 1) * QB], op=ALU.mult),
                             reads=[ps2, rops], writes=[r2])
                        S.op("dve", lambda e: e.tensor_tensor(out=qrT[0:64, g * QB:(g + 1) * QB], in0=r1[:, 0:QB],
                                                              in1=r2[:, 0:QB], op=ALU.add), reads=[r1, r2], writes=[qrT])
                        ev += 2
                    else:
                        _evac(S, ev, qrT[0:64, g * QB:(g + 1) * QB], ps[0:64, 0:QB], reads=[ps], writes=[qrT])
                        ev += 1
                for g in range(L // QB):
                    _attn(S, A,
                          qparts=[(qnT, lambda q0, n: qnT[:, q0:q0 + n]), (qrT, lambda q0, n: qrT[:, q0:q0 + n])],
                          kparts=[(knT, lambda kt: knT[:, kt * 128:(kt + 1) * 128]),
                                  (krT, lambda kt: krT[:, kt * 128:(kt + 1) * 128])],
                          v_ap=lambda kt: (vall, vall[:, kt, h * 128:(h + 1) * 128]),
                          ktl=list(range(nkt)), q0=g * QB, QB=QB, scale=192 ** -0.5, bias_fn=None,
                          out_dst=(brT[0], brT[0][h * 128:(h + 1) * 128, tok0 + g * QB:tok0 + (g + 1) * QB]))
        S.barrier()
    S.scope = S.es


NA_QB_TILES = {0: list(range(0, 6)), 1: list(range(2, 10)), 2: list(range(6, 14)), 3: list(range(10, 16))}
NA_MASK_IDX = {}
_i = 0
for _qb in range(4):
    for _kt in NA_QB_TILES[_qb]:
        NA_MASK_IDX[(_qb, _kt)] = _i
        _i += 1
NA_NMASK = _i


def stage_na(S, l, R):
    ident = R["ident"]
    P_T, P_F, brT = R["P_T"], R["P_F"], R["brT"]
    with ExitStack() as sc:
        S.scope = sc
        A = _attn_res(S)
        psP = [S.psum("psP%d" % i, [128, 512], F32) for i in range(2)]
        qT = S.sbuf("naqT", [128, LL], BF16)
        kT = S.sbuf("nakT", [128, LL + PAST], BF16)
        vall = S.sbuf("navall", [128, (LL + PAST) // 128, 1024], BF16)
        kc_f = [S.sbuf("nakc%d" % i, [128, 1024], F32) for i in range(2)]
        kc_b = S.sbuf("nakcb", [128, 4, 1024], BF16)
        bias = [S.sbuf("nabias%d" % i, [128, 512], F32) for i in range(2)]
        mask = [S.sbuf("namask%d" % i, [128, 512], F32) for i in range(2)]
        fq, _ = F_ROWS["naqT"]
        fk, _ = F_ROWS["nakT"]
        av, _ = T_COLS["nav"]
        seqs = [(s * LC, LC, False) for s in range(NCTX)] + [(NCTX * LC, LL, True)]
        nb = 0
        for si, (tok0, L, latent) in enumerate(seqs):
            Lk = L + (PAST if latent else 0)
            nkt = Lk // 128
            QB = 512 if latent else 256
            for t in range(L // 128):
                g0 = tok0 + t * 128
                S.dma("pool", lambda e: e.dma_start(out=vall[:, t, :], in_=P_T[g0:g0 + 128, av:av + 1024]),
                      reads=[P_T], writes=[vall])
            if latent:
                for t in range(4):
                    S.dma("pool", lambda e: e.dma_start(out=vall[:, L // 128 + t, :],
                                                        in_=R["cache_nav"][l, t * 128:(t + 1) * 128, :]),
                          reads=[R["cache_nav"]], writes=[vall])
                    S.dma("pool", lambda e: e.dma_start(out=kc_b[:, t, :], in_=R["cache_nak"][l, t * 128:(t + 1) * 128, :]),
                          reads=[R["cache_nak"]], writes=[kc_b])
            for h in range(8):
                S.dma("pool", lambda e: e.dma_start(out=qT[:, 0:L], in_=P_F[fq + h * 128:fq + (h + 1) * 128, tok0:tok0 + L]),
                      reads=[P_F], writes=[qT])
                S.dma("pool", lambda e: e.dma_start(out=kT[:, 0:L], in_=P_F[fk + h * 128:fk + (h + 1) * 128, tok0:tok0 + L]),
                      reads=[P_F], writes=[kT])
                if latent:
                    pt = psP[h % 2]
                    ptv = pt[:, :].bitcast(BF16)
                    for t in range(4):
                        S.op("pe", lambda e: e.transpose(ptv[:, t * 128:(t + 1) * 128], kc_b[:, t, h * 128:(h + 1) * 128],
                                                         ident[:]), reads=[kc_b, ident], writes=[pt])
                    _evac(S, h, kT[:, L:L + 512], ptv[:, 0:512], reads=[pt], writes=[kT])
                for g in range(L // QB):
                    if latent:
                        ktl = NA_QB_TILES[g] + [16, 17, 18, 19]

                        def bias_fn(kt, g=g, h=h):
                            if kt >= 16:
                                return None
                            bb, mm = bias[bias_fn.n % 2], mask[bias_fn.n % 2]
                            bias_fn.n += 1
                            for rr in range(2):
                                rp = 2 * kt + rr
                                mm0 = 15 - rp + 8 * g
                                S.dma("sp", lambda e: e.dma_start(
                                    out=bb[rr * 64:(rr + 1) * 64, :],
                                    in_=R["na_ctab"][l, h, :, mm0:mm0 + 8, :].rearrange("c m q -> c (m q)")),
                                    reads=[R["na_ctab"]], writes=[bb])
                            mi = NA_MASK_IDX[(g, kt)]
                            S.dma("sp", lambda e: e.dma_start(out=mm[:], in_=R["na_mask"][mi]), reads=[R["na_mask"]], writes=[mm])
                            S.op("pool", lambda e: e.tensor_tensor(out=bb[:], in0=bb[:], in1=mm[:], op=ALU.add),
                                 reads=[bb, mm], writes=[bb])
                            return bb
                        bias_fn.n = nb
                    else:
                        ktl = list(range(nkt))
                        bias_fn = None
                    _attn(S, A,
                          qparts=[(qT, lambda q0, n: qT[:, q0:q0 + n])],
                          kparts=[(kT, lambda kt: kT[:, kt * 128:(kt + 1) * 128])],
                          v_ap=lambda kt: (vall, vall[:, kt, h * 128:(h + 1) * 128]),
                          ktl=ktl, q0=g * QB, QB=QB, scale=128 ** -0.5, bias_fn=bias_fn,
                          out_dst=(brT[3], brT[3][h * 128:(h + 1) * 128, tok0 + g * QB:tok0 + (g + 1) * QB]))
                    if latent:
                        nb = bias_fn.n
        S.barrier()
    S.scope = S.es


def stage_merge(S, l, R, x_src):
    ident = R["ident"]
    P_T, brT, mrg, xbuf, modd = R["P_T"], R["brT"], R["mrg"], R["xbuf"], R["modd"]
    ag, _ = T_COLS["gate"]
    for b in range(4):
        with ExitStack() as sc:
            S.scope = sc
            wbr = S.sbuf("wbr", [128, 8, 2048], BF16)
            S.dma("pool", lambda e: e.dma_start(out=wbr[:], in_=R["w_branch"][l, b].rearrange("(k p) c -> p k c", p=128)),
                  reads=[R["w_branch"]], writes=[wbr])
            brt = [S.sbuf("brt%d" % i, [128, 8, 128], BF16) for i in range(2)]
            gt = [S.sbuf("gt%d" % i, [128, 2048], F32) for i in range(2)]
            acc = [S.sbuf("acc%d" % i, [128, 2048], F32) for i in range(2)]
            pm = [S.psum("pm%d" % i, [128, 512], F32) for i in range(4)]
            ev = 0
            for t in range(NTILE):
                bt, g, a = brt[t % 2], gt[t % 2], acc[t % 2]
                S.dma("sp", lambda e: e.dma_start(out=bt[:], in_=brT[b][:, t * 128:(t + 1) * 128].rearrange("(k p) t -> p k t", p=128)),
                      reads=[brT[b]], writes=[bt])
                S.dma("sp", lambda e: e.dma_start(out=g[:], in_=P_T[t * 128:(t + 1) * 128, ag + b * 2048:ag + (b + 1) * 2048]),
                      reads=[P_T], writes=[g])
                if b > 0:
                    S.dma("sp", lambda e: e.dma_start(out=a[:], in_=mrg[t * 128:(t + 1) * 128, :]), reads=[R["mrgt"][t]], writes=[a])
                S.op("act", lambda e: e.activation(out=g[:], in_=g[:], func=AF.Sigmoid), reads=[g], writes=[g])
                for nb in range(4):
                    ps = pm[ev % 4]
                    ev += 1
                    for kc in range(8):
                        S.op("pe", lambda e: e.matmul(ps[:, :], lhsT=bt[:, kc, :], rhs=wbr[:, kc, nb * 512:(nb + 1) * 512],
                                                      start=(kc == 0), stop=(kc == 7)), reads=[bt, wbr], writes=[ps])
                    S.op("dve", lambda e: e.tensor_tensor(out=g[:, nb * 512:(nb + 1) * 512], in0=ps[:, :],
                                                          in1=g[:, nb * 512:(nb + 1) * 512], op=ALU.mult),
                         reads=[ps, g], writes=[g])
                if b > 0:
                    S.op("pool", lambda e: e.tensor_tensor(out=g[:], in0=g[:], in1=a[:], op=ALU.add), reads=[g, a], writes=[g])
                S.dma("sp", lambda e: e.dma_start(out=mrg[t * 128:(t + 1) * 128, :], in_=g[:]), reads=[g], writes=[R["mrgt"][t]])
            S.barrier()
    with ExitStack() as sc:
        S.scope = sc
        wout = S.sbuf("wout", [128, 16, 2048], BF16)
        S.dma("pool", lambda e: e.dma_start(out=wout[:], in_=R["w_out"][l].rearrange("(k p) c -> p k c", p=128)),
              reads=[R["w_out"]], writes=[wout])
        g1b = [S.sbuf("g1b%d" % c, [128, 2048], F32) for c in range(2)]
        for c in range(2):
            S.dma("sp", lambda e: e.dma_start(out=g1b[c][:], in_=modd[c, 2 * D:3 * D].partition_broadcast(128)),
                  reads=[modd], writes=[g1b[c]])
        mt = [S.sbuf("mt%d" % i, [128, 2048], F32) for i in range(2)]
        mb = [S.sbuf("mb%d" % i, [128, 2048], BF16) for i in range(2)]
        mT = [S.sbuf("mT%d" % i, [128, 16, 128], BF16) for i in range(2)]
        xt = [S.sbuf("xo%d" % i, [128, 2048], F32) for i in range(2)]
        ptr = [S.psum("optr%d" % i, [128, 8, 128], BF16) for i in range(2)]
        pm = [S.psum("pmo%d" % i, [128, 512], F32) for i in range(4)]
        ev = 0
        for t in range(NTILE):
            c = 0 if t < (NCTX * LC) // 128 else 1
            m, mbb, mTT, x = mt[t % 2], mb[t % 2], mT[t % 2], xt[t % 2]
            S.dma("sp", lambda e: e.dma_start(out=m[:], in_=mrg[t * 128:(t + 1) * 128, :]), reads=[R["mrgt"][t]], writes=[m])
            S.dma("sp", lambda e: e.dma_start(out=x[:], in_=x_src[t * 128:(t + 1) * 128, :]), reads=[R["xbt"][t]], writes=[x])
            S.op("pool", lambda e: e.tensor_copy(out=mbb[:], in_=m[:]), reads=[m], writes=[mbb])
            for half in range(2):
                pt = ptr[half]
                for j in range(8):
                    kc = half * 8 + j
                    S.op("pe", lambda e: e.transpose(pt[:, j, :], mbb[:, kc * 128:(kc + 1) * 128], ident[:]),
                         reads=[mbb, ident], writes=[pt])
                _evac(S, half, mTT[:, half * 8:(half + 1) * 8, :], pt[:, :, :], reads=[pt], writes=[mTT])
            for nb in range(4):
                ps = pm[ev % 4]
                ev += 1
                for kc in range(16):
                    S.op("pe", lambda e: e.matmul(ps[:, :], lhsT=mTT[:, kc, :], rhs=wout[:, kc, nb * 512:(nb + 1) * 512],
                                                  start=(kc == 0), stop=(kc == 15)), reads=[mTT, wout], writes=[ps])
                S.op("dve", lambda e: e.tensor_tensor(out=m[:, nb * 512:(nb + 1) * 512], in0=ps[:, :],
                                                      in1=g1b[c][:, nb * 512:(nb + 1) * 512], op=ALU.mult),
                     reads=[ps, g1b[c]], writes=[m])
            S.op("pool", lambda e: e.tensor_tensor(out=x[:], in0=x[:], in1=m[:], op=ALU.add), reads=[x, m], writes=[x])
            S.dma("sp", lambda e: e.dma_start(out=xbuf[t * 128:(t + 1) * 128, :], in_=x[:]), reads=[x], writes=[R["xbt"][t]])
        S.barrier()
    S.scope = S.es


def stage_peer(S, l, R, last):
    ident = R["ident"]
    xbuf, modd = R["xbuf"], R["modd"]
    with ExitStack() as sc:
        S.scope = sc
        wq = S.sbuf("pwq", [128, 16, 2048], BF16)
        S.dma("pool", lambda e: e.dma_start(out=wq[:], in_=R["peer_wq"][l].rearrange("(k p) c -> p k c", p=128)),
              reads=[R["peer_wq"]], writes=[wq])
        identf = S.sbuf("identf", [128, 128], F32)
        S.dma("sp", lambda e: e.dma_start(out=identf[:], in_=R["ident_in"][:, :]), reads=[R["ident_in"]], writes=[identf])
        keysT = S.sbuf("keysT", [128, 2, 128], F32)
        S.dma("sp", lambda e: e.dma_start(out=keysT[:], in_=R["peer_keysT"][l].rearrange("s c n -> c s n")),
              reads=[R["peer_keysT"]], writes=[keysT])
        gm2 = S.sbuf("gm2", [128, 2048], F32)
        sh2 = S.sbuf("sh2", [128, 2048], F32)
        x1 = S.sbuf("px1", [128, 2048], F32)
        h2 = S.sbuf("ph2", [128, 2048], F32)
        h2b = S.sbuf("ph2b", [128, 2048], BF16)
        h2T = S.sbuf("ph2T", [128, 16, 128], BF16)
        qTf = S.sbuf("pqTf", [128, 16, 128], F32)
        scr = S.sbuf("pscr", [128, 16, 128], F32)
        scw = S.sbuf("pscw", [128, 128], F32)
        vals = S.sbuf("pvals", [128, 16, 16], F32)
        idx = S.sbuf("pidx", [128, 16, 16], U32)
        idxf = S.sbuf("pidxf", [128, 16, 16], F32)
        cand = S.sbuf("pcand", [128, 256], F32)
        candw = S.sbuf("pcandw", [128, 256], F32)
        cid = S.sbuf("pcid", [128, 256], F32)
        bs = S.sbuf("pbs", [128, 8, 16], F32)
        bj = S.sbuf("pbj", [128, 16], U32)
        bjf = S.sbuf("pbjf", [128, 2, 16], F32)
        bja = S.sbuf("pbja", [128, 16], U32)
        bjb = S.sbuf("pbjb", [128, 16], U32)
        eq16 = S.sbuf("peq16", [128, 16, 16], F32)
        sel = S.sbuf("psel", [128, 2, 16], F32)
        iota = S.sbuf("piota", [128, 256], F32)
        S.dma("sp", lambda e: e.dma_start(out=iota[:], in_=R["iota256"][0, :].partition_broadcast(128)),
              reads=[R["iota256"]], writes=[iota])
        eq = S.sbuf("peq", [128, 8, 256], F32)
        eidf = S.sbuf("peidf", [128, 8, 16], F32)
        eidi = S.sbuf("peidi", [128, 128], I32)
        negb = S.sbuf("pnegb", [128, 8], F32)
        zs = S.sbuf("pzs", [128, 8], F32)
        gat = S.sbuf("pgat", [128, 8, 16], F32)
        actv = S.sbuf("pact", [128, 128], F32)
        coef = S.sbuf("pcoef", [128, 128], F32)
        yacc = S.sbuf("pyacc", [128, 2048], F32)
        qf = yacc
        jk = S.sbuf("pjk", [128, 2048], F32)
        st = S.sbuf("pst", [128, 1], F32)
        ug = [S.sbuf("pug%d" % i, [128, 2048], BF16) for i in range(8)]
        dgs = [S.sbuf("pdg%d" % i, [128, 128], BF16) for i in range(4)]
        ptr = [S.psum("pptr%d" % i, [128, 8, 128], BF16) for i in range(2)]
        pm = [S.psum("ppm%d" % i, [128, 512], F32) for i in range(4)]
        cur_c = -1
        for t in range(NTILE_PEER if NTILE_PEER else NTILE):
            c = 0 if t < (NCTX * LC) // 128 else 1
            if c != cur_c:
                cur_c = c
                S.dma("sp", lambda e: e.dma_start(out=sh2[:], in_=modd[c, 3 * D:4 * D].partition_broadcast(128)),
                      reads=[modd], writes=[sh2])
                S.dma("sp", lambda e: e.dma_start(out=gm2[:], in_=modd[c, 4 * D:5 * D].partition_broadcast(128)),
                      reads=[modd], writes=[gm2])
                S.dma("sp", lambda e: e.dma_start(out=jk[:], in_=R["norm2_g"][l, :].partition_broadcast(128)),
                      reads=[R["norm2_g"]], writes=[jk])
                S.op("dve", lambda e: e.scalar_tensor_tensor(out=gm2[:], in0=gm2[:], scalar=1.0, in1=jk[:],
                                                             op0=ALU.add, op1=ALU.mult), reads=[gm2, jk], writes=[gm2])
            S.dma("sp", lambda e: e.dma_start(out=x1[:], in_=xbuf[t * 128:(t + 1) * 128, :]), reads=[R["xbt"][t]], writes=[x1])
            S.op("act", lambda e: e.activation(out=jk[:], in_=x1[:], func=AF.Square, accum_out=st[:]),
                 reads=[x1], writes=[jk, st])
            S.op("act", lambda e: e.activation(out=st[:], in_=st[:], func=AF.Sqrt, scale=1.0 / D, bias=EPS),
                 reads=[st], writes=[st])
            S.op("dve", lambda e: e.reciprocal(out=st[:], in_=st[:]), reads=[st], writes=[st])
            S.op("dve", lambda e: e.scalar_tensor_tensor(out=h2[:], in0=x1[:], scalar=st[:, 0:1], in1=gm2[:],
                                                         op0=ALU.mult, op1=ALU.mult), reads=[x1, st, gm2], writes=[h2])
            S.op("dve", lambda e: e.tensor_tensor(out=h2[:], in0=h2[:], in1=sh2[:], op=ALU.add), reads=[h2, sh2], writes=[h2])
            S.op("act", lambda e: e.activation(out=h2b[:], in_=h2[:], func=AF.Copy), reads=[h2], writes=[h2b])
            for half in range(2):
                pt = ptr[half]
                for j in range(8):
                    kc = half * 8 + j
                    S.op("pe", lambda e: e.transpose(pt[:, j, :], h2b[:, kc * 128:(kc + 1) * 128], ident[:]),
                         reads=[h2b, ident], writes=[pt])
                _evac(S, half, h2T[:, half * 8:(half + 1) * 8, :], pt[:, :, :], reads=[pt], writes=[h2T])
            for nb in range(4):
                ps = pm[nb]
                for kc in range(16):
                    S.op("pe", lambda e: e.matmul(ps[:, :], lhsT=h2T[:, kc, :], rhs=wq[:, kc, nb * 512:(nb + 1) * 512],
                                                  start=(kc == 0), stop=(kc == 15)), reads=[h2T, wq], writes=[ps])
                _evac(S, nb, qf[:, nb * 512:(nb + 1) * 512], ps[:, :], reads=[ps], writes=[qf])
            for b4 in range(4):
                ps = pm[b4]
                for j in range(4):
                    ch = b4 * 4 + j
                    S.op("pe", lambda e: e.transpose(ps[:, j * 128:(j + 1) * 128], qf[:, ch * 128:(ch + 1) * 128], identf[:]),
                         reads=[qf, identf], writes=[ps])
                _evac(S, b4, qTf[:, b4 * 4:(b4 + 1) * 4, :], ps[:, :].rearrange("p (a b) -> p a b", a=4), reads=[ps], writes=[qTf])
            for b4 in range(4):
                ps = pm[b4]
                for j in range(4):
                    ch = b4 * 4 + j
                    S.op("pe", lambda e: e.matmul(ps[:, j * 128:(j + 1) * 128], lhsT=qTf[:, ch, :], rhs=keysT[:, ch % 2, :],
                                                  start=True, stop=True), reads=[qTf, keysT], writes=[ps])
                _evac(S, b4 + 1, scr[:, b4 * 4:(b4 + 1) * 4, :], ps[:, :].rearrange("p (a b) -> p a b", a=4), reads=[ps], writes=[scr])
            for ch in range(16):
                S.op("dve", lambda e: e.max(out=vals[:, ch, 0:8], in_=scr[:, ch, :]), reads=[scr], writes=[vals])
                S.op("dve", lambda e: e.match_replace(out=scw[:, :], in_to_replace=vals[:, ch, 0:8], in_values=scr[:, ch, :],
                                                      imm_value=-1e30), reads=[vals, scr], writes=[scw])
                S.op("dve", lambda e: e.max(out=vals[:, ch, 8:16], in_=scw[:, :]), reads=[scw], writes=[vals])
                S.op("dve", lambda e: e.max_index(out=idx[:, ch, 0:8], in_max=vals[:, ch, 0:8], in_values=scr[:, ch, :]),
                     reads=[vals, scr], writes=[idx])
                S.op("dve", lambda e: e.max_index(out=idx[:, ch, 8:16], in_max=vals[:, ch, 8:16], in_values=scw[:, :]),
                     reads=[vals, scw], writes=[idx])
            S.op("dve", lambda e: e.tensor_copy(out=idxf[:], in_=idx[:]), reads=[idx], writes=[idxf])
            for h in range(8):
                c3 = cand[:, :].rearrange("p (a b) -> p a b", a=16)
                i3 = cid[:, :].rearrange("p (a b) -> p a b", a=16)
                S.op("dve", lambda e: e.tensor_tensor(out=c3, in0=vals[:, 2 * h, :].unsqueeze(2).to_broadcast([128, 16, 16]),
                                                      in1=vals[:, 2 * h + 1, :].unsqueeze(1).to_broadcast([128, 16, 16]),
                                                      op=ALU.add), reads=[vals], writes=[cand])
                S.op("dve", lambda e: e.scalar_tensor_tensor(out=i3, in0=idxf[:, 2 * h, :].unsqueeze(2).to_broadcast([128, 16, 16]),
                                                             scalar=128.0,
                                                             in1=idxf[:, 2 * h + 1, :].unsqueeze(1).to_broadcast([128, 16, 16]),
                                                             op0=ALU.mult, op1=ALU.add), reads=[idxf], writes=[cid])
                S.op("dve", lambda e: e.max(out=bs[:, h, 0:8], in_=cand[:, :]), reads=[cand], writes=[bs])
                S.op("dve", lambda e: e.match_replace(out=candw[:, :], in_to_replace=bs[:, h, 0:8], in_values=cand[:, :],
                                                      imm_value=-1e30), reads=[bs, cand], writes=[candw])
                S.op("dve", lambda e: e.max(out=bs[:, h, 8:16], in_=candw[:, :]), reads=[candw], writes=[bs])
                S.op("dve", lambda e: e.max_index(out=bj[:, 0:8], in_max=bs[:, h, 0:8], in_values=cand[:, :]),
                     reads=[bs, cand], writes=[bj])
                S.op("dve", lambda e: e.max_index(out=bj[:, 8:16], in_max=bs[:, h, 8:16], in_values=candw[:, :]),
                     reads=[bs, candw], writes=[bj])
                S.op("dve", lambda e: e.tensor_single_scalar(out=bja[:], in_=bj[:], scalar=4, op=ALU.logical_shift_right),
                     reads=[bj], writes=[bja])
                S.op("dve", lambda e: e.tensor_single_scalar(out=bjb[:], in_=bj[:], scalar=15, op=ALU.bitwise_and),
                     reads=[bj], writes=[bjb])
                S.op("dve", lambda e: e.tensor_copy(out=bjf[:, 0, :], in_=bja[:]), reads=[bja], writes=[bjf])
                S.op("dve", lambda e: e.tensor_copy(out=bjf[:, 1, :], in_=bjb[:]), reads=[bjb], writes=[bjf])
                for ab in range(2):
                    S.op("dve", lambda e: e.tensor_tensor(out=eq16[:], in0=iota[:, 0:16].unsqueeze(1).to_broadcast([128, 16, 16]),
                                                          in1=bjf[:, ab, :].unsqueeze(2).to_broadcast([128, 16, 16]),
                                                          op=ALU.is_equal), reads=[iota, bjf], writes=[eq16])
                    S.op("dve", lambda e: e.tensor_tensor(out=eq16[:], in0=eq16[:],
                                                          in1=idxf[:, 2 * h + ab, :].unsqueeze(1).to_broadcast([128, 16, 16]),
                                                          op=ALU.mult), reads=[eq16, idxf], writes=[eq16])
                    S.op("dve", lambda e: e.tensor_reduce(out=sel[:, ab, :], in_=eq16[:], axis=AX.X, op=ALU.add),
                         reads=[eq16], writes=[sel])
                S.op("dve", lambda e: e.scalar_tensor_tensor(out=eidf[:, h, :], in0=sel[:, 0, :], scalar=128.0, in1=sel[:, 1, :],
                                                             op0=ALU.mult, op1=ALU.add), reads=[sel], writes=[eidf])
            S.op("dve", lambda e: e.tensor_copy(out=eidi[:, :].rearrange("p (a b) -> p a b", a=8), in_=eidf[:]),
                 reads=[eidf], writes=[eidi])
            S.op("dve", lambda e: e.tensor_scalar(out=negb[:], in0=bs[:, :, 0], scalar1=-1.0, scalar2=None, op0=ALU.mult),
                 reads=[bs], writes=[negb])
            for h in range(8):
                S.op("act", lambda e: e.activation(out=gat[:, h, :], in_=bs[:, h, :], func=AF.Exp, bias=negb[:, h:h + 1],
                                                   accum_out=zs[:, h:h + 1]), reads=[bs, negb], writes=[gat, zs])
            S.op("dve", lambda e: e.reciprocal(out=zs[:], in_=zs[:]), reads=[zs], writes=[zs])
            S.op("dve", lambda e: e.tensor_tensor(out=gat[:], in0=gat[:], in1=zs[:, :].unsqueeze(2).to_broadcast([128, 8, 16]),
                                                  op=ALU.mult), reads=[gat, zs], writes=[gat])
            for s in range(128):
                u = ug[s % 8]
                S.dma("pool", lambda e: e.indirect_dma_start(
                    out=u[:], out_offset=None, in_=R["peer_ub"][l][:, :],
                    in_offset=bass.IndirectOffsetOnAxis(ap=eidi[:, s:s + 1], axis=0)),
                    reads=[R["peer_ub"][l], eidi], writes=[u])
                S.op("dve", lambda e: e.scalar_tensor_tensor(out=jk[:], in0=u[:], scalar=1.0, in1=h2[:],
                                                             op0=ALU.mult, op1=ALU.mult, accum_out=actv[:, s:s + 1]),
                     reads=[u, h2], writes=[jk, actv])
            S.op("act", lambda e: e.activation(out=actv[:], in_=actv[:], func=AF.Gelu), reads=[actv], writes=[actv])
            S.op("dve", lambda e: e.tensor_tensor(out=coef[:], in0=actv[:], in1=gat[:, :, :].rearrange("p a b -> p (a b)"),
                                                  op=ALU.mult), reads=[actv, gat], writes=[coef])
            for s in range(128):
                u = ug[s % 8]
                S.dma("pool", lambda e: e.indirect_dma_start(
                    out=u[:], out_offset=None, in_=R["peer_vb"][l][:, :],
                    in_offset=bass.IndirectOffsetOnAxis(ap=eidi[:, s:s + 1], axis=0)),
                    reads=[R["peer_vb"][l], eidi], writes=[u])
                dg = dgs[s % 4]
                S.op("dve", lambda e: e.tensor_scalar(out=dg[:], in0=ident[:], scalar1=coef[:, s:s + 1], scalar2=None,
                                                      op0=ALU.mult), reads=[ident, coef], writes=[dg])
                for nb in range(4):
                    S.op("pe", lambda e: e.matmul(pm[nb][:, :], lhsT=dg[:, :], rhs=u[:, nb * 512:(nb + 1) * 512],
                                                  start=(s == 0), stop=(s == 127)), reads=[dg, u], writes=[pm[nb]])
            for nb in range(4):
                _evac(S, 0, yacc[:, nb * 512:(nb + 1) * 512], pm[nb][:, :], reads=[pm[nb]], writes=[yacc])
            S.dma("sp", lambda e: e.dma_start(out=jk[:], in_=modd[c, 5 * D:6 * D].partition_broadcast(128)),
                  reads=[modd], writes=[jk])
            S.op("dve", lambda e: e.tensor_tensor(out=yacc[:], in0=yacc[:], in1=jk[:], op=ALU.mult), reads=[yacc, jk], writes=[yacc])
            S.op("dve", lambda e: e.tensor_tensor(out=x1[:], in0=x1[:], in1=yacc[:], op=ALU.add), reads=[x1, yacc], writes=[x1])
            if not last:
                S.dma("sp", lambda e: e.dma_start(out=xbuf[t * 128:(t + 1) * 128, :], in_=x1[:]), reads=[x1], writes=[R["xbt"][t]])
            else:
                S.op("act", lambda e: e.activation(out=jk[:], in_=x1[:], func=AF.Square, accum_out=st[:]),
                     reads=[x1], writes=[jk, st])
                S.op("act", lambda e: e.activation(out=st[:], in_=st[:], func=AF.Sqrt, scale=1.0 / D, bias=EPS),
                     reads=[st], writes=[st])
                S.op("dve", lambda e: e.reciprocal(out=st[:], in_=st[:]), reads=[st], writes=[st])
                S.dma("sp", lambda e: e.dma_start(out=jk[:], in_=R["final_g"][0, :].partition_broadcast(128)),
                      reads=[R["final_g"]], writes=[jk])
                S.op("dve", lambda e: e.scalar_tensor_tensor(out=x1[:], in0=x1[:], scalar=st[:, 0:1], in1=jk[:],
                                                             op0=ALU.mult, op1=ALU.mult), reads=[x1, st, jk], writes=[x1])
                S.dma("sp", lambda e: e.dma_start(out=R["y_out"][t * 128:(t + 1) * 128, :], in_=x1[:]), reads=[x1], writes=[R["y_out"]])
        S.barrier()
    S.scope = S.es


def stage_dn(S, l, R):
    P_T, P_F, brT = R["P_T"], R["P_F"], R["brT"]
    fdn, _ = F_ROWS["dnT"]
    az, _ = T_COLS["z"]
    aa, _ = T_COLS["a"]
    with ExitStack() as sc:
        S.scope = sc
        cst = S.sbuf("dncst", [128, 10, 128], F32)
        S.dma("sp", lambda e: e.dma_start(out=cst[:], in_=R["dn_consts"][:, :, :].rearrange("m p q -> p m q")),
              reads=[R["dn_consts"]], writes=[cst])
        onesf = S.sbuf("dnones", [128, 128], F32)
        S.op("dve", lambda e: e.memset(onesf[:], 1.0), writes=[onesf])
        identf = S.sbuf("dnidentf", [128, 128], F32)
        S.dma("sp", lambda e: e.dma_start(out=identf[:], in_=R["dn_consts"][2]), reads=[R["dn_consts"]], writes=[identf])
        identb = R["ident"]
        cw = S.sbuf("dncw", [128, 24, 5], F32)
        S.dma("sp", lambda e: e.dma_start(out=cw[:], in_=R["dn_convT"][l]), reads=[R["dn_convT"]], writes=[cw])
        alog = S.sbuf("dnalog", [128, 16], F32)
        dtb = S.sbuf("dndtb", [128, 16], F32)
        S.dma("sp", lambda e: e.dma_start(out=alog[:], in_=R["dn_a_log"][l, :].partition_broadcast(128)),
              reads=[R["dn_a_log"]], writes=[alog])
        S.dma("sp", lambda e: e.dma_start(out=dtb[:], in_=R["dn_dt_bias"][l, :].partition_broadcast(128)),
              reads=[R["dn_dt_bias"]], writes=[dtb])
        S.op("act", lambda e: e.activation(out=alog[:], in_=alog[:], func=AF.Exp), reads=[alog], writes=[alog])
        gon = S.sbuf("dngon", [128, 128], F32)
        S.dma("sp", lambda e: e.dma_start(out=gon[:], in_=R["dn_out_norm"][l, :].partition_broadcast(128)),
              reads=[R["dn_out_norm"]], writes=[gon])
        NCH = LL // 128
        xin = S.sbuf("dnxin", [128, LL + 4], F32)
        acc = S.sbuf("dnacc", [128, LL], F32)
        qT = S.sbuf("dnqT", [128, LL], F32)
        kT = S.sbuf("dnkT", [128, LL], F32)
        vT = S.sbuf("dnvT", [128, LL], F32)
        ktok = S.sbuf("dnktok", [128, NCH, 128], F32)
        vtok = S.sbuf("dnvtok", [128, NCH, 128], F32)
        oall = S.sbuf("dnoall", [128, NCH, 1024], F32)
        gt = S.sbuf("dng", [128, NCH, 16], F32)
        bt = S.sbuf("dnb", [128, NCH, 16], F32)
        gc = S.sbuf("dngc", [128, NCH, 16], F32)
        egc = S.sbuf("dnegc", [128, NCH, 16], F32)
        bg = S.sbuf("dnbg", [128, NCH, 16], F32)
        egl = S.sbuf("dnegl", [128, NCH, 16], F32)
        edl = S.sbuf("dnedl", [128, NCH, 16], F32)
        ab = S.sbuf("dnab", [128, 32], F32)
        sq = S.sbuf("dnsq", [128, 512], F32)
        rn = S.sbuf("dnrn", [128, 512], F32)
        Sst = S.sbuf("dnS", [128, 128], F32)
        Bsets = []
        for j_ in range(3):
            B = {"j": j_}
            for nm_ in ("Gm", "EA", "ET", "Am", "Ad", "Aoff", "Boff", "qkT", "wT", "kd"):
                B[nm_] = S.sbuf("dn%s%d" % (nm_, j_), [128, 128], F32)
            B["Bk"] = [S.sbuf("dnB%d_%d" % (i, j_), [128, 128], F32) for i in range(6)]
            B["Ck"] = [S.sbuf("dnC%d_%d" % (i, j_), [128, 128], F32) for i in range(2)]
            B["X"] = S.sbuf("dnX%d" % j_, [128, 256], F32)
            B["Zb"] = S.sbuf("dnZb%d" % j_, [128, 256], F32)
            Bsets.append(B)
        vnew = S.sbuf("dnvnew", [128, 128], F32)
        tmp = S.sbuf("dntmp", [128, 128], F32)
        zt = S.sbuf("dnz", [128, 1024], F32)
        ob = S.sbuf("dnob", [128, 1024], BF16)
        obT = S.sbuf("dnobT", [128, 8, 128], BF16)
        ssq = S.sbuf("dnssq", [128, 8], F32)
        pqb = [S.psum("dnpqb%d" % i, [128, 512], F32) for i in range(7)]
        pbig = pqb[0:2]
        pq = [Buf(pqb[i].t[:, 0:128], "dnpq%d" % i, root=pqb[i]) for i in range(7)]
        pxs = [Buf(pqb[i].t[:, 0:256], "dnpx%d" % i, root=pqb[i]) for i in range(7)]
        ptr = S.psum("dnptr", [128, 8, 128], BF16)
        npq = [0]

        def PQ():
            npq[0] += 1
            return pq[npq[0] % 7]

        def PX():
            npq[0] += 1
            return pxs[npq[0] % 7]

        slot = [0, 0, 0]

        def PQs(j, wide=False):
            slot[j] += 1
            bank = pqb[3 * j + slot[j] % 3]
            w_ = 256 if wide else 128
            return Buf(bank.t[:, 0:w_], "dnps", root=bank)

        seqs = [(s * LC, LC, False) for s in range(NCTX)] + [(NCTX * LC, LL, True)]
        seqs = seqs[:DNSEQ]
        for si, (tok0, L, latent) in enumerate(seqs):
            nch = L // 128
            for n in range(nch):
                g0 = tok0 + n * 128
                S.dma("sp", lambda e: e.dma_start(out=ab[:], in_=P_T[g0:g0 + 128, aa:aa + 32]), reads=[P_T], writes=[ab])
                S.op("dve", lambda e: e.tensor_tensor(out=gt[:, n, :], in0=ab[:, 0:16], in1=dtb[:], op=ALU.add),
                     reads=[ab, dtb], writes=[gt])
                S.op("act", lambda e: e.activation(out=gt[:, n, :], in_=gt[:, n, :], func=AF.Exp), reads=[gt], writes=[gt])
                S.op("act", lambda e: e.activation(out=gt[:, n, :], in_=gt[:, n, :], func=AF.Ln, bias=1.0), reads=[gt], writes=[gt])
                S.op("dve", lambda e: e.scalar_tensor_tensor(out=gt[:, n, :], in0=gt[:, n, :], scalar=-1.0, in1=alog[:],
                                                             op0=ALU.mult, op1=ALU.mult), reads=[gt, alog], writes=[gt])
                S.op("act", lambda e: e.activation(out=bt[:, n, :], in_=ab[:, 16:32], func=AF.Sigmoid), reads=[ab], writes=[bt])
                p1 = PQ()
                for d in range(2):
                    S.op("pe", lambda e: e.matmul(p1[:, d * 8:(d + 1) * 8], lhsT=cst[:, d, :], rhs=gt[:, n, d * 8:(d + 1) * 8],
                                                  start=True, stop=True), reads=[cst, gt], writes=[p1])
                S.op("pe", lambda e: e.matmul(p1[:, 16:32], lhsT=onesf[:, :], rhs=gt[:, n, :], start=True, stop=True),
                     reads=[onesf, gt], writes=[p1])
                S.op("dve", lambda e: e.tensor_copy(out=gc[:, n, :], in_=p1[:, 0:16]), reads=[p1], writes=[gc])
                S.op("act", lambda e: e.activation(out=egc[:, n, :], in_=p1[:, 0:16], func=AF.Exp), reads=[p1], writes=[egc])
                S.op("act", lambda e: e.activation(out=egl[:, n, :], in_=p1[:, 16:32], func=AF.Exp), reads=[p1], writes=[egl])
                S.op("dve", lambda e: e.tensor_tensor(out=edl[:, n, :], in0=p1[:, 16:32], in1=gc[:, n, :], op=ALU.subtract),
                     reads=[p1, gc], writes=[edl])
                S.op("act", lambda e: e.activation(out=edl[:, n, :], in_=edl[:, n, :], func=AF.Exp), reads=[edl], writes=[edl])
                S.op("dve", lambda e: e.tensor_tensor(out=bg[:, n, :], in0=bt[:, n, :], in1=egc[:, n, :], op=ALU.mult),
                     reads=[bt, egc], writes=[bg])
            for h in range(8):
                if DNSTOP < 2:
                    continue
                for which, dst in ((0, qT), (1, kT), (2, vT)):
                    ch = which * 8 + h
                    r0 = fdn + ch * 128
                    S.op("pool", lambda e: e.memset(xin[:, 0:2], 0.0), writes=[xin])
                    S.op("pool", lambda e: e.memset(xin[:, L + 2:L + 4], 0.0), writes=[xin])
                    S.dma("sp", lambda e: e.dma_start(out=xin[:, 2:L + 2], in_=P_F[r0:r0 + 128, tok0:tok0 + L]),
                          reads=[P_F], writes=[xin])
                    S.op("dve", lambda e: e.tensor_scalar(out=acc[:, 0:L], in0=xin[:, 0:L], scalar1=cw[:, ch, 0:1], scalar2=None,
                                                          op0=ALU.mult), reads=[xin, cw], writes=[acc])
                    for kk in range(1, 5):
                        S.op("dve", lambda e: e.scalar_tensor_tensor(out=acc[:, 0:L], in0=xin[:, kk:kk + L],
                                                                     scalar=cw[:, ch, kk:kk + 1], in1=acc[:, 0:L],
                                                                     op0=ALU.mult, op1=ALU.add), reads=[xin, cw, acc], writes=[acc])
                    S.op("act", lambda e: e.activation(out=dst[:, 0:L], in_=acc[:, 0:L], func=AF.Silu), reads=[acc], writes=[dst])
                    if which < 2:
                        for g in range((L + 511) // 512):
                            w = min(512, L - g * 512)
                            pb = pbig[g % 2]
                            S.op("act", lambda e: e.activation(out=sq[:, 0:w], in_=dst[:, g * 512:g * 512 + w], func=AF.Square),
                                 reads=[dst], writes=[sq])
                            S.op("pe", lambda e: e.matmul(pb[:, 0:w], lhsT=onesf[:, :], rhs=sq[:, 0:w], start=True, stop=True),
                                 reads=[onesf, sq], writes=[pb])
                            sc_ = 128.0 if which == 0 else 1.0
                            S.op("act", lambda e: e.activation(out=rn[:, 0:w], in_=pb[:, 0:w], func=AF.Sqrt, scale=sc_,
                                                               bias=sc_ * EPS), reads=[pb], writes=[rn])
                            S.op("dve", lambda e: e.reciprocal(out=rn[:, 0:w], in_=rn[:, 0:w]), reads=[rn], writes=[rn])
                            S.op("dve", lambda e: e.tensor_tensor(out=dst[:, g * 512:g * 512 + w], in0=dst[:, g * 512:g * 512 + w],
                                                                  in1=rn[:, 0:w], op=ALU.mult), reads=[dst, rn], writes=[dst])
                if DNSTOP < 3:
                    continue
                for n in range(nch):
                    for src, dd in ((kT, ktok), (vT, vtok)):
                        p1 = PQ()
                        S.op("pe", lambda e: e.transpose(p1[:, 0:128], src[:, n * 128:(n + 1) * 128], identf[:]),
                             reads=[src, identf], writes=[p1])
                        _evac(S, n, dd[:, n, :], p1[:, 0:128], reads=[p1], writes=[dd])
                for d in range(2):
                    if DNSTOP < 4:
                        continue
                    dh = d * 8 + h
                    if latent:
                        S.dma("sp", lambda e: e.dma_start(out=Sst[:], in_=R["dn_state"][d][l, h, :, :]),
                              reads=[R["dn_state"][d]], writes=[Sst])
                    else:
                        S.op("dve", lambda e: e.memset(Sst[:], 0.0), writes=[Sst])
                    order = list(range(nch)) if d == 0 else list(range(nch - 1, -1, -1))

                    def prep(n, B, d=d, dh=dh):
                        c0 = n * 128
                        Gm, EA, ET, Am, Ad, Aoff, Boff, Bk, Ck, qkT, X, Zb = (B["Gm"], B["EA"], B["ET"], B["Am"], B["Ad"], B["Aoff"],
                                                                             B["Boff"], B["Bk"], B["Ck"], B["qkT"], B["X"], B["Zb"])
                        S.op("dve", lambda e: e.tensor_scalar(out=Gm[:], in0=cst[:, 3 + d, :], scalar1=gt[:, n, dh:dh + 1],
                                                              scalar2=None, op0=ALU.mult), reads=[cst, gt], writes=[Gm])
                        yield
                        J = B["j"]
                        pA, pT_ = PQs(J), PQs(J)
                        S.op("pe", lambda e: e.matmul(pA[:, :], lhsT=cst[:, d, :], rhs=Gm[:, :], start=True, stop=False),
                             reads=[cst, Gm], writes=[pA])
                        S.op("pe", lambda e: e.matmul(pA[:, :], lhsT=cst[:, 2, :], rhs=cst[:, 5 + d, :], start=False, stop=True),
                             reads=[cst], writes=[pA])
                        S.op("pe", lambda e: e.matmul(pT_[:, :], lhsT=Gm[:, :], rhs=cst[:, d, :], start=True, stop=False),
                             reads=[cst, Gm], writes=[pT_])
                        S.op("pe", lambda e: e.matmul(pT_[:, :], lhsT=cst[:, 2, :], rhs=cst[:, 7 + d, :], start=False, stop=True),
                             reads=[cst], writes=[pT_])
                        yield
                        S.op("act", lambda e: e.activation(out=EA[:], in_=pA[:, :], func=AF.Exp), reads=[pA], writes=[EA])
                        S.op("act", lambda e: e.activation(out=ET[:], in_=pT_[:, :], func=AF.Exp), reads=[pT_], writes=[ET])
                        yield
                        pkk, pkq = PQs(J), PQs(J)
                        S.op("pe", lambda e: e.matmul(pkk[:, :], lhsT=kT[:, c0:c0 + 128], rhs=kT[:, c0:c0 + 128], start=True,
                                                      stop=True), reads=[kT], writes=[pkk])
                        S.op("pe", lambda e: e.matmul(pkq[:, :], lhsT=kT[:, c0:c0 + 128], rhs=qT[:, c0:c0 + 128], start=True,
                                                      stop=True), reads=[kT, qT], writes=[pkq])
                        yield
                        S.op("dve", lambda e: e.scalar_tensor_tensor(out=Am[:], in0=pkk[:, :], scalar=bt[:, n, dh:dh + 1], in1=EA[:],
                                                                     op0=ALU.mult, op1=ALU.mult), reads=[pkk, bt, EA], writes=[Am])
                        S.op("dve", lambda e: e.tensor_tensor(out=qkT[:], in0=pkq[:, :], in1=ET[:], op=ALU.mult),
                             reads=[pkq, ET], writes=[qkT])
                        S.op("pool", lambda e: e.tensor_scalar(out=X[:, 0:128], in0=vtok[:, n, :], scalar1=bt[:, n, dh:dh + 1],
                                                               scalar2=None, op0=ALU.mult), reads=[vtok, bt], writes=[X])
                        S.op("pool", lambda e: e.tensor_scalar(out=X[:, 128:256], in0=ktok[:, n, :], scalar1=bg[:, n, dh:dh + 1],
                                                               scalar2=None, op0=ALU.mult), reads=[ktok, bg], writes=[X])
                        yield
                        if DNSTOP < 5:
                            return
                        S.op("dve", lambda e: e.tensor_tensor(out=Ad[:], in0=Am[:], in1=cst[:, 9, :], op=ALU.mult),
                             reads=[Am, cst], writes=[Ad])
                        S.op("pool", lambda e: e.tensor_tensor(out=Aoff[:], in0=Am[:], in1=Ad[:], op=ALU.subtract),
                             reads=[Am, Ad], writes=[Aoff])
                        yield
                        pt1, pt2 = PQs(J), PQs(J)
                        S.op("pe", lambda e: e.transpose(pt1[:, :], Ad[:, :], identf[:]), reads=[Ad, identf], writes=[pt1])
                        S.op("pe", lambda e: e.transpose(pt2[:, :], Aoff[:, :], identf[:]), reads=[Aoff, identf], writes=[pt2])
                        yield
                        S.op("act", lambda e: e.activation(out=Bk[0][:], in_=pt1[:, :], func=AF.Copy), reads=[pt1], writes=[Bk[0]])
                        S.op("act", lambda e: e.activation(out=Boff[:], in_=pt2[:, :], func=AF.Copy), reads=[pt2], writes=[Boff])
                        yield
                        Cc = Ad
                        for k_ in range(6):
                            Bc = Bk[k_]
                            pxx = PQs(J, True)
                            S.op("pe", lambda e: e.matmul(pxx[:, :], lhsT=Bc[:, :], rhs=X[:, :], start=True, stop=True),
                                 reads=[Bc, X], writes=[pxx])
                            if k_ < 5:
                                Bn, Cn = Bk[k_ + 1], Ck[(k_ + 1) % 2]
                                pb_, pc_ = PQs(J), PQs(J)
                                S.op("pe", lambda e: e.matmul(pb_[:, :], lhsT=Cc[:, :], rhs=Bc[:, :], start=True, stop=True),
                                     reads=[Cc, Bc], writes=[pb_])
                                S.op("pe", lambda e: e.matmul(pc_[:, :], lhsT=Bc[:, :], rhs=Cc[:, :], start=True, stop=True),
                                     reads=[Cc, Bc], writes=[pc_])
                            yield
                            S.op("dve", lambda e: e.tensor_tensor(out=X[:], in0=X[:], in1=pxx[:, :],
                                                                  op=(ALU.subtract if k_ == 0 else ALU.add)),
                                 reads=[X, pxx], writes=[X])
                            if k_ < 5:
                                S.op("act", lambda e: e.activation(out=Bn[:], in_=pb_[:, :], func=AF.Copy), reads=[pb_], writes=[Bn])
                                S.op("dve", lambda e: e.tensor_copy(out=Cn[:], in_=pc_[:, :]), reads=[pc_], writes=[Cn])
                                Cc = Cn
                            yield
                        pz = PQs(J, True)
                        S.op("pe", lambda e: e.matmul(pz[:, :], lhsT=Boff[:, :], rhs=X[:, :], start=True, stop=True),
                             reads=[Boff, X], writes=[pz])
                        yield
                        S.op("act", lambda e: e.activation(out=Zb[:], in_=pz[:, :], func=AF.Copy), reads=[pz], writes=[Zb])
                        yield
                        for k_ in range(6):
                            pxx = PQs(J, True)
                            S.op("pe", lambda e: e.matmul(pxx[:, :], lhsT=Bk[k_][:, :], rhs=Zb[:, :], start=True, stop=True),
                                 reads=[Bk[k_], Zb], writes=[pxx])
                            yield
                            S.op("dve", lambda e: e.tensor_tensor(out=Zb[:], in0=Zb[:], in1=pxx[:, :],
                                                                  op=(ALU.subtract if k_ == 0 else ALU.add)),
                                 reads=[Zb, pxx], writes=[Zb])
                            yield
                        S.op("dve", lambda e: e.tensor_tensor(out=X[:], in0=X[:], in1=Zb[:], op=ALU.subtract),
                             reads=[X, Zb], writes=[X])
                        yield
                        pw = PQs(J)
                        S.op("pe", lambda e: e.transpose(pw[:, :], X[:, 128:256], identf[:]), reads=[X, identf], writes=[pw])
                        yield
                        S.op("act", lambda e: e.activation(out=B["wT"][:], in_=pw[:, :], func=AF.Copy), reads=[pw], writes=[B["wT"]])
                        S.op("pool", lambda e: e.tensor_scalar(out=B["kd"][:], in0=ktok[:, n, :], scalar1=edl[:, n, dh:dh + 1],
                                                               scalar2=None, op0=ALU.mult), reads=[ktok, edl], writes=[B["kd"]])

                    def scan(n, B, d=d, dh=dh):
                        c0 = n * 128
                        X, qkT, wT, kd = B["X"], B["qkT"], B["wT"], B["kd"]
                        p1, p2, p3, p4 = PQ(), PQ(), PQ(), PQ()
                        S.op("pe", lambda e: e.matmul(p1[:, :], lhsT=wT[:, :], rhs=Sst[:, :], start=True, stop=True),
                             reads=[wT, Sst], writes=[p1])
                        S.op("pe", lambda e: e.matmul(p2[:, :], lhsT=qT[:, c0:c0 + 128], rhs=Sst[:, :], start=True, stop=True),
                             reads=[qT, Sst], writes=[p2])
                        S.op("dve", lambda e: e.tensor_tensor(out=vnew[:], in0=X[:, 0:128], in1=p1[:, :], op=ALU.subtract),
                             reads=[X, p1], writes=[vnew])
                        S.op("pe", lambda e: e.matmul(p3[:, :], lhsT=qkT[:, :], rhs=vnew[:, :], start=True, stop=True),
                             reads=[qkT, vnew], writes=[p3])
                        S.op("pe", lambda e: e.matmul(p4[:, :], lhsT=kd[:, :], rhs=vnew[:, :], start=True, stop=True),
                             reads=[kd, vnew], writes=[p4])
                        S.op("dve", lambda e: e.scalar_tensor_tensor(out=Sst[:], in0=Sst[:], scalar=egl[:, n, dh:dh + 1], in1=p4[:, :],
                                                                     op0=ALU.mult, op1=ALU.add), reads=[Sst, egl, p4], writes=[Sst])
                        S.op("dve", lambda e: e.tensor_scalar(out=tmp[:], in0=p2[:, :], scalar1=egc[:, n, dh:dh + 1], scalar2=None,
                                                              op0=ALU.mult), reads=[p2, egc], writes=[tmp])
                        osl = oall[:, n, h * 128:(h + 1) * 128]
                        if d == 0:
                            S.op("pool" if False else "dve", lambda e: e.tensor_tensor(out=osl, in0=tmp[:], in1=p3[:, :], op=ALU.add),
                                 reads=[tmp, p3], writes=[oall])
                        else:
                            S.op("dve", lambda e: e.tensor_tensor(out=tmp[:], in0=tmp[:], in1=p3[:, :], op=ALU.add),
                                 reads=[tmp, p3], writes=[tmp])
                            S.op("pool", lambda e: e.tensor_tensor(out=osl, in0=osl, in1=tmp[:], op=ALU.add),
                                 reads=[tmp, oall], writes=[oall])

                    NG = 2
                    for g0 in range(0, nch, NG):
                        grp = order[g0:g0 + NG]
                        gens = [prep(n, Bsets[j]) for j, n in enumerate(grp)]
                        alive = list(gens)
                        while alive:
                            for g_ in list(alive):
                                try:
                                    next(g_)
                                except StopIteration:
                                    alive.remove(g_)
                        if DNSTOP < 6:
                            continue
                        for j, n in enumerate(grp):
                            scan(n, Bsets[j])
                    if not latent:
                        ost = R["o_dn"][d]
                        S.dma("sp", lambda e: e.dma_start(out=ost[si, l, h, :, :], in_=Sst[:]), reads=[Sst], writes=[ost])
            for n in range(nch):
                if DNSTOP < 7:
                    continue
                g0 = tok0 + n * 128
                S.dma("sp", lambda e: e.dma_start(out=zt[:], in_=P_T[g0:g0 + 128, az:az + 1024]), reads=[P_T], writes=[zt])
                S.op("act", lambda e: e.activation(out=zt[:], in_=zt[:], func=AF.Silu), reads=[zt], writes=[zt])
                o3 = oall[:, n, :].rearrange("p (h v) -> p h v", h=8)
                for h in range(8):
                    S.op("act", lambda e: e.activation(out=tmp[:], in_=oall[:, n, h * 128:(h + 1) * 128], func=AF.Square,
                                                       accum_out=ssq[:, h:h + 1]), reads=[oall], writes=[tmp, ssq])
                S.op("act", lambda e: e.activation(out=ssq[:], in_=ssq[:], func=AF.Sqrt, scale=1.0 / 128, bias=EPS),
                     reads=[ssq], writes=[ssq])
                S.op("dve", lambda e: e.reciprocal(out=ssq[:], in_=ssq[:]), reads=[ssq], writes=[ssq])
                S.op("dve", lambda e: e.tensor_tensor(out=o3, in0=o3, in1=ssq[:, :].unsqueeze(2).to_broadcast([128, 8, 128]),
                                                      op=ALU.mult), reads=[oall, ssq], writes=[oall])
                S.op("dve", lambda e: e.tensor_tensor(out=o3, in0=o3, in1=gon[:, :].unsqueeze(1).to_broadcast([128, 8, 128]),
                                                      op=ALU.mult), reads=[oall, gon], writes=[oall])
                S.op("dve", lambda e: e.tensor_tensor(out=ob[:], in0=oall[:, n, :], in1=zt[:], op=ALU.mult),
                     reads=[oall, zt], writes=[ob])
                for h in range(8):
                    S.op("pe", lambda e: e.transpose(ptr[:, h, :], ob[:, h * 128:(h + 1) * 128], identb[:]),
                         reads=[ob, identb], writes=[ptr])
                _evac(S, n, obT[:], ptr[:, :, :], reads=[ptr], writes=[obT])
                S.dma("sp", lambda e: e.dma_start(out=brT[1][:, g0:g0 + 128].rearrange("(h p) t -> p h t", p=128), in_=obT[:]),
                      reads=[obT], writes=[])
        S.barrier()
    S.scope = S.es


def _hy_geom(L):
    nf = L + 1
    KT = (nf + 127) // 128
    NB = (nf + 511) // 512
    return nf, KT, NB


def stage_hyena(S, l, R):
    P_F, brT = R["P_F"], R["brT"]
    ident = R["ident"]
    fhy, _ = F_ROWS["hyT"]
    hyZ, hyY1, hyU, hyPQ, hyYS = R["hyZ"], R["hyY1"], R["hyU"], R["hyPQ"], R["hyYS"]
    PI = float(np.pi)
    seqs = [(s * LC, LC, False) for s in range(NCTX)] + [(NCTX * LC, LL, True)]
    for (Lx, tabi, seq_list) in ((LC, 0, seqs[:NCTX]), (LL, 1, seqs[NCTX:])):
        L = Lx
        nf, KT, NB = _hy_geom(L)
        KU = L // 128
        tab = R["hy_tab"][tabi]
        with ExitStack() as sc:
            S.scope = sc
            w1 = S.sbuf("hyw1", [33, 64], F32)
            w2 = S.sbuf("hyw2", [64, 64], F32)
            w3 = S.sbuf("hyw3", [64, 4096], F32)
            b12 = S.sbuf("hyb12", [64, 2], F32)
            S.dma("sp", lambda e: e.dma_start(out=w1[:], in_=R["hy_w1"][l]), reads=[R["hy_w1"]], writes=[w1])
            S.dma("sp", lambda e: e.dma_start(out=w2[:], in_=R["hy_w2"][l]), reads=[R["hy_w2"]], writes=[w2])
            S.dma("sp", lambda e: e.dma_start(out=w3[:], in_=R["hy_w3"][l]), reads=[R["hy_w3"]], writes=[w3])
            S.dma("sp", lambda e: e.dma_start(out=b12[:], in_=R["hy_b12"][l]), reads=[R["hy_b12"]], writes=[b12])
            zT = S.sbuf("hyzT", [33, L], F32)
            S.dma("sp", lambda e: e.dma_start(out=zT[:], in_=R["hy_zemb"][tabi][:, :]), reads=[R["hy_zemb"][tabi]], writes=[zT])
            wfs = S.sbuf("hywf", [128, KT], F32)
            S.dma("sp", lambda e: e.dma_start(out=wfs[:], in_=R["hy_wf"][tabi][:, :]), reads=[R["hy_wf"][tabi]], writes=[wfs])
            h1 = S.sbuf("hyh1", [64, L], F32)
            h2 = S.sbuf("hyh2", [64, L], F32)
            xa = S.sbuf("hyxa", [64, 512], F32)
            xb_ = S.sbuf("hyxb", [64, 512], F32)
            xc = S.sbuf("hyxc", [64, 512], F32)
            win = S.sbuf("hywin", [128, 1024], F32)
            hs = S.sbuf("hyhs", [128, KU, 1024], BF16)
            hd = S.sbuf("hyhd", [128, KU, 1024], BF16)
            tf = [S.sbuf("hytf%d" % i, [128, 512], F32) for i in range(2)]
            slab = [S.sbuf("hyslab%d" % i, [128, KT, 512], BF16) for i in range(2)]
            pst = S.sbuf("hypst", [128, 512], F32)
            pm = [S.psum("hypm%d" % i, [128, 512], F32) for i in range(4)]
            npm = [0]

            def PM():
                npm[0] += 1
                return pm[npm[0] % 4]

            def sin_layer(dst, wmat, kdim, src, bcol):
                for g in range((L + 511) // 512):
                    w = min(512, L - g * 512)
                    ps = PM()
                    S.op("pe", lambda e: e.matmul(ps[0:64, 0:w], lhsT=wmat[0:kdim, :], rhs=src[0:kdim, g * 512:g * 512 + w],
                                                  start=True, stop=True), reads=[wmat, src], writes=[ps])
                    S.op("dve", lambda e: e.tensor_scalar(out=xa[:, 0:w], in0=ps[0:64, 0:w], scalar1=b12[:, bcol:bcol + 1],
                                                          scalar2=None, op0=ALU.add), reads=[ps, b12], writes=[xa])
                    S.op("dve", lambda e: e.tensor_scalar(out=xb_[:, 0:w], in0=xa[:, 0:w], scalar1=PI, scalar2=-2 * PI,
                                                          op0=ALU.is_gt, op1=ALU.mult), reads=[xa], writes=[xb_])
                    S.op("dve", lambda e: e.tensor_scalar(out=xc[:, 0:w], in0=xa[:, 0:w], scalar1=-PI, scalar2=2 * PI,
                                                          op0=ALU.is_lt, op1=ALU.mult), reads=[xa], writes=[xc])
                    S.op("dve", lambda e: e.tensor_tensor(out=xa[:, 0:w], in0=xa[:, 0:w], in1=xb_[:, 0:w], op=ALU.add),
                         reads=[xa, xb_], writes=[xa])
                    S.op("dve", lambda e: e.tensor_tensor(out=xa[:, 0:w], in0=xa[:, 0:w], in1=xc[:, 0:w], op=ALU.add),
                         reads=[xa, xc], writes=[xa])
                    S.op("act", lambda e: e.activation(out=dst[:, g * 512:g * 512 + w], in_=xa[:, 0:w], func=AF.Sin),
                         reads=[xa], writes=[dst])

            sin_layer(h1, w1, 33, zT, 0)
            sin_layer(h2, w2, 64, h1, 1)
            for o in range(2):
                for t in range(KU):
                    S.dma("sp", lambda e: e.dma_start(out=win[:], in_=R["hy_win"][tabi][t * 128:(t + 1) * 128, :]),
                          reads=[R["hy_win"][tabi]], writes=[win])
                    for cb in range(2):
                        pf, pb = PM(), PM()
                        cf = (o * 2 + 0) * 1024 + cb * 512
                        cbk = (o * 2 + 1) * 1024 + cb * 512
                        S.op("pe", lambda e: e.matmul(pf[:, :], lhsT=h2[:, t * 128:(t + 1) * 128], rhs=w3[:, cf:cf + 512],
                                                      start=True, stop=True), reads=[h2, w3], writes=[pf])
                        S.op("pe", lambda e: e.matmul(pb[:, :], lhsT=h2[:, t * 128:(t + 1) * 128], rhs=w3[:, cbk:cbk + 512],
                                                      start=True, stop=True), reads=[h2, w3], writes=[pb])
                        S.op("dve", lambda e: e.tensor_tensor(out=tf[0][:], in0=pf[:, :], in1=win[:, cb * 512:(cb + 1) * 512],
                                                              op=ALU.mult), reads=[pf, win], writes=[tf[0]])
                        S.op("dve", lambda e: e.tensor_tensor(out=tf[1][:], in0=pb[:, :], in1=win[:, cb * 512:(cb + 1) * 512],
                                                              op=ALU.mult), reads=[pb, win], writes=[tf[1]])
                        if t == 0:
                            S.op("dve", lambda e: e.memset(tf[1][0:1, :], 0.0), writes=[tf[1]])
                        S.op("dve", lambda e: e.tensor_tensor(out=hs[:, t, cb * 512:(cb + 1) * 512], in0=tf[0][:], in1=tf[1][:],
                                                              op=ALU.add), reads=[tf[0], tf[1]], writes=[hs])
                        S.op("pool", lambda e: e.tensor_tensor(out=hd[:, t, cb * 512:(cb + 1) * 512], in0=tf[0][:], in1=tf[1][:],
                                                               op=ALU.subtract), reads=[tf[0], tf[1]], writes=[hd])
                for fb in range(NB):
                    for cs in range(2):
                        S.dma("sp", lambda e: e.dma_start(out=slab[cs][:], in_=tab[cs, fb]), reads=[tab], writes=[slab[cs]])
                    for j in range(4):
                        m = fb * 4 + j
                        if m >= KT:
                            break
                        fm = min(128, nf - m * 128)
                        for cs, src in ((0, hs), (1, hd)):
                            for cb in range(2):
                                ps = PM()
                                for kt in range(KU):
                                    S.op("pe", lambda e: e.matmul(ps[0:fm, :], lhsT=slab[cs][:, kt, j * 128:j * 128 + fm],
                                                                  rhs=src[:, kt, cb * 512:(cb + 1) * 512], start=(kt == 0),
                                                                  stop=(kt == KU - 1)), reads=[slab[cs], src], writes=[ps])
                                S.op("dve", lambda e: e.tensor_scalar(out=pst[0:fm, :], in0=ps[0:fm, :], scalar1=wfs[0:fm, m:m + 1],
                                                                      scalar2=None, op0=ALU.mult), reads=[ps, wfs], writes=[pst])
                                S.dma("sp", lambda e: e.dma_start(
                                    out=hyPQ[tabi][o, cs, m * 128:m * 128 + fm, cb * 512:(cb + 1) * 512], in_=pst[0:fm, :]),
                                    reads=[pst], writes=[])
            S.barrier()
        for (tok0, L_, latent) in seq_list:
            si = tok0 // LC if not latent else NCTX
            with ExitStack() as sc:
                S.scope = sc
                cw = S.sbuf("hycw", [128, 24, 3], F32)
                S.dma("sp", lambda e: e.dma_start(out=cw[:], in_=R["hy_convT"][l]), reads=[R["hy_convT"]], writes=[cw])
                xin = [S.sbuf("hyxin%d" % i, [128, L + 2], F32) for i in range(2)]
                acc = [S.sbuf("hyacc%d" % i, [128, L], F32) for i in range(2)]
                accb = S.sbuf("hyaccb", [128, L], BF16)
                ut = S.sbuf("hyut", [128, KU, 128], BF16)
                ptr = [S.psum("hyptr%d" % i, [128, 8, 128], BF16) for i in range(2)]
                for ch in range(24):
                    xi, ac = xin[ch % 2], acc[ch % 2]
                    r0 = fhy + ch * 128
                    S.op("pool", lambda e: e.memset(xi[:, 0:1], 0.0), writes=[xi])
                    S.op("pool", lambda e: e.memset(xi[:, L + 1:L + 2], 0.0), writes=[xi])
                    S.dma("sp", lambda e: e.dma_start(out=xi[:, 1:L + 1], in_=P_F[r0:r0 + 128, tok0:tok0 + L]), reads=[P_F], writes=[xi])
                    S.op("dve", lambda e: e.tensor_scalar(out=ac[:], in0=xi[:, 0:L], scalar1=cw[:, ch, 0:1], scalar2=None, op0=ALU.mult),
                         reads=[xi, cw], writes=[ac])
                    for kk in (1, 2):
                        S.op("dve", lambda e: e.scalar_tensor_tensor(out=ac[:], in0=xi[:, kk:kk + L], scalar=cw[:, ch, kk:kk + 1], in1=ac[:],
                                                                     op0=ALU.mult, op1=ALU.add), reads=[xi, cw, ac], writes=[ac])
                    S.dma("sp", lambda e: e.dma_start(out=hyZ[ch * 128:(ch + 1) * 128, tok0:tok0 + L], in_=ac[:]), reads=[ac], writes=[])
                    if ch < 8:
                        S.op("act", lambda e: e.activation(out=accb[:], in_=ac[:], func=AF.Copy), reads=[ac], writes=[accb])
                        for t0 in range(0, KU, 8):
                            pt = ptr[(t0 // 8) % 2]
                            nn = min(8, KU - t0)
                            for j in range(nn):
                                S.op("pe", lambda e: e.transpose(pt[:, j, :], accb[:, (t0 + j) * 128:(t0 + j + 1) * 128], ident[:]),
                                     reads=[accb, ident], writes=[pt])
                            _evac(S, t0 // 8, ut[:, t0:t0 + nn, :], pt[:, 0:nn, :], reads=[pt], writes=[ut])
                        S.dma("sp", lambda e: e.dma_start(
                            out=hyU[tok0:tok0 + L, ch * 128:(ch + 1) * 128].rearrange("(k p) c -> p k c", p=128), in_=ut[:]),
                            reads=[ut], writes=[])
                S.barrier()
            for o in range(2):
                with ExitStack() as sc:
                    S.scope = sc
                    u = S.sbuf("hyu", [128, KU, 1024], BF16)
                    S.dma("sp", lambda e: e.dma_start(out=u[:], in_=hyU[tok0:tok0 + L, :].rearrange("(k p) c -> p k c", p=128)),
                          reads=[hyU], writes=[u])
                    slab = [S.sbuf("hyslabf%d" % i, [128, KT, 512], BF16) for i in range(2)]
                    PQt = [S.sbuf("hyPQt%d" % i, [128, 1024], F32) for i in range(2)]
                    ta = S.sbuf("hyta", [128, 512], F32)
                    tb_ = S.sbuf("hytb", [128, 512], F32)
                    yc = S.sbuf("hyyc", [128, 512], BF16)
                    ys = S.sbuf("hyys", [128, 512], BF16)
                    pm = [S.psum("hypmf%d" % i, [128, 512], F32) for i in range(4)]
                    for fb in range(NB):
                        for cs in range(2):
                            S.dma("sp", lambda e: e.dma_start(out=slab[cs][:], in_=tab[cs, fb]), reads=[tab], writes=[slab[cs]])
                        for j in range(4):
                            m = fb * 4 + j
                            if m >= KT:
                                break
                            fm = min(128, nf - m * 128)
                            for cs in range(2):
                                S.dma("sp", lambda e: e.dma_start(out=PQt[cs][0:fm, :], in_=hyPQ[tabi][o, cs, m * 128:m * 128 + fm, :]),
                                      reads=[hyPQ[tabi]], writes=[PQt[cs]])
                            for cb in range(2):
                                pa, pb = pm[(2 * cb) % 4], pm[(2 * cb + 1) % 4]
                                for kt in range(KU):
                                    S.op("pe", lambda e: e.matmul(pa[0:fm, :], lhsT=slab[0][:, kt, j * 128:j * 128 + fm],
                                                                  rhs=u[:, kt, cb * 512:(cb + 1) * 512], start=(kt == 0), stop=(kt == KU - 1)),
                                         reads=[slab[0], u], writes=[pa])
                                for kt in range(KU):
                                    S.op("pe", lambda e: e.matmul(pb[0:fm, :], lhsT=slab[1][:, kt, j * 128:j * 128 + fm],
                                                                  rhs=u[:, kt, cb * 512:(cb + 1) * 512], start=(kt == 0), stop=(kt == KU - 1)),
                                         reads=[slab[1], u], writes=[pb])
                                Pc = PQt[0][0:fm, cb * 512:(cb + 1) * 512]
                                Qc = PQt[1][0:fm, cb * 512:(cb + 1) * 512]
                                S.op("dve", lambda e: e.tensor_tensor(out=ta[0:fm, :], in0=pa[0:fm, :], in1=Pc, op=ALU.mult),
                                     reads=[pa, PQt[0]], writes=[ta])
                                S.op("dve", lambda e: e.tensor_tensor(out=tb_[0:fm, :], in0=pb[0:fm, :], in1=Qc, op=ALU.mult),
                                     reads=[pb, PQt[1]], writes=[tb_])
                                S.op("pool", lambda e: e.tensor_tensor(out=yc[0:fm, :], in0=ta[0:fm, :], in1=tb_[0:fm, :], op=ALU.subtract),
                                     reads=[ta, tb_], writes=[yc])
                                S.op("dve", lambda e: e.tensor_tensor(out=ta[0:fm, :], in0=pa[0:fm, :], in1=Qc, op=ALU.mult),
                                     reads=[pa, PQt[1]], writes=[ta])
                                S.op("dve", lambda e: e.tensor_tensor(out=tb_[0:fm, :], in0=pb[0:fm, :], in1=Pc, op=ALU.mult),
                                     reads=[pb, PQt[0]], writes=[tb_])
                                S.op("pool", lambda e: e.tensor_tensor(out=ys[0:fm, :], in0=ta[0:fm, :], in1=tb_[0:fm, :], op=ALU.add),
                                     reads=[ta, tb_], writes=[ys])
                                S.dma("sp", lambda e: e.dma_start(out=hyYS[0, m * 128:m * 128 + fm, cb * 512:(cb + 1) * 512], in_=yc[0:fm, :]),
                                      reads=[yc], writes=[])
                                S.dma("sp", lambda e: e.dma_start(out=hyYS[1, m * 128:m * 128 + fm, cb * 512:(cb + 1) * 512], in_=ys[0:fm, :]),
                                      reads=[ys], writes=[])
                    S.barrier()
                with ExitStack() as sc:
                    S.scope = sc
                    ycs = [S.sbuf("hyYc%d" % i, [128, KT, 1024], BF16) for i in range(2)]
                    for cs in range(2):
                        S.op("dve", lambda e: e.memset(ycs[cs][:, KT - 1, :], 0.0), writes=[ycs[cs]])
                        S.dma("sp", lambda e: e.dma_start(
                            out=ycs[cs][:, 0:KT - 1, :], in_=hyYS[cs, 0:(KT - 1) * 128, :].rearrange("(k p) c -> p k c", p=128)),
                            reads=[hyYS], writes=[ycs[cs]])
                        S.dma("sp", lambda e: e.dma_start(out=ycs[cs][0:1, KT - 1, :], in_=hyYS[cs, (KT - 1) * 128:(KT - 1) * 128 + 1, :]),
                              reads=[hyYS], writes=[ycs[cs]])
                    slab = [S.sbuf("hyslabi%d" % i, [128, KT, 512], BF16) for i in range(2)]
                    bia = S.sbuf("hybia", [128, 8], F32)
                    S.dma("sp", lambda e: e.dma_start(out=bia[:], in_=R["hy_biasT"][l, o]), reads=[R["hy_biasT"]], writes=[bia])
                    uT = [S.sbuf("hyuT%d" % i, [128, 512], F32) for i in range(2)]
                    gT = [S.sbuf("hygT%d" % i, [128, 512], F32) for i in range(2)]
                    yo = [S.sbuf("hyyo%d" % i, [128, 512], F32) for i in range(2)]
                    yb = [S.sbuf("hyyb%d" % i, [128, 512], BF16) for i in range(2)]
                    ut = [S.sbuf("hyuti%d" % i, [128, 4, 128], BF16) for i in range(2)]
                    pm = [S.psum("hypmi%d" % i, [128, 512], F32) for i in range(4)]
                    ptr = [S.psum("hyptri%d" % i, [128, 8, 128], BF16) for i in range(2)]
                    it = 0
                    for tb in range((L + 511) // 512):
                        tw = min(512, L - tb * 512)
                        for cs in range(2):
                            S.dma("sp", lambda e: e.dma_start(out=slab[cs][:], in_=tab[cs, tb]), reads=[tab], writes=[slab[cs]])
                        for cc in range(8):
                            ps = pm[it % 4]
                            u_, g_, y_, yb_, ut_ = uT[it % 2], gT[it % 2], yo[it % 2], yb[it % 2], ut[it % 2]
                            it += 1
                            usrc = hyZ if o == 0 else hyY1
                            S.dma("sp", lambda e: e.dma_start(out=u_[:, 0:tw], in_=usrc[cc * 128:(cc + 1) * 128, tok0 + tb * 512:tok0 + tb * 512 + tw]),
                                  reads=[usrc], writes=[u_])
                            gr = (1 + o) * 1024 + cc * 128
                            S.dma("sp", lambda e: e.dma_start(out=g_[:, 0:tw], in_=hyZ[gr:gr + 128, tok0 + tb * 512:tok0 + tb * 512 + tw]),
                                  reads=[hyZ], writes=[g_])
                            n_mm = 2 * KT
                            i_mm = 0
                            for cs in range(2):
                                for kt in range(KT):
                                    kp = 128 if kt < KT - 1 else 1
                                    S.op("pe", lambda e: e.matmul(ps[:, 0:tw], lhsT=ycs[cs][0:kp, kt, cc * 128:(cc + 1) * 128],
                                                                  rhs=slab[cs][0:kp, kt, 0:tw], start=(i_mm == 0), stop=(i_mm == n_mm - 1)),
                                         reads=[ycs[cs], slab[cs]], writes=[ps])
                                    i_mm += 1
                            S.op("dve", lambda e: e.scalar_tensor_tensor(out=y_[:, 0:tw], in0=u_[:, 0:tw], scalar=bia[:, cc:cc + 1],
                                                                         in1=ps[:, 0:tw], op0=ALU.mult, op1=ALU.add),
                                 reads=[u_, bia, ps], writes=[y_])
                            if o == 0:
                                S.op("pool", lambda e: e.tensor_tensor(out=y_[:, 0:tw], in0=y_[:, 0:tw], in1=g_[:, 0:tw], op=ALU.mult),
                                     reads=[y_, g_], writes=[y_])
                                S.dma("sp", lambda e: e.dma_start(out=hyY1[cc * 128:(cc + 1) * 128, tok0 + tb * 512:tok0 + tb * 512 + tw],
                                                                  in_=y_[:, 0:tw]), reads=[y_], writes=[])
                                S.op("act", lambda e: e.activation(out=yb_[:, 0:tw], in_=y_[:, 0:tw], func=AF.Copy), reads=[y_], writes=[yb_])
                                pt = ptr[it % 2]
                                nt = tw // 128
                                for j in range(nt):
                                    S.op("pe", lambda e: e.transpose(pt[:, j, :], yb_[:, j * 128:(j + 1) * 128], ident[:]),
                                         reads=[yb_, ident], writes=[pt])
                                _evac(S, it, ut_[:, 0:nt, :], pt[:, 0:nt, :], reads=[pt], writes=[ut_])
                                S.dma("sp", lambda e: e.dma_start(
                                    out=hyU[tok0 + tb * 512:tok0 + tb * 512 + tw, cc * 128:(cc + 1) * 128].rearrange("(k p) c -> p k c", p=128),
                                    in_=ut_[:, 0:nt, :]), reads=[ut_], writes=[])
                            else:
                                S.op("pool", lambda e: e.tensor_tensor(out=yb_[:, 0:tw], in0=y_[:, 0:tw], in1=g_[:, 0:tw], op=ALU.mult),
                                     reads=[y_, g_], writes=[yb_])
                                S.dma("sp", lambda e: e.dma_start(out=brT[2][cc * 128:(cc + 1) * 128, tok0 + tb * 512:tok0 + tb * 512 + tw],
                                                                  in_=yb_[:, 0:tw]), reads=[yb_], writes=[])
                    S.barrier()
    S.scope = S.es


def build_program(dbg=False):
    nc = bass.Bass("TRN2", target_bir_lowering=False)
    k = K()
    k.nc = nc

    def ein(name, shape, dtype=F32):
        return Buf(nc.dram_tensor(name, list(shape), dtype, kind="ExternalInput").ap(), name)

    def eout(name, shape, dtype=F32):
        return Buf(nc.dram_tensor(name, list(shape), dtype, kind="ExternalOutput").ap(), name)

    x_in = ein("x_in", [TT, D])
    c2T = ein("c2T", [128, 16, 2])
    ident_in = ein("ident", [128, 128])
    norm1_g = ein("norm1_g", [DEPTH, D])
    w_ada = ein("w_ada", [DEPTH, D, 6 * D])
    b_ada = ein("b_ada", [DEPTH, 6 * D])
    w_in_T = ein("w_in_T", [DEPTH, D, NT_COLS])
    w_in_F = ein("w_in_F", [DEPTH, D, NF_ROWS])
    mla_kv_norm = ein("mla_kv_norm", [DEPTH, 256])
    R = {}
    R["ident_in"] = ident_in
    R["mla_kv_norm"] = mla_kv_norm
    R["mla_q_norm"] = ein("mla_q_norm", [DEPTH, 512])
    R["w_qb"] = ein("w_qb_p", [DEPTH, 512, 2048])
    R["w_kvb"] = ein("w_kvb_p", [DEPTH, 256, 2048])
    R["cache_ckv"] = ein("cache_ckv", [DEPTH, PAST, 256])
    R["cache_kr"] = ein("cache_kr", [DEPTH, PAST, 64])
    R["cache_nak"] = ein("cache_nak", [DEPTH, PAST, 1024])
    R["cache_nav"] = ein("cache_nav", [DEPTH, PAST, 1024])
    R["ropeT"] = ein("ropeT", [2, 64, LL])
    R["na_ctab"] = ein("na_ctab", [DEPTH, 8, 64, 31, 64])
    R["na_mask"] = ein("na_mask", [NA_NMASK, 128, 512])
    R["w_branch"] = ein("w_branch", [DEPTH, 4, 1024, 2048])
    R["w_out"] = ein("w_out", [DEPTH, D, D])
    R["norm2_g"] = ein("norm2_g", [DEPTH, D])
    R["peer_wq"] = ein("peer_wq", [DEPTH, D, D])
    R["peer_keysT"] = ein("peer_keysT", [DEPTH, 2, 128, 128])
    R["peer_u"] = [ein("peer_u%d" % i, [16384, D]) for i in range(DEPTH)]
    R["peer_v"] = [ein("peer_v%d" % i, [16384, D]) for i in range(DEPTH)]
    R["final_g"] = ein("final_g", [1, D])
    R["iota256"] = ein("iota256", [1, 256])
    _hy_inputs(R, ein)
    R["dn_consts"] = ein("dn_consts", [10, 128, 128])
    R["dn_convT"] = ein("dn_convT", [DEPTH, 128, 24, 5])
    R["dn_a_log"] = ein("dn_a_log", [DEPTH, 16])
    R["dn_dt_bias"] = ein("dn_dt_bias", [DEPTH, 16])
    R["dn_out_norm"] = ein("dn_out_norm", [DEPTH, 128])
    R["dn_state"] = [ein("dn_state%d" % i, [DEPTH, 8, 128, 128]) for i in range(2)]
    if DBG_BR:
        R["dbg_br"] = ein("dbg_br", [DEPTH, 2, 1024, TT], BF16)

    o_ckv = eout("o_ckv", [NCTX, DEPTH, LC, 256])
    o_kr = eout("o_kr", [NCTX, DEPTH, LC, 64])
    o_nak = eout("o_nak", [NCTX, DEPTH, LC, 1024])
    o_nav = eout("o_nav", [NCTX, DEPTH, LC, 1024])
    y_out = eout("y_out", [TT, D])
    o_dn = [eout("o_dn%d" % i, [NCTX, DEPTH, 8, 128, 128]) for i in range(2)]
    R["o_dn"] = o_dn
    outs = [o_ckv, o_kr, o_nak, o_nav, y_out] + o_dn
    R["o_ckv"] = o_ckv
    R["y_out"] = y_out

    with ExitStack() as es:
        S = Sched(nc, es)
        k.S = S
        modd = S.dram("modd", [2, 6 * D])
        P_T = S.dram("P_T", [TT, NT_COLS])
        P_F = S.dram("P_F", [NF_ROWS, TT])
        brT = [S.dram("brT%d" % b_, [1024, TT], BF16) for b_ in range(4)]
        R.update(P_T=P_T, P_F=P_F, brT=brT, modd=modd)
        R["mrg"] = S.dram("mrg", [TT, D])
        R["xbuf"] = S.dram("xbuf", [TT, D])
        R["mrgt"] = [Buf(R["mrg"].t[t_ * 128:(t_ + 1) * 128, :], "mrgt%d" % t_) for t_ in range(NTILE)]
        R["xbt"] = [Buf(R["xbuf"].t[t_ * 128:(t_ + 1) * 128, :], "xbt%d" % t_) for t_ in range(NTILE)]
        _hy_scratch(S, R)
        R["peer_ub"] = [S.dram("peer_ub%d" % i, [16384, D], BF16) for i in range(DEPTH)]
        R["peer_vb"] = [S.dram("peer_vb%d" % i, [16384, D], BF16) for i in range(DEPTH)]

        ident = S.sbuf("identb", [128, 128], BF16)
        S.dma("pool", lambda e: e.dma_start(out=ident[:], in_=ident_in[:, :]), reads=[ident_in], writes=[ident])
        R["ident"] = ident

        for l in range(1 if DBG_BR else DEPTH):
            with ExitStack() as sc:
                S.scope = sc
                cT = S.sbuf("cT", [128, 16, 2], F32)
                sT = S.sbuf("sT", [128, 16, 2], BF16)
                S.dma("sp", lambda e: e.dma_start(out=cT[:], in_=c2T[:, :, :]), reads=[c2T], writes=[cT])
                S.op("act", lambda e: e.activation(out=sT[:], in_=cT[:], func=AF.Silu), reads=[cT], writes=[sT])
                wts = [S.sbuf("adaw%d" % i, [128, 16, 512], BF16) for i in range(2)]
                bts = [S.sbuf("adab%d" % i, [2, 512], F32) for i in range(2)]
                mts = [S.sbuf("adam%d" % i, [2, 512], F32) for i in range(2)]
                pss = [S.psum("adap%d" % i, [2, 512], F32) for i in range(2)]
                for cb in range(24):
                    wt, bt, mt, ps = wts[cb % 2], bts[cb % 2], mts[cb % 2], pss[cb % 2]
                    c0 = cb * 512
                    S.dma("pool", lambda e: e.dma_start(
                        out=wt[:], in_=w_ada[l, :, c0:c0 + 512].rearrange("(k p) c -> p k c", p=128)),
                        reads=[w_ada], writes=[wt])
                    S.dma("sp", lambda e: e.dma_start(out=bt[:], in_=b_ada[l, c0:c0 + 512].partition_broadcast(2)),
                          reads=[b_ada], writes=[bt])
                    for kc in range(16):
                        S.op("pe", lambda e: e.matmul(ps[:, :], lhsT=sT[:, kc, :], rhs=wt[:, kc, :],
                                                      start=(kc == 0), stop=(kc == 15)),
                             reads=[sT, wt], writes=[ps])
                    S.op("dve", lambda e: e.tensor_tensor(out=mt[:], in0=ps[:], in1=bt[:], op=ALU.add),
                         reads=[ps, bt], writes=[mt])
                    S.dma("sp", lambda e: e.dma_start(out=modd[:, c0:c0 + 512], in_=mt[:]), reads=[mt], writes=[modd])
            S.barrier()

            x_cur = x_in if l == 0 else R["xbuf"]
            with ExitStack() as sc:
                S.scope = sc
                hT = S.sbuf("hT", [128, 16, TT], BF16)
                with ExitStack() as sc2:
                    S.scope = sc2
                    gm = [S.sbuf("gm%d" % c, [128, D], F32) for c in range(2)]
                    sh = [S.sbuf("sh%d" % c, [128, D], F32) for c in range(2)]
                    gt = S.sbuf("gt", [128, D], F32)
                    S.dma("sp", lambda e: e.dma_start(out=gt[:], in_=norm1_g[l, :].partition_broadcast(128)),
                          reads=[norm1_g], writes=[gt])
                    for c in range(2):
                        S.dma("sp", lambda e: e.dma_start(out=sh[c][:], in_=modd[c, 0:D].partition_broadcast(128)),
                              reads=[modd], writes=[sh[c]])
                        S.dma("sp", lambda e: e.dma_start(out=gm[c][:], in_=modd[c, D:2 * D].partition_broadcast(128)),
                              reads=[modd], writes=[gm[c]])
                        S.op("dve", lambda e: e.scalar_tensor_tensor(out=gm[c][:], in0=gm[c][:], scalar=1.0, in1=gt[:],
                                                                     op0=ALU.add, op1=ALU.mult),
                             reads=[gm[c], gt], writes=[gm[c]])
                    xts = [S.sbuf("xt%d" % i, [128, D], F32) for i in range(2)]
                    junk = S.sbuf("junk", [128, D], F32)
                    hb = [S.sbuf("hb%d" % i, [128, D], BF16) for i in range(2)]
                    ss = [S.sbuf("ss%d" % i, [128, 1], F32) for i in range(2)]
                    rs = [S.sbuf("rs%d" % i, [128, 1], F32) for i in range(2)]
                    ptr = [S.psum("ptr%d" % i, [128, 8, 128], BF16) for i in range(2)]
                    for t in range(NTILE):
                        c = 0 if t < (NCTX * LC) // 128 else 1
                        xt, hbt, sst, rst = xts[t % 2], hb[t % 2], ss[t % 2], rs[t % 2]
                        S.dma("sp", lambda e: e.dma_start(out=xt[:], in_=x_cur[t * 128:(t + 1) * 128, :]),
                              reads=[x_cur], writes=[xt])
                        S.op("act", lambda e: e.activation(out=junk[:], in_=xt[:], func=AF.Square, accum_out=sst[:]),
                             reads=[xt], writes=[junk, sst])
                        S.op("act", lambda e: e.activation(out=rst[:], in_=sst[:], func=AF.Sqrt, scale=1.0 / D, bias=EPS),
                             reads=[sst], writes=[rst])
                        S.op("dve", lambda e: e.reciprocal(out=rst[:], in_=rst[:]), reads=[rst], writes=[rst])
                        S.op("dve", lambda e: e.scalar_tensor_tensor(out=xt[:], in0=xt[:], scalar=rst[:, 0:1], in1=gm[c][:],
                                                                     op0=ALU.mult, op1=ALU.mult),
                             reads=[xt, rst, gm[c]], writes=[xt])
                        S.op("pool", lambda e: e.tensor_tensor(out=hbt[:], in0=xt[:], in1=sh[c][:], op=ALU.add),
                             reads=[xt, sh[c]], writes=[hbt])
                        for half in range(2):
                            pt = ptr[half]
                            for j in range(8):
                                kc = half * 8 + j
                                S.op("pe", lambda e: e.transpose(pt[:, j, :], hbt[:, kc * 128:(kc + 1) * 128], ident[:]),
                                     reads=[hbt, ident], writes=[pt])
                            _evac(S, half, hT[:, half * 8:(half + 1) * 8, t * 128:(t + 1) * 128], pt[:, :, :],
                                  reads=[pt], writes=[hT])
                    S.barrier()
                S.scope = sc
                wts = [S.sbuf("wint%d" % i, [128, 16, 512], BF16) for i in range(2)]
                stg = [S.sbuf("stg%d" % i, [128, 512], F32) for i in range(4)]
                pmm = [S.psum("pmm%d" % i, [128, 512], F32) for i in range(4)]
                nblk = (NT_COLS + 511) // 512
                ev = 0
                for cb in range(nblk):
                    c0 = cb * 512
                    cw = min(512, NT_COLS - c0)
                    wt = wts[cb % 2]
                    S.dma("pool", lambda e: e.dma_start(
                        out=wt[:, :, 0:cw], in_=w_in_T[l, :, c0:c0 + cw].rearrange("(k p) c -> p k c", p=128)),
                        reads=[w_in_T], writes=[wt])
                    for t in range(NTILE):
                        ps, st = pmm[ev % 4], stg[ev % 4]
                        for kc in range(16):
                            S.op("pe", lambda e: e.matmul(ps[:, 0:cw], lhsT=hT[:, kc, t * 128:(t + 1) * 128],
                                                          rhs=wt[:, kc, 0:cw], start=(kc == 0), stop=(kc == 15)),
                                 reads=[hT, wt], writes=[ps])
                        _evac(S, ev, st[:, 0:cw], ps[:, 0:cw], reads=[ps], writes=[st])
                        S.dma("sp", lambda e: e.dma_start(out=P_T[t * 128:(t + 1) * 128, c0:c0 + cw], in_=st[:, 0:cw]),
                              reads=[st], writes=[])
                        ev += 1
                nblk = (NF_ROWS + 511) // 512
                for cb in range(nblk):
                    c0 = cb * 512
                    cw = min(512, NF_ROWS - c0)
                    wt = wts[cb % 2]
                    S.dma("pool", lambda e: e.dma_start(
                        out=wt[:, :, 0:cw], in_=w_in_F[l, :, c0:c0 + cw].rearrange("(k p) c -> p k c", p=128)),
                        reads=[w_in_F], writes=[wt])
                    for sb in range((cw + 127) // 128):
                        r0 = c0 + sb * 128
                        rw = min(128, NF_ROWS - r0)
                        for g in range(TT // 512):
                            ps, st = pmm[ev % 4], stg[ev % 4]
                            for kc in range(16):
                                S.op("pe", lambda e: e.matmul(ps[0:rw, :], lhsT=wt[:, kc, sb * 128:sb * 128 + rw],
                                                              rhs=hT[:, kc, g * 512:(g + 1) * 512],
                                                              start=(kc == 0), stop=(kc == 15)),
                                     reads=[hT, wt], writes=[ps])
                            _evac(S, ev, st[0:rw, :], ps[0:rw, :], reads=[ps], writes=[st])
                            S.dma("sp", lambda e: e.dma_start(out=P_F[r0:r0 + rw, g * 512:(g + 1) * 512], in_=st[0:rw, :]),
                                  reads=[st], writes=[])
                            ev += 1
            S.barrier()
            S.scope = es

            for s_ in range(NCTX):
                for (nm, ob) in (("kr", o_kr), ("nak", o_nak), ("nav", o_nav)):
                    a0, aw = T_COLS[nm]
                    S.dma("sp", lambda e: e.dma_start(out=ob[s_, l, :, :], in_=P_T[s_ * LC:(s_ + 1) * LC, a0:a0 + aw]),
                          reads=[P_T], writes=[ob])
            if DBG_BR:
                for b_ in DBG_FILL:
                    S.dma("sp", lambda e: e.dma_start(out=brT[b_][:, :], in_=R["dbg_br"][l, b_ - 1]),
                          reads=[R["dbg_br"]], writes=[brT[b_]])
            if not DBG_BR:
                for src_, dst_ in ((R["peer_u"][l], R["peer_ub"][l]), (R["peer_v"][l], R["peer_vb"][l])):
                    for r_ in range(0, 16384, 1024):
                        S.dma("pool", lambda e: e.dma_start(out=dst_[r_:r_ + 1024, :], in_=src_[r_:r_ + 1024, :]),
                              reads=[src_], writes=[dst_])
            stage_dn(S, l, R)
            if not DBG_BR:
                stage_hyena(S, l, R)
                stage_mla(S, l, R)
                stage_na(S, l, R)
            if DBG_BR:
                for nm_, b_ in (("d_brT1", 1),):
                    db = eout(nm_, [1024, TT], BF16)
                    outs.append(db)
                    S.dma("sp", lambda e: e.dma_start(out=db[:, :], in_=brT[b_][:, :]), reads=[brT[b_]], writes=[db])
            if not DBG_BR:
                stage_merge(S, l, R, x_in if l == 0 else R["xbuf"])
                stage_peer(S, l, R, last=(l == DEPTH - 1))

        for b in outs:
            if b.last_w is not None:
                S._wait("sp", b.last_w[0], b.last_w[1])
        S.barrier()
        k.ninstr = S.ninstr
    return nc, k


def _prep_weights(inp):
    w_in = inp["w_in"]
    tcols = np.concatenate([
        np.arange(O_CQ, O_CQ + 512), np.arange(O_CKV, O_CKV + 256), np.arange(O_KR, O_KR + 64),
        np.arange(O_Z, O_Z + 1024), np.arange(O_A, O_A + 16), np.arange(O_B, O_B + 16),
        np.arange(O_NA + 1024, O_NA + 2048), np.arange(O_NA + 2048, O_NA + 3072),
        np.arange(O_GATE, O_GATE + 8192)])
    sw = np.concatenate([np.arange(16, 32), np.arange(0, 16), np.arange(48, 64), np.arange(32, 48)])
    fcols = np.concatenate([
        np.arange(O_KR, O_KR + 64), O_KR + sw, np.arange(O_DN, O_DN + 3072), np.arange(O_HY, O_HY + 3072),
        np.arange(O_NA, O_NA + 1024), np.arange(O_NA + 1024, O_NA + 2048)])
    assert len(tcols) == NT_COLS and len(fcols) == NF_ROWS
    return np.ascontiguousarray(w_in[:, :, tcols]), np.ascontiguousarray(w_in[:, :, fcols])


def _prep_consts(inp):
    C = {}
    sw = np.concatenate([np.arange(16, 32), np.arange(0, 16), np.arange(48, 64), np.arange(32, 48)])
    nope = np.concatenate([np.arange(h * 192, h * 192 + 128) for h in range(8)])
    rope = np.concatenate([np.arange(h * 192 + 128, h * 192 + 192) for h in range(8)])
    ropesw = np.concatenate([h * 192 + 128 + sw for h in range(8)])
    C["w_qb_p"] = np.ascontiguousarray(inp["mla_w_qb"][:, :, np.concatenate([nope, rope, ropesw])])
    kn = np.concatenate([np.arange(h * 256, h * 256 + 128) for h in range(8)])
    vv = np.concatenate([np.arange(h * 256 + 128, h * 256 + 256) for h in range(8)])
    C["w_kvb_p"] = np.ascontiguousarray(inp["mla_w_kvb"][:, :, np.concatenate([kn, vv])])
    t = np.arange(LL)
    pos = [t // 64, t % 64]
    inv = 10000.0 ** (-np.arange(0, 32, 2, dtype=np.float32) / 32.0)
    cosT = np.zeros((64, LL), np.float32)
    sinT = np.zeros((64, LL), np.float32)
    for d in range(64):
        half, j = d // 32, d % 32
        ang = pos[half].astype(np.float32) * inv[j % 16]
        cosT[d] = np.cos(ang)
        sinT[d] = (-np.sin(ang)) if j < 16 else np.sin(ang)
    C["ropeT"] = np.stack([cosT, sinT], 0).astype(np.float32)
    rpb = inp["na_rpb"]
    NEG = np.float32(-30000.0)
    cp = np.arange(64)[:, None]
    c = np.arange(64)[None, :]
    cstart = np.clip(c - 8, 0, 48)
    cvalid = (cp >= cstart) & (cp < cstart + 16)
    dcol = np.clip(cp - c + 15, 0, 30)
    ctab = np.full((DEPTH, 8, 64, 31, 64), NEG, np.float32)
    for mm in range(31):
        dr = 22 - mm
        if 0 <= dr <= 14:
            g = rpb[:, :, dr, :][:, :, dcol]
            ctab[:, :, :, mm, :] = np.where(cvalid[None, None], g, NEG)
    C["na_ctab"] = ctab
    mask = np.full((NA_NMASK, 128, 512), NEG, np.float32)
    for (qb, kt), mi in NA_MASK_IDX.items():
        for rr in range(2):
            rp = 2 * kt + rr
            for j in range(8):
                r = 8 * qb + j
                st_ = min(max(r - 4, 0), 24)
                if st_ <= rp < st_ + 8:
                    mask[mi, rr * 64:(rr + 1) * 64, j * 64:(j + 1) * 64] = 0.0
    C["na_mask"] = mask
    C["peer_keysT"] = np.ascontiguousarray(inp["peer_keys"].transpose(0, 1, 3, 2))
    ii = np.arange(128)[:, None]
    jj = np.arange(128)[None, :]
    NEGM = np.float32(-30000.0)
    dc = np.zeros((10, 128, 128), np.float32)
    dc[9] = ((ii // 64) == (jj // 64))
    dc[0] = (ii <= jj)
    dc[1] = (ii >= jj)
    dc[2] = np.eye(128)
    dc[3] = (ii > jj)
    dc[4] = (ii < jj)
    dc[5] = np.where(ii > jj, 0.0, NEGM)
    dc[6] = np.where(ii < jj, 0.0, NEGM)
    dc[7] = np.where(jj >= ii, 0.0, NEGM)
    dc[8] = np.where(jj <= ii, 0.0, NEGM)
    C["dn_consts"] = dc
    C["dn_convT"] = np.ascontiguousarray(inp["dn_conv"].reshape(DEPTH, 5, 24, 128).transpose(0, 3, 2, 1))
    return C


_CACHE = {}
_DBG = {}


def kernel(**inp):
    inp = {k_: np.asarray(v) for k_, v in inp.items()}
    if "prog" not in _CACHE:
        _CACHE["prog"] = build_program()
    nc, kk = _CACHE["prog"]
    w_in_T, w_in_F = _prep_weights(inp)
    C = _prep_consts(inp)
    H = _hy_host(inp)
    ident = np.eye(128, dtype=np.float32)
    in_maps = []
    for i in range(NCORE):
        b = i // 2
        x_in = np.concatenate([inp["x_prompt"][2 * i].reshape(LC, D), inp["x_prompt"][2 * i + 1].reshape(LC, D),
                               inp["x_sample"][b].reshape(LL, D)], axis=0)
        c2 = np.stack([inp["c_ctx"], inp["c"][b]], axis=0)
        c2T = np.ascontiguousarray(c2.reshape(2, 16, 128).transpose(2, 1, 0))
        in_maps.append({
            "x_in": np.ascontiguousarray(x_in), "c2T": c2T, "ident": ident,
            "norm1_g": inp["norm1_g"], "w_ada": inp["w_ada"], "b_ada": inp["b_ada"],
            "w_in_T": w_in_T, "w_in_F": w_in_F, "mla_kv_norm": inp["mla_kv_norm"],
            "mla_q_norm": inp["mla_q_norm"], "w_qb_p": C["w_qb_p"], "w_kvb_p": C["w_kvb_p"],
            "cache_ckv": inp["cache_mla_ckv"][b], "cache_kr": inp["cache_mla_krope"][b],
            "cache_nak": np.ascontiguousarray(inp["cache_na_k"][b].reshape(DEPTH, PAST, 1024)),
            "cache_nav": np.ascontiguousarray(inp["cache_na_v"][b].reshape(DEPTH, PAST, 1024)),
            "ropeT": C["ropeT"], "na_ctab": C["na_ctab"], "na_mask": C["na_mask"],
            "w_branch": inp["w_branch"], "w_out": inp["w_out"], "norm2_g": inp["norm2_g"],
            "peer_wq": inp["peer_wq"], "peer_keysT": C["peer_keysT"],
            "peer_u0": inp["peer_u"][0], "peer_u1": inp["peer_u"][1],
            "peer_v0": inp["peer_v"][0], "peer_v1": inp["peer_v"][1],
            "final_g": inp["final_g"].reshape(1, D), "iota256": np.arange(256, dtype=np.float32).reshape(1, 256),
            "dn_consts": C["dn_consts"], "dn_convT": C["dn_convT"],
            "dn_a_log": inp["dn_a_log"].reshape(DEPTH, 16), "dn_dt_bias": inp["dn_dt_bias"].reshape(DEPTH, 16),
            "dn_out_norm": inp["dn_out_norm"],
            "dn_state0": inp["state_dn_fwd"][b], "dn_state1": inp["state_dn_bwd"][b],
        })
        in_maps[-1].update(H)
        if DBG_BR:
            in_maps[-1]["dbg_br"] = _DBG["br"]
    import os
    ndev = int(os.environ.get("KDEV_CORES", NCORE))
    res = run_bass_kernel_spmd(nc, in_maps[:ndev], core_ids=list(range(ndev)))
    r = list(res.results) + [res.results[0]] * (NCORE - ndev)
    _DBG["res"] = res.results[0]
    B = 16
    y_prompt = np.concatenate([r[i]["y_out"][:NCTX * LC].reshape(NCTX, LC, D) for i in range(NCORE)], axis=0)
    y_sample = np.stack([r[2 * j]["y_out"][NCTX * LC:] for j in range(4)], axis=0)
    new_ckv = np.concatenate([r[i]["o_ckv"] for i in range(NCORE)], axis=0)
    new_kr = np.concatenate([r[i]["o_kr"] for i in range(NCORE)], axis=0)
    new_nak = np.concatenate([r[i]["o_nak"] for i in range(NCORE)], axis=0).reshape(B, DEPTH, LC, 8, 128)
    new_nav = np.concatenate([r[i]["o_nav"] for i in range(NCORE)], axis=0).reshape(B, DEPTH, LC, 8, 128)
    new_dnf = np.concatenate([r[i]["o_dn0"] for i in range(NCORE)], axis=0)
    new_dnb = np.concatenate([r[i]["o_dn1"] for i in range(NCORE)], axis=0)
    return (y_prompt, y_sample, new_ckv, new_kr, new_nak, new_nav, new_dnf, new_dnb)
```

```python
import numpy as np
from contextlib import ExitStack
import concourse.bass as bass
import concourse.mybir as mybir
from concourse.bass_utils import run_bass_kernel_spmd

F32 = mybir.dt.float32
BF16 = mybir.dt.bfloat16
I32 = mybir.dt.int32
U32 = mybir.dt.uint32
U16 = mybir.dt.uint16
AF = mybir.ActivationFunctionType
ALU = mybir.AluOpType
AX = mybir.AxisListType

D = 2048
DEPTH = 2
NCORE = 8
LC = 256
LL = 2048
PAST = 512
NCTX = 2
TT = NCTX * LC + LL
NTILE = TT // 128
EPS = 1e-6

T_COLS = {}
_o = 0
for _n, _s in (("cq", 512), ("ckv", 256), ("kr", 64), ("z", 1024), ("a", 16), ("b", 16),
               ("nak", 1024), ("nav", 1024), ("gate", 8192)):
    T_COLS[_n] = (_o, _s)
    _o += _s
NT_COLS = _o
F_ROWS = {}
_o = 0
for _n, _s in (("krT", 64), ("krswT", 64), ("dnT", 3072), ("hyT", 3072), ("naqT", 1024), ("nakT", 1024)):
    F_ROWS[_n] = (_o, _s)
    _o += _s
NF_ROWS = _o

IN_SIZES = (512, 256, 64, 3072, 1024, 16, 16, 3072, 3072, 8192)
IN_OFF = np.concatenate([[0], np.cumsum(IN_SIZES)]).astype(int)
(O_CQ, O_CKV, O_KR, O_DN, O_Z, O_A, O_B, O_HY, O_NA, O_GATE) = [int(v) for v in IN_OFF[:-1]]


class Buf:
    __slots__ = ("t", "last_w", "readers", "name", "root")

    def __init__(self, t, name, root=None):
        self.t = t
        self.name = name
        self.last_w = None
        self.readers = {}
        self.root = root if root is not None else self

    def __getitem__(self, idx):
        return self.t[idx]


class Sched:
    ENG = ("pe", "act", "dve", "pool", "sp")
    NDMA = 10

    def __init__(self, nc, es):
        self.nc = nc
        self.es = es
        self.eng = {"pe": nc.tensor, "act": nc.scalar, "dve": nc.vector, "pool": nc.gpsimd, "sp": nc.sync}
        self.sem = {}
        self.cnt = {}
        for e in self.ENG:
            self.sem[e] = es.enter_context(nc.semaphore("sem_" + e))
            self.cnt[e] = 0
        self.dq = {}
        for q in ("sp", "pool"):
            sl = []
            for i in range(self.NDMA):
                key = ("dma", q, i)
                self.sem[key] = es.enter_context(nc.semaphore("dsem_%s_%d" % (q, i)))
                self.cnt[key] = 0
                sl.append(key)
            self.dq[q] = [sl, 0]
        self.seen = {e: {} for e in self.ENG}
        self.ninstr = 0
        self.scope = es

    def _nm(self, name):
        self.nid = getattr(self, "nid", 0) + 1
        return "%s_%d" % (name, self.nid)

    def sbuf(self, name, shape, dtype=F32):
        name = self._nm(name)
        return Buf(self.scope.enter_context(self.nc.sbuf_tensor(name, list(shape), dtype)), name)

    def psum(self, name, shape, dtype=F32):
        name = self._nm(name)
        return Buf(self.scope.enter_context(self.nc.psum_tensor(name, list(shape), dtype)), name)

    def dram(self, name, shape, dtype=F32, kind="Internal"):
        t = self.nc.dram_tensor(name, list(shape), dtype, kind=kind)
        return Buf(t.ap(), name)

    def _wait(self, e, key, val):
        if self.seen[e].get(key, 0) >= val:
            return
        self.eng[e].wait_ge(self.sem[key], val)
        self.seen[e][key] = val
        self.ninstr += 1

    def _deps(self, e, reads, writes):
        reads = [b.root for b in reads]
        writes = [b.root for b in writes]
        need = {}
        for b in list(reads) + list(writes):
            if b.last_w is not None:
                k, v = b.last_w
                if need.get(k, 0) < v:
                    need[k] = v
        for b in writes:
            for k, v in b.readers.items():
                if need.get(k, 0) < v:
                    need[k] = v
        for k, v in need.items():
            if k == "pe" and e == "pe":
                continue
            self._wait(e, k, v)

    def _mark(self, ev, reads, writes):
        reads = [b.root for b in reads]
        writes = [b.root for b in writes]
        k, v = ev
        for b in writes:
            b.last_w = ev
            b.readers = {}
        for b in reads:
            if b.readers.get(k, 0) < v:
                b.readers[k] = v

    def op(self, e, fn, reads=(), writes=()):
        self._deps(e, reads, writes)
        ins = fn(self.eng[e])
        self.cnt[e] += 1
        ins.then_inc(self.sem[e], 1)
        self._mark((e, self.cnt[e]), reads, writes)
        self.ninstr += 1
        return ins

    def dma(self, q, fn, reads=(), writes=()):
        sl, i = self.dq[q]
        key = sl[i % self.NDMA]
        self.dq[q][1] = i + 1
        if self.cnt[key] > 0:
            self._wait(q, key, self.cnt[key])
        self._deps(q, reads, writes)
        ins = fn(self.eng[q])
        self.cnt[key] += 16
        ins.then_inc(self.sem[key], 16)
        self._mark((key, self.cnt[key]), reads, writes)
        self.ninstr += 1
        return ins

    def barrier(self):
        for e in self.ENG:
            for key, v in self.cnt.items():
                if v > 0:
                    self._wait(e, key, v)


import os as _os
DBG_BR = bool(int(_os.environ.get("KDBG_BR", "0")))
DBG_FILL = (2,)
NTILE_PEER = 0
NT_LAST = (NCTX * LC) // 128 + (LL // 128) // 2
DNSTOP = int(_os.environ.get("KDN_STOP", "9"))
DNVAR = int(_os.environ.get("KDN_VAR", "0"))
DNSEQ = int(_os.environ.get("KDN_SEQ", "3"))


def _hy_inputs(R, ein):
    R["hy_tab"] = []
    R["hy_zemb"] = []
    R["hy_wf"] = []
    R["hy_win"] = []
    for i, L in enumerate((LC, LL)):
        nf, KT, NB = (L + 1), (L + 1 + 127) // 128, (L + 1 + 511) // 512
        R["hy_tab"].append(ein("hy_tab%d" % i, [2, NB, 128, KT, 512], BF16))
        R["hy_zemb"].append(ein("hy_zemb%d" % i, [33, L]))
        R["hy_wf"].append(ein("hy_wf%d" % i, [128, KT]))
        R["hy_win"].append(ein("hy_win%d" % i, [L, 1024]))
    R["hy_w1"] = ein("hy_w1", [DEPTH, 33, 64])
    R["hy_w2"] = ein("hy_w2", [DEPTH, 64, 64])
    R["hy_w3"] = ein("hy_w3", [DEPTH, 64, 4096])
    R["hy_b12"] = ein("hy_b12", [DEPTH, 64, 2])
    R["hy_convT"] = ein("hy_convT", [DEPTH, 128, 24, 3])
    R["hy_biasT"] = ein("hy_biasT", [DEPTH, 2, 128, 8])


def _hy_scratch(S, R):
    R["hyZ"] = S.dram("hyZ", [3072, TT])
    R["hyY1"] = S.dram("hyY1", [1024, TT])
    R["hyU"] = S.dram("hyU", [TT, 1024], BF16)
    R["hyPQ"] = [S.dram("hyPQ%d" % i, [2, 2, ((L + 1 + 127) // 128) * 128, 1024]) for i, L in enumerate((LC, LL))]
    R["hyYS"] = S.dram("hyYS", [2, ((LL + 1 + 127) // 128) * 128, 1024], BF16)


def _hy_host(inp):
    import ml_dtypes
    H = {}
    for i, L in enumerate((LC, LL)):
        nf, KT, NB = (L + 1), (L + 1 + 127) // 128, (L + 1 + 511) // 512
        r = np.arange(KT * 128, dtype=np.int64)
        q = np.arange(NB * 512, dtype=np.int64)
        prod = (r[:, None] * q[None, :]) % (2 * L)
        ang = np.pi * prod.astype(np.float64) / L
        valid = (r[:, None] <= L) & (q[None, :] <= L)
        tabs = []
        for fn in (np.cos, np.sin):
            T = np.where(valid, fn(ang), 0.0).astype(np.float32)
            T = T.reshape(KT, 128, NB, 512).transpose(2, 1, 0, 3)
            tabs.append(T)
        H["hy_tab%d" % i] = np.ascontiguousarray(np.stack(tabs, 0)).astype(ml_dtypes.bfloat16)
        f = np.arange(KT * 128)
        wf = np.where((f == 0) | (f == L), 1.0, np.where(f < L, 2.0, 0.0)) / (2.0 * L)
        H["hy_wf%d" % i] = np.ascontiguousarray(wf.reshape(KT, 128).T).astype(np.float32)
        t01 = np.linspace(0.0, 1.0, L, dtype=np.float32)[:, None]
        w = (np.float32(2.0 * np.pi) * np.arange(L, dtype=np.float32)[:, None] / np.float32(L)).astype(np.float32)
        fr = np.linspace(1e-4, 15.0, 16, dtype=np.float32)[None, :]
        z = np.concatenate([t01, np.cos(fr * w), -np.sin(fr * w)], axis=-1).astype(np.float32)
        H["hy_zemb%d" % i] = np.ascontiguousarray(z.T)
        max_decay = np.log(1e-2) / 0.3
        min_decay = np.log(1e-2) / 1.5
        deltas = np.linspace(min_decay, max_decay, 1024, dtype=np.float32)
        H["hy_win%d" % i] = np.exp(-t01 * np.abs(deltas)[None, :]).astype(np.float32)
    H["hy_w1"] = inp["hy_w1"]
    H["hy_w2"] = inp["hy_w2"]
    H["hy_w3"] = inp["hy_w3"]
    H["hy_b12"] = np.ascontiguousarray(np.stack([inp["hy_b1"], inp["hy_b2"]], axis=-1))
    H["hy_convT"] = np.ascontiguousarray(inp["hy_conv"].reshape(DEPTH, 3, 24, 128).transpose(0, 3, 2, 1))
    H["hy_biasT"] = np.ascontiguousarray(inp["hy_bias"].reshape(DEPTH, 2, 8, 128).transpose(0, 1, 3, 2))
    return H


class K:
    pass


def _evac(S, i, out_ap, in_ap, reads, writes):
    if i % 2 == 0:
        S.op("act", lambda e: e.activation(out=out_ap, in_=in_ap, func=AF.Copy), reads=reads, writes=writes)
    else:
        S.op("dve", lambda e: e.tensor_copy(out=out_ap, in_=in_ap), reads=reads, writes=writes)


def _rmsnorm_rows(S, xt, g, n, jk, st):
    S.op("act", lambda e: e.activation(out=jk[:, 0:n], in_=xt[:, 0:n], func=AF.Square, accum_out=st[:]),
         reads=[xt], writes=[jk, st])
    S.op("act", lambda e: e.activation(out=st[:], in_=st[:], func=AF.Sqrt, scale=1.0 / n, bias=EPS),
         reads=[st], writes=[st])
    S.op("dve", lambda e: e.reciprocal(out=st[:], in_=st[:]), reads=[st], writes=[st])
    S.op("dve", lambda e: e.scalar_tensor_tensor(out=xt[:, 0:n], in0=xt[:, 0:n], scalar=st[:, 0:1], in1=g[:, 0:n],
                                                 op0=ALU.mult, op1=ALU.mult),
         reads=[xt, st, g], writes=[xt])


def _attn(S, A, qparts, kparts, v_ap, ktl, q0, QB, scale, bias_fn, out_dst):
    psO, psD = A["psO"][A["n"] % 2], A["psD"][A["n"] % 2]
    A["n"] += 1
    n = len(ktl)
    for i, kt in enumerate(ktl):
        ps = A["psS"][A["ns"] % 2]
        pT = A["pT"][A["ns"] % 3]
        A["ns"] += 1
        for pi in range(len(qparts)):
            kb, kf = kparts[pi]
            qb_, qf = qparts[pi]
            S.op("pe", lambda e: e.matmul(ps[:, 0:QB], lhsT=kf(kt), rhs=qf(q0, QB), start=(pi == 0),
                                          stop=(pi == len(qparts) - 1)), reads=[kb, qb_], writes=[ps])
        bb = bias_fn(kt) if bias_fn is not None else None
        if bb is not None:
            tf = A["tf"][A["ns"] % 2]
            S.op("dve", lambda e: e.scalar_tensor_tensor(out=tf[:, 0:QB], in0=ps[:, 0:QB], scalar=float(scale),
                                                         in1=bb[:, 0:QB], op0=ALU.mult, op1=ALU.add),
                 reads=[ps, bb], writes=[tf])
            S.op("act", lambda e: e.activation(out=pT[:, 0:QB], in_=tf[:, 0:QB], func=AF.Exp), reads=[tf], writes=[pT])
        else:
            S.op("act", lambda e: e.activation(out=pT[:, 0:QB], in_=ps[:, 0:QB], func=AF.Exp, scale=float(scale)),
                 reads=[ps], writes=[pT])
        vb, va = v_ap(kt)
        S.op("pe", lambda e: e.matmul(psO[:, 0:QB], lhsT=va, rhs=pT[:, 0:QB], start=(i == 0), stop=(i == n - 1)),
             reads=[vb, pT], writes=[psO])
        S.op("pe", lambda e: e.matmul(psD[:, 0:QB], lhsT=A["ones"][:, :], rhs=pT[:, 0:QB], start=(i == 0),
                                      stop=(i == n - 1)), reads=[A["ones"], pT], writes=[psD])
    rd = A["rd"]
    ob = A["ob"][A["n"] % 2]
    S.op("dve", lambda e: e.reciprocal(out=rd[:, 0:QB], in_=psD[:, 0:QB]), reads=[psD], writes=[rd])
    S.op("dve", lambda e: e.tensor_tensor(out=ob[:, 0:QB], in0=psO[:, 0:QB], in1=rd[:, 0:QB], op=ALU.mult),
         reads=[psO, rd], writes=[ob])
    dbuf, dap = out_dst
    S.dma("sp", lambda e: e.dma_start(out=dap, in_=ob[:, 0:QB]), reads=[ob], writes=[])


def _attn_res(S):
    A = {"n": 0, "ns": 0}
    A["psS"] = [S.psum("psS%d" % i, [128, 512], F32) for i in range(2)]
    A["psO"] = [S.psum("psO%d" % i, [128, 512], F32) for i in range(2)]
    A["psD"] = [S.psum("psD%d" % i, [128, 512], F32) for i in range(2)]
    A["pT"] = [S.sbuf("pT%d" % i, [128, 512], BF16) for i in range(3)]
    A["tf"] = [S.sbuf("tf%d" % i, [128, 512], F32) for i in range(2)]
    A["rd"] = S.sbuf("rd", [128, 512], F32)
    A["ob"] = [S.sbuf("ob%d" % i, [128, 512], BF16) for i in range(2)]
    ones = S.sbuf("onesb", [128, 128], BF16)
    S.op("dve", lambda e: e.memset(ones[:], 1.0), writes=[ones])
    A["ones"] = ones
    return A


def _transpose_rows(S, src, ncol, dst, dst_fn, ident, ptr, ev0=0):
    nch = ncol // 128
    for c0 in range(0, nch, 8):
        pt = ptr[(ev0 + c0 // 8) % 2]
        nn = min(8, nch - c0)
        for j in range(nn):
            S.op("pe", lambda e: e.transpose(pt[:, j, :], src[:, (c0 + j) * 128:(c0 + j + 1) * 128], ident[:]),
                 reads=[src, ident], writes=[pt])
        for j in range(nn):
            _evac(S, j, dst_fn(c0 + j), pt[:, j, :], reads=[pt], writes=[dst])


def stage_mla(S, l, R):
    ident = R["ident"]
    P_T, P_F, brT = R["P_T"], R["P_F"], R["brT"]
    with ExitStack() as sc:
        S.scope = sc
        A = _attn_res(S)
        psP = [S.psum("psP%d" % i, [128, 512], F32) for i in range(2)]
        wqb = S.sbuf("wqb", [128, 4, 2048], BF16)
        wkvb = S.sbuf("wkvb", [128, 2, 2048], BF16)
        S.dma("pool", lambda e: e.dma_start(out=wqb[:], in_=R["w_qb"][l].rearrange("(k p) c -> p k c", p=128)),
              reads=[R["w_qb"]], writes=[wqb])
        S.dma("pool", lambda e: e.dma_start(out=wkvb[:], in_=R["w_kvb"][l].rearrange("(k p) c -> p k c", p=128)),
              reads=[R["w_kvb"]], writes=[wkvb])
        gq = S.sbuf("gq", [128, 512], F32)
        gk = S.sbuf("gk", [128, 256], F32)
        S.dma("sp", lambda e: e.dma_start(out=gq[:], in_=R["mla_q_norm"][l, :].partition_broadcast(128)),
              reads=[R["mla_q_norm"]], writes=[gq])
        S.dma("sp", lambda e: e.dma_start(out=gk[:], in_=R["mla_kv_norm"][l, :].partition_broadcast(128)),
              reads=[R["mla_kv_norm"]], writes=[gk])
        ropc = S.sbuf("ropc", [64, LL], F32)
        rops = S.sbuf("rops", [64, LL], F32)
        S.dma("sp", lambda e: e.dma_start(out=ropc[:], in_=R["ropeT"][0]), reads=[R["ropeT"]], writes=[ropc])
        S.dma("sp", lambda e: e.dma_start(out=rops[:], in_=R["ropeT"][1]), reads=[R["ropeT"]], writes=[rops])
        cqT = S.sbuf("cqT", [128, 4, LL], BF16)
        ckvT = S.sbuf("ckvT", [128, 2, LL + PAST], BF16)
        krT = S.sbuf("krT", [128, LL + PAST], BF16)
        vall = S.sbuf("vall", [128, (LL + PAST) // 128, 1024], BF16)
        knT = S.sbuf("knT", [128, LL + PAST], BF16)
        qnT = S.sbuf("qnT", [128, LL], BF16)
        qrT = S.sbuf("qrT", [128, LL], BF16)
        S.op("dve", lambda e: e.memset(krT[64:128, :], 0.0), writes=[krT])
        S.op("dve", lambda e: e.memset(qrT[64:128, :], 0.0), writes=[qrT])
        xt = [S.sbuf("mx%d" % i, [128, 512], F32) for i in range(2)]
        xb = [S.sbuf("mxb%d" % i, [128, 512], BF16) for i in range(2)]
        jk = S.sbuf("mjk", [128, 512], F32)
        st = [S.sbuf("mst%d" % i, [128, 1], F32) for i in range(2)]
        r1 = S.sbuf("mr1", [64, 512], F32)
        r2 = S.sbuf("mr2", [64, 512], F32)
        ptr = [S.psum("mptr%d" % i, [128, 8, 128], BF16) for i in range(0)]
        aq, _ = T_COLS["cq"]
        ak, _ = T_COLS["ckv"]
        akr, _ = T_COLS["kr"]
        fkr, _ = F_ROWS["krT"]
        fks, _ = F_ROWS["krswT"]

        def tr_bf(src, ncol, dst, dst_fn, ev):
            nch = ncol // 128
            pt = psP[ev % 2]
            ptv = pt[:, :].bitcast(BF16)
            for j in range(nch):
                S.op("pe", lambda e: e.transpose(ptv[:, j * 128:(j + 1) * 128], src[:, j * 128:(j + 1) * 128], ident[:]),
                     reads=[src, ident], writes=[pt])
            for j in range(nch):
                _evac(S, j, dst_fn(j), ptv[:, j * 128:(j + 1) * 128], reads=[pt], writes=[dst])

        seqs = [(s * LC, LC, False) for s in range(NCTX)] + [(NCTX * LC, LL, True)]
        for si, (tok0, L, latent) in enumerate(seqs):
            Lk = L + (PAST if latent else 0)
            nkt = Lk // 128
            QB = 512 if latent else 256
            for t in range(L // 128):
                g0 = tok0 + t * 128
                x1, x1b, s1 = xt[t % 2], xb[t % 2], st[t % 2]
                S.dma("sp", lambda e: e.dma_start(out=x1[:, 0:512], in_=P_T[g0:g0 + 128, aq:aq + 512]),
                      reads=[P_T], writes=[x1])
                _rmsnorm_rows(S, x1, gq, 512, jk, s1)
                S.op("pool", lambda e: e.tensor_copy(out=x1b[:, 0:512], in_=x1[:, 0:512]), reads=[x1], writes=[x1b])
                tr_bf(x1b, 512, cqT, lambda j: cqT[:, j, t * 128:(t + 1) * 128], t)
            for t in range(nkt):
                x1, x1b, s1 = xt[t % 2], xb[t % 2], st[t % 2]
                if t < L // 128:
                    g0 = tok0 + t * 128
                    S.dma("sp", lambda e: e.dma_start(out=x1[:, 0:256], in_=P_T[g0:g0 + 128, ak:ak + 256]),
                          reads=[P_T], writes=[x1])
                    _rmsnorm_rows(S, x1, gk, 256, jk, s1)
                    if not latent:
                        S.dma("sp", lambda e: e.dma_start(out=R["o_ckv"][si, l, t * 128:(t + 1) * 128, :], in_=x1[:, 0:256]),
                              reads=[x1], writes=[R["o_ckv"]])
                else:
                    p0 = (t - L // 128) * 128
                    S.dma("sp", lambda e: e.dma_start(out=x1[:, 0:256], in_=R["cache_ckv"][l, p0:p0 + 128, :]),
                          reads=[R["cache_ckv"]], writes=[x1])
                S.op("pool", lambda e: e.tensor_copy(out=x1b[:, 0:256], in_=x1[:, 0:256]), reads=[x1], writes=[x1b])
                tr_bf(x1b, 256, ckvT, lambda j: ckvT[:, j, t * 128:(t + 1) * 128], t)
                if t >= L // 128:
                    p0 = (t - L // 128) * 128
                    S.dma("sp", lambda e: e.dma_start(out=x1[:, 256:320], in_=R["cache_kr"][l, p0:p0 + 128, :]),
                          reads=[R["cache_kr"]], writes=[x1])
                    S.op("pool", lambda e: e.tensor_copy(out=x1b[:, 256:320], in_=x1[:, 256:320]), reads=[x1], writes=[x1b])
                    pt = psP[(t + 1) % 2]
                    ptv = pt[:, :].bitcast(BF16)
                    S.op("pe", lambda e: e.transpose(ptv[0:64, 0:128], x1b[:, 256:320], ident[:]),
                         reads=[x1b, ident], writes=[pt])
                    _evac(S, t, krT[0:64, t * 128:(t + 1) * 128], ptv[0:64, 0:128], reads=[pt], writes=[krT])
            for g in range(L // QB):
                g0 = tok0 + g * QB
                S.dma("sp", lambda e: e.dma_start(out=r1[:, 0:QB], in_=P_F[fkr:fkr + 64, g0:g0 + QB]), reads=[P_F], writes=[r1])
                if latent:
                    S.dma("sp", lambda e: e.dma_start(out=r2[:, 0:QB], in_=P_F[fks:fks + 64, g0:g0 + QB]),
                          reads=[P_F], writes=[r2])
                    S.op("dve", lambda e: e.tensor_tensor(out=r1[:, 0:QB], in0=r1[:, 0:QB], in1=ropc[:, g * QB:(g + 1) * QB],
                                                          op=ALU.mult), reads=[r1, ropc], writes=[r1])
                    S.op("dve", lambda e: e.tensor_tensor(out=r2[:, 0:QB], in0=r2[:, 0:QB], in1=rops[:, g * QB:(g + 1) * QB],
                                                          op=ALU.mult), reads=[r2, rops], writes=[r2])
                    S.op("dve", lambda e: e.tensor_tensor(out=krT[0:64, g * QB:(g + 1) * QB], in0=r1[:, 0:QB], in1=r2[:, 0:QB],
                                                          op=ALU.add), reads=[r1, r2], writes=[krT])
                else:
                    S.op("dve", lambda e: e.tensor_copy(out=krT[0:64, g * QB:(g + 1) * QB], in_=r1[:, 0:QB]),
                         reads=[r1], writes=[krT])
            ev = 0
            for t in range(nkt):
                for hb in range(2):
                    ps = psP[ev % 2]
                    for kc in range(2):
                        S.op("pe", lambda e: e.matmul(ps[:, :], lhsT=ckvT[:, kc, t * 128:(t + 1) * 128],
                                                      rhs=wkvb[:, kc, 1024 + hb * 512:1024 + (hb + 1) * 512],
                                                      start=(kc == 0), stop=(kc == 1)), reads=[ckvT, wkvb], writes=[ps])
                    _evac(S, ev, vall[:, t, hb * 512:(hb + 1) * 512], ps[:, :], reads=[ps], writes=[vall])
                    ev += 1
            for h in range(8):
                for g in range((Lk + 511) // 512):
                    w = min(512, Lk - g * 512)
                    ps = psP[ev % 2]
                    for kc in range(2):
                        S.op("pe", lambda e: e.matmul(ps[:, 0:w], lhsT=wkvb[:, kc, h * 128:(h + 1) * 128],
                                                      rhs=ckvT[:, kc, g * 512:g * 512 + w], start=(kc == 0), stop=(kc == 1)),
                             reads=[ckvT, wkvb], writes=[ps])
                    _evac(S, ev, knT[:, g * 512:g * 512 + w], ps[:, 0:w], reads=[ps], writes=[knT])
                    ev += 1
                for g in range(L // QB):
                    ps = psP[ev % 2]
                    for kc in range(4):
                        S.op("pe", lambda e: e.matmul(ps[:, 0:QB], lhsT=wqb[:, kc, h * 128:(h + 1) * 128],
                                                      rhs=cqT[:, kc, g * QB:(g + 1) * QB], start=(kc == 0), stop=(kc == 3)),
                             reads=[cqT, wqb], writes=[ps])
                    _evac(S, ev, qnT[:, g * QB:(g + 1) * QB], ps[:, 0:QB], reads=[ps], writes=[qnT])
                    ev += 1
                    ps = psP[ev % 2]
                    for kc in range(4):
                        S.op("pe", lambda e: e.matmul(ps[0:64, 0:QB], lhsT=wqb[:, kc, 1024 + h * 64:1024 + (h + 1) * 64],
                                                      rhs=cqT[:, kc, g * QB:(g + 1) * QB], start=(kc == 0), stop=(kc == 3)),
                             reads=[cqT, wqb], writes=[ps])
                    if latent:
                        ps2 = psP[(ev + 1) % 2]
                        for kc in range(4):
                            S.op("pe", lambda e: e.matmul(ps2[0:64, 0:QB], lhsT=wqb[:, kc, 1536 + h * 64:1536 + (h + 1) * 64],
                                                          rhs=cqT[:, kc, g * QB:(g + 1) * QB], start=(kc == 0), stop=(kc == 3)),
                                 reads=[cqT, wqb], writes=[ps2])
                        S.op("dve", lambda e: e.tensor_tensor(out=r1[:, 0:QB], in0=ps[0:64, 0:QB],
                                                              in1=ropc[:, g * QB:(g + 1) * QB], op=ALU.mult),
                             reads=[ps, ropc], writes=[r1])
                        S.op("dve", lambda e: e.tensor_tensor(out=r2[:, 0:QB], in0=ps2[0:64, 0:QB],
                                                              in1=rops[:, g * QB:(g + 1) * QB], op=ALU.mult),
                             reads=[ps2, rops], writes=[r2])
                        S.op("dve", lambda e: e.tensor_tensor(out=qrT[0:64, g * QB:(g + 1) * QB], in0=r1[:, 0:QB],
                                                              in1=r2[:, 0:QB], op=ALU.add), reads=[r1, r2], writes=[qrT])
                        ev += 2
                    else:
                        _evac(S, ev, qrT[0:64, g * QB:(g + 1) * QB], ps[0:64, 0:QB], reads=[ps], writes=[qrT])
                        ev += 1
                for g in range(L // QB):
                    _attn(S, A,
                          qparts=[(qnT, lambda q0, n: qnT[:, q0:q0 + n]), (qrT, lambda q0, n: qrT[:, q0:q0 + n])],
                          kparts=[(knT, lambda kt: knT[:, kt * 128:(kt + 1) * 128]),
                                  (krT, lambda kt: krT[:, kt * 128:(kt + 1) * 128])],
                          v_ap=lambda kt: (vall, vall[:, kt, h * 128:(h + 1) * 128]),
                          ktl=list(range(nkt)), q0=g * QB, QB=QB, scale=192 ** -0.5, bias_fn=None,
                          out_dst=(brT[0], brT[0][h * 128:(h + 1) * 128, tok0 + g * QB:tok0 + (g + 1) * QB]))
        S.barrier()
    S.scope = S.es


NA_QB_TILES = {0: list(range(0, 6)), 1: list(range(2, 10)), 2: list(range(6, 14)), 3: list(range(10, 16))}
NA_MASK_IDX = {}
_i = 0
for _qb in range(4):
    for _kt in NA_QB_TILES[_qb]:
        NA_MASK_IDX[(_qb, _kt)] = _i
        _i += 1
NA_NMASK = _i


def stage_na(S, l, R):
    ident = R["ident"]
    P_T, P_F, brT = R["P_T"], R["P_F"], R["brT"]
    with ExitStack() as sc:
        S.scope = sc
        A = _attn_res(S)
        psP = [S.psum("psP%d" % i, [128, 512], F32) for i in range(2)]
        qT = S.sbuf("naqT", [128, LL], BF16)
        kT = S.sbuf("nakT", [128, LL + PAST], BF16)
        vall = S.sbuf("navall", [128, (LL + PAST) // 128, 1024], BF16)
        kc_f = [S.sbuf("nakc%d" % i, [128, 1024], F32) for i in range(2)]
        kc_b = S.sbuf("nakcb", [128, 4, 1024], BF16)
        bias = [S.sbuf("nabias%d" % i, [128, 512], F32) for i in range(2)]
        mask = [S.sbuf("namask%d" % i, [128, 512], F32) for i in range(2)]
        fq, _ = F_ROWS["naqT"]
        fk, _ = F_ROWS["nakT"]
        av, _ = T_COLS["nav"]
        seqs = [(s * LC, LC, False) for s in range(NCTX)] + [(NCTX * LC, LL, True)]
        nb = 0
        for si, (tok0, L, latent) in enumerate(seqs):
            Lk = L + (PAST if latent else 0)
            nkt = Lk // 128
            QB = 512 if latent else 256
            for t in range(L // 128):
                g0 = tok0 + t * 128
                S.dma("pool", lambda e: e.dma_start(out=vall[:, t, :], in_=P_T[g0:g0 + 128, av:av + 1024]),
                      reads=[P_T], writes=[vall])
            if latent:
                for t in range(4):
                    S.dma("pool", lambda e: e.dma_start(out=vall[:, L // 128 + t, :],
                                                        in_=R["cache_nav"][l, t * 128:(t + 1) * 128, :]),
                          reads=[R["cache_nav"]], writes=[vall])
                    S.dma("pool", lambda e: e.dma_start(out=kc_b[:, t, :], in_=R["cache_nak"][l, t * 128:(t + 1) * 128, :]),
                          reads=[R["cache_nak"]], writes=[kc_b])
            for h in range(8):
                S.dma("pool", lambda e: e.dma_start(out=qT[:, 0:L], in_=P_F[fq + h * 128:fq + (h + 1) * 128, tok0:tok0 + L]),
                      reads=[P_F], writes=[qT])
                S.dma("pool", lambda e: e.dma_start(out=kT[:, 0:L], in_=P_F[fk + h * 128:fk + (h + 1) * 128, tok0:tok0 + L]),
                      reads=[P_F], writes=[kT])
                if latent:
                    pt = psP[h % 2]
                    ptv = pt[:, :].bitcast(BF16)
                    for t in range(4):
                        S.op("pe", lambda e: e.transpose(ptv[:, t * 128:(t + 1) * 128], kc_b[:, t, h * 128:(h + 1) * 128],
                                                         ident[:]), reads=[kc_b, ident], writes=[pt])
                    _evac(S, h, kT[:, L:L + 512], ptv[:, 0:512], reads=[pt], writes=[kT])
                for g in range(L // QB):
                    if latent:
                        ktl = NA_QB_TILES[g] + [16, 17, 18, 19]

                        def bias_fn(kt, g=g, h=h):
                            if kt >= 16:
                                return None
                            bb, mm = bias[bias_fn.n % 2], mask[bias_fn.n % 2]
                            bias_fn.n += 1
                            for rr in range(2):
                                rp = 2 * kt + rr
                                mm0 = 15 - rp + 8 * g
                                S.dma("sp", lambda e: e.dma_start(
                                    out=bb[rr * 64:(rr + 1) * 64, :],
                                    in_=R["na_ctab"][l, h, :, mm0:mm0 + 8, :].rearrange("c m q -> c (m q)")),
                                    reads=[R["na_ctab"]], writes=[bb])
                            mi = NA_MASK_IDX[(g, kt)]
                            S.dma("sp", lambda e: e.dma_start(out=mm[:], in_=R["na_mask"][mi]), reads=[R["na_mask"]], writes=[mm])
                            S.op("pool", lambda e: e.tensor_tensor(out=bb[:], in0=bb[:], in1=mm[:], op=ALU.add),
                                 reads=[bb, mm], writes=[bb])
                            return bb
                        bias_fn.n = nb
                    else:
                        ktl = list(range(nkt))
                        bias_fn = None
                    _attn(S, A,
                          qparts=[(qT, lambda q0, n: qT[:, q0:q0 + n])],
                          kparts=[(kT, lambda kt: kT[:, kt * 128:(kt + 1) * 128])],
                          v_ap=lambda kt: (vall, vall[:, kt, h * 128:(h + 1) * 128]),
                          ktl=ktl, q0=g * QB, QB=QB, scale=128 ** -0.5, bias_fn=bias_fn,
                          out_dst=(brT[3], brT[3][h * 128:(h + 1) * 128, tok0 + g * QB:tok0 + (g + 1) * QB]))
                    if latent:
                        nb = bias_fn.n
        S.barrier()
    S.scope = S.es


def stage_merge(S, l, R, x_src):
    ident = R["ident"]
    P_T, brT, mrg, xbuf, modd = R["P_T"], R["brT"], R["mrg"], R["xbuf"], R["modd"]
    ag, _ = T_COLS["gate"]
    for b in range(4):
        with ExitStack() as sc:
            S.scope = sc
            wbr = S.sbuf("wbr", [128, 8, 2048], BF16)
            S.dma("pool", lambda e: e.dma_start(out=wbr[:], in_=R["w_branch"][l, b].rearrange("(k p) c -> p k c", p=128)),
                  reads=[R["w_branch"]], writes=[wbr])
            brt = [S.sbuf("brt%d" % i, [128, 8, 128], BF16) for i in range(2)]
            gt = [S.sbuf("gt%d" % i, [128, 2048], F32) for i in range(2)]
            acc = [S.sbuf("acc%d" % i, [128, 2048], F32) for i in range(2)]
            pm = [S.psum("pm%d" % i, [128, 512], F32) for i in range(4)]
            ev = 0
            for t in range(NTILE):
                bt, g, a = brt[t % 2], gt[t % 2], acc[t % 2]
                S.dma("sp", lambda e: e.dma_start(out=bt[:], in_=brT[b][:, t * 128:(t + 1) * 128].rearrange("(k p) t -> p k t", p=128)),
                      reads=[brT[b]], writes=[bt])
                S.dma("sp", lambda e: e.dma_start(out=g[:], in_=P_T[t * 128:(t + 1) * 128, ag + b * 2048:ag + (b + 1) * 2048]),
                      reads=[P_T], writes=[g])
                if b > 0:
                    S.dma("sp", lambda e: e.dma_start(out=a[:], in_=mrg[t * 128:(t + 1) * 128, :]), reads=[R["mrgt"][t]], writes=[a])
                S.op("act", lambda e: e.activation(out=g[:], in_=g[:], func=AF.Sigmoid), reads=[g], writes=[g])
                for nb in range(4):
                    ps = pm[ev % 4]
                    ev += 1
                    for kc in range(8):
                        S.op("pe", lambda e: e.matmul(ps[:, :], lhsT=bt[:, kc, :], rhs=wbr[:, kc, nb * 512:(nb + 1) * 512],
                                                      start=(kc == 0), stop=(kc == 7)), reads=[bt, wbr], writes=[ps])
                    S.op("dve", lambda e: e.tensor_tensor(out=g[:, nb * 512:(nb + 1) * 512], in0=ps[:, :],
                                                          in1=g[:, nb * 512:(nb + 1) * 512], op=ALU.mult),
                         reads=[ps, g], writes=[g])
                if b > 0:
                    S.op("pool", lambda e: e.tensor_tensor(out=g[:], in0=g[:], in1=a[:], op=ALU.add), reads=[g, a], writes=[g])
                S.dma("sp", lambda e: e.dma_start(out=mrg[t * 128:(t + 1) * 128, :], in_=g[:]), reads=[g], writes=[R["mrgt"][t]])
            S.barrier()
    with ExitStack() as sc:
        S.scope = sc
        wout = S.sbuf("wout", [128, 16, 2048], BF16)
        S.dma("pool", lambda e: e.dma_start(out=wout[:], in_=R["w_out"][l].rearrange("(k p) c -> p k c", p=128)),
              reads=[R["w_out"]], writes=[wout])
        g1b = [S.sbuf("g1b%d" % c, [128, 2048], F32) for c in range(2)]
        for c in range(2):
            S.dma("sp", lambda e: e.dma_start(out=g1b[c][:], in_=modd[c, 2 * D:3 * D].partition_broadcast(128)),
                  reads=[modd], writes=[g1b[c]])
        mt = [S.sbuf("mt%d" % i, [128, 2048], F32) for i in range(2)]
        mb = [S.sbuf("mb%d" % i, [128, 2048], BF16) for i in range(2)]
        mT = [S.sbuf("mT%d" % i, [128, 16, 128], BF16) for i in range(2)]
        xt = [S.sbuf("xo%d" % i, [128, 2048], F32) for i in range(2)]
        ptr = [S.psum("optr%d" % i, [128, 8, 128], BF16) for i in range(2)]
        pm = [S.psum("pmo%d" % i, [128, 512], F32) for i in range(4)]
        ev = 0
        for t in range(NTILE):
            c = 0 if t < (NCTX * LC) // 128 else 1
            m, mbb, mTT, x = mt[t % 2], mb[t % 2], mT[t % 2], xt[t % 2]
            S.dma("sp", lambda e: e.dma_start(out=m[:], in_=mrg[t * 128:(t + 1) * 128, :]), reads=[R["mrgt"][t]], writes=[m])
            S.dma("sp", lambda e: e.dma_start(out=x[:], in_=x_src[t * 128:(t + 1) * 128, :]), reads=[R["xbt"][t]], writes=[x])
            S.op("pool", lambda e: e.tensor_copy(out=mbb[:], in_=m[:]), reads=[m], writes=[mbb])
            for half in range(2):
                pt = ptr[half]
                for j in range(8):
                    kc = half * 8 + j
                    S.op("pe", lambda e: e.transpose(pt[:, j, :], mbb[:, kc * 128:(kc + 1) * 128], ident[:]),
                         reads=[mbb, ident], writes=[pt])
                _evac(S, half, mTT[:, half * 8:(half + 1) * 8, :], pt[:, :, :], reads=[pt], writes=[mTT])
            for nb in range(4):
                ps = pm[ev % 4]
                ev += 1
                for kc in range(16):
                    S.op("pe", lambda e: e.matmul(ps[:, :], lhsT=mTT[:, kc, :], rhs=wout[:, kc, nb * 512:(nb + 1) * 512],
                                                  start=(kc == 0), stop=(kc == 15)), reads=[mTT, wout], writes=[ps])
                S.op("dve", lambda e: e.tensor_tensor(out=m[:, nb * 512:(nb + 1) * 512], in0=ps[:, :],
                                                      in1=g1b[c][:, nb * 512:(nb + 1) * 512], op=ALU.mult),
                     reads=[ps, g1b[c]], writes=[m])
            S.op("pool", lambda e: e.tensor_tensor(out=x[:], in0=x[:], in1=m[:], op=ALU.add), reads=[x, m], writes=[x])
            S.dma("sp", lambda e: e.dma_start(out=xbuf[t * 128:(t + 1) * 128, :], in_=x[:]), reads=[x], writes=[R["xbt"][t]])
        S.barrier()
    S.scope = S.es


def stage_peer(S, l, R, last):
    ident = R["ident"]
    xbuf, modd = R["xbuf"], R["modd"]
    with ExitStack() as sc:
        S.scope = sc
        wq = S.sbuf("pwq", [128, 16, 2048], BF16)
        S.dma("pool", lambda e: e.dma_start(out=wq[:], in_=R["peer_wq"][l].rearrange("(k p) c -> p k c", p=128)),
              reads=[R["peer_wq"]], writes=[wq])
        identf = S.sbuf("identf", [128, 128], F32)
        S.dma("sp", lambda e: e.dma_start(out=identf[:], in_=R["ident_in"][:, :]), reads=[R["ident_in"]], writes=[identf])
        keysT = S.sbuf("keysT", [128, 2, 128], F32)
        S.dma("sp", lambda e: e.dma_start(out=keysT[:], in_=R["peer_keysT"][l].rearrange("s c n -> c s n")),
              reads=[R["peer_keysT"]], writes=[keysT])
        gm2 = S.sbuf("gm2", [128, 2048], F32)
        sh2 = S.sbuf("sh2", [128, 2048], F32)
        x1 = S.sbuf("px1", [128, 2048], F32)
        h2 = S.sbuf("ph2", [128, 2048], F32)
        h2b = S.sbuf("ph2b", [128, 2048], BF16)
        h2T = S.sbuf("ph2T", [128, 16, 128], BF16)
        qTf = S.sbuf("pqTf", [128, 16, 128], F32)
        scr = S.sbuf("pscr", [128, 16, 128], F32)
        scw = S.sbuf("pscw", [128, 128], F32)
        vals = S.sbuf("pvals", [128, 16, 16], F32)
        idx = S.sbuf("pidx", [128, 16, 16], U32)
        idxf = S.sbuf("pidxf", [128, 16, 16], F32)
        cand = S.sbuf("pcand", [128, 256], F32)
        candw = S.sbuf("pcandw", [128, 256], F32)
        cid = S.sbuf("pcid", [128, 256], F32)
        bs = S.sbuf("pbs", [128, 8, 16], F32)
        bj = S.sbuf("pbj", [128, 16], U32)
        bjf = S.sbuf("pbjf", [128, 2, 16], F32)
        bja = S.sbuf("pbja", [128, 16], U32)
        bjb = S.sbuf("pbjb", [128, 16], U32)
        eq16 = S.sbuf("peq16", [128, 16, 16], F32)
        sel = S.sbuf("psel", [128, 2, 16], F32)
        iota = S.sbuf("piota", [128, 256], F32)
        S.dma("sp", lambda e: e.dma_start(out=iota[:], in_=R["iota256"][0, :].partition_broadcast(128)),
              reads=[R["iota256"]], writes=[iota])
        eq = S.sbuf("peq", [128, 8, 256], F32)
        eidf = S.sbuf("peidf", [128, 8, 16], F32)
        eidi = S.sbuf("peidi", [128, 128], I32)
        negb = S.sbuf("pnegb", [128, 8], F32)
        zs = S.sbuf("pzs", [128, 8], F32)
        gat = S.sbuf("pgat", [128, 8, 16], F32)
        actv = S.sbuf("pact", [128, 128], F32)
        coef = S.sbuf("pcoef", [128, 128], F32)
        yacc = S.sbuf("pyacc", [128, 2048], F32)
        qf = yacc
        jk = S.sbuf("pjk", [128, 2048], F32)
        st = S.sbuf("pst", [128, 1], F32)
        ug = [S.sbuf("pug%d" % i, [128, 2048], BF16) for i in range(8)]
        dgs = [S.sbuf("pdg%d" % i, [128, 128], BF16) for i in range(4)]
        ptr = [S.psum("pptr%d" % i, [128, 8, 128], BF16) for i in range(2)]
        pm = [S.psum("ppm%d" % i, [128, 512], F32) for i in range(4)]
        cur_c = -1
        if last:
            ptok = S.sbuf("pptok", [128, NT_LAST], I32)
            S.dma("sp", lambda e: e.dma_start(out=ptok[:], in_=R["ptok"][:, :]), reads=[R["ptok"]], writes=[ptok])
        for t in range(NT_LAST if last else (NTILE_PEER if NTILE_PEER else NTILE)):
            c = 0 if t < (NCTX * LC) // 128 else 1
            if c != cur_c:
                cur_c = c
                S.dma("sp", lambda e: e.dma_start(out=sh2[:], in_=modd[c, 3 * D:4 * D].partition_broadcast(128)),
                      reads=[modd], writes=[sh2])
                S.dma("sp", lambda e: e.dma_start(out=gm2[:], in_=modd[c, 4 * D:5 * D].partition_broadcast(128)),
                      reads=[modd], writes=[gm2])
                S.dma("sp", lambda e: e.dma_start(out=jk[:], in_=R["norm2_g"][l, :].partition_broadcast(128)),
                      reads=[R["norm2_g"]], writes=[jk])
                S.op("dve", lambda e: e.scalar_tensor_tensor(out=gm2[:], in0=gm2[:], scalar=1.0, in1=jk[:],
                                                             op0=ALU.add, op1=ALU.mult), reads=[gm2, jk], writes=[gm2])
            if last:
                S.dma("pool", lambda e: e.indirect_dma_start(
                    out=x1[:], out_offset=None, in_=xbuf[:, :],
                    in_offset=bass.IndirectOffsetOnAxis(ap=ptok[:, t:t + 1], axis=0)),
                    reads=[xbuf, ptok] + R["xbt"], writes=[x1])
            else:
                S.dma("sp", lambda e: e.dma_start(out=x1[:], in_=xbuf[t * 128:(t + 1) * 128, :]), reads=[R["xbt"][t]], writes=[x1])
            S.op("act", lambda e: e.activation(out=jk[:], in_=x1[:], func=AF.Square, accum_out=st[:]),
                 reads=[x1], writes=[jk, st])
            S.op("act", lambda e: e.activation(out=st[:], in_=st[:], func=AF.Sqrt, scale=1.0 / D, bias=EPS),
                 reads=[st], writes=[st])
            S.op("dve", lambda e: e.reciprocal(out=st[:], in_=st[:]), reads=[st], writes=[st])
            S.op("dve", lambda e: e.scalar_tensor_tensor(out=h2[:], in0=x1[:], scalar=st[:, 0:1], in1=gm2[:],
                                                         op0=ALU.mult, op1=ALU.mult), reads=[x1, st, gm2], writes=[h2])
            S.op("dve", lambda e: e.tensor_tensor(out=h2[:], in0=h2[:], in1=sh2[:], op=ALU.add), reads=[h2, sh2], writes=[h2])
            S.op("act", lambda e: e.activation(out=h2b[:], in_=h2[:], func=AF.Copy), reads=[h2], writes=[h2b])
            for half in range(2):
                pt = ptr[half]
                for j in range(8):
                    kc = half * 8 + j
                    S.op("pe", lambda e: e.transpose(pt[:, j, :], h2b[:, kc * 128:(kc + 1) * 128], ident[:]),
                         reads=[h2b, ident], writes=[pt])
                _evac(S, half, h2T[:, half * 8:(half + 1) * 8, :], pt[:, :, :], reads=[pt], writes=[h2T])
            for nb in range(4):
                ps = pm[nb]
                for kc in range(16):
                    S.op("pe", lambda e: e.matmul(ps[:, :], lhsT=h2T[:, kc, :], rhs=wq[:, kc, nb * 512:(nb + 1) * 512],
                                                  start=(kc == 0), stop=(kc == 15)), reads=[h2T, wq], writes=[ps])
                _evac(S, nb, qf[:, nb * 512:(nb + 1) * 512], ps[:, :], reads=[ps], writes=[qf])
            for b4 in range(4):
                ps = pm[b4]
                for j in range(4):
                    ch = b4 * 4 + j
                    S.op("pe", lambda e: e.transpose(ps[:, j * 128:(j + 1) * 128], qf[:, ch * 128:(ch + 1) * 128], identf[:]),
                         reads=[qf, identf], writes=[ps])
                _evac(S, b4, qTf[:, b4 * 4:(b4 + 1) * 4, :], ps[:, :].rearrange("p (a b) -> p a b", a=4), reads=[ps], writes=[qTf])
            for b4 in range(4):
                ps = pm[b4]
                for j in range(4):
                    ch = b4 * 4 + j
                    S.op("pe", lambda e: e.matmul(ps[:, j * 128:(j + 1) * 128], lhsT=qTf[:, ch, :], rhs=keysT[:, ch % 2, :],
                                                  start=True, stop=True), reads=[qTf, keysT], writes=[ps])
                _evac(S, b4 + 1, scr[:, b4 * 4:(b4 + 1) * 4, :], ps[:, :].rearrange("p (a b) -> p a b", a=4), reads=[ps], writes=[scr])
            for ch in range(16):
                S.op("dve", lambda e: e.max(out=vals[:, ch, 0:8], in_=scr[:, ch, :]), reads=[scr], writes=[vals])
                S.op("dve", lambda e: e.match_replace(out=scw[:, :], in_to_replace=vals[:, ch, 0:8], in_values=scr[:, ch, :],
                                                      imm_value=-1e30), reads=[vals, scr], writes=[scw])
                S.op("dve", lambda e: e.max(out=vals[:, ch, 8:16], in_=scw[:, :]), reads=[scw], writes=[vals])
                S.op("dve", lambda e: e.max_index(out=idx[:, ch, 0:8], in_max=vals[:, ch, 0:8], in_values=scr[:, ch, :]),
                     reads=[vals, scr], writes=[idx])
                S.op("dve", lambda e: e.max_index(out=idx[:, ch, 8:16], in_max=vals[:, ch, 8:16], in_values=scw[:, :]),
                     reads=[vals, scw], writes=[idx])
            S.op("dve", lambda e: e.tensor_copy(out=idxf[:], in_=idx[:]), reads=[idx], writes=[idxf])
            for h in range(8):
                c3 = cand[:, :].rearrange("p (a b) -> p a b", a=16)
                i3 = cid[:, :].rearrange("p (a b) -> p a b", a=16)
                S.op("dve", lambda e: e.tensor_tensor(out=c3, in0=vals[:, 2 * h, :].unsqueeze(2).to_broadcast([128, 16, 16]),
                                                      in1=vals[:, 2 * h + 1, :].unsqueeze(1).to_broadcast([128, 16, 16]),
                                                      op=ALU.add), reads=[vals], writes=[cand])
                S.op("dve", lambda e: e.scalar_tensor_tensor(out=i3, in0=idxf[:, 2 * h, :].unsqueeze(2).to_broadcast([128, 16, 16]),
                                                             scalar=128.0,
                                                             in1=idxf[:, 2 * h + 1, :].unsqueeze(1).to_broadcast([128, 16, 16]),
                                                             op0=ALU.mult, op1=ALU.add), reads=[idxf], writes=[cid])
                S.op("dve", lambda e: e.max(out=bs[:, h, 0:8], in_=cand[:, :]), reads=[cand], writes=[bs])
                S.op("dve", lambda e: e.match_replace(out=candw[:, :], in_to_replace=bs[:, h, 0:8], in_values=cand[:, :],
                                                      imm_value=-1e30), reads=[bs, cand], writes=[candw])
                S.op("dve", lambda e: e.max(out=bs[:, h, 8:16], in_=candw[:, :]), reads=[candw], writes=[bs])
                S.op("dve", lambda e: e.max_index(out=bj[:, 0:8], in_max=bs[:, h, 0:8], in_values=cand[:, :]),
                     reads=[bs, cand], writes=[bj])
                S.op("dve", lambda e: e.max_index(out=bj[:, 8:16], in_max=bs[:, h, 8:16], in_values=candw[:, :]),
                     reads=[bs, candw], writes=[bj])
                S.op("dve", lambda e: e.tensor_single_scalar(out=bja[:], in_=bj[:], scalar=4, op=ALU.logical_shift_right),
                     reads=[bj], writes=[bja])
                S.op("dve", lambda e: e.tensor_single_scalar(out=bjb[:], in_=bj[:], scalar=15, op=ALU.bitwise_and),
                     reads=[bj], writes=[bjb])
                S.op("dve", lambda e: e.tensor_copy(out=bjf[:, 0, :], in_=bja[:]), reads=[bja], writes=[bjf])
                S.op("dve", lambda e: e.tensor_copy(out=bjf[:, 1, :], in_=bjb[:]), reads=[bjb], writes=[bjf])
                for ab in range(2):
                    S.op("dve", lambda e: e.tensor_tensor(out=eq16[:], in0=iota[:, 0:16].unsqueeze(1).to_broadcast([128, 16, 16]),
                                                          in1=bjf[:, ab, :].unsqueeze(2).to_broadcast([128, 16, 16]),
                                                          op=ALU.is_equal), reads=[iota, bjf], writes=[eq16])
                    S.op("dve", lambda e: e.tensor_tensor(out=eq16[:], in0=eq16[:],
                                                          in1=idxf[:, 2 * h + ab, :].unsqueeze(1).to_broadcast([128, 16, 16]),
                                                          op=ALU.mult), reads=[eq16, idxf], writes=[eq16])
                    S.op("dve", lambda e: e.tensor_reduce(out=sel[:, ab, :], in_=eq16[:], axis=AX.X, op=ALU.add),
                         reads=[eq16], writes=[sel])
                S.op("dve", lambda e: e.scalar_tensor_tensor(out=eidf[:, h, :], in0=sel[:, 0, :], scalar=128.0, in1=sel[:, 1, :],
                                                             op0=ALU.mult, op1=ALU.add), reads=[sel], writes=[eidf])
            S.op("dve", lambda e: e.tensor_copy(out=eidi[:, :].rearrange("p (a b) -> p a b", a=8), in_=eidf[:]),
                 reads=[eidf], writes=[eidi])
            S.op("dve", lambda e: e.tensor_scalar(out=negb[:], in0=bs[:, :, 0], scalar1=-1.0, scalar2=None, op0=ALU.mult),
                 reads=[bs], writes=[negb])
            for h in range(8):
                S.op("act", lambda e: e.activation(out=gat[:, h, :], in_=bs[:, h, :], func=AF.Exp, bias=negb[:, h:h + 1],
                                                   accum_out=zs[:, h:h + 1]), reads=[bs, negb], writes=[gat, zs])
            S.op("dve", lambda e: e.reciprocal(out=zs[:], in_=zs[:]), reads=[zs], writes=[zs])
            S.op("dve", lambda e: e.tensor_tensor(out=gat[:], in0=gat[:], in1=zs[:, :].unsqueeze(2).to_broadcast([128, 8, 16]),
                                                  op=ALU.mult), reads=[gat, zs], writes=[gat])
            for s in range(128):
                u = ug[s % 8]
                S.dma("pool", lambda e: e.indirect_dma_start(
                    out=u[:], out_offset=None, in_=R["peer_ub"][l][:, :],
                    in_offset=bass.IndirectOffsetOnAxis(ap=eidi[:, s:s + 1], axis=0)),
                    reads=[R["peer_ub"][l], eidi], writes=[u])
                S.op("dve", lambda e: e.scalar_tensor_tensor(out=jk[:], in0=u[:], scalar=1.0, in1=h2[:],
                                                             op0=ALU.mult, op1=ALU.mult, accum_out=actv[:, s:s + 1]),
                     reads=[u, h2], writes=[jk, actv])
            S.op("act", lambda e: e.activation(out=actv[:], in_=actv[:], func=AF.Gelu), reads=[actv], writes=[actv])
            S.op("dve", lambda e: e.tensor_tensor(out=coef[:], in0=actv[:], in1=gat[:, :, :].rearrange("p a b -> p (a b)"),
                                                  op=ALU.mult), reads=[actv, gat], writes=[coef])
            for s in range(128):
                u = ug[s % 8]
                S.dma("pool", lambda e: e.indirect_dma_start(
                    out=u[:], out_offset=None, in_=R["peer_vb"][l][:, :],
                    in_offset=bass.IndirectOffsetOnAxis(ap=eidi[:, s:s + 1], axis=0)),
                    reads=[R["peer_vb"][l], eidi], writes=[u])
                dg = dgs[s % 4]
                S.op("dve", lambda e: e.tensor_scalar(out=dg[:], in0=ident[:], scalar1=coef[:, s:s + 1], scalar2=None,
                                                      op0=ALU.mult), reads=[ident, coef], writes=[dg])
                for nb in range(4):
                    S.op("pe", lambda e: e.matmul(pm[nb][:, :], lhsT=dg[:, :], rhs=u[:, nb * 512:(nb + 1) * 512],
                                                  start=(s == 0), stop=(s == 127)), reads=[dg, u], writes=[pm[nb]])
            for nb in range(4):
                _evac(S, 0, yacc[:, nb * 512:(nb + 1) * 512], pm[nb][:, :], reads=[pm[nb]], writes=[yacc])
            S.dma("sp", lambda e: e.dma_start(out=jk[:], in_=modd[c, 5 * D:6 * D].partition_broadcast(128)),
                  reads=[modd], writes=[jk])
            S.op("dve", lambda e: e.tensor_tensor(out=yacc[:], in0=yacc[:], in1=jk[:], op=ALU.mult), reads=[yacc, jk], writes=[yacc])
            S.op("dve", lambda e: e.tensor_tensor(out=x1[:], in0=x1[:], in1=yacc[:], op=ALU.add), reads=[x1, yacc], writes=[x1])
            if not last:
                S.dma("sp", lambda e: e.dma_start(out=xbuf[t * 128:(t + 1) * 128, :], in_=x1[:]), reads=[x1], writes=[R["xbt"][t]])
            else:
                S.op("act", lambda e: e.activation(out=jk[:], in_=x1[:], func=AF.Square, accum_out=st[:]),
                     reads=[x1], writes=[jk, st])
                S.op("act", lambda e: e.activation(out=st[:], in_=st[:], func=AF.Sqrt, scale=1.0 / D, bias=EPS),
                     reads=[st], writes=[st])
                S.op("dve", lambda e: e.reciprocal(out=st[:], in_=st[:]), reads=[st], writes=[st])
                S.dma("sp", lambda e: e.dma_start(out=jk[:], in_=R["final_g"][0, :].partition_broadcast(128)),
                      reads=[R["final_g"]], writes=[jk])
                S.op("dve", lambda e: e.scalar_tensor_tensor(out=x1[:], in0=x1[:], scalar=st[:, 0:1], in1=jk[:],
                                                             op0=ALU.mult, op1=ALU.mult), reads=[x1, st, jk], writes=[x1])
                S.dma("sp", lambda e: e.dma_start(out=R["y_out"][t * 128:(t + 1) * 128, :], in_=x1[:]), reads=[x1], writes=[R["y_out"]])
        S.barrier()
    S.scope = S.es


def stage_dn(S, l, R):
    P_T, P_F, brT = R["P_T"], R["P_F"], R["brT"]
    fdn, _ = F_ROWS["dnT"]
    az, _ = T_COLS["z"]
    aa, _ = T_COLS["a"]
    with ExitStack() as sc:
        S.scope = sc
        cst = S.sbuf("dncst", [128, 10, 128], F32)
        S.dma("sp", lambda e: e.dma_start(out=cst[:], in_=R["dn_consts"][:, :, :].rearrange("m p q -> p m q")),
              reads=[R["dn_consts"]], writes=[cst])
        onesf = S.sbuf("dnones", [128, 128], F32)
        S.op("dve", lambda e: e.memset(onesf[:], 1.0), writes=[onesf])
        identf = S.sbuf("dnidentf", [128, 128], F32)
        S.dma("sp", lambda e: e.dma_start(out=identf[:], in_=R["dn_consts"][2]), reads=[R["dn_consts"]], writes=[identf])
        identb = R["ident"]
        cw = S.sbuf("dncw", [128, 24, 5], F32)
        S.dma("sp", lambda e: e.dma_start(out=cw[:], in_=R["dn_convT"][l]), reads=[R["dn_convT"]], writes=[cw])
        alog = S.sbuf("dnalog", [128, 16], F32)
        dtb = S.sbuf("dndtb", [128, 16], F32)
        S.dma("sp", lambda e: e.dma_start(out=alog[:], in_=R["dn_a_log"][l, :].partition_broadcast(128)),
              reads=[R["dn_a_log"]], writes=[alog])
        S.dma("sp", lambda e: e.dma_start(out=dtb[:], in_=R["dn_dt_bias"][l, :].partition_broadcast(128)),
              reads=[R["dn_dt_bias"]], writes=[dtb])
        S.op("act", lambda e: e.activation(out=alog[:], in_=alog[:], func=AF.Exp), reads=[alog], writes=[alog])
        gon = S.sbuf("dngon", [128, 128], F32)
        S.dma("sp", lambda e: e.dma_start(out=gon[:], in_=R["dn_out_norm"][l, :].partition_broadcast(128)),
              reads=[R["dn_out_norm"]], writes=[gon])
        NCH = LL // 128
        xin = S.sbuf("dnxin", [128, LL + 4], F32)
        acc = S.sbuf("dnacc", [128, LL], F32)
        qT = S.sbuf("dnqT", [128, LL], F32)
        kT = S.sbuf("dnkT", [128, LL], F32)
        vT = S.sbuf("dnvT", [128, LL], F32)
        ktok = S.sbuf("dnktok", [128, NCH, 128], F32)
        vtok = S.sbuf("dnvtok", [128, NCH, 128], F32)
        oall = S.sbuf("dnoall", [128, NCH, 1024], F32)
        gt = S.sbuf("dng", [128, NCH, 16], F32)
        bt = S.sbuf("dnb", [128, NCH, 16], F32)
        gc = S.sbuf("dngc", [128, NCH, 16], F32)
        egc = S.sbuf("dnegc", [128, NCH, 16], F32)
        bg = S.sbuf("dnbg", [128, NCH, 16], F32)
        egl = S.sbuf("dnegl", [128, NCH, 16], F32)
        edl = S.sbuf("dnedl", [128, NCH, 16], F32)
        ab = S.sbuf("dnab", [128, 32], F32)
        sq = S.sbuf("dnsq", [128, 512], F32)
        rn = S.sbuf("dnrn", [128, 512], F32)
        Sst = S.sbuf("dnS", [128, 128], F32)
        Bsets = []
        for j_ in range(3):
            B = {"j": j_}
            for nm_ in ("Gm", "EA", "ET", "Am", "Ad", "Aoff", "Boff", "qkT", "wT", "kd"):
                B[nm_] = S.sbuf("dn%s%d" % (nm_, j_), [128, 128], F32)
            B["Bk"] = [S.sbuf("dnB%d_%d" % (i, j_), [128, 128], F32) for i in range(6)]
            B["Ck"] = [S.sbuf("dnC%d_%d" % (i, j_), [128, 128], F32) for i in range(2)]
            B["X"] = S.sbuf("dnX%d" % j_, [128, 256], F32)
            B["Zb"] = S.sbuf("dnZb%d" % j_, [128, 256], F32)
            Bsets.append(B)
        vnew = S.sbuf("dnvnew", [128, 128], F32)
        tmp = S.sbuf("dntmp", [128, 128], F32)
        zt = S.sbuf("dnz", [128, 1024], F32)
        ob = S.sbuf("dnob", [128, 1024], BF16)
        obT = S.sbuf("dnobT", [128, 8, 128], BF16)
        ssq = S.sbuf("dnssq", [128, 8], F32)
        pqb = [S.psum("dnpqb%d" % i, [128, 512], F32) for i in range(7)]
        pbig = pqb[0:2]
        pq = [Buf(pqb[i].t[:, 0:128], "dnpq%d" % i, root=pqb[i]) for i in range(7)]
        pxs = [Buf(pqb[i].t[:, 0:256], "dnpx%d" % i, root=pqb[i]) for i in range(7)]
        ptr = S.psum("dnptr", [128, 8, 128], BF16)
        npq = [0]

        def PQ():
            npq[0] += 1
            return pq[npq[0] % 7]

        def PX():
            npq[0] += 1
            return pxs[npq[0] % 7]

        slot = [0, 0, 0]

        def PQs(j, wide=False):
            slot[j] += 1
            bank = pqb[3 * j + slot[j] % 3]
            w_ = 256 if wide else 128
            return Buf(bank.t[:, 0:w_], "dnps", root=bank)

        seqs = [(s * LC, LC, False) for s in range(NCTX)] + [(NCTX * LC, LL, True)]
        seqs = seqs[:DNSEQ]
        for si, (tok0, L, latent) in enumerate(seqs):
            nch = L // 128
            for n in range(nch):
                g0 = tok0 + n * 128
                S.dma("sp", lambda e: e.dma_start(out=ab[:], in_=P_T[g0:g0 + 128, aa:aa + 32]), reads=[P_T], writes=[ab])
                S.op("dve", lambda e: e.tensor_tensor(out=gt[:, n, :], in0=ab[:, 0:16], in1=dtb[:], op=ALU.add),
                     reads=[ab, dtb], writes=[gt])
                S.op("act", lambda e: e.activation(out=gt[:, n, :], in_=gt[:, n, :], func=AF.Exp), reads=[gt], writes=[gt])
                S.op("act", lambda e: e.activation(out=gt[:, n, :], in_=gt[:, n, :], func=AF.Ln, bias=1.0), reads=[gt], writes=[gt])
                S.op("dve", lambda e: e.scalar_tensor_tensor(out=gt[:, n, :], in0=gt[:, n, :], scalar=-1.0, in1=alog[:],
                                                             op0=ALU.mult, op1=ALU.mult), reads=[gt, alog], writes=[gt])
                S.op("act", lambda e: e.activation(out=bt[:, n, :], in_=ab[:, 16:32], func=AF.Sigmoid), reads=[ab], writes=[bt])
                p1 = PQ()
                for d in range(2):
                    S.op("pe", lambda e: e.matmul(p1[:, d * 8:(d + 1) * 8], lhsT=cst[:, d, :], rhs=gt[:, n, d * 8:(d + 1) * 8],
                                                  start=True, stop=True), reads=[cst, gt], writes=[p1])
                S.op("pe", lambda e: e.matmul(p1[:, 16:32], lhsT=onesf[:, :], rhs=gt[:, n, :], start=True, stop=True),
                     reads=[onesf, gt], writes=[p1])
                S.op("dve", lambda e: e.tensor_copy(out=gc[:, n, :], in_=p1[:, 0:16]), reads=[p1], writes=[gc])
                S.op("act", lambda e: e.activation(out=egc[:, n, :], in_=p1[:, 0:16], func=AF.Exp), reads=[p1], writes=[egc])
                S.op("act", lambda e: e.activation(out=egl[:, n, :], in_=p1[:, 16:32], func=AF.Exp), reads=[p1], writes=[egl])
                S.op("dve", lambda e: e.tensor_tensor(out=edl[:, n, :], in0=p1[:, 16:32], in1=gc[:, n, :], op=ALU.subtract),
                     reads=[p1, gc], writes=[edl])
                S.op("act", lambda e: e.activation(out=edl[:, n, :], in_=edl[:, n, :], func=AF.Exp), reads=[edl], writes=[edl])
                S.op("dve", lambda e: e.tensor_tensor(out=bg[:, n, :], in0=bt[:, n, :], in1=egc[:, n, :], op=ALU.mult),
                     reads=[bt, egc], writes=[bg])
            for h in range(8):
                if DNSTOP < 2:
                    continue
                for which, dst in ((0, qT), (1, kT), (2, vT)):
                    ch = which * 8 + h
                    r0 = fdn + ch * 128
                    S.op("pool", lambda e: e.memset(xin[:, 0:2], 0.0), writes=[xin])
                    S.op("pool", lambda e: e.memset(xin[:, L + 2:L + 4], 0.0), writes=[xin])
                    S.dma("sp", lambda e: e.dma_start(out=xin[:, 2:L + 2], in_=P_F[r0:r0 + 128, tok0:tok0 + L]),
                          reads=[P_F], writes=[xin])
                    S.op("dve", lambda e: e.tensor_scalar(out=acc[:, 0:L], in0=xin[:, 0:L], scalar1=cw[:, ch, 0:1], scalar2=None,
                                                          op0=ALU.mult), reads=[xin, cw], writes=[acc])
                    for kk in range(1, 5):
                        S.op("dve", lambda e: e.scalar_tensor_tensor(out=acc[:, 0:L], in0=xin[:, kk:kk + L],
                                                                     scalar=cw[:, ch, kk:kk + 1], in1=acc[:, 0:L],
                                                                     op0=ALU.mult, op1=ALU.add), reads=[xin, cw, acc], writes=[acc])
                    S.op("act", lambda e: e.activation(out=dst[:, 0:L], in_=acc[:, 0:L], func=AF.Silu), reads=[acc], writes=[dst])
                    if which < 2:
                        for g in range((L + 511) // 512):
                            w = min(512, L - g * 512)
                            pb = pbig[g % 2]
                            S.op("act", lambda e: e.activation(out=sq[:, 0:w], in_=dst[:, g * 512:g * 512 + w], func=AF.Square),
                                 reads=[dst], writes=[sq])
                            S.op("pe", lambda e: e.matmul(pb[:, 0:w], lhsT=onesf[:, :], rhs=sq[:, 0:w], start=True, stop=True),
                                 reads=[onesf, sq], writes=[pb])
                            sc_ = 128.0 if which == 0 else 1.0
                            S.op("act", lambda e: e.activation(out=rn[:, 0:w], in_=pb[:, 0:w], func=AF.Sqrt, scale=sc_,
                                                               bias=sc_ * EPS), reads=[pb], writes=[rn])
                            S.op("dve", lambda e: e.reciprocal(out=rn[:, 0:w], in_=rn[:, 0:w]), reads=[rn], writes=[rn])
                            S.op("dve", lambda e: e.tensor_tensor(out=dst[:, g * 512:g * 512 + w], in0=dst[:, g * 512:g * 512 + w],
                                                                  in1=rn[:, 0:w], op=ALU.mult), reads=[dst, rn], writes=[dst])
                if DNSTOP < 3:
                    continue
                for n in range(nch):
                    for src, dd in ((kT, ktok), (vT, vtok)):
                        p1 = PQ()
                        S.op("pe", lambda e: e.transpose(p1[:, 0:128], src[:, n * 128:(n + 1) * 128], identf[:]),
                             reads=[src, identf], writes=[p1])
                        _evac(S, n, dd[:, n, :], p1[:, 0:128], reads=[p1], writes=[dd])
                for d in range(2):
                    if DNSTOP < 4:
                        continue
                    dh = d * 8 + h
                    if latent:
                        S.dma("sp", lambda e: e.dma_start(out=Sst[:], in_=R["dn_state"][d][l, h, :, :]),
                              reads=[R["dn_state"][d]], writes=[Sst])
                    else:
                        S.op("dve", lambda e: e.memset(Sst[:], 0.0), writes=[Sst])
                    order = list(range(nch)) if d == 0 else list(range(nch - 1, -1, -1))

                    def prep(n, B, d=d, dh=dh):
                        c0 = n * 128
                        Gm, EA, ET, Am, Ad, Aoff, Boff, Bk, Ck, qkT, X, Zb = (B["Gm"], B["EA"], B["ET"], B["Am"], B["Ad"], B["Aoff"],
                                                                             B["Boff"], B["Bk"], B["Ck"], B["qkT"], B["X"], B["Zb"])
                        S.op("dve", lambda e: e.tensor_scalar(out=Gm[:], in0=cst[:, 3 + d, :], scalar1=gt[:, n, dh:dh + 1],
                                                              scalar2=None, op0=ALU.mult), reads=[cst, gt], writes=[Gm])
                        yield
                        J = B["j"]
                        pA, pT_ = PQs(J), PQs(J)
                        S.op("pe", lambda e: e.matmul(pA[:, :], lhsT=cst[:, d, :], rhs=Gm[:, :], start=True, stop=False),
                             reads=[cst, Gm], writes=[pA])
                        S.op("pe", lambda e: e.matmul(pA[:, :], lhsT=cst[:, 2, :], rhs=cst[:, 5 + d, :], start=False, stop=True),
                             reads=[cst], writes=[pA])
                        S.op("pe", lambda e: e.matmul(pT_[:, :], lhsT=Gm[:, :], rhs=cst[:, d, :], start=True, stop=False),
                             reads=[cst, Gm], writes=[pT_])
                        S.op("pe", lambda e: e.matmul(pT_[:, :], lhsT=cst[:, 2, :], rhs=cst[:, 7 + d, :], start=False, stop=True),
                             reads=[cst], writes=[pT_])
                        yield
                        S.op("act", lambda e: e.activation(out=EA[:], in_=pA[:, :], func=AF.Exp), reads=[pA], writes=[EA])
                        S.op("act", lambda e: e.activation(out=ET[:], in_=pT_[:, :], func=AF.Exp), reads=[pT_], writes=[ET])
                        yield
                        pkk, pkq = PQs(J), PQs(J)
                        S.op("pe", lambda e: e.matmul(pkk[:, :], lhsT=kT[:, c0:c0 + 128], rhs=kT[:, c0:c0 + 128], start=True,
                                                      stop=True), reads=[kT], writes=[pkk])
                        S.op("pe", lambda e: e.matmul(pkq[:, :], lhsT=kT[:, c0:c0 + 128], rhs=qT[:, c0:c0 + 128], start=True,
                                                      stop=True), reads=[kT, qT], writes=[pkq])
                        yield
                        S.op("dve", lambda e: e.scalar_tensor_tensor(out=Am[:], in0=pkk[:, :], scalar=bt[:, n, dh:dh + 1], in1=EA[:],
                                                                     op0=ALU.mult, op1=ALU.mult), reads=[pkk, bt, EA], writes=[Am])
                        S.op("dve", lambda e: e.tensor_tensor(out=qkT[:], in0=pkq[:, :], in1=ET[:], op=ALU.mult),
                             reads=[pkq, ET], writes=[qkT])
                        S.op("pool", lambda e: e.tensor_scalar(out=X[:, 0:128], in0=vtok[:, n, :], scalar1=bt[:, n, dh:dh + 1],
                                                               scalar2=None, op0=ALU.mult), reads=[vtok, bt], writes=[X])
                        S.op("pool", lambda e: e.tensor_scalar(out=X[:, 128:256], in0=ktok[:, n, :], scalar1=bg[:, n, dh:dh + 1],
                                                               scalar2=None, op0=ALU.mult), reads=[ktok, bg], writes=[X])
                        yield
                        if DNSTOP < 5:
                            return
                        S.op("dve", lambda e: e.tensor_tensor(out=Ad[:], in0=Am[:], in1=cst[:, 9, :], op=ALU.mult),
                             reads=[Am, cst], writes=[Ad])
                        S.op("pool", lambda e: e.tensor_tensor(out=Aoff[:], in0=Am[:], in1=Ad[:], op=ALU.subtract),
                             reads=[Am, Ad], writes=[Aoff])
                        yield
                        pt1, pt2 = PQs(J), PQs(J)
                        S.op("pe", lambda e: e.transpose(pt1[:, :], Ad[:, :], identf[:]), reads=[Ad, identf], writes=[pt1])
                        S.op("pe", lambda e: e.transpose(pt2[:, :], Aoff[:, :], identf[:]), reads=[Aoff, identf], writes=[pt2])
                        yield
                        S.op("act", lambda e: e.activation(out=Bk[0][:], in_=pt1[:, :], func=AF.Copy), reads=[pt1], writes=[Bk[0]])
                        S.op("act", lambda e: e.activation(out=Boff[:], in_=pt2[:, :], func=AF.Copy), reads=[pt2], writes=[Boff])
                        yield
                        Cc = Ad
                        for k_ in range(6):
                            Bc = Bk[k_]
                            pxx = PQs(J, True)
                            S.op("pe", lambda e: e.matmul(pxx[:, :], lhsT=Bc[:, :], rhs=X[:, :], start=True, stop=True),
                                 reads=[Bc, X], writes=[pxx])
                            if k_ < 5:
                                Bn, Cn = Bk[k_ + 1], Ck[(k_ + 1) % 2]
                                pb_, pc_ = PQs(J), PQs(J)
                                S.op("pe", lambda e: e.matmul(pb_[:, :], lhsT=Cc[:, :], rhs=Bc[:, :], start=True, stop=True),
                                     reads=[Cc, Bc], writes=[pb_])
                                S.op("pe", lambda e: e.matmul(pc_[:, :], lhsT=Bc[:, :], rhs=Cc[:, :], start=True, stop=True),
                                     reads=[Cc, Bc], writes=[pc_])
                            yield
                            S.op("dve", lambda e: e.tensor_tensor(out=X[:], in0=X[:], in1=pxx[:, :],
                                                                  op=(ALU.subtract if k_ == 0 else ALU.add)),
                                 reads=[X, pxx], writes=[X])
                            if k_ < 5:
                                S.op("act", lambda e: e.activation(out=Bn[:], in_=pb_[:, :], func=AF.Copy), reads=[pb_], writes=[Bn])
                                S.op("dve", lambda e: e.tensor_copy(out=Cn[:], in_=pc_[:, :]), reads=[pc_], writes=[Cn])
                                Cc = Cn
                            yield
                        pz = PQs(J, True)
                        S.op("pe", lambda e: e.matmul(pz[:, :], lhsT=Boff[:, :], rhs=X[:, :], start=True, stop=True),
                             reads=[Boff, X], writes=[pz])
                        yield
                        S.op("act", lambda e: e.activation(out=Zb[:], in_=pz[:, :], func=AF.Copy), reads=[pz], writes=[Zb])
                        yield
                        for k_ in range(6):
                            pxx = PQs(J, True)
                            S.op("pe", lambda e: e.matmul(pxx[:, :], lhsT=Bk[k_][:, :], rhs=Zb[:, :], start=True, stop=True),
                                 reads=[Bk[k_], Zb], writes=[pxx])
                            yield
                            S.op("dve", lambda e: e.tensor_tensor(out=Zb[:], in0=Zb[:], in1=pxx[:, :],
                                                                  op=(ALU.subtract if k_ == 0 else ALU.add)),
                                 reads=[Zb, pxx], writes=[Zb])
                            yield
                        S.op("dve", lambda e: e.tensor_tensor(out=X[:], in0=X[:], in1=Zb[:], op=ALU.subtract),
                             reads=[X, Zb], writes=[X])
                        yield
                        pw = PQs(J)
                        S.op("pe", lambda e: e.transpose(pw[:, :], X[:, 128:256], identf[:]), reads=[X, identf], writes=[pw])
                        yield
                        S.op("act", lambda e: e.activation(out=B["wT"][:], in_=pw[:, :], func=AF.Copy), reads=[pw], writes=[B["wT"]])
                        S.op("pool", lambda e: e.tensor_scalar(out=B["kd"][:], in0=ktok[:, n, :], scalar1=edl[:, n, dh:dh + 1],
                                                               scalar2=None, op0=ALU.mult), reads=[ktok, edl], writes=[B["kd"]])

                    def scan(n, B, d=d, dh=dh):
                        c0 = n * 128
                        X, qkT, wT, kd = B["X"], B["qkT"], B["wT"], B["kd"]
                        p1, p2, p3, p4 = PQ(), PQ(), PQ(), PQ()
                        S.op("pe", lambda e: e.matmul(p1[:, :], lhsT=wT[:, :], rhs=Sst[:, :], start=True, stop=True),
                             reads=[wT, Sst], writes=[p1])
                        S.op("pe", lambda e: e.matmul(p2[:, :], lhsT=qT[:, c0:c0 + 128], rhs=Sst[:, :], start=True, stop=True),
                             reads=[qT, Sst], writes=[p2])
                        S.op("dve", lambda e: e.tensor_tensor(out=vnew[:], in0=X[:, 0:128], in1=p1[:, :], op=ALU.subtract),
                             reads=[X, p1], writes=[vnew])
                        S.op("pe", lambda e: e.matmul(p3[:, :], lhsT=qkT[:, :], rhs=vnew[:, :], start=True, stop=True),
                             reads=[qkT, vnew], writes=[p3])
                        S.op("pe", lambda e: e.matmul(p4[:, :], lhsT=kd[:, :], rhs=vnew[:, :], start=True, stop=True),
                             reads=[kd, vnew], writes=[p4])
                        S.op("dve", lambda e: e.scalar_tensor_tensor(out=Sst[:], in0=Sst[:], scalar=egl[:, n, dh:dh + 1], in1=p4[:, :],
                                                                     op0=ALU.mult, op1=ALU.add), reads=[Sst, egl, p4], writes=[Sst])
                        S.op("dve", lambda e: e.tensor_scalar(out=tmp[:], in0=p2[:, :], scalar1=egc[:, n, dh:dh + 1], scalar2=None,
                                                              op0=ALU.mult), reads=[p2, egc], writes=[tmp])
                        osl = oall[:, n, h * 128:(h + 1) * 128]
                        if d == 0:
                            S.op("pool" if False else "dve", lambda e: e.tensor_tensor(out=osl, in0=tmp[:], in1=p3[:, :], op=ALU.add),
                                 reads=[tmp, p3], writes=[oall])
                        else:
                            S.op("dve", lambda e: e.tensor_tensor(out=tmp[:], in0=tmp[:], in1=p3[:, :], op=ALU.add),
                                 reads=[tmp, p3], writes=[tmp])
                            S.op("pool", lambda e: e.tensor_tensor(out=osl, in0=osl, in1=tmp[:], op=ALU.add),
                                 reads=[tmp, oall], writes=[oall])

                    NG = 2
                    for g0 in range(0, nch, NG):
                        grp = order[g0:g0 + NG]
                        gens = [prep(n, Bsets[j]) for j, n in enumerate(grp)]
                        alive = list(gens)
                        while alive:
                            for g_ in list(alive):
                                try:
                                    next(g_)
                                except StopIteration:
                                    alive.remove(g_)
                        if DNSTOP < 6:
                            continue
                        for j, n in enumerate(grp):
                            scan(n, Bsets[j])
                    if not latent:
                        ost = R["o_dn"][d]
                        S.dma("sp", lambda e: e.dma_start(out=ost[si, l, h, :, :], in_=Sst[:]), reads=[Sst], writes=[ost])
            for n in range(nch):
                if DNSTOP < 7:
                    continue
                g0 = tok0 + n * 128
                S.dma("sp", lambda e: e.dma_start(out=zt[:], in_=P_T[g0:g0 + 128, az:az + 1024]), reads=[P_T], writes=[zt])
                S.op("act", lambda e: e.activation(out=zt[:], in_=zt[:], func=AF.Silu), reads=[zt], writes=[zt])
                o3 = oall[:, n, :].rearrange("p (h v) -> p h v", h=8)
                for h in range(8):
                    S.op("act", lambda e: e.activation(out=tmp[:], in_=oall[:, n, h * 128:(h + 1) * 128], func=AF.Square,
                                                       accum_out=ssq[:, h:h + 1]), reads=[oall], writes=[tmp, ssq])
                S.op("act", lambda e: e.activation(out=ssq[:], in_=ssq[:], func=AF.Sqrt, scale=1.0 / 128, bias=EPS),
                     reads=[ssq], writes=[ssq])
                S.op("dve", lambda e: e.reciprocal(out=ssq[:], in_=ssq[:]), reads=[ssq], writes=[ssq])
                S.op("dve", lambda e: e.tensor_tensor(out=o3, in0=o3, in1=ssq[:, :].unsqueeze(2).to_broadcast([128, 8, 128]),
                                                      op=ALU.mult), reads=[oall, ssq], writes=[oall])
                S.op("dve", lambda e: e.tensor_tensor(out=o3, in0=o3, in1=gon[:, :].unsqueeze(1).to_broadcast([128, 8, 128]),
                                                      op=ALU.mult), reads=[oall, gon], writes=[oall])
                S.op("dve", lambda e: e.tensor_tensor(out=ob[:], in0=oall[:, n, :], in1=zt[:], op=ALU.mult),
                     reads=[oall, zt], writes=[ob])
                for h in range(8):
                    S.op("pe", lambda e: e.transpose(ptr[:, h, :], ob[:, h * 128:(h + 1) * 128], identb[:]),
                         reads=[ob, identb], writes=[ptr])
                _evac(S, n, obT[:], ptr[:, :, :], reads=[ptr], writes=[obT])
                S.dma("sp", lambda e: e.dma_start(out=brT[1][:, g0:g0 + 128].rearrange("(h p) t -> p h t", p=128), in_=obT[:]),
                      reads=[obT], writes=[])
        S.barrier()
    S.scope = S.es


def _hy_geom(L):
    nf = L + 1
    KT = (nf + 127) // 128
    NB = (nf + 511) // 512
    return nf, KT, NB


def stage_hyena(S, l, R):
    P_F, brT = R["P_F"], R["brT"]
    ident = R["ident"]
    fhy, _ = F_ROWS["hyT"]
    hyZ, hyY1, hyU, hyPQ, hyYS = R["hyZ"], R["hyY1"], R["hyU"], R["hyPQ"], R["hyYS"]
    PI = float(np.pi)
    seqs = [(s * LC, LC, False) for s in range(NCTX)] + [(NCTX * LC, LL, True)]
    for (Lx, tabi, seq_list) in ((LC, 0, seqs[:NCTX]), (LL, 1, seqs[NCTX:])):
        L = Lx
        nf, KT, NB = _hy_geom(L)
        KU = L // 128
        tab = R["hy_tab"][tabi]
        with ExitStack() as sc:
            S.scope = sc
            w1 = S.sbuf("hyw1", [33, 64], F32)
            w2 = S.sbuf("hyw2", [64, 64], F32)
            w3 = S.sbuf("hyw3", [64, 4096], F32)
            b12 = S.sbuf("hyb12", [64, 2], F32)
            S.dma("sp", lambda e: e.dma_start(out=w1[:], in_=R["hy_w1"][l]), reads=[R["hy_w1"]], writes=[w1])
            S.dma("sp", lambda e: e.dma_start(out=w2[:], in_=R["hy_w2"][l]), reads=[R["hy_w2"]], writes=[w2])
            S.dma("sp", lambda e: e.dma_start(out=w3[:], in_=R["hy_w3"][l]), reads=[R["hy_w3"]], writes=[w3])
            S.dma("sp", lambda e: e.dma_start(out=b12[:], in_=R["hy_b12"][l]), reads=[R["hy_b12"]], writes=[b12])
            zT = S.sbuf("hyzT", [33, L], F32)
            S.dma("sp", lambda e: e.dma_start(out=zT[:], in_=R["hy_zemb"][tabi][:, :]), reads=[R["hy_zemb"][tabi]], writes=[zT])
            wfs = S.sbuf("hywf", [128, KT], F32)
            S.dma("sp", lambda e: e.dma_start(out=wfs[:], in_=R["hy_wf"][tabi][:, :]), reads=[R["hy_wf"][tabi]], writes=[wfs])
            h1 = S.sbuf("hyh1", [64, L], F32)
            h2 = S.sbuf("hyh2", [64, L], F32)
            xa = S.sbuf("hyxa", [64, 512], F32)
            xb_ = S.sbuf("hyxb", [64, 512], F32)
            xc = S.sbuf("hyxc", [64, 512], F32)
            win = S.sbuf("hywin", [128, 1024], F32)
            hs = S.sbuf("hyhs", [128, KU, 1024], BF16)
            hd = S.sbuf("hyhd", [128, KU, 1024], BF16)
            tf = [S.sbuf("hytf%d" % i, [128, 512], F32) for i in range(2)]
            slab = [S.sbuf("hyslab%d" % i, [128, KT, 512], BF16) for i in range(2)]
            pst = S.sbuf("hypst", [128, 512], F32)
            pm = [S.psum("hypm%d" % i, [128, 512], F32) for i in range(4)]
            npm = [0]

            def PM():
                npm[0] += 1
                return pm[npm[0] % 4]

            def sin_layer(dst, wmat, kdim, src, bcol):
                for g in range((L + 511) // 512):
                    w = min(512, L - g * 512)
                    ps = PM()
                    S.op("pe", lambda e: e.matmul(ps[0:64, 0:w], lhsT=wmat[0:kdim, :], rhs=src[0:kdim, g * 512:g * 512 + w],
                                                  start=True, stop=True), reads=[wmat, src], writes=[ps])
                    S.op("dve", lambda e: e.tensor_scalar(out=xa[:, 0:w], in0=ps[0:64, 0:w], scalar1=b12[:, bcol:bcol + 1],
                                                          scalar2=None, op0=ALU.add), reads=[ps, b12], writes=[xa])
                    S.op("dve", lambda e: e.tensor_scalar(out=xb_[:, 0:w], in0=xa[:, 0:w], scalar1=PI, scalar2=-2 * PI,
                                                          op0=ALU.is_gt, op1=ALU.mult), reads=[xa], writes=[xb_])
                    S.op("dve", lambda e: e.tensor_scalar(out=xc[:, 0:w], in0=xa[:, 0:w], scalar1=-PI, scalar2=2 * PI,
                                                          op0=ALU.is_lt, op1=ALU.mult), reads=[xa], writes=[xc])
                    S.op("dve", lambda e: e.tensor_tensor(out=xa[:, 0:w], in0=xa[:, 0:w], in1=xb_[:, 0:w], op=ALU.add),
                         reads=[xa, xb_], writes=[xa])
                    S.op("dve", lambda e: e.tensor_tensor(out=xa[:, 0:w], in0=xa[:, 0:w], in1=xc[:, 0:w], op=ALU.add),
                         reads=[xa, xc], writes=[xa])
                    S.op("act", lambda e: e.activation(out=dst[:, g * 512:g * 512 + w], in_=xa[:, 0:w], func=AF.Sin),
                         reads=[xa], writes=[dst])

            sin_layer(h1, w1, 33, zT, 0)
            sin_layer(h2, w2, 64, h1, 1)
            for o in range(2):
                for t in range(KU):
                    S.dma("sp", lambda e: e.dma_start(out=win[:], in_=R["hy_win"][tabi][t * 128:(t + 1) * 128, :]),
                          reads=[R["hy_win"][tabi]], writes=[win])
                    for cb in range(2):
                        pf, pb = PM(), PM()
                        cf = (o * 2 + 0) * 1024 + cb * 512
                        cbk = (o * 2 + 1) * 1024 + cb * 512
                        S.op("pe", lambda e: e.matmul(pf[:, :], lhsT=h2[:, t * 128:(t + 1) * 128], rhs=w3[:, cf:cf + 512],
                                                      start=True, stop=True), reads=[h2, w3], writes=[pf])
                        S.op("pe", lambda e: e.matmul(pb[:, :], lhsT=h2[:, t * 128:(t + 1) * 128], rhs=w3[:, cbk:cbk + 512],
                                                      start=True, stop=True), reads=[h2, w3], writes=[pb])
                        S.op("dve", lambda e: e.tensor_tensor(out=tf[0][:], in0=pf[:, :], in1=win[:, cb * 512:(cb + 1) * 512],
                                                              op=ALU.mult), reads=[pf, win], writes=[tf[0]])
                        S.op("dve", lambda e: e.tensor_tensor(out=tf[1][:], in0=pb[:, :], in1=win[:, cb * 512:(cb + 1) * 512],
                                                              op=ALU.mult), reads=[pb, win], writes=[tf[1]])
                        if t == 0:
                            S.op("dve", lambda e: e.memset(tf[1][0:1, :], 0.0), writes=[tf[1]])
                        S.op("dve", lambda e: e.tensor_tensor(out=hs[:, t, cb * 512:(cb + 1) * 512], in0=tf[0][:], in1=tf[1][:],
                                                              op=ALU.add), reads=[tf[0], tf[1]], writes=[hs])
                        S.op("pool", lambda e: e.tensor_tensor(out=hd[:, t, cb * 512:(cb + 1) * 512], in0=tf[0][:], in1=tf[1][:],
                                                               op=ALU.subtract), reads=[tf[0], tf[1]], writes=[hd])
                for fb in range(NB):
                    for cs in range(2):
                        S.dma("sp", lambda e: e.dma_start(out=slab[cs][:], in_=tab[cs, fb]), reads=[tab], writes=[slab[cs]])
                    for j in range(4):
                        m = fb * 4 + j
                        if m >= KT:
                            break
                        fm = min(128, nf - m * 128)
                        for cs, src in ((0, hs), (1, hd)):
                            for cb in range(2):
                                ps = PM()
                                for kt in range(KU):
                                    S.op("pe", lambda e: e.matmul(ps[0:fm, :], lhsT=slab[cs][:, kt, j * 128:j * 128 + fm],
                                                                  rhs=src[:, kt, cb * 512:(cb + 1) * 512], start=(kt == 0),
                                                                  stop=(kt == KU - 1)), reads=[slab[cs], src], writes=[ps])
                                S.op("dve", lambda e: e.tensor_scalar(out=pst[0:fm, :], in0=ps[0:fm, :], scalar1=wfs[0:fm, m:m + 1],
                                                                      scalar2=None, op0=ALU.mult), reads=[ps, wfs], writes=[pst])
                                S.dma("sp", lambda e: e.dma_start(
                                    out=hyPQ[tabi][o, cs, m * 128:m * 128 + fm, cb * 512:(cb + 1) * 512], in_=pst[0:fm, :]),
                                    reads=[pst], writes=[])
            S.barrier()
        for (tok0, L_, latent) in seq_list:
            si = tok0 // LC if not latent else NCTX
            with ExitStack() as sc:
                S.scope = sc
                cw = S.sbuf("hycw", [128, 24, 3], F32)
                S.dma("sp", lambda e: e.dma_start(out=cw[:], in_=R["hy_convT"][l]), reads=[R["hy_convT"]], writes=[cw])
                xin = [S.sbuf("hyxin%d" % i, [128, L + 2], F32) for i in range(2)]
                acc = [S.sbuf("hyacc%d" % i, [128, L], F32) for i in range(2)]
                accb = S.sbuf("hyaccb", [128, L], BF16)
                ut = S.sbuf("hyut", [128, KU, 128], BF16)
                ptr = [S.psum("hyptr%d" % i, [128, 8, 128], BF16) for i in range(2)]
                for ch in range(24):
                    xi, ac = xin[ch % 2], acc[ch % 2]
                    r0 = fhy + ch * 128
                    S.op("pool", lambda e: e.memset(xi[:, 0:1], 0.0), writes=[xi])
                    S.op("pool", lambda e: e.memset(xi[:, L + 1:L + 2], 0.0), writes=[xi])
                    S.dma("sp", lambda e: e.dma_start(out=xi[:, 1:L + 1], in_=P_F[r0:r0 + 128, tok0:tok0 + L]), reads=[P_F], writes=[xi])
                    S.op("dve", lambda e: e.tensor_scalar(out=ac[:], in0=xi[:, 0:L], scalar1=cw[:, ch, 0:1], scalar2=None, op0=ALU.mult),
                         reads=[xi, cw], writes=[ac])
                    for kk in (1, 2):
                        S.op("dve", lambda e: e.scalar_tensor_tensor(out=ac[:], in0=xi[:, kk:kk + L], scalar=cw[:, ch, kk:kk + 1], in1=ac[:],
                                                                     op0=ALU.mult, op1=ALU.add), reads=[xi, cw, ac], writes=[ac])
                    S.dma("sp", lambda e: e.dma_start(out=hyZ[ch * 128:(ch + 1) * 128, tok0:tok0 + L], in_=ac[:]), reads=[ac], writes=[])
                    if ch < 8:
                        S.op("act", lambda e: e.activation(out=accb[:], in_=ac[:], func=AF.Copy), reads=[ac], writes=[accb])
                        for t0 in range(0, KU, 8):
                            pt = ptr[(t0 // 8) % 2]
                            nn = min(8, KU - t0)
                            for j in range(nn):
                                S.op("pe", lambda e: e.transpose(pt[:, j, :], accb[:, (t0 + j) * 128:(t0 + j + 1) * 128], ident[:]),
                                     reads=[accb, ident], writes=[pt])
                            _evac(S, t0 // 8, ut[:, t0:t0 + nn, :], pt[:, 0:nn, :], reads=[pt], writes=[ut])
                        S.dma("sp", lambda e: e.dma_start(
                            out=hyU[tok0:tok0 + L, ch * 128:(ch + 1) * 128].rearrange("(k p) c -> p k c", p=128), in_=ut[:]),
                            reads=[ut], writes=[])
                S.barrier()
            for o in range(2):
                with ExitStack() as sc:
                    S.scope = sc
                    u = S.sbuf("hyu", [128, KU, 1024], BF16)
                    S.dma("sp", lambda e: e.dma_start(out=u[:], in_=hyU[tok0:tok0 + L, :].rearrange("(k p) c -> p k c", p=128)),
                          reads=[hyU], writes=[u])
                    slab = [S.sbuf("hyslabf%d" % i, [128, KT, 512], BF16) for i in range(2)]
                    PQt = [S.sbuf("hyPQt%d" % i, [128, 1024], F32) for i in range(2)]
                    ta = S.sbuf("hyta", [128, 512], F32)
                    tb_ = S.sbuf("hytb", [128, 512], F32)
                    yc = S.sbuf("hyyc", [128, 512], BF16)
                    ys = S.sbuf("hyys", [128, 512], BF16)
                    pm = [S.psum("hypmf%d" % i, [128, 512], F32) for i in range(4)]
                    for fb in range(NB):
                        for cs in range(2):
                            S.dma("sp", lambda e: e.dma_start(out=slab[cs][:], in_=tab[cs, fb]), reads=[tab], writes=[slab[cs]])
                        for j in range(4):
                            m = fb * 4 + j
                            if m >= KT:
                                break
                            fm = min(128, nf - m * 128)
                            for cs in range(2):
                                S.dma("sp", lambda e: e.dma_start(out=PQt[cs][0:fm, :], in_=hyPQ[tabi][o, cs, m * 128:m * 128 + fm, :]),
                                      reads=[hyPQ[tabi]], writes=[PQt[cs]])
                            for cb in range(2):
                                pa, pb = pm[(2 * cb) % 4], pm[(2 * cb + 1) % 4]
                                for kt in range(KU):
                                    S.op("pe", lambda e: e.matmul(pa[0:fm, :], lhsT=slab[0][:, kt, j * 128:j * 128 + fm],
                                                                  rhs=u[:, kt, cb * 512:(cb + 1) * 512], start=(kt == 0), stop=(kt == KU - 1)),
                                         reads=[slab[0], u], writes=[pa])
                                for kt in range(KU):
                                    S.op("pe", lambda e: e.matmul(pb[0:fm, :], lhsT=slab[1][:, kt, j * 128:j * 128 + fm],
                                                                  rhs=u[:, kt, cb * 512:(cb + 1) * 512], start=(kt == 0), stop=(kt == KU - 1)),
                                         reads=[slab[1], u], writes=[pb])
                                Pc = PQt[0][0:fm, cb * 512:(cb + 1) * 512]
                                Qc = PQt[1][0:fm, cb * 512:(cb + 1) * 512]
                                S.op("dve", lambda e: e.tensor_tensor(out=ta[0:fm, :], in0=pa[0:fm, :], in1=Pc, op=ALU.mult),
                                     reads=[pa, PQt[0]], writes=[ta])
                                S.op("dve", lambda e: e.tensor_tensor(out=tb_[0:fm, :], in0=pb[0:fm, :], in1=Qc, op=ALU.mult),
                                     reads=[pb, PQt[1]], writes=[tb_])
                                S.op("pool", lambda e: e.tensor_tensor(out=yc[0:fm, :], in0=ta[0:fm, :], in1=tb_[0:fm, :], op=ALU.subtract),
                                     reads=[ta, tb_], writes=[yc])
                                S.op("dve", lambda e: e.tensor_tensor(out=ta[0:fm, :], in0=pa[0:fm, :], in1=Qc, op=ALU.mult),
                                     reads=[pa, PQt[1]], writes=[ta])
                                S.op("dve", lambda e: e.tensor_tensor(out=tb_[0:fm, :], in0=pb[0:fm, :], in1=Pc, op=ALU.mult),
                                     reads=[pb, PQt[0]], writes=[tb_])
                                S.op("pool", lambda e: e.tensor_tensor(out=ys[0:fm, :], in0=ta[0:fm, :], in1=tb_[0:fm, :], op=ALU.add),
                                     reads=[ta, tb_], writes=[ys])
                                S.dma("sp", lambda e: e.dma_start(out=hyYS[0, m * 128:m * 128 + fm, cb * 512:(cb + 1) * 512], in_=yc[0:fm, :]),
                                      reads=[yc], writes=[])
                                S.dma("sp", lambda e: e.dma_start(out=hyYS[1, m * 128:m * 128 + fm, cb * 512:(cb + 1) * 512], in_=ys[0:fm, :]),
                                      reads=[ys], writes=[])
                    S.barrier()
                with ExitStack() as sc:
                    S.scope = sc
                    ycs = [S.sbuf("hyYc%d" % i, [128, KT, 1024], BF16) for i in range(2)]
                    for cs in range(2):
                        S.op("dve", lambda e: e.memset(ycs[cs][:, KT - 1, :], 0.0), writes=[ycs[cs]])
                        S.dma("sp", lambda e: e.dma_start(
                            out=ycs[cs][:, 0:KT - 1, :], in_=hyYS[cs, 0:(KT - 1) * 128, :].rearrange("(k p) c -> p k c", p=128)),
                            reads=[hyYS], writes=[ycs[cs]])
                        S.dma("sp", lambda e: e.dma_start(out=ycs[cs][0:1, KT - 1, :], in_=hyYS[cs, (KT - 1) * 128:(KT - 1) * 128 + 1, :]),
                              reads=[hyYS], writes=[ycs[cs]])
                    slab = [S.sbuf("hyslabi%d" % i, [128, KT, 512], BF16) for i in range(2)]
                    bia = S.sbuf("hybia", [128, 8], F32)
                    S.dma("sp", lambda e: e.dma_start(out=bia[:], in_=R["hy_biasT"][l, o]), reads=[R["hy_biasT"]], writes=[bia])
                    uT = [S.sbuf("hyuT%d" % i, [128, 512], F32) for i in range(2)]
                    gT = [S.sbuf("hygT%d" % i, [128, 512], F32) for i in range(2)]
                    yo = [S.sbuf("hyyo%d" % i, [128, 512], F32) for i in range(2)]
                    yb = [S.sbuf("hyyb%d" % i, [128, 512], BF16) for i in range(2)]
                    ut = [S.sbuf("hyuti%d" % i, [128, 4, 128], BF16) for i in range(2)]
                    pm = [S.psum("hypmi%d" % i, [128, 512], F32) for i in range(4)]
                    ptr = [S.psum("hyptri%d" % i, [128, 8, 128], BF16) for i in range(2)]
                    it = 0
                    for tb in range((L + 511) // 512):
                        tw = min(512, L - tb * 512)
                        for cs in range(2):
                            S.dma("sp", lambda e: e.dma_start(out=slab[cs][:], in_=tab[cs, tb]), reads=[tab], writes=[slab[cs]])
                        for cc in range(8):
                            ps = pm[it % 4]
                            u_, g_, y_, yb_, ut_ = uT[it % 2], gT[it % 2], yo[it % 2], yb[it % 2], ut[it % 2]
                            it += 1
                            usrc = hyZ if o == 0 else hyY1
                            S.dma("sp", lambda e: e.dma_start(out=u_[:, 0:tw], in_=usrc[cc * 128:(cc + 1) * 128, tok0 + tb * 512:tok0 + tb * 512 + tw]),
                                  reads=[usrc], writes=[u_])
                            gr = (1 + o) * 1024 + cc * 128
                            S.dma("sp", lambda e: e.dma_start(out=g_[:, 0:tw], in_=hyZ[gr:gr + 128, tok0 + tb * 512:tok0 + tb * 512 + tw]),
                                  reads=[hyZ], writes=[g_])
                            n_mm = 2 * KT
                            i_mm = 0
                            for cs in range(2):
                                for kt in range(KT):
                                    kp = 128 if kt < KT - 1 else 1
                                    S.op("pe", lambda e: e.matmul(ps[:, 0:tw], lhsT=ycs[cs][0:kp, kt, cc * 128:(cc + 1) * 128],
                                                                  rhs=slab[cs][0:kp, kt, 0:tw], start=(i_mm == 0), stop=(i_mm == n_mm - 1)),
                                         reads=[ycs[cs], slab[cs]], writes=[ps])
                                    i_mm += 1
                            S.op("dve", lambda e: e.scalar_tensor_tensor(out=y_[:, 0:tw], in0=u_[:, 0:tw], scalar=bia[:, cc:cc + 1],
                                                                         in1=ps[:, 0:tw], op0=ALU.mult, op1=ALU.add),
                                 reads=[u_, bia, ps], writes=[y_])
                            if o == 0:
                                S.op("pool", lambda e: e.tensor_tensor(out=y_[:, 0:tw], in0=y_[:, 0:tw], in1=g_[:, 0:tw], op=ALU.mult),
                                     reads=[y_, g_], writes=[y_])
                                S.dma("sp", lambda e: e.dma_start(out=hyY1[cc * 128:(cc + 1) * 128, tok0 + tb * 512:tok0 + tb * 512 + tw],
                                                                  in_=y_[:, 0:tw]), reads=[y_], writes=[])
                                S.op("act", lambda e: e.activation(out=yb_[:, 0:tw], in_=y_[:, 0:tw], func=AF.Copy), reads=[y_], writes=[yb_])
                                pt = ptr[it % 2]
                                nt = tw // 128
                                for j in range(nt):
                                    S.op("pe", lambda e: e.transpose(pt[:, j, :], yb_[:, j * 128:(j + 1) * 128], ident[:]),
                                         reads=[yb_, ident], writes=[pt])
                                _evac(S, it, ut_[:, 0:nt, :], pt[:, 0:nt, :], reads=[pt], writes=[ut_])
                                S.dma("sp", lambda e: e.dma_start(
                                    out=hyU[tok0 + tb * 512:tok0 + tb * 512 + tw, cc * 128:(cc + 1) * 128].rearrange("(k p) c -> p k c", p=128),
                                    in_=ut_[:, 0:nt, :]), reads=[ut_], writes=[])
                            else:
                                S.op("pool", lambda e: e.tensor_tensor(out=yb_[:, 0:tw], in0=y_[:, 0:tw], in1=g_[:, 0:tw], op=ALU.mult),
                                     reads=[y_, g_], writes=[yb_])
                                S.dma("sp", lambda e: e.dma_start(out=brT[2][cc * 128:(cc + 1) * 128, tok0 + tb * 512:tok0 + tb * 512 + tw],
                                                                  in_=yb_[:, 0:tw]), reads=[yb_], writes=[])
                    S.barrier()
    S.scope = S.es


def build_program(dbg=False):
    nc = bass.Bass("TRN2", target_bir_lowering=False)
    k = K()
    k.nc = nc

    def ein(name, shape, dtype=F32):
        return Buf(nc.dram_tensor(name, list(shape), dtype, kind="ExternalInput").ap(), name)

    def eout(name, shape, dtype=F32):
        return Buf(nc.dram_tensor(name, list(shape), dtype, kind="ExternalOutput").ap(), name)

    x_in = ein("x_in", [TT, D])
    c2T = ein("c2T", [128, 16, 2])
    ident_in = ein("ident", [128, 128])
    norm1_g = ein("norm1_g", [DEPTH, D])
    w_ada = ein("w_ada", [DEPTH, D, 6 * D])
    b_ada = ein("b_ada", [DEPTH, 6 * D])
    w_in_T = ein("w_in_T", [DEPTH, D, NT_COLS])
    w_in_F = ein("w_in_F", [DEPTH, D, NF_ROWS])
    mla_kv_norm = ein("mla_kv_norm", [DEPTH, 256])
    R = {}
    R["ident_in"] = ident_in
    R["mla_kv_norm"] = mla_kv_norm
    R["mla_q_norm"] = ein("mla_q_norm", [DEPTH, 512])
    R["w_qb"] = ein("w_qb_p", [DEPTH, 512, 2048])
    R["w_kvb"] = ein("w_kvb_p", [DEPTH, 256, 2048])
    R["cache_ckv"] = ein("cache_ckv", [DEPTH, PAST, 256])
    R["cache_kr"] = ein("cache_kr", [DEPTH, PAST, 64])
    R["cache_nak"] = ein("cache_nak", [DEPTH, PAST, 1024])
    R["cache_nav"] = ein("cache_nav", [DEPTH, PAST, 1024])
    R["ropeT"] = ein("ropeT", [2, 64, LL])
    R["na_ctab"] = ein("na_ctab", [DEPTH, 8, 64, 31, 64])
    R["na_mask"] = ein("na_mask", [NA_NMASK, 128, 512])
    R["w_branch"] = ein("w_branch", [DEPTH, 4, 1024, 2048])
    R["w_out"] = ein("w_out", [DEPTH, D, D])
    R["norm2_g"] = ein("norm2_g", [DEPTH, D])
    R["peer_wq"] = ein("peer_wq", [DEPTH, D, D])
    R["peer_keysT"] = ein("peer_keysT", [DEPTH, 2, 128, 128])
    R["peer_u"] = [ein("peer_u%d" % i, [16384, D]) for i in range(DEPTH)]
    R["peer_v"] = [ein("peer_v%d" % i, [16384, D]) for i in range(DEPTH)]
    R["final_g"] = ein("final_g", [1, D])
    R["iota256"] = ein("iota256", [1, 256])
    _hy_inputs(R, ein)
    R["dn_consts"] = ein("dn_consts", [10, 128, 128])
    R["dn_convT"] = ein("dn_convT", [DEPTH, 128, 24, 5])
    R["dn_a_log"] = ein("dn_a_log", [DEPTH, 16])
    R["dn_dt_bias"] = ein("dn_dt_bias", [DEPTH, 16])
    R["dn_out_norm"] = ein("dn_out_norm", [DEPTH, 128])
    R["dn_state"] = [ein("dn_state%d" % i, [DEPTH, 8, 128, 128]) for i in range(2)]
    if DBG_BR:
        R["dbg_br"] = ein("dbg_br", [DEPTH, 2, 1024, TT], BF16)

    o_ckv = eout("o_ckv", [NCTX, DEPTH, LC, 256])
    o_kr = eout("o_kr", [NCTX, DEPTH, LC, 64])
    o_nak = eout("o_nak", [NCTX, DEPTH, LC, 1024])
    o_nav = eout("o_nav", [NCTX, DEPTH, LC, 1024])
    y_out = eout("y_out", [NT_LAST * 128, D])
    R["ptok"] = ein("ptok", [128, NT_LAST], I32)
    o_dn = [eout("o_dn%d" % i, [NCTX, DEPTH, 8, 128, 128]) for i in range(2)]
    R["o_dn"] = o_dn
    outs = [o_ckv, o_kr, o_nak, o_nav, y_out] + o_dn
    R["o_ckv"] = o_ckv
    R["y_out"] = y_out

    with ExitStack() as es:
        S = Sched(nc, es)
        k.S = S
        modd = S.dram("modd", [2, 6 * D])
        P_T = S.dram("P_T", [TT, NT_COLS])
        P_F = S.dram("P_F", [NF_ROWS, TT])
        brT = [S.dram("brT%d" % b_, [1024, TT], BF16) for b_ in range(4)]
        R.update(P_T=P_T, P_F=P_F, brT=brT, modd=modd)
        R["mrg"] = S.dram("mrg", [TT, D])
        R["xbuf"] = S.dram("xbuf", [TT, D])
        R["mrgt"] = [Buf(R["mrg"].t[t_ * 128:(t_ + 1) * 128, :], "mrgt%d" % t_) for t_ in range(NTILE)]
        R["xbt"] = [Buf(R["xbuf"].t[t_ * 128:(t_ + 1) * 128, :], "xbt%d" % t_) for t_ in range(NTILE)]
        _hy_scratch(S, R)
        R["peer_ub"] = [S.dram("peer_ub%d" % i, [16384, D], BF16) for i in range(DEPTH)]
        R["peer_vb"] = [S.dram("peer_vb%d" % i, [16384, D], BF16) for i in range(DEPTH)]

        ident = S.sbuf("identb", [128, 128], BF16)
        S.dma("pool", lambda e: e.dma_start(out=ident[:], in_=ident_in[:, :]), reads=[ident_in], writes=[ident])
        R["ident"] = ident

        for l in range(1 if DBG_BR else DEPTH):
            with ExitStack() as sc:
                S.scope = sc
                cT = S.sbuf("cT", [128, 16, 2], F32)
                sT = S.sbuf("sT", [128, 16, 2], BF16)
                S.dma("sp", lambda e: e.dma_start(out=cT[:], in_=c2T[:, :, :]), reads=[c2T], writes=[cT])
                S.op("act", lambda e: e.activation(out=sT[:], in_=cT[:], func=AF.Silu), reads=[cT], writes=[sT])
                wts = [S.sbuf("adaw%d" % i, [128, 16, 512], BF16) for i in range(2)]
                bts = [S.sbuf("adab%d" % i, [2, 512], F32) for i in range(2)]
                mts = [S.sbuf("adam%d" % i, [2, 512], F32) for i in range(2)]
                pss = [S.psum("adap%d" % i, [2, 512], F32) for i in range(2)]
                for cb in range(24):
                    wt, bt, mt, ps = wts[cb % 2], bts[cb % 2], mts[cb % 2], pss[cb % 2]
                    c0 = cb * 512
                    S.dma("pool", lambda e: e.dma_start(
                        out=wt[:], in_=w_ada[l, :, c0:c0 + 512].rearrange("(k p) c -> p k c", p=128)),
                        reads=[w_ada], writes=[wt])
                    S.dma("sp", lambda e: e.dma_start(out=bt[:], in_=b_ada[l, c0:c0 + 512].partition_broadcast(2)),
                          reads=[b_ada], writes=[bt])
                    for kc in range(16):
                        S.op("pe", lambda e: e.matmul(ps[:, :], lhsT=sT[:, kc, :], rhs=wt[:, kc, :],
                                                      start=(kc == 0), stop=(kc == 15)),
                             reads=[sT, wt], writes=[ps])
                    S.op("dve", lambda e: e.tensor_tensor(out=mt[:], in0=ps[:], in1=bt[:], op=ALU.add),
                         reads=[ps, bt], writes=[mt])
                    S.dma("sp", lambda e: e.dma_start(out=modd[:, c0:c0 + 512], in_=mt[:]), reads=[mt], writes=[modd])
            S.barrier()

            x_cur = x_in if l == 0 else R["xbuf"]
            with ExitStack() as sc:
                S.scope = sc
                hT = S.sbuf("hT", [128, 16, TT], BF16)
                with ExitStack() as sc2:
                    S.scope = sc2
                    gm = [S.sbuf("gm%d" % c, [128, D], F32) for c in range(2)]
                    sh = [S.sbuf("sh%d" % c, [128, D], F32) for c in range(2)]
                    gt = S.sbuf("gt", [128, D], F32)
                    S.dma("sp", lambda e: e.dma_start(out=gt[:], in_=norm1_g[l, :].partition_broadcast(128)),
                          reads=[norm1_g], writes=[gt])
                    for c in range(2):
                        S.dma("sp", lambda e: e.dma_start(out=sh[c][:], in_=modd[c, 0:D].partition_broadcast(128)),
                              reads=[modd], writes=[sh[c]])
                        S.dma("sp", lambda e: e.dma_start(out=gm[c][:], in_=modd[c, D:2 * D].partition_broadcast(128)),
                              reads=[modd], writes=[gm[c]])
                        S.op("dve", lambda e: e.scalar_tensor_tensor(out=gm[c][:], in0=gm[c][:], scalar=1.0, in1=gt[:],
                                                                     op0=ALU.add, op1=ALU.mult),
                             reads=[gm[c], gt], writes=[gm[c]])
                    xts = [S.sbuf("xt%d" % i, [128, D], F32) for i in range(2)]
                    junk = S.sbuf("junk", [128, D], F32)
                    hb = [S.sbuf("hb%d" % i, [128, D], BF16) for i in range(2)]
                    ss = [S.sbuf("ss%d" % i, [128, 1], F32) for i in range(2)]
                    rs = [S.sbuf("rs%d" % i, [128, 1], F32) for i in range(2)]
                    ptr = [S.psum("ptr%d" % i, [128, 8, 128], BF16) for i in range(2)]
                    for t in range(NTILE):
                        c = 0 if t < (NCTX * LC) // 128 else 1
                        xt, hbt, sst, rst = xts[t % 2], hb[t % 2], ss[t % 2], rs[t % 2]
                        S.dma("sp", lambda e: e.dma_start(out=xt[:], in_=x_cur[t * 128:(t + 1) * 128, :]),
                              reads=[x_cur], writes=[xt])
                        S.op("act", lambda e: e.activation(out=junk[:], in_=xt[:], func=AF.Square, accum_out=sst[:]),
                             reads=[xt], writes=[junk, sst])
                        S.op("act", lambda e: e.activation(out=rst[:], in_=sst[:], func=AF.Sqrt, scale=1.0 / D, bias=EPS),
                             reads=[sst], writes=[rst])
                        S.op("dve", lambda e: e.reciprocal(out=rst[:], in_=rst[:]), reads=[rst], writes=[rst])
                        S.op("dve", lambda e: e.scalar_tensor_tensor(out=xt[:], in0=xt[:], scalar=rst[:, 0:1], in1=gm[c][:],
                                                                     op0=ALU.mult, op1=ALU.mult),
                             reads=[xt, rst, gm[c]], writes=[xt])
                        S.op("pool", lambda e: e.tensor_tensor(out=hbt[:], in0=xt[:], in1=sh[c][:], op=ALU.add),
                             reads=[xt, sh[c]], writes=[hbt])
                        for half in range(2):
                            pt = ptr[half]
                            for j in range(8):
                                kc = half * 8 + j
                                S.op("pe", lambda e: e.transpose(pt[:, j, :], hbt[:, kc * 128:(kc + 1) * 128], ident[:]),
                                     reads=[hbt, ident], writes=[pt])
                            _evac(S, half, hT[:, half * 8:(half + 1) * 8, t * 128:(t + 1) * 128], pt[:, :, :],
                                  reads=[pt], writes=[hT])
                    S.barrier()
                S.scope = sc
                wts = [S.sbuf("wint%d" % i, [128, 16, 512], BF16) for i in range(2)]
                stg = [S.sbuf("stg%d" % i, [128, 512], F32) for i in range(4)]
                pmm = [S.psum("pmm%d" % i, [128, 512], F32) for i in range(4)]
                nblk = (NT_COLS + 511) // 512
                ev = 0
                for cb in range(nblk):
                    c0 = cb * 512
                    cw = min(512, NT_COLS - c0)
                    wt = wts[cb % 2]
                    S.dma("pool", lambda e: e.dma_start(
                        out=wt[:, :, 0:cw], in_=w_in_T[l, :, c0:c0 + cw].rearrange("(k p) c -> p k c", p=128)),
                        reads=[w_in_T], writes=[wt])
                    for t in range(NTILE):
                        ps, st = pmm[ev % 4], stg[ev % 4]
                        for kc in range(16):
                            S.op("pe", lambda e: e.matmul(ps[:, 0:cw], lhsT=hT[:, kc, t * 128:(t + 1) * 128],
                                                          rhs=wt[:, kc, 0:cw], start=(kc == 0), stop=(kc == 15)),
                                 reads=[hT, wt], writes=[ps])
                        _evac(S, ev, st[:, 0:cw], ps[:, 0:cw], reads=[ps], writes=[st])
                        S.dma("sp", lambda e: e.dma_start(out=P_T[t * 128:(t + 1) * 128, c0:c0 + cw], in_=st[:, 0:cw]),
                              reads=[st], writes=[])
                        ev += 1
                nblk = (NF_ROWS + 511) // 512
                for cb in range(nblk):
                    c0 = cb * 512
                    cw = min(512, NF_ROWS - c0)
                    wt = wts[cb % 2]
                    S.dma("pool", lambda e: e.dma_start(
                        out=wt[:, :, 0:cw], in_=w_in_F[l, :, c0:c0 + cw].rearrange("(k p) c -> p k c", p=128)),
                        reads=[w_in_F], writes=[wt])
                    for sb in range((cw + 127) // 128):
                        r0 = c0 + sb * 128
                        rw = min(128, NF_ROWS - r0)
                        for g in range(TT // 512):
                            ps, st = pmm[ev % 4], stg[ev % 4]
                            for kc in range(16):
                                S.op("pe", lambda e: e.matmul(ps[0:rw, :], lhsT=wt[:, kc, sb * 128:sb * 128 + rw],
                                                              rhs=hT[:, kc, g * 512:(g + 1) * 512],
                                                              start=(kc == 0), stop=(kc == 15)),
                                     reads=[hT, wt], writes=[ps])
                            _evac(S, ev, st[0:rw, :], ps[0:rw, :], reads=[ps], writes=[st])
                            S.dma("sp", lambda e: e.dma_start(out=P_F[r0:r0 + rw, g * 512:(g + 1) * 512], in_=st[0:rw, :]),
                                  reads=[st], writes=[])
                            ev += 1
            S.barrier()
            S.scope = es

            for s_ in range(NCTX):
                for (nm, ob) in (("kr", o_kr), ("nak", o_nak), ("nav", o_nav)):
                    a0, aw = T_COLS[nm]
                    S.dma("sp", lambda e: e.dma_start(out=ob[s_, l, :, :], in_=P_T[s_ * LC:(s_ + 1) * LC, a0:a0 + aw]),
                          reads=[P_T], writes=[ob])
            if DBG_BR:
                for b_ in DBG_FILL:
                    S.dma("sp", lambda e: e.dma_start(out=brT[b_][:, :], in_=R["dbg_br"][l, b_ - 1]),
                          reads=[R["dbg_br"]], writes=[brT[b_]])
            if not DBG_BR:
                for src_, dst_ in ((R["peer_u"][l], R["peer_ub"][l]), (R["peer_v"][l], R["peer_vb"][l])):
                    for r_ in range(0, 16384, 1024):
                        S.dma("pool", lambda e: e.dma_start(out=dst_[r_:r_ + 1024, :], in_=src_[r_:r_ + 1024, :]),
                              reads=[src_], writes=[dst_])
            stage_dn(S, l, R)
            if not DBG_BR:
                stage_hyena(S, l, R)
                stage_mla(S, l, R)
                stage_na(S, l, R)
            if DBG_BR:
                for nm_, b_ in (("d_brT1", 1),):
                    db = eout(nm_, [1024, TT], BF16)
                    outs.append(db)
                    S.dma("sp", lambda e: e.dma_start(out=db[:, :], in_=brT[b_][:, :]), reads=[brT[b_]], writes=[db])
            if not DBG_BR:
                stage_merge(S, l, R, x_in if l == 0 else R["xbuf"])
                stage_peer(S, l, R, last=(l == DEPTH - 1))

        for b in outs:
            if b.last_w is not None:
                S._wait("sp", b.last_w[0], b.last_w[1])
        S.barrier()
        k.ninstr = S.ninstr
    return nc, k


def _prep_weights(inp):
    w_in = inp["w_in"]
    tcols = np.concatenate([
        np.arange(O_CQ, O_CQ + 512), np.arange(O_CKV, O_CKV + 256), np.arange(O_KR, O_KR + 64),
        np.arange(O_Z, O_Z + 1024), np.arange(O_A, O_A + 16), np.arange(O_B, O_B + 16),
        np.arange(O_NA + 1024, O_NA + 2048), np.arange(O_NA + 2048, O_NA + 3072),
        np.arange(O_GATE, O_GATE + 8192)])
    sw = np.concatenate([np.arange(16, 32), np.arange(0, 16), np.arange(48, 64), np.arange(32, 48)])
    fcols = np.concatenate([
        np.arange(O_KR, O_KR + 64), O_KR + sw, np.arange(O_DN, O_DN + 3072), np.arange(O_HY, O_HY + 3072),
        np.arange(O_NA, O_NA + 1024), np.arange(O_NA + 1024, O_NA + 2048)])
    assert len(tcols) == NT_COLS and len(fcols) == NF_ROWS
    return np.ascontiguousarray(w_in[:, :, tcols]), np.ascontiguousarray(w_in[:, :, fcols])


def _prep_consts(inp):
    C = {}
    sw = np.concatenate([np.arange(16, 32), np.arange(0, 16), np.arange(48, 64), np.arange(32, 48)])
    nope = np.concatenate([np.arange(h * 192, h * 192 + 128) for h in range(8)])
    rope = np.concatenate([np.arange(h * 192 + 128, h * 192 + 192) for h in range(8)])
    ropesw = np.concatenate([h * 192 + 128 + sw for h in range(8)])
    C["w_qb_p"] = np.ascontiguousarray(inp["mla_w_qb"][:, :, np.concatenate([nope, rope, ropesw])])
    kn = np.concatenate([np.arange(h * 256, h * 256 + 128) for h in range(8)])
    vv = np.concatenate([np.arange(h * 256 + 128, h * 256 + 256) for h in range(8)])
    C["w_kvb_p"] = np.ascontiguousarray(inp["mla_w_kvb"][:, :, np.concatenate([kn, vv])])
    t = np.arange(LL)
    pos = [t // 64, t % 64]
    inv = 10000.0 ** (-np.arange(0, 32, 2, dtype=np.float32) / 32.0)
    cosT = np.zeros((64, LL), np.float32)
    sinT = np.zeros((64, LL), np.float32)
    for d in range(64):
        half, j = d // 32, d % 32
        ang = pos[half].astype(np.float32) * inv[j % 16]
        cosT[d] = np.cos(ang)
        sinT[d] = (-np.sin(ang)) if j < 16 else np.sin(ang)
    C["ropeT"] = np.stack([cosT, sinT], 0).astype(np.float32)
    rpb = inp["na_rpb"]
    NEG = np.float32(-30000.0)
    cp = np.arange(64)[:, None]
    c = np.arange(64)[None, :]
    cstart = np.clip(c - 8, 0, 48)
    cvalid = (cp >= cstart) & (cp < cstart + 16)
    dcol = np.clip(cp - c + 15, 0, 30)
    ctab = np.full((DEPTH, 8, 64, 31, 64), NEG, np.float32)
    for mm in range(31):
        dr = 22 - mm
        if 0 <= dr <= 14:
            g = rpb[:, :, dr, :][:, :, dcol]
            ctab[:, :, :, mm, :] = np.where(cvalid[None, None], g, NEG)
    C["na_ctab"] = ctab
    mask = np.full((NA_NMASK, 128, 512), NEG, np.float32)
    for (qb, kt), mi in NA_MASK_IDX.items():
        for rr in range(2):
            rp = 2 * kt + rr
            for j in range(8):
                r = 8 * qb + j
                st_ = min(max(r - 4, 0), 24)
                if st_ <= rp < st_ + 8:
                    mask[mi, rr * 64:(rr + 1) * 64, j * 64:(j + 1) * 64] = 0.0
    C["na_mask"] = mask
    C["peer_keysT"] = np.ascontiguousarray(inp["peer_keys"].transpose(0, 1, 3, 2))
    ii = np.arange(128)[:, None]
    jj = np.arange(128)[None, :]
    NEGM = np.float32(-30000.0)
    dc = np.zeros((10, 128, 128), np.float32)
    dc[9] = ((ii // 64) == (jj // 64))
    dc[0] = (ii <= jj)
    dc[1] = (ii >= jj)
    dc[2] = np.eye(128)
    dc[3] = (ii > jj)
    dc[4] = (ii < jj)
    dc[5] = np.where(ii > jj, 0.0, NEGM)
    dc[6] = np.where(ii < jj, 0.0, NEGM)
    dc[7] = np.where(jj >= ii, 0.0, NEGM)
    dc[8] = np.where(jj <= ii, 0.0, NEGM)
    C["dn_consts"] = dc
    C["dn_convT"] = np.ascontiguousarray(inp["dn_conv"].reshape(DEPTH, 5, 24, 128).transpose(0, 3, 2, 1))
    return C


_CACHE = {}
_DBG = {}


def kernel(**inp):
    inp = {k_: np.asarray(v) for k_, v in inp.items()}
    if "prog" not in _CACHE:
        _CACHE["prog"] = build_program()
    nc, kk = _CACHE["prog"]
    w_in_T, w_in_F = _prep_weights(inp)
    C = _prep_consts(inp)
    H = _hy_host(inp)
    ident = np.eye(128, dtype=np.float32)
    in_maps = []
    for i in range(NCORE):
        b = i // 2
        x_in = np.concatenate([inp["x_prompt"][2 * i].reshape(LC, D), inp["x_prompt"][2 * i + 1].reshape(LC, D),
                               inp["x_sample"][b].reshape(LL, D)], axis=0)
        c2 = np.stack([inp["c_ctx"], inp["c"][b]], axis=0)
        c2T = np.ascontiguousarray(c2.reshape(2, 16, 128).transpose(2, 1, 0))
        in_maps.append({
            "x_in": np.ascontiguousarray(x_in), "c2T": c2T, "ident": ident,
            "norm1_g": inp["norm1_g"], "w_ada": inp["w_ada"], "b_ada": inp["b_ada"],
            "w_in_T": w_in_T, "w_in_F": w_in_F, "mla_kv_norm": inp["mla_kv_norm"],
            "mla_q_norm": inp["mla_q_norm"], "w_qb_p": C["w_qb_p"], "w_kvb_p": C["w_kvb_p"],
            "cache_ckv": inp["cache_mla_ckv"][b], "cache_kr": inp["cache_mla_krope"][b],
            "cache_nak": np.ascontiguousarray(inp["cache_na_k"][b].reshape(DEPTH, PAST, 1024)),
            "cache_nav": np.ascontiguousarray(inp["cache_na_v"][b].reshape(DEPTH, PAST, 1024)),
            "ropeT": C["ropeT"], "na_ctab": C["na_ctab"], "na_mask": C["na_mask"],
            "w_branch": inp["w_branch"], "w_out": inp["w_out"], "norm2_g": inp["norm2_g"],
            "peer_wq": inp["peer_wq"], "peer_keysT": C["peer_keysT"],
            "peer_u0": inp["peer_u"][0], "peer_u1": inp["peer_u"][1],
            "peer_v0": inp["peer_v"][0], "peer_v1": inp["peer_v"][1],
            "final_g": inp["final_g"].reshape(1, D), "iota256": np.arange(256, dtype=np.float32).reshape(1, 256),
            "dn_consts": C["dn_consts"], "dn_convT": C["dn_convT"],
            "dn_a_log": inp["dn_a_log"].reshape(DEPTH, 16), "dn_dt_bias": inp["dn_dt_bias"].reshape(DEPTH, 16),
            "dn_out_norm": inp["dn_out_norm"],
            "dn_state0": inp["state_dn_fwd"][b], "dn_state1": inp["state_dn_bwd"][b],
        })
        in_maps[-1].update(H)
        half = (i % 2) * (LL // 2)
        rows = np.concatenate([np.arange(NCTX * LC), NCTX * LC + half + np.arange(LL // 2)]).astype(np.int32)
        in_maps[-1]["ptok"] = np.ascontiguousarray(rows.reshape(NT_LAST, 128).T)
        if DBG_BR:
            in_maps[-1]["dbg_br"] = _DBG["br"]
    import os
    ndev = int(os.environ.get("KDEV_CORES", NCORE))
    res = run_bass_kernel_spmd(nc, in_maps[:ndev], core_ids=list(range(ndev)))
    r = list(res.results) + [res.results[0]] * (NCORE - ndev)
    _DBG["res"] = res.results[0]
    B = 16
    y_prompt = np.concatenate([r[i]["y_out"][:NCTX * LC].reshape(NCTX, LC, D) for i in range(NCORE)], axis=0)
    y_sample = np.stack([np.concatenate([r[2 * j]["y_out"][NCTX * LC:], r[2 * j + 1]["y_out"][NCTX * LC:]], axis=0)
                         for j in range(4)], axis=0)
    new_ckv = np.concatenate([r[i]["o_ckv"] for i in range(NCORE)], axis=0)
    new_kr = np.concatenate([r[i]["o_kr"] for i in range(NCORE)], axis=0)
    new_nak = np.concatenate([r[i]["o_nak"] for i in range(NCORE)], axis=0).reshape(B, DEPTH, LC, 8, 128)
    new_nav = np.concatenate([r[i]["o_nav"] for i in range(NCORE)], axis=0).reshape(B, DEPTH, LC, 8, 128)
    new_dnf = np.concatenate([r[i]["o_dn0"] for i in range(NCORE)], axis=0)
    new_dnb = np.concatenate([r[i]["o_dn1"] for i in range(NCORE)], axis=0)
    return (y_prompt, y_sample, new_ckv, new_kr, new_nak, new_nav, new_dnf, new_dnb)
```
